# Optimizing a Trainium2 kernel written in Bass

```python
import jax, jax.numpy as jnp
from jax import lax
import numpy as np

D_MODEL = 1024
BATCH = 4
SEQ = 8192
DEPTH = 1

N_HEADS = 16
HEAD_DIM = 64
ATTN_WIDTH = N_HEADS * HEAD_DIM
MOBA_BLOCK = 256
MOBA_TOPK = 3
MOBA_QBLOCK = 32
SSD_EXPAND = 2
SSD_INNER = SSD_EXPAND * D_MODEL
SSD_HEAD_DIM = 64
SSD_HEADS = SSD_INNER // SSD_HEAD_DIM
SSD_GROUPS = 8
SSD_STATE = 128
SSD_CONV = 4
SSD_CHUNK = 256
SSD_GN = SSD_GROUPS * SSD_STATE
SSD_CONV_CH = SSD_INNER + 2 * SSD_GN
PEER_HEADS = 8
PEER_NKEYS = 128
PEER_EXPERTS = PEER_NKEYS * PEER_NKEYS
PEER_QDIM = 256
PEER_TOPK = 16
PEER_TBLOCK = 128
N_BRANCH = 2
EPS = 1e-6
IN_COLS = 3 * ATTN_WIDTH + SSD_INNER + SSD_CONV_CH + SSD_HEADS + N_BRANCH * D_MODEL

kernel_name = "hybrid_moba_ssd_peer_block"


def rms_norm(x, g):
    x32 = x.astype(jnp.float32)
    y = x32 * lax.rsqrt(jnp.mean(x32 * x32, axis=-1, keepdims=True) + EPS)
    return (y * g.astype(jnp.float32)).astype(x.dtype)


def alibi_slopes(n):
    return jnp.exp2(-8.0 * jnp.arange(1, n + 1, dtype=jnp.float32) / n)


def moba_attention(q, k, v):
    B_, S_, H, hd = q.shape
    f32 = jnp.float32
    nblk = -(-S_ // MOBA_BLOCK)
    pad = nblk * MOBA_BLOCK - S_
    q = q.transpose(0, 2, 1, 3)
    padk = ((0, 0), (0, 0), (0, pad), (0, 0))
    kb = jnp.pad(k.transpose(0, 2, 1, 3), padk).reshape(B_, H, nblk, MOBA_BLOCK, hd)
    vb = jnp.pad(v.transpose(0, 2, 1, 3), padk).reshape(B_, H, nblk, MOBA_BLOCK, hd)
    slopes = alibi_slopes(H)
    kmean = jnp.mean(kb.astype(f32), axis=3).astype(q.dtype)
    t_all = jnp.arange(S_, dtype=jnp.int32)
    fully_past = jnp.arange(nblk, dtype=jnp.int32)[None, :] < (t_all // MOBA_BLOCK)[:, None]
    gate = jnp.einsum('bhsd,bhnd->bhsn', q, kmean).astype(f32)
    gate = jnp.where(fully_past, gate, -jnp.inf)
    k_sel = min(MOBA_TOPK, nblk)
    _, sel = lax.top_k(gate, k_sel)
    nq = S_ // MOBA_QBLOCK
    q_chunks = q.reshape(B_, H, nq, MOBA_QBLOCK, hd).transpose(2, 0, 1, 3, 4)
    sel_chunks = sel.reshape(B_, H, nq, MOBA_QBLOCK, k_sel).transpose(2, 0, 1, 3, 4)
    gather = jax.vmap(jax.vmap(lambda blocks, idx: blocks[idx]))
    scale = HEAD_DIM ** -0.5
    offs = jnp.arange(MOBA_BLOCK, dtype=jnp.int32)
    n_sel_keys = k_sel * MOBA_BLOCK

    def step(args):
        qc, sc, ci = args
        t = ci * MOBA_QBLOCK + jnp.arange(MOBA_QBLOCK, dtype=jnp.int32)
        kg = gather(kb, sc)
        vg = gather(vb, sc)
        s_sel = jnp.einsum('bhqd,bhqjld->bhqjl', qc, kg).astype(f32) * scale
        pos_sel = sc[..., None] * MOBA_BLOCK + offs
        dist_sel = (t[:, None, None] - pos_sel).astype(f32)
        valid = jnp.arange(k_sel, dtype=jnp.int32)[None, :] < (t // MOBA_BLOCK)[:, None]
        s_sel = jnp.where(valid[:, :, None], s_sel - slopes[:, None, None, None] * dist_sel, -jnp.inf)
        own = (ci * MOBA_QBLOCK) // MOBA_BLOCK
        k_own = lax.dynamic_index_in_dim(kb, own, axis=2, keepdims=False)
        v_own = lax.dynamic_index_in_dim(vb, own, axis=2, keepdims=False)
        dist_own = t[:, None] - (own * MOBA_BLOCK + offs)[None, :]
        s_own = jnp.einsum('bhqd,bhld->bhql', qc, k_own).astype(f32) * scale
        s_own = jnp.where(dist_own >= 0, s_own - slopes[:, None, None] * dist_own.astype(f32), -jnp.inf)
        s = jnp.concatenate([s_sel.reshape(B_, H, MOBA_QBLOCK, n_sel_keys), s_own], axis=-1)
        p = jax.nn.softmax(s, axis=-1).astype(vb.dtype)
        out = jnp.einsum('bhqm,bhqmd->bhqd', p[..., :n_sel_keys],
                         vg.reshape(B_, H, MOBA_QBLOCK, n_sel_keys, hd))
        out = out + jnp.einsum('bhql,bhld->bhqd', p[..., n_sel_keys:], v_own)
        return out

    outs = lax.map(step, (q_chunks, sel_chunks, jnp.arange(nq, dtype=jnp.int32)))
    return outs.transpose(1, 0, 3, 2, 4).reshape(B_, S_, H * hd)


def causal_depthwise_conv(u, w, b):
    out = lax.conv_general_dilated(u, w[:, None, :], window_strides=(1,),
                                   padding=[(SSD_CONV - 1, 0)],
                                   dimension_numbers=('NWC', 'WIO', 'NWC'),
                                   feature_group_count=u.shape[-1])
    return out + b


def ssd_mixer(z, xbc, dt_raw, conv_w, conv_b, dt_bias, a_log, d_skip, norm_g):
    out_dtype = z.dtype
    f32 = jnp.float32
    z, xbc, dt_raw = z.astype(f32), xbc.astype(f32), dt_raw.astype(f32)
    B_, S_, _ = xbc.shape
    G, R, P, N = SSD_GROUPS, SSD_HEADS // SSD_GROUPS, SSD_HEAD_DIM, SSD_STATE
    xbc = jax.nn.silu(causal_depthwise_conv(xbc, conv_w.astype(f32), conv_b.astype(f32)))
    xs = xbc[..., :SSD_INNER].reshape(B_, S_, G, R, P)
    bm = xbc[..., SSD_INNER:SSD_INNER + SSD_GN].reshape(B_, S_, G, N)
    cm = xbc[..., SSD_INNER + SSD_GN:].reshape(B_, S_, G, N)
    dt = jax.nn.softplus(dt_raw + dt_bias.astype(f32)).reshape(B_, S_, G, R)
    a = dt * (-jnp.exp(a_log.astype(f32))).reshape(G, R)
    nc = -(-S_ // SSD_CHUNK)
    pad = nc * SSD_CHUNK - S_

    def chunks(u):
        u = jnp.pad(u, [(0, 0), (0, pad)] + [(0, 0)] * (u.ndim - 2))
        return jnp.moveaxis(u.reshape((B_, nc, SSD_CHUNK) + u.shape[2:]), 1, 0)

    causal = jnp.tril(jnp.ones((SSD_CHUNK, SSD_CHUNK), dtype=bool))

    def step(state, inp):
        xc, ac, dtc, bc, cc = inp
        acum = jnp.cumsum(ac, axis=1)
        seg = acum[:, :, None] - acum[:, None, :]
        lmat = jnp.exp(jnp.where(causal[None, :, :, None, None], seg, -jnp.inf))
        xdt = xc * dtc[..., None]
        cb = jnp.einsum('btgn,bsgn->btsg', cc, bc)
        y_diag = jnp.einsum('btsg,btsgr,bsgrp->btgrp', cb, lmat, xdt)
        y_off = jnp.einsum('btgn,bgrpn->btgrp', cc, state) * jnp.exp(acum)[..., None]
        decay_end = jnp.exp(acum[:, -1:] - acum)
        new_state = state * jnp.exp(acum[:, -1])[..., None, None] + \
            jnp.einsum('bsgn,bsgr,bsgrp->bgrpn', bc, decay_end, xdt)
        return new_state, y_diag + y_off

    state0 = jnp.zeros((B_, G, R, P, N), f32)
    _, ys = lax.scan(step, state0, (chunks(xs), chunks(a), chunks(dt), chunks(bm), chunks(cm)))
    y = jnp.moveaxis(ys, 0, 1).reshape(B_, nc * SSD_CHUNK, G, R, P)[:, :S_]
    y = y + d_skip.astype(f32).reshape(G, R)[..., None] * xs
    y = y.reshape(B_, S_, SSD_INNER) * jax.nn.silu(z)
    yg = y.reshape(B_, S_, G, SSD_INNER // G)
    yg = yg * lax.rsqrt(jnp.mean(yg * yg, axis=-1, keepdims=True) + EPS)
    y = yg.reshape(B_, S_, SSD_INNER) * norm_g.astype(f32)
    return y.astype(out_dtype)


def peer_ffn(xn, w_q, keys1, keys2, u_tab, v_tab):
    B_, S_, D = xn.shape
    T = B_ * S_
    f32 = jnp.float32
    half = PEER_QDIM // 2
    xt = xn.reshape(T, D)
    q = (xt @ w_q).reshape(T, PEER_HEADS, PEER_QDIM)
    s1 = jnp.einsum('thd,hkd->thk', q[..., :half], keys1).astype(f32)
    s2 = jnp.einsum('thd,hkd->thk', q[..., half:], keys2).astype(f32)
    v1, i1 = lax.top_k(s1, PEER_TOPK)
    v2, i2 = lax.top_k(s2, PEER_TOPK)
    cand = (v1[..., :, None] + v2[..., None, :]).reshape(T, PEER_HEADS, PEER_TOPK * PEER_TOPK)
    cidx = (i1[..., :, None] * PEER_NKEYS + i2[..., None, :]).reshape(T, PEER_HEADS, PEER_TOPK * PEER_TOPK)
    sv, pos = lax.top_k(cand, PEER_TOPK)
    eidx = jnp.take_along_axis(cidx, pos, axis=-1)
    gw = jax.nn.softmax(sv, axis=-1)
    nt = T // PEER_TBLOCK

    def step(args):
        xc, ec, gc = args
        u = jnp.take(u_tab, ec, axis=0)
        act = jnp.einsum('td,thkd->thk', xc, u).astype(f32)
        hact = (jax.nn.gelu(act, approximate=False) * gc).astype(xc.dtype)
        return jnp.einsum('thk,thkd->td', hact, jnp.take(v_tab, ec, axis=0))

    y = lax.map(step, (xt.reshape(nt, PEER_TBLOCK, D),
                       eidx.reshape(nt, PEER_TBLOCK, PEER_HEADS, PEER_TOPK),
                       gw.reshape(nt, PEER_TBLOCK, PEER_HEADS, PEER_TOPK)))
    return y.reshape(B_, S_, D).astype(xn.dtype)


def setup_inputs(seed: int = 0) -> dict:
    key = jax.random.key(seed)
    ks = jax.random.split(key, 20)
    f32 = jnp.float32
    L = DEPTH

    def nrm(k, shape, scale):
        return jax.random.normal(k, shape, f32) * scale

    dt0 = jnp.exp(jax.random.uniform(ks[7], (L, SSD_HEADS), f32, np.log(1e-3), np.log(1e-1)))
    dt_bias = dt0 + jnp.log(-jnp.expm1(-dt0))
    return {
        "x": nrm(ks[0], (BATCH, SEQ, D_MODEL), 1.0),
        "norm1_g": 1.0 + nrm(ks[1], (L, D_MODEL), 0.02),
        "w_in": nrm(ks[2], (L, D_MODEL, IN_COLS), D_MODEL ** -0.5),
        "q_norm_g": 1.0 + nrm(ks[3], (L, HEAD_DIM), 0.02),
        "k_norm_g": 1.0 + nrm(ks[4], (L, HEAD_DIM), 0.02),
        "conv_w": nrm(ks[5], (L, SSD_CONV, SSD_CONV_CH), SSD_CONV ** -0.5),
        "conv_b": nrm(ks[6], (L, SSD_CONV_CH), 0.02),
        "dt_bias": dt_bias,
        "a_log": jnp.log(jax.random.uniform(ks[8], (L, SSD_HEADS), f32, 1.0, 16.0)),
        "d_skip": 1.0 + nrm(ks[9], (L, SSD_HEADS), 0.1),
        "ssd_norm_g": 1.0 + nrm(ks[10], (L, SSD_INNER), 0.02),
        "w_attn_o": nrm(ks[11], (L, ATTN_WIDTH, D_MODEL), ATTN_WIDTH ** -0.5),
        "w_ssd_o": nrm(ks[12], (L, SSD_INNER, D_MODEL), SSD_INNER ** -0.5),
        "w_out": nrm(ks[13], (L, D_MODEL, D_MODEL), D_MODEL ** -0.5),
        "norm2_g": 1.0 + nrm(ks[14], (L, D_MODEL), 0.02),
        "w_peer_q": nrm(ks[15], (L, D_MODEL, PEER_HEADS * PEER_QDIM), D_MODEL ** -0.5),
        "peer_keys1": nrm(ks[16], (L, PEER_HEADS, PEER_NKEYS, PEER_QDIM // 2), (PEER_QDIM // 2) ** -0.5),
        "peer_keys2": nrm(ks[17], (L, PEER_HEADS, PEER_NKEYS, PEER_QDIM // 2), (PEER_QDIM // 2) ** -0.5),
        "peer_u": nrm(ks[18], (L, PEER_EXPERTS, D_MODEL), D_MODEL ** -0.5),
        "peer_v": nrm(ks[19], (L, PEER_EXPERTS, D_MODEL), (PEER_HEADS * PEER_TOPK) ** -0.5),
    }


def reference(x, norm1_g, w_in, q_norm_g, k_norm_g, conv_w, conv_b, dt_bias, a_log, d_skip,
              ssd_norm_g, w_attn_o, w_ssd_o, w_out, norm2_g, w_peer_q, peer_keys1, peer_keys2,
              peer_u, peer_v):
    B_, S_, _ = x.shape
    sizes = [ATTN_WIDTH, ATTN_WIDTH, ATTN_WIDTH, SSD_INNER, SSD_CONV_CH, SSD_HEADS, D_MODEL, D_MODEL]
    split_at = [int(c) for c in np.cumsum(sizes)[:-1]]
    for l in range(DEPTH):
        h = rms_norm(x, norm1_g[l])
        proj = h @ w_in[l]
        q, k, v, z, xbc, dt_raw, g_attn, g_ssd = jnp.split(proj, split_at, axis=-1)
        q = rms_norm(q.reshape(B_, S_, N_HEADS, HEAD_DIM), q_norm_g[l])
        k = rms_norm(k.reshape(B_, S_, N_HEADS, HEAD_DIM), k_norm_g[l])
        v = v.reshape(B_, S_, N_HEADS, HEAD_DIM)
        y_attn = moba_attention(q, k, v) @ w_attn_o[l]
        y_ssd = ssd_mixer(z, xbc, dt_raw, conv_w[l], conv_b[l], dt_bias[l], a_log[l],
                          d_skip[l], ssd_norm_g[l]) @ w_ssd_o[l]
        mixed = jax.nn.sigmoid(g_attn) * y_attn + jax.nn.sigmoid(g_ssd) * y_ssd
        x = x + mixed @ w_out[l]
        x = x + peer_ffn(rms_norm(x, norm2_g[l]), w_peer_q[l], peer_keys1[l], peer_keys2[l],
                         peer_u[l], peer_v[l])
    return x
```

```python
from contextlib import ExitStack
import ml_dtypes
import numpy as np
import concourse.bass as bass
import concourse.mybir as mybir

F32 = mybir.dt.float32
BF16 = mybir.dt.bfloat16
AF = mybir.ActivationFunctionType
ALU = mybir.AluOpType
AX = mybir.AxisListType

SAME_ENGINE_SYNC = True
N_DMA_SEMS = 32


class Sched:
    ENG = ("sp", "act", "dve", "pool", "pe")

    def __init__(self, nc, stack):
        self.nc = nc
        self.ops = []
        self.esem = {e: stack.enter_context(nc.semaphore("s_" + e)) for e in self.ENG}
        self.ecnt = {e: 0 for e in self.ENG}
        self.dsem = [stack.enter_context(nc.semaphore("d%d" % i)) for i in range(N_DMA_SEMS)]
        self.dcnt = [0] * N_DMA_SEMS
        self.downer = [None] * N_DMA_SEMS
        self.dnext = 0
        self.last_w = {}
        self.readers = {}
        self.waited = {e: {} for e in self.ENG}
        self.nblocks = 0
        self.nops = 0

    def _need(self, eng, ev, waits):
        if ev is None:
            return
        sem, val, src_eng, is_dma = ev
        if (not is_dma) and src_eng == eng and (eng == "pe" or not SAME_ENGINE_SYNC):
            return
        key = id(sem)
        if self.waited[eng].get(key, 0) >= val:
            return
        cur = waits.get(key)
        if cur is None or cur[1] < val:
            waits[key] = (sem, val)

    def op(self, eng, fn, reads=(), writes=(), dma=False):
        writes = list(writes) + [k for k in reads if isinstance(k, str) and k.startswith("ps") and k not in writes]
        waits = {}
        for k in reads:
            self._need(eng, self.last_w.get(k), waits)
        for k in writes:
            self._need(eng, self.last_w.get(k), waits)
            for ev in self.readers.get(k, ()):
                self._need(eng, ev, waits)
        if dma:
            half = N_DMA_SEMS // 2
            base = 0 if eng == "sp" else half
            self.dnx = getattr(self, "dnx", {})
            i = base + self.dnx.get(eng, 0)
            self.dnx[eng] = (self.dnx.get(eng, 0) + 1) % half
            if self.dcnt[i] > 0:
                self._need(eng, (self.dsem[i], 16 * self.dcnt[i], self.downer[i], True), waits)
            self.dcnt[i] += 1
            self.downer[i] = eng
            ev = (self.dsem[i], 16 * self.dcnt[i], eng, True)
            inc = (self.dsem[i], 16)
        else:
            self.ecnt[eng] += 1
            ev = (self.esem[eng], self.ecnt[eng], eng, False)
            inc = (self.esem[eng], 1)
        for (sem, val) in waits.values():
            self.waited[eng][id(sem)] = val
        for k in reads:
            self.readers.setdefault(k, []).append(ev)
        for k in writes:
            self.last_w[k] = ev
            self.readers[k] = []
        self.ops.append((eng, fn, list(waits.values()), inc))
        self.nops += 1

    def flush(self):
        fin = {}
        for i in range(N_DMA_SEMS):
            if self.dcnt[i] > 0:
                e = self.downer[i]
                if self.waited[e].get(id(self.dsem[i]), 0) < 16 * self.dcnt[i]:
                    fin.setdefault(e, []).append((self.dsem[i], 16 * self.dcnt[i]))
                    self.waited[e][id(self.dsem[i])] = 16 * self.dcnt[i]
        ops = self.ops
        self.ops = []
        if not ops and not fin:
            return
        nc = self.nc
        with nc.Block() as block:
            deco = {"sp": block.sync, "act": block.scalar, "dve": block.vector,
                    "pool": block.gpsimd, "pe": block.tensor}
            for e in self.ENG:
                mine = [o for o in ops if o[0] == e]
                tail = fin.get(e, [])
                if not mine and not tail:
                    continue

                def body(engine, mine=mine, tail=tail):
                    for (_, fn, waits, inc) in mine:
                        for (sem, val) in waits:
                            engine.wait_ge(sem, val)
                        ins = fn(engine)
                        ins.then_inc(inc[0], inc[1])
                    for (sem, val) in tail:
                        engine.wait_ge(sem, val)

                deco[e](body)
        self.nblocks += 1
        self.last_w = {}
        self.readers = {}

    def dma(self, eng, out, in_, reads=(), writes=(), **kw):
        self.op(eng, lambda e: e.dma_start(out=out, in_=in_, **kw), reads, writes, dma=True)

    def mm(self, out, lhsT, rhs, start, stop, reads=(), writes=()):
        self.op("pe", lambda e: e.matmul(out, lhsT, rhs, start=start, stop=stop), reads, writes)

    def tr(self, out, in_, ident, reads=(), writes=()):
        self.op("pe", lambda e: e.transpose(out, in_, ident), reads, writes)

    def act(self, out, in_, func, reads=(), writes=(), **kw):
        self.op("act", lambda e: e.activation(out=out, in_=in_, func=func, **kw), reads, writes)

    def tt(self, eng, out, in0, in1, op, reads=(), writes=()):
        self.op(eng, lambda e: e.tensor_tensor(out=out, in0=in0, in1=in1, op=op), reads, writes)

    def ts(self, eng, out, in0, s1, op0, s2=None, op1=None, reads=(), writes=(), **kw):
        if op1 is None:
            if op0 == ALU.pow:
                self.op(eng, lambda e: e.tensor_scalar(out=out, in0=in0, scalar1=0.0, scalar2=s1, op0=ALU.add, op1=ALU.pow, **kw),
                        reads, writes)
            else:
                self.op(eng, lambda e: e.tensor_scalar(out=out, in0=in0, scalar1=s1, scalar2=None, op0=op0, **kw),
                        reads, writes)
        else:
            self.op(eng, lambda e: e.tensor_scalar(out=out, in0=in0, scalar1=s1, scalar2=s2, op0=op0, op1=op1, **kw),
                    reads, writes)

    def stt(self, eng, out, in0, scalar, in1, op0, op1, reads=(), writes=()):
        self.op(eng, lambda e: e.scalar_tensor_tensor(out=out, in0=in0, scalar=scalar, in1=in1, op0=op0, op1=op1),
                reads, writes)

    def copy(self, eng, out, in_, reads=(), writes=()):
        if eng == "act":
            self.op(eng, lambda e: e.activation(out=out, in_=in_, func=AF.Copy), reads, writes)
        else:
            self.op(eng, lambda e: e.tensor_copy(out=out, in_=in_), reads, writes)

    def memset(self, eng, ap, val, writes=()):
        self.op(eng, lambda e: e.memset(ap, val), (), writes)


D = 1024
NH = 16
HD = 64
SH = 32
SP = 64
SG = 8
SN = 128
EPS = 1e-6
NEG = -30000.0
C_Q, C_K, C_V, C_Z, C_X, C_B, C_C, C_DT, C_GA, C_GS = 0, 1024, 2048, 3072, 5120, 7168, 8192, 9216, 9248, 10272
IN_COLS = 11296


class Ctx:
    pass


def dram(C, name, shape, dt):
    kind = "ExternalOutput" if name in C.debug else "Internal"
    return C.nc.dram_tensor(name, list(shape), dt, kind=kind).ap()


def setup(nc, NV, debug=()):
    C = Ctx()
    C.nc = nc
    C.NV = NV
    C.NO = NV // 2
    C.debug = set(debug)
    C.inp = {}

    def inp(name, shape, dt=F32):
        C.inp[name] = nc.dram_tensor(name, list(shape), dt, kind="ExternalInput").ap()
        return C.inp[name]
    NV_, NO = NV, C.NO
    NB = NV // 256
    C.NB = NB
    inp("xv", [NV, D])
    inp("norm1_g", [1, D]); inp("w_in", [D, IN_COLS]); inp("q_norm_g", [1, HD]); inp("k_norm_g", [1, HD])
    inp("conv_wT", [4096, 4]); inp("conv_b", [4096, 1]); inp("dt_bias", [1, SH]); inp("a_log", [1, SH])
    inp("d_skip", [1, SH]); inp("ssd_norm_g", [1, 2048]); inp("w_attn_o", [D, D]); inp("w_ssd_o", [2048, D])
    inp("w_out", [D, D]); inp("norm2_g", [1, D]); inp("w_peer_q", [D, 2048])
    inp("keys1T", [8, 128, 128]); inp("keys2T", [8, 128, 128])
    inp("peer_uT", [D, 16384]); inp("peer_v", [16384, D])
    inp("ident_bf", [128, 128], BF16); inp("ident_f", [128, 128], F32)
    inp("pv", [1, 1])
    C.hT = dram(C, "hT_s", [8, 128, NV], BF16)
    C.qT = dram(C, "qT_s", [D, NO], BF16)
    C.kT = dram(C, "kT_s", [D, NV], BF16)
    C.kmT = dram(C, "kmT_s", [8, 128, NB], F32)
    C.v = dram(C, "v_s", [NV, D], BF16)
    C.z = dram(C, "z_s", [NO, 2048], BF16)
    C.xsT = dram(C, "xsT_s", [2048, NV], BF16)
    C.BT = dram(C, "BT_s", [1024, NV], BF16)
    C.CT = dram(C, "CT_s", [1024, NO], BF16)
    C.da = dram(C, "da_s", [NV, 64], F32)
    C.sga = dram(C, "sga_s", [NO, D], BF16)
    C.sgs = dram(C, "sgs_s", [NO, D], BF16)
    return C


def phase_norm(C, S, xin, gname, hT, ntok, tag):
    nc = C.nc
    with ExitStack() as st:
        sb = lambda name, shape, dt: st.enter_context(nc.sbuf_tensor(tag + name, shape, dt))
        ident = sb("ident", [128, 128], BF16)
        gT = sb("gT", [128, 8], F32)
        S.dma("sp", ident[:], C.inp["ident_bf"][:, :], writes=["ident"])
        S.dma("sp", gT[:], C.inp[gname][0, :].rearrange("(c p) -> p c", p=128), writes=["gT"],
              allow_slow_non_contiguous=True)
        xt = [sb("xt%d" % i, [128, D], F32) for i in range(2)]
        junk = sb("junk", [128, D], F32)
        ss = [sb("ss%d" % i, [128, 1], F32) for i in range(2)]
        xb = [sb("xb%d" % i, [128, D], BF16) for i in range(2)]
        ho = [sb("ho%d" % i, [128, 8, 128], BF16) for i in range(2)]
        for t in range(ntok // 128):
            i = t % 2
            pT = C.ps[t % 2][:].bitcast(BF16)[:, 0:1024].rearrange("p (c t) -> p c t", t=128)
            pk = "ps%d" % (t % 2)
            S.dma("sp", xt[i][:], xin[t * 128:(t + 1) * 128, :], writes=["xt%d" % i])
            S.act(junk[:], xt[i][:], AF.Square, reads=["xt%d" % i], writes=["junk", "ss%d" % i], accum_out=ss[i][:])
            S.act(ss[i][:], ss[i][:], AF.Ln, reads=["ss%d" % i], writes=["ss%d" % i], scale=1.0 / D, bias=EPS)
            S.act(ss[i][:], ss[i][:], AF.Exp, reads=["ss%d" % i], writes=["ss%d" % i], scale=-0.5)
            S.act(xb[i][:], xt[i][:], AF.Copy, reads=["xt%d" % i, "ss%d" % i], writes=["xb%d" % i], scale=ss[i][:])
            for c in range(8):
                S.tr(pT[:, c, :], xb[i][:, c * 128:(c + 1) * 128], ident[:], reads=["xb%d" % i, "ident"], writes=[pk])
            S.tt("dve", ho[i][:], pT, gT[:].unsqueeze(2).to_broadcast([128, 8, 128]), ALU.mult,
                 reads=[pk, "gT"], writes=["ho%d" % i])
            S.dma("pool", hT[:, :, t * 128:(t + 1) * 128].rearrange("c p t -> p c t"), ho[i][:], reads=["ho%d" % i])
        S.flush()


def phase_inproj(C, S):
    nc = C.nc
    NV, NO = C.NV, C.NO
    NT = NV // 512
    NTO = NO // 512
    W = C.inp["w_in"]
    with ExitStack() as st:
        sb = lambda name, shape, dt: st.enter_context(nc.sbuf_tensor("ip_" + name, shape, dt))
        ident = sb("ident", [128, 128], BF16)
        S.dma("sp", ident[:], C.inp["ident_bf"][:, :], writes=["ident"])
        gq = sb("gq", [128, HD], F32); gk = sb("gk", [128, HD], F32)
        S.dma("sp", gq[:], C.inp["q_norm_g"][0:1, :].partition_broadcast(128), writes=["gq"])
        S.dma("sp", gk[:], C.inp["k_norm_g"][0:1, :].partition_broadcast(128), writes=["gk"])
        S.ts("dve", gq[:], gq[:], HD ** -0.5, ALU.mult, reads=["gq"], writes=["gq"])
        dtb = sb("dtb", [128, SH], F32); An = sb("An", [128, SH], F32)
        S.dma("sp", dtb[:], C.inp["dt_bias"][0:1, :].partition_broadcast(128), writes=["dtb"])
        S.dma("sp", An[:], C.inp["a_log"][0:1, :].partition_broadcast(128), writes=["An"])
        S.act(An[:], An[:], AF.Exp, reads=["An"], writes=["An"])
        S.ts("dve", An[:], An[:], -1.0, ALU.mult, reads=["An"], writes=["An"])
        cw = sb("cw", [128, 32, 4], F32); cb = sb("cb", [128, 32], F32)
        S.dma("sp", cw[:], C.inp["conv_wT"].rearrange("(c p) k -> p c k", p=128), writes=["cw"])
        S.dma("sp", cb[:], C.inp["conv_b"].rearrange("(c p) o -> p (c o)", p=128), writes=["cb"],
              allow_slow_non_contiguous=True)
        kmT = sb("kmT", [128, 8, C.NB], F32)
        S.memset("pool", kmT[:], 0.0, writes=["kmT"])
        wf = [sb("wf%d" % i, [128, 8, 512], F32) for i in range(2)]
        wb = [sb("wb%d" % i, [128, 8, 512], BF16) for i in range(2)]
        hb = [sb("hb%d" % i, [128, 8, 512], BF16) for i in range(3)]
        ev = [sb("ev%d" % i, [128, 512], F32) for i in range(2)]
        sq = sb("sq", [128, 512], F32)
        ssq = sb("ssq", [128, 8], F32)
        ob = [sb("ob%d" % i, [128, 512], BF16) for i in range(2)]
        tb = [sb("tb%d" % i, [128, 4, 128], BF16) for i in range(2)]
        kr = sb("kr", [128, 4], F32)
        cbuf = [sb("cbuf%d" % i, [128, 515], F32) for i in range(4)]
        cacc = sb("cacc", [128, 512], F32)
        dab = [sb("dab%d" % i, [128, 64], F32) for i in range(2)]

        blocks = []
        for j in range(2): blocks.append(("q", C_Q + 512 * j, 512, NT - NTO, j))
        for j in range(2): blocks.append(("k", C_K + 512 * j, 512, 0, j))
        for j in range(2): blocks.append(("v", C_V + 512 * j, 512, 0, j))
        for j in range(4): blocks.append(("z", C_Z + 512 * j, 512, NT - NTO, j))
        for j in range(4): blocks.append(("xs", C_X + 512 * j, 512, 0, j))
        for j in range(2): blocks.append(("B", C_B + 512 * j, 512, 0, j))
        for j in range(2): blocks.append(("C", C_C + 512 * j, 512, NT - NTO - 1, j))
        blocks.append(("dt", C_DT, 32, 0, 0))
        for j in range(2): blocks.append(("ga", C_GA + 512 * j, 512, NT - NTO, j))
        for j in range(2): blocks.append(("gs", C_GS + 512 * j, 512, NT - NTO, j))

        cnt = {"h": 0, "ps": 0, "ev": 0, "ob": 0, "tb": 0, "da": 0}
        def load_w(bi):
            kind_, c0_, ncol_, _, _ = blocks[bi]
            wi_ = bi % 2
            S.dma("sp", wf[wi_][:, :, 0:ncol_], W[:, c0_:c0_ + ncol_].rearrange("(c p) n -> p c n", p=128),
                  writes=["wf%d" % wi_])
            S.copy("pool", wb[wi_][:, :, 0:ncol_], wf[wi_][:, :, 0:ncol_], reads=["wf%d" % wi_], writes=["wb%d" % wi_])
        load_w(0)
        deferred = []
        for bi, (kind, c0, ncol, t0, j) in enumerate(blocks):
            wi = bi % 2
            if bi + 1 < len(blocks):
                load_w(bi + 1)
            if kind in ("xs", "B", "C"):
                for s4 in range(4):
                    S.memset("pool", cbuf[s4][:, 0:3], 0.0, writes=["cbuf%d" % s4])
            for t in range(t0, NT):
                hi = cnt["h"] % 3; cnt["h"] += 1
                S.dma("sp", hb[hi][:], C.hT[:, :, t * 512:(t + 1) * 512].rearrange("c p t -> p c t"),
                      writes=["hb%d" % hi])
                to = t - (NT - NTO)
                for s4 in range(4):
                    pi = cnt["ps"] % 4; cnt["ps"] += 1
                    ps = C.ps[pi]; pk = "ps%d" % pi
                    tok = t * 512 + s4 * 128
                    if kind in ("xs", "B", "C"):
                        for c in range(8):
                            S.mm(ps[:, 0:512], wb[wi][:, c, s4 * 128:(s4 + 1) * 128], hb[hi][:, c, :], c == 0, c == 7,
                                 reads=["wb%d" % wi, "hb%d" % hi], writes=[pk])
                        ck = "cbuf%d" % s4
                        S.copy("act", cbuf[s4][:, 3:515], ps[:, 0:512], reads=[pk], writes=[ck])
                        chn = (c0 - C_X) // 128 + s4
                        S.ts("dve", cacc[:], cbuf[s4][:, 0:512], cw[:, chn, 0:1], ALU.mult, cb[:, chn:chn + 1], ALU.add,
                             reads=[ck, "cw", "cb"], writes=["cacc"])
                        for k in range(1, 4):
                            S.stt("dve", cacc[:], cbuf[s4][:, k:k + 512], cw[:, chn, k:k + 1], cacc[:], ALU.mult, ALU.add,
                                  reads=[ck, "cacc"], writes=["cacc"])
                        S.copy("pool", cbuf[s4][:, 0:3], cbuf[s4][:, 512:515], reads=[ck], writes=[ck])
                        oi = cnt["ob"] % 2; cnt["ob"] += 1
                        S.act(ob[oi][:], cacc[:], AF.Silu, reads=["cacc"], writes=["ob%d" % oi])
                        r0 = (c0 - {"xs": C_X, "B": C_B, "C": C_C}[kind]) + s4 * 128
                        if kind == "xs":
                            S.dma("pool", C.xsT[r0:r0 + 128, t * 512:(t + 1) * 512], ob[oi][:], reads=["ob%d" % oi])
                        elif kind == "B":
                            S.dma("pool", C.BT[r0:r0 + 128, t * 512:(t + 1) * 512], ob[oi][:], reads=["ob%d" % oi])
                        elif to >= 0:
                            S.dma("pool", C.CT[r0:r0 + 128, to * 512:(to + 1) * 512], ob[oi][:], reads=["ob%d" % oi])
                        continue
                    for c in range(8):
                        S.mm(ps[:, 0:ncol], hb[hi][:, c, s4 * 128:(s4 + 1) * 128], wb[wi][:, c, 0:ncol], c == 0, c == 7,
                             reads=["wb%d" % wi, "hb%d" % hi], writes=[pk])
                    while deferred:
                        deferred.pop(0)()
                    if kind in ("q", "k"):
                        ei = cnt["ev"] % 2; cnt["ev"] += 1
                        ek = "ev%d" % ei
                        S.copy("act", ev[ei][:], ps[:, 0:512], reads=[pk], writes=[ek])
                        S.tt("dve", sq[:], ev[ei][:], ev[ei][:], ALU.mult, reads=[ek], writes=["sq"])
                        S.op("dve", lambda e: e.tensor_reduce(out=ssq[:], in_=sq[:].rearrange("p (a b) -> p a b", b=HD),
                                                             axis=AX.X, op=ALU.add), reads=["sq"], writes=["ssq"])
                        S.act(ssq[:], ssq[:], AF.Ln, reads=["ssq"], writes=["ssq"], scale=1.0 / HD, bias=EPS)
                        S.act(ssq[:], ssq[:], AF.Exp, reads=["ssq"], writes=["ssq"], scale=-0.5)
                        e3 = ev[ei][:].rearrange("p (a b) -> p a b", b=HD)
                        S.tt("dve", e3, e3, ssq[:].unsqueeze(2).to_broadcast([128, 8, HD]), ALU.mult,
                             reads=[ek, "ssq"], writes=[ek])
                        oi = cnt["ob"] % 2; cnt["ob"] += 1
                        g_ = gq if kind == "q" else gk
                        S.tt("dve", ob[oi][:].rearrange("p (a b) -> p a b", b=HD), e3,
                             g_[:].unsqueeze(1).to_broadcast([128, 8, HD]), ALU.mult,
                             reads=[ek, "gq", "gk"], writes=["ob%d" % oi])
                        def fin(kind=kind, oi=oi, j=j, to=to, s4=s4, tok=tok):
                            pti = 4 + cnt["tb"] % 2
                            ti = cnt["tb"] % 2; cnt["tb"] += 1
                            pT = C.ps[pti][:].bitcast(BF16)[:, 0:512].rearrange("p (c t) -> p c t", t=128)
                            for c in range(4):
                                S.tr(pT[:, c, :], ob[oi][:, c * 128:(c + 1) * 128], ident[:], reads=["ob%d" % oi, "ident"],
                                     writes=["ps%d" % pti])
                            S.copy("act", tb[ti][:], pT, reads=["ps%d" % pti], writes=["tb%d" % ti])
                            rows = slice(j * 512, (j + 1) * 512)
                            if kind == "q":
                                S.dma("pool", C.qT[rows, to * 512 + s4 * 128: to * 512 + (s4 + 1) * 128].rearrange("(c p) t -> p c t", p=128),
                                      tb[ti][:], reads=["tb%d" % ti])
                            else:
                                S.dma("pool", C.kT[rows, tok:tok + 128].rearrange("(c p) t -> p c t", p=128),
                                      tb[ti][:], reads=["tb%d" % ti])
                                S.op("dve", lambda e, ti=ti: e.tensor_reduce(out=kr[:], in_=tb[ti][:], axis=AX.X, op=ALU.add),
                                     reads=["tb%d" % ti], writes=["kr"])
                                blk = tok // 256
                                S.stt("dve", kmT[:, j * 4:(j + 1) * 4, blk], kr[:], 1.0 / 256, kmT[:, j * 4:(j + 1) * 4, blk],
                                      ALU.mult, ALU.add, reads=["kr", "kmT"], writes=["kmT"])
                        deferred.append(fin)
                    elif kind == "dt":
                        di = cnt["da"] % 2; cnt["da"] += 1
                        dk = "dab%d" % di
                        S.tt("dve", dab[di][:, 0:32], ps[:, 0:32], dtb[:], ALU.add, reads=[pk, "dtb"], writes=[dk])
                        S.act(dab[di][:, 0:32], dab[di][:, 0:32], AF.Exp, reads=[dk], writes=[dk])
                        S.act(dab[di][:, 0:32], dab[di][:, 0:32], AF.Ln, reads=[dk], writes=[dk], bias=1.0)
                        S.tt("dve", dab[di][:, 32:64], dab[di][:, 0:32], An[:], ALU.mult, reads=[dk, "An"], writes=[dk])
                        S.dma("pool", C.da[tok:tok + 128, :], dab[di][:], reads=[dk])
                    else:
                        oi = cnt["ob"] % 2; cnt["ob"] += 1
                        fn = {"v": AF.Copy, "z": AF.Silu, "ga": AF.Sigmoid, "gs": AF.Sigmoid}[kind]
                        S.act(ob[oi][:], ps[:, 0:512], fn, reads=[pk], writes=["ob%d" % oi])
                        if kind == "v":
                            dst = C.v[tok:tok + 128, j * 512:(j + 1) * 512]
                        else:
                            otok = to * 512 + s4 * 128
                            dst = {"z": C.z, "ga": C.sga, "gs": C.sgs}[kind][otok:otok + 128, j * 512:(j + 1) * 512]
                        S.dma("pool", dst, ob[oi][:], reads=["ob%d" % oi])
        while deferred:
            deferred.pop(0)()
        S.dma("pool", C.kmT.rearrange("c p n -> p c n"), kmT[:], reads=["kmT"])
        S.flush()


def moba_setup(C):
    nc = C.nc
    NV, NO, NB = C.NV, C.NO, C.NB

    def inp(name, shape, dt=F32):
        C.inp[name] = nc.dram_tensor(name, list(shape), dt, kind="ExternalInput").ap()
    inp("kaug_c", [33, NV], BF16)
    inp("cq", [NH, NO], BF16)
    inp("kbias", [128, NH, NV // 128])
    NBO = NO // 256
    inp("gbp", [1, NBO * 32]); inp("A01", [1, NBO * 32]); inp("Bt", [1, NBO * 32])
    inp("cm", [128, 2, 256], BF16)
    inp("sel65", [65, 64])
    C.aT = dram(C, "aT_s", [D, NO], BF16)


def moba_consts(NV, r):
    bf = ml_dtypes.bfloat16
    NO = NV // 2
    NB = NV // 256
    NBO = NO // 256
    slopes = np.exp2(-8.0 * np.arange(1, NH + 1, dtype=np.float32) / NH).astype(np.float32)
    out = {}
    ka = np.zeros((33, NV), np.float32)
    for n in range(NB):
        ka[n, n * 256:(n + 1) * 256] = 1
    ka[32] = 1
    out["kaug_c"] = ka.astype(bf)
    pos_q = (NO + np.arange(NO)).astype(np.float32)
    out["cq"] = (-slopes[:, None] * pos_q[None, :]).astype(bf)
    pos_k = (np.arange(NV // 128)[None, :] * 128 + np.arange(128)[:, None]).astype(np.float32)
    out["kbias"] = np.ascontiguousarray((slopes[None, :, None] * pos_k[:, None, :]).astype(np.float32))
    valid = np.ones(32, bool)
    valid[NB:] = False
    if r == 0:
        valid[:NB // 2] = False
    gbp = np.full((NBO, 32), NEG, np.float32); A01 = np.zeros((NBO, 32), np.float32); Bt = np.full((NBO, 32), NEG, np.float32)
    for mo in range(NBO):
        m = NB // 2 + mo
        for n in range(32):
            if n < m and valid[n]:
                gbp[mo, n] = 0; A01[mo, n] = 1
            if n == m:
                Bt[mo, n] = 0
    out["gbp"] = gbp.reshape(1, -1); out["A01"] = A01.reshape(1, -1); out["Bt"] = Bt.reshape(1, -1)
    cm = np.zeros((128, 2, 256), np.float32)
    kk = np.arange(128)[:, None]; qq = np.arange(256)[None, :]
    cm[:, 0, :] = (qq >= kk); cm[:, 1, :] = (qq >= kk + 128)
    out["cm"] = ((cm - 1.0) * 30000.0).astype(bf)
    s = np.zeros((65, 64), np.float32); s[64] = 1
    out["sel65"] = s
    return out


def phase_moba(C, S):
    nc = C.nc
    NV, NO, NB = C.NV, C.NO, C.NB
    NKT = NV // 128
    NQ = NO // 512
    NBO = NO // 256
    with ExitStack() as st:
        sb = lambda name, shape, dt: st.enter_context(nc.sbuf_tensor("mb_" + name, shape, dt))
        ident = sb("ident", [128, 128], BF16)
        S.dma("sp", ident[:], C.inp["ident_bf"][:, :], writes=["ident"])
        kaT = [sb("kaT%d" % i, [97, NV], BF16) for i in range(2)]
        qaT = [sb("qaT%d" % i, [97, NO], BF16) for i in range(2)]
        for i in range(2):
            S.dma("sp", kaT[i][64:97, :], C.inp["kaug_c"][:, :], writes=["kaT%d" % i])
        vt = sb("vt", [128, NKT, 8, 65], BF16)
        kbias = sb("kbias", [128, NH, NKT], F32)
        S.dma("sp", kbias[:], C.inp["kbias"][:, :, :], writes=["kbias"])
        gbp = sb("gbp", [128, NBO, 32], F32); A01 = sb("A01", [128, NBO, 32], F32); Bt = sb("Bt", [128, NBO, 32], F32)
        for nm, tl in (("gbp", gbp), ("A01", A01), ("Bt", Bt)):
            S.dma("sp", tl[:].rearrange("p a b -> p (a b)"), C.inp[nm][0:1, :].partition_broadcast(128), writes=[nm])
        cm = sb("cm", [128, 2, 256], BF16)
        S.dma("sp", cm[:], C.inp["cm"][:, :, :], writes=["cm"])
        sel65 = sb("sel65", [65, 64], F32)
        S.dma("sp", sel65[:], C.inp["sel65"][:, :], writes=["sel65"])
        kmf = sb("kmf", [64, NB], F32)
        kmb = sb("kmb", [64, 32], BF16)
        S.memset("pool", kmb[:], 0.0, writes=["kmb"])
        gm = sb("gm", [128, 32], F32); top8 = sb("top8", [128, 8], F32); f1 = sb("f1", [128, 32], F32)
        mbt = [sb("mbt%d" % i, [128, 96], BF16) for i in range(2)]
        for i in range(2):
            S.memset("pool", mbt[i][:], 0.0, writes=["mbt%d" % i])
        pt = [sb("pt%d" % i, [128, 512], BF16) for i in range(4)]
        oT = sb("oT", [65, 512], F32); rd = sb("rd", [64, 512], F32)
        ao = [sb("ao%d" % i, [64, 512], BF16) for i in range(2)]
        psS = [C.ps[0], C.ps[1]]; psO = [C.ps[2], C.ps[3]]; psG = C.ps[4]; psT = C.ps[5]; psD = C.ps[6]
        cnt = {"s": 0, "pt": 0, "o": 0, "ao": 0, "mb": 0}
        def load_v(g):
            S.memset("pool", vt[:, :, :, 64:65], 1.0, writes=["vt"])
            for kt0 in range(NKT):
                S.dma("sp", vt[:, kt0, :, 0:64],
                      C.v[kt0 * 128:(kt0 + 1) * 128, g * 512:(g + 1) * 512].rearrange("p (a d) -> p a d", d=64),
                      writes=["vt"])

        def prep_steps(h):
            hb = h % 2
            steps = []

            def loads():
                S.dma("sp", kaT[hb][0:64, :], C.kT[h * 64:(h + 1) * 64, :], writes=["kaT%d" % hb])
                S.dma("sp", qaT[hb][0:64, :], C.qT[h * 64:(h + 1) * 64, :], writes=["qaT%d" % hb])
                S.dma("sp", qaT[hb][96:97, :], C.inp["cq"][h:h + 1, :], writes=["qaT%d" % hb])
                S.dma("sp", kmf[:], C.kmT[h // 2, (h % 2) * 64:(h % 2) * 64 + 64, :], writes=["kmf"])
                S.copy("act", kmb[:, 0:NB], kmf[:], reads=["kmf"], writes=["kmb"])
            steps.append(loads)
            nq = NO // 128
            st1, st2, st3 = [], [], []
            for qs in range(nq):
                mo = qs // 2
                mi = qs % 2

                def s1(qs=qs, mo=mo, mi=mi):
                    S.mm(psG[:, 0:32], qaT[hb][0:64, qs * 128:(qs + 1) * 128], kmb[:], True, True,
                         reads=["qaT%d" % hb, "kmb"], writes=["psG"])
                    S.tt("dve", gm[:], psG[:, 0:32], gbp[:, mo, :], ALU.add, reads=["psG", "gbp"], writes=["gm"])
                    S.op("dve", lambda e: e.max(out=top8[:], in_=gm[:]), reads=["gm"], writes=["top8"])
                    S.ts("dve", f1[:], gm[:], top8[:, 2:3], ALU.is_ge, -NEG, ALU.mult, reads=["gm", "top8"], writes=["f1"])
                    S.tt("dve", f1[:], f1[:], A01[:, mo, :], ALU.mult, reads=["f1", "A01"], writes=["f1"])
                    S.tt("dve", mbt[mi][:, 64:96], f1[:], Bt[:, mo, :], ALU.add, reads=["f1", "Bt"], writes=["mbt%d" % mi])

                def s2(qs=qs, mi=mi):
                    pTt = psT[:].bitcast(BF16)[0:96, 0:128]
                    S.tr(pTt, mbt[mi][:], ident[:], reads=["mbt%d" % mi, "ident"], writes=["psT"])

                def s3(qs=qs):
                    S.copy("act", qaT[hb][64:96, qs * 128:(qs + 1) * 128], psT[:].bitcast(BF16)[64:96, 0:128],
                           reads=["psT"], writes=["qaT%d" % hb])
                st1.append(s1); st2.append(s2); st3.append(s3)
            for k in range(nq + 2):
                if 0 <= k - 2 < nq: steps.append(st3[k - 2])
                if 0 <= k - 1 < nq: steps.append(st2[k - 1])
                if k < nq: steps.append(st1[k])
            return steps

        def pairs(h, pending):
            hb = h % 2
            hl = h % 8
            for j in range(NQ):
                oi = cnt["o"] % 2; cnt["o"] += 1
                ok = "ps%d" % (2 + oi)
                b0 = (NO + 512 * j) // 256
                nkt = NKT // 2 + 4 * j + 4
                def emitS(kt):
                    si = cnt["s"] % 2; cnt["s"] += 1
                    sk = "ps%d" % si
                    n = kt // 2
                    lk = kaT[hb][0:97, kt * 128:(kt + 1) * 128]
                    if n >= b0:
                        c0 = (n - b0) * 256
                        c1 = 256 - c0
                        S.mm(psS[si][:, c1:c1 + 256], lk, qaT[hb][0:97, j * 512 + c1:j * 512 + c1 + 256],
                             True, True, reads=["kaT%d" % hb, "qaT%d" % hb], writes=[sk])
                        S.mm(psS[si][:, c0:c0 + 256], lk, qaT[hb][0:97, j * 512 + c0:j * 512 + c0 + 256],
                             True, False, reads=["kaT%d" % hb, "qaT%d" % hb], writes=[sk])
                        S.mm(psS[si][:, c0:c0 + 256], ident[:], cm[:, kt % 2, :],
                             False, True, reads=["ident", "cm"], writes=[sk])
                    else:
                        S.mm(psS[si][:, 0:512], lk, qaT[hb][0:97, j * 512:(j + 1) * 512],
                             True, True, reads=["kaT%d" % hb, "qaT%d" % hb], writes=[sk])
                    pi = cnt["pt"] % 4; cnt["pt"] += 1
                    pk = "pt%d" % pi
                    S.act(pt[pi][:], psS[si][:, 0:512], AF.Exp, reads=[sk, "kbias"], writes=[pk], bias=kbias[:, h, kt:kt + 1])
                    return pi
                pis = {0: emitS(0)}
                for kt in range(nkt):
                    if kt + 1 < nkt:
                        pis[kt + 1] = emitS(kt + 1)
                    pi = pis.pop(kt)
                    S.mm(psO[oi][0:65, 0:512], vt[:, kt, hl, :], pt[pi][:], kt == 0, kt == nkt - 1,
                         reads=["vt", "pt%d" % pi], writes=[ok])
                    if pending and kt % 2 == 1:
                        pending.pop(0)()
                S.copy("act", oT[:], psO[oi][0:65, 0:512], reads=[ok], writes=["oT"])
                S.mm(psD[0:64, 0:512], sel65[:], oT[:], True, True, reads=["sel65", "oT"], writes=["psD"])
                S.op("dve", lambda e: e.reciprocal(out=rd[:], in_=psD[0:64, 0:512]), reads=["psD"], writes=["rd"])
                ai = cnt["ao"] % 2; cnt["ao"] += 1
                S.tt("dve", ao[ai][:], oT[0:64, :], rd[:], ALU.mult, reads=["oT", "rd"], writes=["ao%d" % ai])
                S.dma("pool", C.aT[h * 64:(h + 1) * 64, j * 512:(j + 1) * 512], ao[ai][:], reads=["ao%d" % ai])

        for f in prep_steps(0):
            f()
        for h in range(NH):
            if h % 8 == 0:
                load_v(h // 8)
            pending = prep_steps(h + 1) if h + 1 < NH else []
            pairs(h, pending)
            while pending:
                pending.pop(0)()
        S.flush()


def ssd_setup(C):
    nc = C.nc

    def inp(name, shape, dt=F32):
        C.inp[name] = nc.dram_tensor(name, list(shape), dt, kind="ExternalInput").ap()
    inp("tri_f", [128, 128]); inp("ones_f", [128, 128]); inp("trineg_f", [128, 128])
    C.ynT = dram(C, "ynT_s", [2048, C.NO], BF16)


def ssd_consts():
    s = np.arange(128)[:, None]; t = np.arange(128)[None, :]
    return {"tri_f": (s <= t).astype(np.float32), "ones_f": np.ones((128, 128), np.float32),
            "trineg_f": np.where(t >= s, 0.0, NEG).astype(np.float32)}


def phase_ssd(C, S):
    nc = C.nc
    C.ssd_stop = getattr(C, "ssd_stop", 99)
    C.ssd_sub = getattr(C, "ssd_sub", 99)
    NV, NO = C.NV, C.NO
    NCH = NV // 256
    with ExitStack() as st:
        sb = lambda name, shape, dt: st.enter_context(nc.sbuf_tensor("sd_" + name, shape, dt))
        ident = sb("ident", [128, 128], BF16); identf = sb("identf", [128, 128], F32)
        tri = sb("tri", [128, 128], F32); ones = sb("ones", [128, 128], F32); trineg = sb("trineg", [128, 128], F32)
        for tl, nm in ((ident, "ident_bf"), (identf, "ident_f"), (tri, "tri_f"), (ones, "ones_f"), (trineg, "trineg_f")):
            S.dma("sp", tl[:], C.inp[nm][:, :], writes=["consts"])
        dsk = sb("dsk", [128, SH], F32); gn = sb("gn", [128, 2048], F32); pv = sb("pv", [128, 1], F32)
        S.dma("sp", dsk[:], C.inp["d_skip"][0:1, :].partition_broadcast(128), writes=["consts"])
        S.dma("sp", gn[:], C.inp["ssd_norm_g"][0:1, :].partition_broadcast(128), writes=["consts"])
        S.dma("sp", pv[:], C.inp["pv"][0:1, :].partition_broadcast(128), writes=["consts"])
        state = sb("state", [128, 8, 256], F32); stb = sb("stb", [128, 8, 256], BF16)
        S.memset("pool", state[:], 0.0, writes=["state"])
        S.memset("pool", stb[:], 0.0, writes=["stb"])
        xsT = sb("xsT", [128, 16, 256], BF16); BTt = sb("BTt", [128, 8, 256], BF16); CTt = sb("CTt", [128, 8, 256], BF16)
        da = sb("da", [128, 2, 64], F32); zt = sb("zt", [128, 2, 2048], BF16)
        xdt = sb("xdt", [128, 2, 2048], BF16); xdtd = sb("xdtd", [128, 2, 2048], BF16); xtm = sb("xtm", [128, 2, 2048], BF16)
        Btm = sb("Btm", [128, 2, 8, 128], BF16)
        acum = sb("acum", [128, 2, 32], F32); nacum = sb("nacum", [128, 2, 32], F32); eA = sb("eA", [128, 2, 32], F32)
        dend = sb("dend", [128, 2, 32], F32); eTot = sb("eTot", [128, 32], F32); tot = sb("tot", [128, 32], F32)
        Lt = [sb("Lt%d" % i, [128, 384], F32) for i in range(2)]
        Mt = [sb("Mt%d" % i, [128, 384], BF16) for i in range(4)]
        ysb = sb("ysb", [128, 2, 2048], F32); ytmp = sb("ytmp", [128, 2048], F32)
        RA = sb("RA", [128, 3, 4, 128], F32)
        ssg = sb("ssg", [128, 8], F32); ynb = sb("ynb", [128, 2048], BF16); ynT = sb("ynT", [128, 16, 128], BF16)
        ps = C.ps
        for c in range(NCH):
            own = c >= NCH // 2
            t0 = c * 256
            to0 = t0 - NO
            S.dma("sp", xsT[:], C.xsT[:, t0:t0 + 256].rearrange("(c p) t -> p c t", p=128), writes=["xsT"])
            S.dma("sp", BTt[:], C.BT[:, t0:t0 + 256].rearrange("(c p) t -> p c t", p=128), writes=["BTt"])
            S.dma("sp", da[:], C.da[t0:t0 + 256, :].rearrange("(i p) c -> p i c", p=128), writes=["da"])
            if own:
                S.dma("sp", CTt[:], C.CT[:, to0:to0 + 256].rearrange("(c p) t -> p c t", p=128), writes=["CTt"])
                S.dma("sp", zt[:], C.z[to0:to0 + 256, :].rearrange("(i p) c -> p i c", p=128), writes=["zt"])
            S.mm(ps[6][:, 0:32], tri[:], da[:, 0, 32:64], True, True, reads=["consts", "da"], writes=["ps6"])
            S.mm(ps[6][:, 32:64], tri[:], da[:, 1, 32:64], True, False, reads=["consts", "da"], writes=["ps6"])
            S.mm(ps[6][:, 32:64], ones[:], da[:, 0, 32:64], False, True, reads=["consts", "da"], writes=["ps6"])
            S.mm(ps[6][:, 64:96], ones[:], da[:, 0, 32:64], True, False, reads=["consts", "da"], writes=["ps6"])
            S.mm(ps[6][:, 64:96], ones[:], da[:, 1, 32:64], False, True, reads=["consts", "da"], writes=["ps6"])
            S.copy("dve", acum[:].rearrange("p i h -> p (i h)"), ps[6][:, 0:64], reads=["ps6"], writes=["acum"])
            S.copy("dve", tot[:], ps[6][:, 64:96], reads=["ps6"], writes=["tot"])
            S.ts("dve", nacum[:], acum[:], -1.0, ALU.mult, reads=["acum"], writes=["nacum"])
            S.act(eA[:], acum[:], AF.Exp, reads=["acum"], writes=["eA"])
            S.act(eTot[:], tot[:], AF.Exp, reads=["tot"], writes=["eTot"])
            S.tt("dve", dend[:], nacum[:], tot[:].unsqueeze(1).to_broadcast([128, 2, 32]), ALU.add,
                 reads=["nacum", "tot"], writes=["dend"])
            S.act(dend[:], dend[:], AF.Exp, reads=["dend"], writes=["dend"])
            if C.ssd_stop <= 1:
                continue
            for i in range(2):
                pT = ps[7][:].bitcast(BF16)[:, 0:1024].rearrange("p (c t) -> p c t", t=128)
                for half in range(2):
                    for cc in range(8):
                        S.tr(pT[:, cc, :], xsT[:, half * 8 + cc, i * 128:(i + 1) * 128], ident[:],
                             reads=["xsT", "consts"], writes=["ps7"])
                    hs = slice(half * 16, half * 16 + 16)
                    dst = xdt[:, i, half * 1024:(half + 1) * 1024].rearrange("p (h d) -> p h d", d=64)
                    src = pT.rearrange("p c (h d) -> p (c h) d", d=64)
                    S.tt("dve", dst, src, da[:, i, hs].unsqueeze(2).to_broadcast([128, 16, 64]), ALU.mult,
                         reads=["ps7", "da"], writes=["xdt"])
                    if own and C.ssd_sub >= 1:
                        S.copy("act", xtm[:, i, half * 1024:(half + 1) * 1024], pT.rearrange("p c t -> p (c t)"),
                               reads=["ps7"], writes=["xtm"])
                if C.ssd_sub < 2:
                    continue
                S.tt("dve", xdtd[:, i, :].rearrange("p (h d) -> p h d", d=64), xdt[:, i, :].rearrange("p (h d) -> p h d", d=64),
                     dend[:, i, :].unsqueeze(2).to_broadcast([128, 32, 64]), ALU.mult, reads=["xdt", "dend"], writes=["xdtd"])
                if C.ssd_sub < 3:
                    continue
                pT = ps[7][:].bitcast(BF16)[:, 0:1024].rearrange("p (c t) -> p c t", t=128)
                for g in range(8):
                    S.tr(pT[:, g, :], BTt[:, g, i * 128:(i + 1) * 128], ident[:], reads=["BTt", "consts"], writes=["ps7"])
                S.copy("act", Btm[:, i, :, :], pT, reads=["ps7"], writes=["Btm"])
            if C.ssd_stop <= 2:
                continue
            if own:
                for g in range(8):
                    if C.ssd_stop <= 3 and g > 0:
                        continue
                    S.mm(ps[4][:, 0:256], BTt[:, g, 0:128], CTt[:, g, 0:256], True, True, reads=["BTt", "CTt"], writes=["ps4"])
                    S.mm(ps[4][:, 256:384], BTt[:, g, 128:256], CTt[:, g, 128:256], True, True, reads=["BTt", "CTt"], writes=["ps4"])
                    for h4 in range(4):
                        h = g * 4 + h4
                        pL = ps[h4]; lk = "ps%d" % h4
                        if h4 == 0:
                            for q_, (src_i, mat) in enumerate(((0, tri), (0, ones), (1, tri))):
                                S.tt("dve", RA[:, q_, :, :], da[:, src_i, 32 + g * 4:36 + g * 4].unsqueeze(2).to_broadcast([128, 4, 128]),
                                     mat[:].unsqueeze(1).to_broadcast([128, 4, 128]), ALU.mult, reads=["da", "consts"], writes=["RA"])
                        S.mm(pL[:, 0:128], ones[:], RA[:, 0, h4, :], True, False, reads=["RA", "consts"], writes=[lk])
                        S.mm(pL[:, 0:128], identf[:], trineg[:], False, True, reads=["consts"], writes=[lk])
                        S.mm(pL[:, 128:256], ones[:], RA[:, 1, h4, :], True, False, reads=["RA", "consts"], writes=[lk])
                        S.mm(pL[:, 128:256], ones[:], RA[:, 2, h4, :], False, True, reads=["RA", "consts"], writes=[lk])
                        S.mm(pL[:, 256:384], ones[:], RA[:, 1, h4, :], True, False, reads=["RA", "consts"], writes=[lk])
                        S.mm(pL[:, 256:384], ones[:], RA[:, 2, h4, :], False, False, reads=["RA", "consts"], writes=[lk])
                        S.mm(pL[:, 256:384], identf[:], trineg[:], False, True, reads=["consts"], writes=[lk])
                        li = h % 2
                        S.act(Lt[li][:, 0:256], pL[:, 0:256], AF.Exp, reads=[lk, "nacum"], writes=["Lt%d" % li],
                              bias=nacum[:, 0, h:h + 1])
                        S.act(Lt[li][:, 256:384], pL[:, 256:384], AF.Exp, reads=[lk, "nacum"], writes=["Lt%d" % li],
                              bias=nacum[:, 1, h:h + 1])
                        S.tt("dve", Mt[h4][:], Lt[li][:], ps[4][:, 0:384], ALU.mult, reads=["Lt%d" % li, "ps4"],
                             writes=["Mt%d" % h4])
                    if C.ssd_stop <= 4:
                        continue
                    pY = ps[5][:, 0:512].rearrange("p (i c) -> p i c", i=2)
                    for h4 in range(4):
                        h = g * 4 + h4
                        cs = slice(h4 * 64, (h4 + 1) * 64)
                        S.mm(pY[:, 0, cs], Mt[h4][:, 0:128], xdt[:, 0, h * 64:(h + 1) * 64], True, True,
                             reads=["Mt%d" % h4, "xdt"], writes=["ps5"])
                        S.mm(pY[:, 1, cs], Mt[h4][:, 128:256], xdt[:, 0, h * 64:(h + 1) * 64], True, False,
                             reads=["Mt%d" % h4, "xdt"], writes=["ps5"])
                        S.mm(pY[:, 1, cs], Mt[h4][:, 256:384], xdt[:, 1, h * 64:(h + 1) * 64], False, True,
                             reads=["Mt%d" % h4, "xdt"], writes=["ps5"])
                    pO = ps[6][:, 0:512].rearrange("p (i c) -> p i c", i=2)
                    for i in range(2):
                        S.mm(pO[:, i, :], CTt[:, g, i * 128:(i + 1) * 128], stb[:, g, :], True, True,
                             reads=["CTt", "stb"], writes=["ps6"])
                    for i in range(2):
                        yg = ysb[:, i, g * 256:(g + 1) * 256].rearrange("p (h d) -> p h d", d=64)
                        S.tt("dve", yg, pO[:, i, :].rearrange("p (h d) -> p h d", d=64),
                             eA[:, i, g * 4:(g + 1) * 4].unsqueeze(2).to_broadcast([128, 4, 64]), ALU.mult,
                             reads=["ps6", "eA"], writes=["ysb"])
                        S.tt("dve", ysb[:, i, g * 256:(g + 1) * 256], ysb[:, i, g * 256:(g + 1) * 256], pY[:, i, :], ALU.add,
                             reads=["ysb", "ps5"], writes=["ysb"])
            if C.ssd_stop <= 5:
                continue
            for g in range(8):
                for i in range(2):
                    S.mm(ps[5][:, 0:256], Btm[:, i, g, :], xdtd[:, i, g * 256:(g + 1) * 256], i == 0, i == 1,
                         reads=["Btm", "xdtd"], writes=["ps5"])
                sg = state[:, g, :].rearrange("p (h d) -> p h d", d=64)
                S.tt("dve", sg, sg, eTot[:, g * 4:(g + 1) * 4].unsqueeze(2).to_broadcast([128, 4, 64]), ALU.mult,
                     reads=["state", "eTot"], writes=["state"])
                S.tt("dve", state[:, g, :], state[:, g, :], ps[5][:, 0:256], ALU.add, reads=["state", "ps5"], writes=["state"])
            if c == NCH // 2 - 1:
                S.ts("dve", state[:].rearrange("p g c -> p (g c)"), state[:].rearrange("p g c -> p (g c)"), pv[:, 0:1], ALU.mult,
                     reads=["state", "consts"], writes=["state"])
            S.copy("act", stb[:].rearrange("p g c -> p (g c)"), state[:].rearrange("p g c -> p (g c)"), reads=["state"], writes=["stb"])
            if C.ssd_stop <= 6:
                continue
            if own:
                for i in range(2):
                    y = ysb[:, i, :]
                    y3 = y.rearrange("p (h d) -> p h d", d=64)
                    S.tt("dve", ytmp[:].rearrange("p (h d) -> p h d", d=64), xtm[:, i, :].rearrange("p (h d) -> p h d", d=64),
                         dsk[:].unsqueeze(2).to_broadcast([128, 32, 64]), ALU.mult, reads=["xtm", "consts"], writes=["ytmp"])
                    S.tt("dve", y, y, ytmp[:], ALU.add, reads=["ysb", "ytmp"], writes=["ysb"])
                    S.tt("dve", y, y, zt[:, i, :], ALU.mult, reads=["ysb", "zt"], writes=["ysb"])
                    S.tt("dve", ytmp[:], y, y, ALU.mult, reads=["ysb"], writes=["ytmp"])
                    S.op("dve", lambda e: e.tensor_reduce(out=ssg[:], in_=ytmp[:].rearrange("p (g c) -> p g c", c=256),
                                                         axis=AX.X, op=ALU.add), reads=["ytmp"], writes=["ssg"])
                    S.act(ssg[:], ssg[:], AF.Ln, reads=["ssg"], writes=["ssg"], scale=1.0 / 256, bias=EPS)
                    S.act(ssg[:], ssg[:], AF.Exp, reads=["ssg"], writes=["ssg"], scale=-0.5)
                    S.tt("dve", ytmp[:].rearrange("p (g c) -> p g c", c=256), y.rearrange("p (g c) -> p g c", c=256),
                         ssg[:].unsqueeze(2).to_broadcast([128, 8, 256]), ALU.mult, reads=["ysb", "ssg"], writes=["ytmp"])
                    S.tt("dve", ynb[:], ytmp[:], gn[:], ALU.mult, reads=["ytmp", "consts"], writes=["ynb"])
                    for half in range(2):
                        pT = ps[7][:].bitcast(BF16)[:, 0:1024].rearrange("p (c t) -> p c t", t=128)
                        for cc in range(8):
                            S.tr(pT[:, cc, :], ynb[:, (half * 8 + cc) * 128:(half * 8 + cc + 1) * 128], ident[:],
                                 reads=["ynb", "consts"], writes=["ps7"])
                        S.copy("act", ynT[:, half * 8:(half + 1) * 8, :], pT, reads=["ps7"], writes=["ynT"])
                    tok = to0 + i * 128
                    S.dma("pool", C.ynT[:, tok:tok + 128].rearrange("(c p) t -> p c t", p=128), ynT[:], reads=["ynT"])
        S.flush()


def peer_setup(C):
    nc = C.nc

    def inp(name, shape, dt=F32):
        C.inp[name] = nc.dram_tensor(name, list(shape), dt, kind="ExternalInput").ap()
    inp("R1", [128, 32, 512], BF16); inp("R2", [128, 512], BF16)
    C.x2 = dram(C, "x2_s", [C.NO, D], F32)
    C.hT2 = dram(C, "hT2_s", [8, 128, C.NO], BF16)
    C.out = nc.dram_tensor("out", [C.NO, D], F32, kind="ExternalOutput").ap()


def peer_consts():
    bf = ml_dtypes.bfloat16
    R1 = np.zeros((128, 32, 4, 128), np.float32)
    for c in range(32):
        for j in range(4):
            R1[4 * c + j, c, j, :] = 1
    R2 = np.tile(np.eye(128, dtype=np.float32), (1, 4))
    return {"R1": R1.reshape(128, 32, 512).astype(bf), "R2": R2.astype(bf)}


def phase_peer(C, S):
    nc = C.nc
    NO = C.NO
    UT = C.inp["peer_uT"]; VT = C.inp["peer_v"]; WQ = C.inp["w_peer_q"]
    with ExitStack() as st:
        sb = lambda name, shape, dt: st.enter_context(nc.sbuf_tensor("pr_" + name, shape, dt))
        ident = sb("ident", [128, 128], BF16)
        S.dma("sp", ident[:], C.inp["ident_bf"][:, :], writes=["ident"])
        R1 = sb("R1", [128, 32, 512], BF16); R2 = sb("R2", [128, 512], BF16)
        S.dma("sp", R1[:], C.inp["R1"][:, :, :], writes=["R1"])
        S.dma("sp", R2[:], C.inp["R2"][:, :], writes=["R2"])
        stg = sb("stg", [128, 4096], F32)
        stg2 = sb("stg2", [128, 4096], F32)
        stg2_v = stg2[:].rearrange("p (j d) -> p j d", d=1024)
        stg_u = stg[:].rearrange("p (c n) -> p c n", n=512)
        stg_v = stg[:].rearrange("p (j d) -> p j d", d=1024)
        kT = sb("kT", [128, 16, 128], BF16)
        for hh in range(2):
            S.dma("sp", stg_u[:, :, 0:128], C.inp["keys%dT" % (hh + 1)].rearrange("h d k -> d h k"), writes=["stg"])
            S.copy("pool", kT[:].rearrange("p (h two) k -> p h two k", two=2)[:, :, hh, :], stg_u[:, :, 0:128],
                   reads=["stg"], writes=["kT"])
        ub = [sb("ub%d" % i, [128, 8, 512], BF16) for i in range(2)]
        vb = [sb("vb%d" % i, [128, 4, 1024], BF16) for i in range(2)]
        xnT = sb("xnT", [128, 8, 512], BF16)
        shr = sb("shr", [128, 8192], BF16)
        qTr = shr[:].rearrange("p (c t) -> p c t", t=512)
        sb16 = sb("sb16", [128, 16, 128], BF16); sf = sb("sf", [128, 16, 128], F32); swk = sb("swk", [128, 128], F32)
        v16 = sb("v16", [128, 16, 16], F32)
        cand = sb("cand", [128, 8, 256], F32); cwk = stg2[:, 0:2048].rearrange("p (h k) -> p h k", k=256)
        t8 = sb("t8", [128, 8, 8], F32); t8b = sb("t8b", [128, 8, 8], F32)
        thr = sb("thr", [128, 4, 8], F32); nb = sb("nb", [128, 4, 8], F32); zz = sb("zz", [128, 8], F32)
        sT = sb("sT", [128, 4, 16, 128], BF16)
        tau = sb("tau", [128, 4, 8], F32); taub = sb("taub", [128, 8], BF16)
        Eb = [sb("Eb%d" % i, [128, 512], BF16) for i in range(3)]
        Em8 = [sb("Em80", [128, 8, 512], BF16), shr[:, 0:4096].rearrange("p (h e) -> p h e", e=512)]
        gsb = [sb("gsb%d" % i, [128, 4, 512], BF16) for i in range(2)]
        hact = [sb("hact%d" % i, [128, 512], BF16) for i in range(2)]
        hTt = [sb("hTt%d" % i, [128, 4, 128], BF16) for i in range(2)]
        yacc = sb("yacc", [128, 4, 1024], F32)
        ps = C.ps
        cnt = {"e": 0, "u": 0, "it": 0}

        def peer_iter(it, c, s4, ui, vi):
            ts_ = slice(s4 * 128, (s4 + 1) * 128)
            b = it % 2
            pW = ps[4]; wk = "ps4"
            A = []
            for h in range(8):
                def ah(h=h):
                    ei = cnt["e"] % 3; cnt["e"] += 1
                    pE = ps[(2, 3, 1)[ei]]; ek = "ps%d" % (2, 3, 1)[ei]
                    S.mm(pE[:, 0:512], sT[:, s4, 2 * h, :], R1[:, c, :], True, False, reads=["sT", "R1"], writes=[ek])
                    S.mm(pE[:, 0:512], sT[:, s4, 2 * h + 1, :], R2[:], False, True, reads=["sT", "R2"], writes=[ek])
                    S.act(Eb[ei][:], pE[:, 0:512], AF.Exp, reads=[ek, "nb"], writes=["Eb%d" % ei], bias=nb[:, s4, h:h + 1])
                    S.stt("dve", Em8[b][:, h, :], pE[:, 0:512], thr[:, s4, h:h + 1], Eb[ei][:], ALU.is_ge, ALU.mult,
                          reads=[ek, "thr", "Eb%d" % ei], writes=["Em8%d" % b])
                A.append(ah)

            def b1a():
                for h in range(8):
                    S.mm(pW[:, 0:512], ident[:], Em8[b][:, h, :], h == 0, h == 7, reads=["Em8%d" % b, "ident"], writes=[wk])

            def b1():
                pass

            def b2():
                pass

            def b3():
                S.tt("dve", hact[b][:], gsb[c % 2][:, s4, :], pW[:, 0:512], ALU.mult, reads=["gsb%d" % (c % 2), wk],
                     writes=["hact%d" % b])

            def b4():
                pT = ps[0][:].bitcast(BF16)[:, 0:512].rearrange("p (j t) -> p j t", t=128)
                for j in range(4):
                    S.tr(pT[:, j, :], hact[b][:, j * 128:(j + 1) * 128], ident[:], reads=["hact%d" % b, "ident"], writes=["ps0"])

            def b5():
                pT = ps[0][:].bitcast(BF16)[:, 0:512].rearrange("p (j t) -> p j t", t=128)
                S.copy("act", hTt[b][:], pT, reads=["ps0"], writes=["hTt%d" % b])

            def b6():
                for half in range(2):
                    for j in range(4):
                        S.mm(ps[6 + half][:, 0:512], hTt[b][:, j, :], vb[vi][:, j, half * 512:(half + 1) * 512], j == 0, j == 3,
                             reads=["hTt%d" % b, "vb%d" % vi], writes=["ps%d" % (6 + half)])

            def b7():
                for half in range(2):
                    ya = yacc[:, s4, half * 512:(half + 1) * 512]
                    S.tt("dve", ya, ya, ps[6 + half][:, 0:512], ALU.add, reads=["yacc", "ps%d" % (6 + half)], writes=["yacc"])
            return A, [b1a, b1, b2, b3, b4, b5, b6, b7]

        def gelu_chunk(c, ui):
            for s4 in range(4):
                pa = ps[5] if s4 % 2 == 0 else ps[0]; pak = "ps5" if s4 % 2 == 0 else "ps0"
                for kc in range(8):
                    S.mm(pa[:, 0:512], xnT[:, kc, s4 * 128:(s4 + 1) * 128], ub[ui][:, kc, :], kc == 0, kc == 7,
                         reads=["ub%d" % ui, "xnT"], writes=[pak])
                S.act(gsb[c % 2][:, s4, :], pa[:, 0:512], AF.Gelu, reads=[pak], writes=["gsb%d" % (c % 2)])

        prevB = []
        for rd in range(NO // 512):
            S.dma("sp", xnT[:], C.hT2[:, :, rd * 512:(rd + 1) * 512].rearrange("c p t -> p c t"), writes=["xnT"])
            S.memset("pool", yacc[:], 0.0, writes=["yacc"])
            for pc in range(4):
                S.dma("sp", stg_u, WQ[:, pc * 512:(pc + 1) * 512].rearrange("(c p) n -> p c n", p=128), writes=["stg"])
                ui = cnt["u"] % 2; cnt["u"] += 1
                S.copy("pool", ub[ui][:], stg_u, reads=["stg"], writes=["ub%d" % ui])
                for cc in range(4):
                    for kc in range(8):
                        S.mm(ps[0][:, 0:512], ub[ui][:, kc, cc * 128:(cc + 1) * 128], xnT[:, kc, :],
                             kc == 0, kc == 7, reads=["ub%d" % ui, "xnT"], writes=["ps0"])
                    S.copy("act", qTr[:, pc * 4 + cc, :], ps[0][:, 0:512], reads=["ps0"], writes=["Em81"])
            for s4 in range(4):
                ts_ = slice(s4 * 128, (s4 + 1) * 128)
                for g4 in range(4):
                    for cc in range(4):
                        ch = g4 * 4 + cc
                        S.mm(ps[1][:, cc * 128:(cc + 1) * 128], qTr[:, ch, ts_], kT[:, ch, :], True, True,
                             reads=["Em81", "kT"], writes=["ps1"])
                    S.copy("act", sb16[:, g4 * 4:(g4 + 1) * 4, :], ps[1][:, 0:512].rearrange("p (c k) -> p c k", k=128),
                           reads=["ps1"], writes=["sb16"])
                S.copy("dve", sf[:], sb16[:], reads=["sb16"], writes=["sf"])
                for ch in range(16):
                    S.op("dve", lambda e, ch=ch: e.max(out=v16[:, ch, 0:8], in_=sf[:, ch, :]), reads=["sf"], writes=["v16"])
                    S.op("dve", lambda e, ch=ch: e.match_replace(out=swk[:], in_to_replace=v16[:, ch, 0:8], in_values=sf[:, ch, :],
                                                                 imm_value=-1e30), reads=["sf", "v16"], writes=["swk"])
                    S.op("dve", lambda e, ch=ch: e.max(out=v16[:, ch, 8:16], in_=swk[:]), reads=["swk"], writes=["v16"])
                v4 = v16[:].rearrange("p (h two) k -> p h two k", two=2)
                c4 = cand[:].rearrange("p h (a b) -> p h a b", b=16)
                S.tt("dve", c4, v4[:, :, 0, :].unsqueeze(3).to_broadcast([128, 8, 16, 16]),
                     v4[:, :, 1, :].unsqueeze(2).to_broadcast([128, 8, 16, 16]), ALU.add, reads=["v16"], writes=["cand"])
                for h in range(8):
                    S.op("dve", lambda e, h=h: e.max(out=t8[:, h, :], in_=cand[:, h, :]), reads=["cand"], writes=["t8"])
                    S.op("dve", lambda e, h=h: e.match_replace(out=cwk[:, h, :], in_to_replace=t8[:, h, :], in_values=cand[:, h, :],
                                                               imm_value=-1e30), reads=["cand", "t8"], writes=["stg2"])
                    S.op("dve", lambda e, h=h: e.max(out=t8b[:, h, :], in_=cwk[:, h, :]), reads=["stg2"], writes=["t8b"])
                S.copy("dve", thr[:, s4, :], t8b[:, :, 7], reads=["t8b"], writes=["thr"])
                S.tt("dve", cwk, cand[:], t8[:, :, 0:1].to_broadcast([128, 8, 256]), ALU.subtract, reads=["cand", "t8"], writes=["stg2"])
                S.act(cwk, cwk, AF.Exp, reads=["stg2"], writes=["stg2"])
                S.tt("dve", cand[:], cand[:], thr[:, s4, :].unsqueeze(2).to_broadcast([128, 8, 256]), ALU.is_ge,
                     reads=["cand", "thr"], writes=["cand"])
                S.tt("dve", cwk, cwk, cand[:], ALU.mult, reads=["stg2", "cand"], writes=["stg2"])
                S.op("dve", lambda e: e.tensor_reduce(out=zz[:], in_=cwk, axis=AX.X, op=ALU.add), reads=["stg2"], writes=["zz"])
                S.act(zz[:], zz[:], AF.Ln, reads=["zz"], writes=["zz"])
                S.tt("dve", zz[:], zz[:], t8[:, :, 0], ALU.add, reads=["zz", "t8"], writes=["zz"])
                S.ts("dve", nb[:, s4, :], zz[:], -1.0, ALU.mult, reads=["zz"], writes=["nb"])
                for half in range(2):
                    pT = ps[1][:].bitcast(BF16)[:, 0:1024].rearrange("p (c t) -> p c t", t=128)
                    for cc in range(8):
                        S.tr(pT[:, cc, :], sb16[:, half * 8 + cc, :], ident[:], reads=["sb16", "ident"], writes=["ps1"])
                    S.copy("act", sT[:, s4, half * 8:(half + 1) * 8, :], pT, reads=["ps1"], writes=["sT"])
            def load_u(c):
                S.dma("sp", stg_u, UT[:, c * 512:(c + 1) * 512].rearrange("(kc p) n -> p kc n", p=128), writes=["stg"])
                ui_ = cnt["u"] % 2; cnt["u"] += 1
                S.copy("pool", ub[ui_][:], stg_u, reads=["stg"], writes=["ub%d" % ui_])
                return ui_
            ui_next = load_u(0)
            gelu_chunk(0, ui_next)
            for c in range(32):
                ui = ui_next
                if c + 1 < 32:
                    ui_next = load_u(c + 1)
                S.dma("sp", stg2_v, VT[c * 512:(c + 1) * 512, :].rearrange("(j p) d -> p j d", p=128), writes=["stg2"])
                vi = c % 2
                S.copy("pool", vb[vi][:], stg2_v, reads=["stg2"], writes=["vb%d" % vi])
                for s4 in range(4):
                    if s4 == 2 and c + 1 < 32:
                        gelu_chunk(c + 1, ui_next)
                    it = cnt["it"]; cnt["it"] += 1
                    a_steps, b_steps = peer_iter(it, c, s4, ui, vi)
                    order = ["a0", "a1", "b0", "a2", "b1", "b2", "a3", "b3", "a4", "b4", "a5", "b5", "a6", "b6", "a7", "b7"]
                    for o in order:
                        k = int(o[1])
                        if o[0] == "a":
                            a_steps[k]()
                        elif k < len(prevB):
                            prevB[k]()
                    prevB = b_steps
            for f in prevB:
                f()
            prevB = []
            for s4 in range(4):
                tok = rd * 512 + s4 * 128
                S.dma("sp", stg[:, 0:1024], C.x2[tok:tok + 128, :], writes=["stg"])
                S.tt("dve", yacc[:, s4, :], yacc[:, s4, :], stg[:, 0:1024], ALU.add, reads=["yacc", "stg"], writes=["yacc"])
                S.dma("pool", C.out[tok:tok + 128, :], yacc[:, s4, :], reads=["yacc"])
        S.flush()


def phase_outproj(C, S):
    nc = C.nc
    NV, NO = C.NV, C.NO
    with ExitStack() as st:
        sb = lambda name, shape, dt: st.enter_context(nc.sbuf_tensor("op_" + name, shape, dt))
        ident = sb("ident", [128, 128], BF16)
        S.dma("sp", ident[:], C.inp["ident_bf"][:, :], writes=["ident"])
        stg = sb("stg", [128, 4, 1024], F32)
        Wa = sb("Wa", [128, 8, 1024], BF16); Ws = sb("Ws", [128, 16, 1024], BF16); Wo = sb("Wo", [128, 8, 1024], BF16)
        for nm, tl, nch in (("w_attn_o", Wa, 8), ("w_ssd_o", Ws, 16), ("w_out", Wo, 8)):
            for c0 in range(0, nch, 4):
                S.dma("sp", stg[:], C.inp[nm][c0 * 128:(c0 + 4) * 128, :].rearrange("(c p) n -> p c n", p=128), writes=["stg"])
                S.copy("pool", tl[:, c0:c0 + 4, :], stg[:], reads=["stg"], writes=[nm])
        aTt = [sb("aTt%d" % i, [128, 8, 128], BF16) for i in range(2)]
        yTt = [sb("yTt%d" % i, [128, 16, 128], BF16) for i in range(2)]
        ga = [sb("ga%d" % i, [128, 1024], BF16) for i in range(2)]
        gs = [sb("gs%d" % i, [128, 1024], BF16) for i in range(2)]
        xt = [sb("xt%d" % i, [128, 1024], F32) for i in range(2)]
        m1 = sb("m1", [128, 1024], F32); m2 = sb("m2", [128, 1024], F32); mb = sb("mb", [128, 1024], BF16)
        mT = sb("mT", [128, 8, 128], BF16)
        xo = [sb("xo%d" % i, [128, 1024], F32) for i in range(2)]
        ps = C.ps
        for t in range(NO // 128):
            i = t % 2
            tok = t * 128
            S.dma("sp", aTt[i][:], C.aT[:, tok:tok + 128].rearrange("(c p) t -> p c t", p=128), writes=["aTt%d" % i])
            S.dma("sp", yTt[i][:], C.ynT[:, tok:tok + 128].rearrange("(c p) t -> p c t", p=128), writes=["yTt%d" % i])
            S.dma("sp", ga[i][:], C.sga[tok:tok + 128, :], writes=["ga%d" % i])
            S.dma("sp", gs[i][:], C.sgs[tok:tok + 128, :], writes=["gs%d" % i])
            S.dma("sp", xt[i][:], C.inp["xv"][NO + tok:NO + tok + 128, :], writes=["xt%d" % i])
            for half in range(2):
                hs = slice(half * 512, (half + 1) * 512)
                for c in range(8):
                    S.mm(ps[half][:, 0:512], aTt[i][:, c, :], Wa[:, c, hs], c == 0, c == 7,
                         reads=["aTt%d" % i, "w_attn_o"], writes=["ps%d" % half])
                for c in range(16):
                    S.mm(ps[2 + half][:, 0:512], yTt[i][:, c, :], Ws[:, c, hs], c == 0, c == 15,
                         reads=["yTt%d" % i, "w_ssd_o"], writes=["ps%d" % (2 + half)])
                S.tt("dve", m1[:, hs], ps[half][:, 0:512], ga[i][:, hs], ALU.mult, reads=["ps%d" % half, "ga%d" % i], writes=["m1"])
                S.tt("dve", m2[:, hs], ps[2 + half][:, 0:512], gs[i][:, hs], ALU.mult, reads=["ps%d" % (2 + half), "gs%d" % i], writes=["m2"])
            S.tt("dve", mb[:], m1[:], m2[:], ALU.add, reads=["m1", "m2"], writes=["mb"])
            pT = ps[4][:].bitcast(BF16)[:, 0:1024].rearrange("p (c t) -> p c t", t=128)
            for c in range(8):
                S.tr(pT[:, c, :], mb[:, c * 128:(c + 1) * 128], ident[:], reads=["mb", "ident"], writes=["ps4"])
            S.copy("act", mT[:], pT, reads=["ps4"], writes=["mT"])
            for half in range(2):
                hs = slice(half * 512, (half + 1) * 512)
                for c in range(8):
                    S.mm(ps[5 + half][:, 0:512], mT[:, c, :], Wo[:, c, hs], c == 0, c == 7,
                         reads=["mT", "w_out"], writes=["ps%d" % (5 + half)])
                S.tt("dve", xo[i][:, hs], ps[5 + half][:, 0:512], xt[i][:, hs], ALU.add,
                     reads=["ps%d" % (5 + half), "xt%d" % i], writes=["xo%d" % i])
            S.dma("pool", C.x2[tok:tok + 128, :], xo[i][:], reads=["xo%d" % i])
        S.flush()


def build_all(nc, NV, st, debug=()):
    C = setup(nc, NV, debug)
    moba_setup(C); ssd_setup(C); peer_setup(C)
    S = Sched(nc, st)
    C.ps = [st.enter_context(nc.psum_tensor("ps%d" % i, [128, 512], F32)) for i in range(8)]
    phase_norm(C, S, C.inp["xv"], "norm1_g", C.hT, NV, "n1")
    phase_inproj(C, S)
    phase_moba(C, S)
    phase_ssd(C, S)
    phase_outproj(C, S)
    phase_norm(C, S, C.x2, "norm2_g", C.hT2, C.NO, "n2")
    phase_peer(C, S)
    return C, S


def make_inputs(inputs, NV, b, r, full_seq):
    bf = ml_dtypes.bfloat16
    NO = NV // 2
    f32 = lambda a: np.ascontiguousarray(np.asarray(a, dtype=np.float32))
    x = np.asarray(inputs["x"])
    ins = {}
    xv = np.zeros((NV, D), np.float32)
    if r == 0:
        xv[NO:] = x[b, 0:NO]
    else:
        xv[:] = x[b, 0:NV]
    ins["xv"] = xv
    ins["pv"] = np.full((1, 1), float(r), np.float32)
    ins["norm1_g"] = f32(inputs["norm1_g"][0:1]); ins["w_in"] = f32(inputs["w_in"][0])
    ins["q_norm_g"] = f32(inputs["q_norm_g"][0:1]); ins["k_norm_g"] = f32(inputs["k_norm_g"][0:1])
    ins["conv_wT"] = f32(np.asarray(inputs["conv_w"][0]).T); ins["conv_b"] = f32(np.asarray(inputs["conv_b"][0]).reshape(4096, 1))
    ins["dt_bias"] = f32(inputs["dt_bias"][0:1]); ins["a_log"] = f32(inputs["a_log"][0:1]); ins["d_skip"] = f32(inputs["d_skip"][0:1])
    ins["ssd_norm_g"] = f32(inputs["ssd_norm_g"][0:1]); ins["w_attn_o"] = f32(inputs["w_attn_o"][0])
    ins["w_ssd_o"] = f32(inputs["w_ssd_o"][0]); ins["w_out"] = f32(inputs["w_out"][0]); ins["norm2_g"] = f32(inputs["norm2_g"][0:1])
    ins["w_peer_q"] = f32(inputs["w_peer_q"][0])
    ins["keys1T"] = f32(np.asarray(inputs["peer_keys1"][0]).transpose(0, 2, 1))
    ins["keys2T"] = f32(np.asarray(inputs["peer_keys2"][0]).transpose(0, 2, 1))
    ins["peer_uT"] = f32(np.asarray(inputs["peer_u"][0]).T); ins["peer_v"] = f32(inputs["peer_v"][0])
    ins["ident_bf"] = np.eye(128).astype(bf); ins["ident_f"] = np.eye(128, dtype=np.float32)
    ins.update(moba_consts(NV, r)); ins.update(ssd_consts()); ins.update(peer_consts())
    return ins


NV_FULL = 8192


def kernel(**inputs):
    from concourse.bass_utils import run_bass_kernel_spmd
    nc = bass.Bass("TRN2", target_bir_lowering=False)
    with ExitStack() as st:
        C, S = build_all(nc, NV_FULL, st)
    x = np.asarray(inputs["x"])
    B = x.shape[0]
    in_maps = []
    for b in range(B):
        for r in range(2):
            in_maps.append(make_inputs(inputs, NV_FULL, b, r, NV_FULL))
    res = run_bass_kernel_spmd(nc, in_maps, core_ids=list(range(len(in_maps)))).results
    NO = NV_FULL // 2
    out = np.empty((B, NV_FULL, D), np.float32)
    for b in range(B):
        for r in range(2):
            out[b, r * NO:(r + 1) * NO] = np.asarray(res[b * 2 + r]["out"], dtype=np.float32)
    return out
```

```python
from contextlib import ExitStack
import ml_dtypes
import numpy as np
import concourse.bass as bass
import concourse.mybir as mybir

F32 = mybir.dt.float32
BF16 = mybir.dt.bfloat16
AF = mybir.ActivationFunctionType
ALU = mybir.AluOpType
AX = mybir.AxisListType

SAME_ENGINE_SYNC = True
N_DMA_SEMS = 32


class Sched:
    ENG = ("sp", "act", "dve", "pool", "pe")

    def __init__(self, nc, stack):
        self.nc = nc
        self.ops = []
        self.esem = {e: stack.enter_context(nc.semaphore("s_" + e)) for e in self.ENG}
        self.ecnt = {e: 0 for e in self.ENG}
        self.dsem = [stack.enter_context(nc.semaphore("d%d" % i)) for i in range(N_DMA_SEMS)]
        self.dcnt = [0] * N_DMA_SEMS
        self.downer = [None] * N_DMA_SEMS
        self.dnext = 0
        self.last_w = {}
        self.readers = {}
        self.waited = {e: {} for e in self.ENG}
        self.nblocks = 0
        self.nops = 0

    def _need(self, eng, ev, waits):
        if ev is None:
            return
        sem, val, src_eng, is_dma = ev
        if (not is_dma) and src_eng == eng and (eng == "pe" or not SAME_ENGINE_SYNC):
            return
        key = id(sem)
        if self.waited[eng].get(key, 0) >= val:
            return
        cur = waits.get(key)
        if cur is None or cur[1] < val:
            waits[key] = (sem, val)

    def op(self, eng, fn, reads=(), writes=(), dma=False):
        writes = list(writes) + [k for k in reads if isinstance(k, str) and k.startswith("ps") and k not in writes]
        waits = {}
        for k in reads:
            self._need(eng, self.last_w.get(k), waits)
        for k in writes:
            self._need(eng, self.last_w.get(k), waits)
            for ev in self.readers.get(k, ()):
                self._need(eng, ev, waits)
        if dma:
            half = N_DMA_SEMS // 2
            base = 0 if eng == "sp" else half
            self.dnx = getattr(self, "dnx", {})
            i = base + self.dnx.get(eng, 0)
            self.dnx[eng] = (self.dnx.get(eng, 0) + 1) % half
            if self.dcnt[i] > 0:
                self._need(eng, (self.dsem[i], 16 * self.dcnt[i], self.downer[i], True), waits)
            self.dcnt[i] += 1
            self.downer[i] = eng
            ev = (self.dsem[i], 16 * self.dcnt[i], eng, True)
            inc = (self.dsem[i], 16)
        else:
            self.ecnt[eng] += 1
            ev = (self.esem[eng], self.ecnt[eng], eng, False)
            inc = (self.esem[eng], 1)
        for (sem, val) in waits.values():
            self.waited[eng][id(sem)] = val
        for k in reads:
            self.readers.setdefault(k, []).append(ev)
        for k in writes:
            self.last_w[k] = ev
            self.readers[k] = []
        self.ops.append((eng, fn, list(waits.values()), inc))
        self.nops += 1

    def flush(self):
        fin = {}
        for i in range(N_DMA_SEMS):
            if self.dcnt[i] > 0:
                e = self.downer[i]
                if self.waited[e].get(id(self.dsem[i]), 0) < 16 * self.dcnt[i]:
                    fin.setdefault(e, []).append((self.dsem[i], 16 * self.dcnt[i]))
                    self.waited[e][id(self.dsem[i])] = 16 * self.dcnt[i]
        ops = self.ops
        self.ops = []
        if not ops and not fin:
            return
        nc = self.nc
        with nc.Block() as block:
            deco = {"sp": block.sync, "act": block.scalar, "dve": block.vector,
                    "pool": block.gpsimd, "pe": block.tensor}
            for e in self.ENG:
                mine = [o for o in ops if o[0] == e]
                tail = fin.get(e, [])
                if not mine and not tail:
                    continue

                def body(engine, mine=mine, tail=tail):
                    for (_, fn, waits, inc) in mine:
                        for (sem, val) in waits:
                            engine.wait_ge(sem, val)
                        ins = fn(engine)
                        ins.then_inc(inc[0], inc[1])
                    for (sem, val) in tail:
                        engine.wait_ge(sem, val)

                deco[e](body)
        self.nblocks += 1
        self.last_w = {}
        self.readers = {}

    def dma(self, eng, out, in_, reads=(), writes=(), **kw):
        self.op(eng, lambda e: e.dma_start(out=out, in_=in_, **kw), reads, writes, dma=True)

    def mm(self, out, lhsT, rhs, start, stop, reads=(), writes=()):
        self.op("pe", lambda e: e.matmul(out, lhsT, rhs, start=start, stop=stop), reads, writes)

    def tr(self, out, in_, ident, reads=(), writes=()):
        self.op("pe", lambda e: e.transpose(out, in_, ident), reads, writes)

    def act(self, out, in_, func, reads=(), writes=(), **kw):
        self.op("act", lambda e: e.activation(out=out, in_=in_, func=func, **kw), reads, writes)

    def tt(self, eng, out, in0, in1, op, reads=(), writes=()):
        self.op(eng, lambda e: e.tensor_tensor(out=out, in0=in0, in1=in1, op=op), reads, writes)

    def ts(self, eng, out, in0, s1, op0, s2=None, op1=None, reads=(), writes=(), **kw):
        if op1 is None:
            if op0 == ALU.pow:
                self.op(eng, lambda e: e.tensor_scalar(out=out, in0=in0, scalar1=0.0, scalar2=s1, op0=ALU.add, op1=ALU.pow, **kw),
                        reads, writes)
            else:
                self.op(eng, lambda e: e.tensor_scalar(out=out, in0=in0, scalar1=s1, scalar2=None, op0=op0, **kw),
                        reads, writes)
        else:
            self.op(eng, lambda e: e.tensor_scalar(out=out, in0=in0, scalar1=s1, scalar2=s2, op0=op0, op1=op1, **kw),
                    reads, writes)

    def stt(self, eng, out, in0, scalar, in1, op0, op1, reads=(), writes=()):
        self.op(eng, lambda e: e.scalar_tensor_tensor(out=out, in0=in0, scalar=scalar, in1=in1, op0=op0, op1=op1),
                reads, writes)

    def copy(self, eng, out, in_, reads=(), writes=()):
        if eng == "act":
            self.op(eng, lambda e: e.activation(out=out, in_=in_, func=AF.Copy), reads, writes)
        else:
            self.op(eng, lambda e: e.tensor_copy(out=out, in_=in_), reads, writes)

    def memset(self, eng, ap, val, writes=()):
        self.op(eng, lambda e: e.memset(ap, val), (), writes)


D = 1024
NH = 16
HD = 64
SH = 32
SP = 64
SG = 8
SN = 128
EPS = 1e-6
NEG = -30000.0
import os
PEER_DUMMY = int(os.environ.get('PEER_DUMMY', '0')) if 'PEER_DUMMY' in os.environ else 0
C_Q, C_K, C_V, C_Z, C_X, C_B, C_C, C_DT, C_GA, C_GS = 0, 1024, 2048, 3072, 5120, 7168, 8192, 9216, 9248, 10272
IN_COLS = 11296


class Ctx:
    pass


def dram(C, name, shape, dt):
    kind = "ExternalOutput" if name in C.debug else "Internal"
    return C.nc.dram_tensor(name, list(shape), dt, kind=kind).ap()


def setup(nc, NV, debug=()):
    C = Ctx()
    C.nc = nc
    C.NV = NV
    C.NO = NV // 2
    C.debug = set(debug)
    C.inp = {}

    def inp(name, shape, dt=F32):
        C.inp[name] = nc.dram_tensor(name, list(shape), dt, kind="ExternalInput").ap()
        return C.inp[name]
    NV_, NO = NV, C.NO
    NB = NV // 256
    C.NB = NB
    inp("xv", [NV, D])
    inp("norm1_g", [1, D]); inp("w_in", [D, IN_COLS]); inp("q_norm_g", [1, HD]); inp("k_norm_g", [1, HD])
    inp("conv_wT", [4096, 4]); inp("conv_b", [4096, 1]); inp("dt_bias", [1, SH]); inp("a_log", [1, SH])
    inp("d_skip", [1, SH]); inp("ssd_norm_g", [1, 2048]); inp("w_attn_o", [D, D]); inp("w_ssd_o", [2048, D])
    inp("w_out", [D, D]); inp("norm2_g", [1, D]); inp("w_peer_q", [D, 2048])
    inp("keys1T", [8, 128, 128]); inp("keys2T", [8, 128, 128])
    inp("peer_uT", [D, 16384]); inp("peer_v", [16384, D])
    inp("ident_bf", [128, 128], BF16); inp("ident_f", [128, 128], F32)
    inp("pv", [1, 1])
    C.hT = dram(C, "hT_s", [NV // 512, 128, 8, 512], BF16)
    C.qT = dram(C, "qT_s", [D, NO], BF16)
    C.kT = dram(C, "kT_s", [D, NV], BF16)
    C.kmT = dram(C, "kmT_s", [8, 128, NB], F32)
    C.v = dram(C, "v_s", [NV, D], BF16)
    C.z = dram(C, "z_s", [NO, 2048], BF16)
    C.xsT = dram(C, "xsT_s", [2048, NV], BF16)
    C.BT = dram(C, "BT_s", [1024, NV], BF16)
    C.CT = dram(C, "CT_s", [1024, NO], BF16)
    C.da = dram(C, "da_s", [NV, 64], F32)
    C.sga = dram(C, "sga_s", [NO, D], BF16)
    C.sgs = dram(C, "sgs_s", [NO, D], BF16)
    return C


def phase_norm(C, S, xin, gname, hT, ntok, tag):
    nc = C.nc
    with ExitStack() as st:
        sb = lambda name, shape, dt: st.enter_context(nc.sbuf_tensor(tag + name, shape, dt))
        ident = sb("ident", [128, 128], BF16)
        gT = sb("gT", [128, 8], F32)
        S.dma("sp", ident[:], C.inp["ident_bf"][:, :], writes=["ident"])
        S.dma("sp", gT[:], C.inp[gname][0, :].rearrange("(c p) -> p c", p=128), writes=["gT"],
              allow_slow_non_contiguous=True)
        xt = [sb("xt%d" % i, [128, D], F32) for i in range(2)]
        junk = sb("junk", [128, D], F32)
        ss = [sb("ss%d" % i, [128, 1], F32) for i in range(2)]
        xb = [sb("xb%d" % i, [128, D], BF16) for i in range(2)]
        ho = [sb("ho%d" % i, [128, 8, 128], BF16) for i in range(2)]
        for t in range(ntok // 128):
            i = t % 2
            pT = C.ps[t % 2][:].bitcast(BF16)[:, 0:1024].rearrange("p (c t) -> p c t", t=128)
            pk = "ps%d" % (t % 2)
            S.dma("sp", xt[i][:], xin[t * 128:(t + 1) * 128, :], writes=["xt%d" % i])
            S.act(junk[:], xt[i][:], AF.Square, reads=["xt%d" % i], writes=["junk", "ss%d" % i], accum_out=ss[i][:])
            S.act(ss[i][:], ss[i][:], AF.Ln, reads=["ss%d" % i], writes=["ss%d" % i], scale=1.0 / D, bias=EPS)
            S.act(ss[i][:], ss[i][:], AF.Exp, reads=["ss%d" % i], writes=["ss%d" % i], scale=-0.5)
            S.act(xb[i][:], xt[i][:], AF.Copy, reads=["xt%d" % i, "ss%d" % i], writes=["xb%d" % i], scale=ss[i][:])
            for c in range(8):
                S.tr(pT[:, c, :], xb[i][:, c * 128:(c + 1) * 128], ident[:], reads=["xb%d" % i, "ident"], writes=[pk])
            S.tt("dve", ho[i][:], pT, gT[:].unsqueeze(2).to_broadcast([128, 8, 128]), ALU.mult,
                 reads=[pk, "gT"], writes=["ho%d" % i])
            S.dma("pool", hT[t // 4, :, :, (t % 4) * 128:(t % 4 + 1) * 128], ho[i][:], reads=["ho%d" % i])
        S.flush()


def phase_inproj(C, S):
    nc = C.nc
    NV, NO = C.NV, C.NO
    NT = NV // 512
    NTO = NO // 512
    W = C.inp["w_in"]
    with ExitStack() as st:
        sb = lambda name, shape, dt: st.enter_context(nc.sbuf_tensor("ip_" + name, shape, dt))
        ident = sb("ident", [128, 128], BF16)
        S.dma("sp", ident[:], C.inp["ident_bf"][:, :], writes=["ident"])
        gq = sb("gq", [128, HD], F32); gk = sb("gk", [128, HD], F32)
        S.dma("sp", gq[:], C.inp["q_norm_g"][0:1, :].partition_broadcast(128), writes=["gq"])
        S.dma("sp", gk[:], C.inp["k_norm_g"][0:1, :].partition_broadcast(128), writes=["gk"])
        S.ts("dve", gq[:], gq[:], HD ** -0.5, ALU.mult, reads=["gq"], writes=["gq"])
        dtb = sb("dtb", [128, SH], F32); An = sb("An", [128, SH], F32)
        S.dma("sp", dtb[:], C.inp["dt_bias"][0:1, :].partition_broadcast(128), writes=["dtb"])
        S.dma("sp", An[:], C.inp["a_log"][0:1, :].partition_broadcast(128), writes=["An"])
        S.act(An[:], An[:], AF.Exp, reads=["An"], writes=["An"])
        S.ts("dve", An[:], An[:], -1.0, ALU.mult, reads=["An"], writes=["An"])
        cw = sb("cw", [128, 32, 4], F32); cb = sb("cb", [128, 32], F32)
        S.dma("sp", cw[:], C.inp["conv_wT"].rearrange("(c p) k -> p c k", p=128), writes=["cw"])
        S.dma("sp", cb[:], C.inp["conv_b"].rearrange("(c p) o -> p (c o)", p=128), writes=["cb"],
              allow_slow_non_contiguous=True)
        kmT = sb("kmT", [128, 8, C.NB], F32)
        S.memset("pool", kmT[:], 0.0, writes=["kmT"])
        wf = [sb("wf%d" % i, [128, 8, 512], F32) for i in range(2)]
        wb = [sb("wb%d" % i, [128, 8, 512], BF16) for i in range(2)]
        hb = [sb("hb%d" % i, [128, 8, 512], BF16) for i in range(3)]
        ev = [sb("ev%d" % i, [128, 512], F32) for i in range(2)]
        sq = sb("sq", [128, 512], F32)
        ssq = sb("ssq", [128, 8], F32)
        ob = [sb("ob%d" % i, [128, 512], BF16) for i in range(2)]
        tb = [sb("tb%d" % i, [128, 4, 128], BF16) for i in range(2)]
        kr = sb("kr", [128, 4], F32)
        cbuf = [sb("cbuf%d" % i, [128, 515], F32) for i in range(4)]
        caccs = [sb("cacc%d" % i, [128, 512], F32) for i in range(2)]
        dab = [sb("dab%d" % i, [128, 64], F32) for i in range(2)]

        blocks = []
        for j in range(2): blocks.append(("q", C_Q + 512 * j, 512, NT - NTO, j))
        for j in range(2): blocks.append(("k", C_K + 512 * j, 512, 0, j))
        for j in range(2): blocks.append(("v", C_V + 512 * j, 512, 0, j))
        for j in range(4): blocks.append(("z", C_Z + 512 * j, 512, NT - NTO, j))
        for j in range(4): blocks.append(("xs", C_X + 512 * j, 512, 0, j))
        for j in range(2): blocks.append(("B", C_B + 512 * j, 512, 0, j))
        for j in range(2): blocks.append(("C", C_C + 512 * j, 512, NT - NTO - 1, j))
        blocks.append(("dt", C_DT, 32, 0, 0))
        for j in range(2): blocks.append(("ga", C_GA + 512 * j, 512, NT - NTO, j))
        for j in range(2): blocks.append(("gs", C_GS + 512 * j, 512, NT - NTO, j))

        cnt = {"h": 0, "ps": 0, "ev": 0, "ob": 0, "tb": 0, "da": 0, "ca": 0}
        def load_w(bi):
            kind_, c0_, ncol_, _, _ = blocks[bi]
            wi_ = bi % 2
            S.dma("sp", wf[wi_][:, :, 0:ncol_], W[:, c0_:c0_ + ncol_].rearrange("(c p) n -> p c n", p=128),
                  writes=["wf%d" % wi_])
            S.copy("pool", wb[wi_][:, :, 0:ncol_], wf[wi_][:, :, 0:ncol_], reads=["wf%d" % wi_], writes=["wb%d" % wi_])
        load_w(0)
        deferred = []
        for bi, (kind, c0, ncol, t0, j) in enumerate(blocks):
            wi = bi % 2
            if bi + 1 < len(blocks):
                load_w(bi + 1)
            if kind in ("xs", "B", "C"):
                for s4 in range(4):
                    S.memset("pool", cbuf[s4][:, 0:3], 0.0, writes=["cbuf%d" % s4])
            for t in range(t0, NT):
                hi = cnt["h"] % 3; cnt["h"] += 1
                S.dma("sp", hb[hi][:], C.hT[t, :, :, :], writes=["hb%d" % hi])
                to = t - (NT - NTO)
                for s4 in range(4):
                    pi = cnt["ps"] % 4; cnt["ps"] += 1
                    ps = C.ps[pi]; pk = "ps%d" % pi
                    tok = t * 512 + s4 * 128
                    if kind in ("xs", "B", "C"):
                        for c in range(8):
                            S.mm(ps[:, 0:512], wb[wi][:, c, s4 * 128:(s4 + 1) * 128], hb[hi][:, c, :], c == 0, c == 7,
                                 reads=["wb%d" % wi, "hb%d" % hi], writes=[pk])
                        ck = "cbuf%d" % s4
                        cai = cnt["ca"] % 2; cnt["ca"] += 1
                        cacc = caccs[cai]; cak = "cacc%d" % cai
                        S.copy("act", cbuf[s4][:, 3:515], ps[:, 0:512], reads=[pk], writes=[ck])
                        chn = (c0 - C_X) // 128 + s4
                        S.ts("dve", cacc[:], cbuf[s4][:, 0:512], cw[:, chn, 0:1], ALU.mult, cb[:, chn:chn + 1], ALU.add,
                             reads=[ck, "cw", "cb"], writes=[cak])
                        for k in range(1, 4):
                            S.stt("dve", cacc[:], cbuf[s4][:, k:k + 512], cw[:, chn, k:k + 1], cacc[:], ALU.mult, ALU.add,
                                  reads=[ck, cak], writes=[cak])
                        S.copy("pool", cbuf[s4][:, 0:3], cbuf[s4][:, 512:515], reads=[ck], writes=[ck])
                        oi = cnt["ob"] % 2; cnt["ob"] += 1
                        S.act(ob[oi][:], cacc[:], AF.Silu, reads=[cak], writes=["ob%d" % oi])
                        r0 = (c0 - {"xs": C_X, "B": C_B, "C": C_C}[kind]) + s4 * 128
                        if kind == "xs":
                            S.dma("pool", C.xsT[r0:r0 + 128, t * 512:(t + 1) * 512], ob[oi][:], reads=["ob%d" % oi])
                        elif kind == "B":
                            S.dma("pool", C.BT[r0:r0 + 128, t * 512:(t + 1) * 512], ob[oi][:], reads=["ob%d" % oi])
                        elif to >= 0:
                            S.dma("pool", C.CT[r0:r0 + 128, to * 512:(to + 1) * 512], ob[oi][:], reads=["ob%d" % oi])
                        continue
                    for c in range(8):
                        S.mm(ps[:, 0:ncol], hb[hi][:, c, s4 * 128:(s4 + 1) * 128], wb[wi][:, c, 0:ncol], c == 0, c == 7,
                             reads=["wb%d" % wi, "hb%d" % hi], writes=[pk])
                    while deferred:
                        deferred.pop(0)()
                    if kind in ("q", "k"):
                        ei = cnt["ev"] % 2; cnt["ev"] += 1
                        ek = "ev%d" % ei
                        S.copy("act", ev[ei][:], ps[:, 0:512], reads=[pk], writes=[ek])
                        S.tt("dve", sq[:], ev[ei][:], ev[ei][:], ALU.mult, reads=[ek], writes=["sq"])
                        S.op("dve", lambda e: e.tensor_reduce(out=ssq[:], in_=sq[:].rearrange("p (a b) -> p a b", b=HD),
                                                             axis=AX.X, op=ALU.add), reads=["sq"], writes=["ssq"])
                        S.act(ssq[:], ssq[:], AF.Ln, reads=["ssq"], writes=["ssq"], scale=1.0 / HD, bias=EPS)
                        S.act(ssq[:], ssq[:], AF.Exp, reads=["ssq"], writes=["ssq"], scale=-0.5)
                        e3 = ev[ei][:].rearrange("p (a b) -> p a b", b=HD)
                        S.tt("dve", e3, e3, ssq[:].unsqueeze(2).to_broadcast([128, 8, HD]), ALU.mult,
                             reads=[ek, "ssq"], writes=[ek])
                        oi = cnt["ob"] % 2; cnt["ob"] += 1
                        g_ = gq if kind == "q" else gk
                        S.tt("dve", ob[oi][:].rearrange("p (a b) -> p a b", b=HD), e3,
                             g_[:].unsqueeze(1).to_broadcast([128, 8, HD]), ALU.mult,
                             reads=[ek, "gq", "gk"], writes=["ob%d" % oi])
                        def fin(kind=kind, oi=oi, j=j, to=to, s4=s4, tok=tok):
                            pti = 4 + cnt["tb"] % 2
                            ti = cnt["tb"] % 2; cnt["tb"] += 1
                            pT = C.ps[pti][:].bitcast(BF16)[:, 0:512].rearrange("p (c t) -> p c t", t=128)
                            for c in range(4):
                                S.tr(pT[:, c, :], ob[oi][:, c * 128:(c + 1) * 128], ident[:], reads=["ob%d" % oi, "ident"],
                                     writes=["ps%d" % pti])
                            S.copy("act", tb[ti][:], pT, reads=["ps%d" % pti], writes=["tb%d" % ti])
                            rows = slice(j * 512, (j + 1) * 512)
                            if kind == "q":
                                S.dma("pool", C.qT[rows, to * 512 + s4 * 128: to * 512 + (s4 + 1) * 128].rearrange("(c p) t -> p c t", p=128),
                                      tb[ti][:], reads=["tb%d" % ti])
                            else:
                                S.dma("pool", C.kT[rows, tok:tok + 128].rearrange("(c p) t -> p c t", p=128),
                                      tb[ti][:], reads=["tb%d" % ti])
                                S.op("dve", lambda e, ti=ti: e.tensor_reduce(out=kr[:], in_=tb[ti][:], axis=AX.X, op=ALU.add),
                                     reads=["tb%d" % ti], writes=["kr"])
                                blk = tok // 256
                                S.stt("dve", kmT[:, j * 4:(j + 1) * 4, blk], kr[:], 1.0 / 256, kmT[:, j * 4:(j + 1) * 4, blk],
                                      ALU.mult, ALU.add, reads=["kr", "kmT"], writes=["kmT"])
                        deferred.append(fin)
                    elif kind == "dt":
                        di = cnt["da"] % 2; cnt["da"] += 1
                        dk = "dab%d" % di
                        S.tt("dve", dab[di][:, 0:32], ps[:, 0:32], dtb[:], ALU.add, reads=[pk, "dtb"], writes=[dk])
                        S.act(dab[di][:, 0:32], dab[di][:, 0:32], AF.Exp, reads=[dk], writes=[dk])
                        S.act(dab[di][:, 0:32], dab[di][:, 0:32], AF.Ln, reads=[dk], writes=[dk], bias=1.0)
                        S.tt("dve", dab[di][:, 32:64], dab[di][:, 0:32], An[:], ALU.mult, reads=[dk, "An"], writes=[dk])
                        S.dma("pool", C.da[tok:tok + 128, :], dab[di][:], reads=[dk])
                    else:
                        oi = cnt["ob"] % 2; cnt["ob"] += 1
                        fn = {"v": AF.Copy, "z": AF.Silu, "ga": AF.Sigmoid, "gs": AF.Sigmoid}[kind]
                        S.act(ob[oi][:], ps[:, 0:512], fn, reads=[pk], writes=["ob%d" % oi])
                        if kind == "v":
                            dst = C.v[tok:tok + 128, j * 512:(j + 1) * 512]
                        else:
                            otok = to * 512 + s4 * 128
                            dst = {"z": C.z, "ga": C.sga, "gs": C.sgs}[kind][otok:otok + 128, j * 512:(j + 1) * 512]
                        S.dma("pool", dst, ob[oi][:], reads=["ob%d" % oi])
        while deferred:
            deferred.pop(0)()
        S.dma("pool", C.kmT.rearrange("c p n -> p c n"), kmT[:], reads=["kmT"])
        S.flush()


def moba_setup(C):
    nc = C.nc
    NV, NO, NB = C.NV, C.NO, C.NB

    def inp(name, shape, dt=F32):
        C.inp[name] = nc.dram_tensor(name, list(shape), dt, kind="ExternalInput").ap()
    inp("kaug_c", [33, NV], BF16)
    inp("cq", [NH, NO], BF16)
    inp("kbias", [128, NH, NV // 128])
    NBO = NO // 256
    inp("gbp", [1, NBO * 32]); inp("A01", [1, NBO * 32]); inp("Bt", [1, NBO * 32])
    inp("cm", [128, 2, 256], BF16)
    inp("sel65", [65, 64])
    C.aT = dram(C, "aT_s", [D, NO], BF16)


def moba_consts(NV, r):
    bf = ml_dtypes.bfloat16
    NO = NV // 2
    NB = NV // 256
    NBO = NO // 256
    slopes = np.exp2(-8.0 * np.arange(1, NH + 1, dtype=np.float32) / NH).astype(np.float32)
    out = {}
    ka = np.zeros((33, NV), np.float32)
    for n in range(NB):
        ka[n, n * 256:(n + 1) * 256] = 1
    ka[32] = 1
    out["kaug_c"] = ka.astype(bf)
    pos_q = (NO + np.arange(NO)).astype(np.float32)
    out["cq"] = (-slopes[:, None] * pos_q[None, :]).astype(bf)
    pos_k = (np.arange(NV // 128)[None, :] * 128 + np.arange(128)[:, None]).astype(np.float32)
    out["kbias"] = np.ascontiguousarray((slopes[None, :, None] * pos_k[:, None, :]).astype(np.float32))
    valid = np.ones(32, bool)
    valid[NB:] = False
    if r == 0:
        valid[:NB // 2] = False
    gbp = np.full((NBO, 32), NEG, np.float32); A01 = np.zeros((NBO, 32), np.float32); Bt = np.full((NBO, 32), NEG, np.float32)
    for mo in range(NBO):
        m = NB // 2 + mo
        for n in range(32):
            if n < m and valid[n]:
                gbp[mo, n] = 0; A01[mo, n] = 1
            if n == m:
                Bt[mo, n] = 0
    out["gbp"] = gbp.reshape(1, -1); out["A01"] = A01.reshape(1, -1); out["Bt"] = Bt.reshape(1, -1)
    cm = np.zeros((128, 2, 256), np.float32)
    kk = np.arange(128)[:, None]; qq = np.arange(256)[None, :]
    cm[:, 0, :] = (qq >= kk); cm[:, 1, :] = (qq >= kk + 128)
    out["cm"] = ((cm - 1.0) * 30000.0).astype(bf)
    s = np.zeros((65, 64), np.float32); s[64] = 1
    out["sel65"] = s
    return out


def phase_moba(C, S):
    nc = C.nc
    NV, NO, NB = C.NV, C.NO, C.NB
    NKT = NV // 128
    NQ = NO // 512
    NBO = NO // 256
    with ExitStack() as st:
        sb = lambda name, shape, dt: st.enter_context(nc.sbuf_tensor("mb_" + name, shape, dt))
        ident = sb("ident", [128, 128], BF16)
        S.dma("sp", ident[:], C.inp["ident_bf"][:, :], writes=["ident"])
        kaT = [sb("kaT%d" % i, [97, NV], BF16) for i in range(2)]
        qaT = [sb("qaT%d" % i, [97, NO], BF16) for i in range(2)]
        for i in range(2):
            S.dma("sp", kaT[i][64:97, :], C.inp["kaug_c"][:, :], writes=["kaT%d" % i])
        vt = sb("vt", [128, NKT, 8, 65], BF16)
        kbias = sb("kbias", [128, NH, NKT], F32)
        S.dma("sp", kbias[:], C.inp["kbias"][:, :, :], writes=["kbias"])
        gbp = sb("gbp", [128, NBO, 32], F32); A01 = sb("A01", [128, NBO, 32], F32); Bt = sb("Bt", [128, NBO, 32], F32)
        for nm, tl in (("gbp", gbp), ("A01", A01), ("Bt", Bt)):
            S.dma("sp", tl[:].rearrange("p a b -> p (a b)"), C.inp[nm][0:1, :].partition_broadcast(128), writes=[nm])
        cm = sb("cm", [128, 2, 256], BF16)
        S.dma("sp", cm[:], C.inp["cm"][:, :, :], writes=["cm"])
        sel65 = sb("sel65", [65, 64], F32)
        S.dma("sp", sel65[:], C.inp["sel65"][:, :], writes=["sel65"])
        kmf = sb("kmf", [64, NB], F32)
        kmb = sb("kmb", [64, 32], BF16)
        S.memset("pool", kmb[:], 0.0, writes=["kmb"])
        gm = sb("gm", [128, 32], F32); top8 = sb("top8", [128, 8], F32); f1 = sb("f1", [128, 32], F32)
        mbt = [sb("mbt%d" % i, [128, 96], BF16) for i in range(2)]
        for i in range(2):
            S.memset("pool", mbt[i][:], 0.0, writes=["mbt%d" % i])
        pt = [sb("pt%d" % i, [128, 512], BF16) for i in range(4)]
        oT = sb("oT", [65, 512], F32); rd = sb("rd", [64, 512], F32)
        ao = [sb("ao%d" % i, [64, 512], BF16) for i in range(2)]
        psS = [C.ps[0], C.ps[1]]; psO = [C.ps[2], C.ps[3]]; psG = C.ps[4]; psT = C.ps[5]; psD = C.ps[6]
        cnt = {"s": 0, "pt": 0, "o": 0, "ao": 0, "mb": 0}
        def load_v(g):
            S.memset("pool", vt[:, :, :, 64:65], 1.0, writes=["vt"])
            for kt0 in range(NKT):
                S.dma("sp", vt[:, kt0, :, 0:64],
                      C.v[kt0 * 128:(kt0 + 1) * 128, g * 512:(g + 1) * 512].rearrange("p (a d) -> p a d", d=64),
                      writes=["vt"])

        def prep_steps(h):
            hb = h % 2
            steps = []

            def loads():
                S.dma("sp", kaT[hb][0:64, :], C.kT[h * 64:(h + 1) * 64, :], writes=["kaT%d" % hb])
                S.dma("sp", qaT[hb][0:64, :], C.qT[h * 64:(h + 1) * 64, :], writes=["qaT%d" % hb])
                S.dma("sp", qaT[hb][96:97, :], C.inp["cq"][h:h + 1, :], writes=["qaT%d" % hb])
                S.dma("sp", kmf[:], C.kmT[h // 2, (h % 2) * 64:(h % 2) * 64 + 64, :], writes=["kmf"])
                S.copy("act", kmb[:, 0:NB], kmf[:], reads=["kmf"], writes=["kmb"])
            steps.append(loads)
            nq = NO // 128
            st1, st2, st3 = [], [], []
            for qs in range(nq):
                mo = qs // 2
                mi = qs % 2

                def s1(qs=qs, mo=mo, mi=mi):
                    S.mm(psG[:, 0:32], qaT[hb][0:64, qs * 128:(qs + 1) * 128], kmb[:], True, True,
                         reads=["qaT%d" % hb, "kmb"], writes=["psG"])
                    S.tt("dve", gm[:], psG[:, 0:32], gbp[:, mo, :], ALU.add, reads=["psG", "gbp"], writes=["gm"])
                    S.op("dve", lambda e: e.max(out=top8[:], in_=gm[:]), reads=["gm"], writes=["top8"])
                    S.ts("dve", f1[:], gm[:], top8[:, 2:3], ALU.is_ge, -NEG, ALU.mult, reads=["gm", "top8"], writes=["f1"])
                    S.tt("dve", f1[:], f1[:], A01[:, mo, :], ALU.mult, reads=["f1", "A01"], writes=["f1"])
                    S.tt("dve", mbt[mi][:, 64:96], f1[:], Bt[:, mo, :], ALU.add, reads=["f1", "Bt"], writes=["mbt%d" % mi])

                def s2(qs=qs, mi=mi):
                    pTt = psT[:].bitcast(BF16)[0:96, 0:128]
                    S.tr(pTt, mbt[mi][:], ident[:], reads=["mbt%d" % mi, "ident"], writes=["psT"])

                def s3(qs=qs):
                    S.copy("act", qaT[hb][64:96, qs * 128:(qs + 1) * 128], psT[:].bitcast(BF16)[64:96, 0:128],
                           reads=["psT"], writes=["qaT%d" % hb])
                st1.append(s1); st2.append(s2); st3.append(s3)
            for k in range(nq + 2):
                if 0 <= k - 2 < nq: steps.append(st3[k - 2])
                if 0 <= k - 1 < nq: steps.append(st2[k - 1])
                if k < nq: steps.append(st1[k])
            return steps

        def pairs(h, pending):
            hb = h % 2
            hl = h % 8
            for j in range(NQ):
                oi = cnt["o"] % 2; cnt["o"] += 1
                ok = "ps%d" % (2 + oi)
                b0 = (NO + 512 * j) // 256
                nkt = NKT // 2 + 4 * j + 4
                def emitS(kt):
                    si = cnt["s"] % 2; cnt["s"] += 1
                    sk = "ps%d" % si
                    n = kt // 2
                    lk = kaT[hb][0:97, kt * 128:(kt + 1) * 128]
                    if n >= b0:
                        c0 = (n - b0) * 256
                        c1 = 256 - c0
                        S.mm(psS[si][:, c1:c1 + 256], lk, qaT[hb][0:97, j * 512 + c1:j * 512 + c1 + 256],
                             True, True, reads=["kaT%d" % hb, "qaT%d" % hb], writes=[sk])
                        S.mm(psS[si][:, c0:c0 + 256], lk, qaT[hb][0:97, j * 512 + c0:j * 512 + c0 + 256],
                             True, False, reads=["kaT%d" % hb, "qaT%d" % hb], writes=[sk])
                        S.mm(psS[si][:, c0:c0 + 256], ident[:], cm[:, kt % 2, :],
                             False, True, reads=["ident", "cm"], writes=[sk])
                    else:
                        S.mm(psS[si][:, 0:512], lk, qaT[hb][0:97, j * 512:(j + 1) * 512],
                             True, True, reads=["kaT%d" % hb, "qaT%d" % hb], writes=[sk])
                    pi = cnt["pt"] % 4; cnt["pt"] += 1
                    pk = "pt%d" % pi
                    S.act(pt[pi][:], psS[si][:, 0:512], AF.Exp, reads=[sk, "kbias"], writes=[pk], bias=kbias[:, h, kt:kt + 1])
                    return pi
                pis = {0: emitS(0)}
                for kt in range(nkt):
                    if kt + 1 < nkt:
                        pis[kt + 1] = emitS(kt + 1)
                    pi = pis.pop(kt)
                    S.mm(psO[oi][0:65, 0:512], vt[:, kt, hl, :], pt[pi][:], kt == 0, kt == nkt - 1,
                         reads=["vt", "pt%d" % pi], writes=[ok])
                    if pending and kt % 2 == 1:
                        pending.pop(0)()
                S.copy("act", oT[:], psO[oi][0:65, 0:512], reads=[ok], writes=["oT"])
                S.mm(psD[0:64, 0:512], sel65[:], oT[:], True, True, reads=["sel65", "oT"], writes=["psD"])
                S.op("dve", lambda e: e.reciprocal(out=rd[:], in_=psD[0:64, 0:512]), reads=["psD"], writes=["rd"])
                ai = cnt["ao"] % 2; cnt["ao"] += 1
                S.tt("dve", ao[ai][:], oT[0:64, :], rd[:], ALU.mult, reads=["oT", "rd"], writes=["ao%d" % ai])
                S.dma("pool", C.aT[h * 64:(h + 1) * 64, j * 512:(j + 1) * 512], ao[ai][:], reads=["ao%d" % ai])

        for f in prep_steps(0):
            f()
        for h in range(NH):
            if h % 8 == 0:
                load_v(h // 8)
            pending = prep_steps(h + 1) if h + 1 < NH else []
            pairs(h, pending)
            while pending:
                pending.pop(0)()
        S.flush()


def ssd_setup(C):
    nc = C.nc

    def inp(name, shape, dt=F32):
        C.inp[name] = nc.dram_tensor(name, list(shape), dt, kind="ExternalInput").ap()
    inp("tri_f", [128, 128]); inp("ones_f", [128, 128]); inp("trineg_f", [128, 128])
    C.ynT = dram(C, "ynT_s", [2048, C.NO], BF16)


def ssd_consts():
    s = np.arange(128)[:, None]; t = np.arange(128)[None, :]
    return {"tri_f": (s <= t).astype(np.float32), "ones_f": np.ones((128, 128), np.float32),
            "trineg_f": np.where(t >= s, 0.0, NEG).astype(np.float32)}


def phase_ssd(C, S):
    nc = C.nc
    C.ssd_stop = getattr(C, "ssd_stop", 99)
    C.ssd_sub = getattr(C, "ssd_sub", 99)
    NV, NO = C.NV, C.NO
    NCH = NV // 256
    with ExitStack() as st:
        sb = lambda name, shape, dt: st.enter_context(nc.sbuf_tensor("sd_" + name, shape, dt))
        ident = sb("ident", [128, 128], BF16); identf = sb("identf", [128, 128], F32)
        tri = sb("tri", [128, 128], F32); ones = sb("ones", [128, 128], F32); trineg = sb("trineg", [128, 128], F32)
        for tl, nm in ((ident, "ident_bf"), (identf, "ident_f"), (tri, "tri_f"), (ones, "ones_f"), (trineg, "trineg_f")):
            S.dma("sp", tl[:], C.inp[nm][:, :], writes=["consts"])
        dsk = sb("dsk", [128, SH], F32); gn = sb("gn", [128, 2048], F32); pv = sb("pv", [128, 1], F32)
        S.dma("sp", dsk[:], C.inp["d_skip"][0:1, :].partition_broadcast(128), writes=["consts"])
        S.dma("sp", gn[:], C.inp["ssd_norm_g"][0:1, :].partition_broadcast(128), writes=["consts"])
        S.dma("sp", pv[:], C.inp["pv"][0:1, :].partition_broadcast(128), writes=["consts"])
        state = sb("state", [128, 8, 256], F32); stb = sb("stb", [128, 8, 256], BF16)
        S.memset("pool", state[:], 0.0, writes=["state"])
        S.memset("pool", stb[:], 0.0, writes=["stb"])
        xsT = sb("xsT", [128, 16, 256], BF16); BTt = sb("BTt", [128, 8, 256], BF16); CTt = sb("CTt", [128, 8, 256], BF16)
        da = sb("da", [128, 2, 64], F32); zt = sb("zt", [128, 2, 2048], BF16)
        xdt = sb("xdt", [128, 2, 2048], BF16); xdtd = sb("xdtd", [128, 2, 2048], BF16); xtm = sb("xtm", [128, 2, 2048], BF16)
        Btm = sb("Btm", [128, 2, 8, 128], BF16)
        acum = sb("acum", [128, 2, 32], F32); nacum = sb("nacum", [128, 2, 32], F32); eA = sb("eA", [128, 2, 32], F32)
        dend = sb("dend", [128, 2, 32], F32); eTot = sb("eTot", [128, 32], F32); tot = sb("tot", [128, 32], F32)
        Lt = [sb("Lt%d" % i, [128, 384], F32) for i in range(2)]
        Mt = [sb("Mt%d" % i, [128, 384], BF16) for i in range(4)]
        ysb = sb("ysb", [128, 2, 2048], F32); ytmp = sb("ytmp", [128, 2048], F32)
        RA = sb("RA", [128, 3, 4, 128], F32)
        ssg = sb("ssg", [128, 8], F32); ynb = sb("ynb", [128, 2048], BF16); ynT = sb("ynT", [128, 16, 128], BF16)
        ps = C.ps
        for c in range(NCH):
            own = c >= NCH // 2
            t0 = c * 256
            to0 = t0 - NO
            S.dma("sp", xsT[:], C.xsT[:, t0:t0 + 256].rearrange("(c p) t -> p c t", p=128), writes=["xsT"])
            S.dma("sp", BTt[:], C.BT[:, t0:t0 + 256].rearrange("(c p) t -> p c t", p=128), writes=["BTt"])
            S.dma("sp", da[:], C.da[t0:t0 + 256, :].rearrange("(i p) c -> p i c", p=128), writes=["da"])
            if own:
                S.dma("sp", CTt[:], C.CT[:, to0:to0 + 256].rearrange("(c p) t -> p c t", p=128), writes=["CTt"])
                S.dma("sp", zt[:], C.z[to0:to0 + 256, :].rearrange("(i p) c -> p i c", p=128), writes=["zt"])
            S.mm(ps[6][:, 0:32], tri[:], da[:, 0, 32:64], True, True, reads=["consts", "da"], writes=["ps6"])
            S.mm(ps[6][:, 32:64], tri[:], da[:, 1, 32:64], True, False, reads=["consts", "da"], writes=["ps6"])
            S.mm(ps[6][:, 32:64], ones[:], da[:, 0, 32:64], False, True, reads=["consts", "da"], writes=["ps6"])
            S.mm(ps[6][:, 64:96], ones[:], da[:, 0, 32:64], True, False, reads=["consts", "da"], writes=["ps6"])
            S.mm(ps[6][:, 64:96], ones[:], da[:, 1, 32:64], False, True, reads=["consts", "da"], writes=["ps6"])
            S.copy("dve", acum[:].rearrange("p i h -> p (i h)"), ps[6][:, 0:64], reads=["ps6"], writes=["acum"])
            S.copy("dve", tot[:], ps[6][:, 64:96], reads=["ps6"], writes=["tot"])
            S.ts("dve", nacum[:], acum[:], -1.0, ALU.mult, reads=["acum"], writes=["nacum"])
            S.act(eA[:], acum[:], AF.Exp, reads=["acum"], writes=["eA"])
            S.act(eTot[:], tot[:], AF.Exp, reads=["tot"], writes=["eTot"])
            S.tt("dve", dend[:], nacum[:], tot[:].unsqueeze(1).to_broadcast([128, 2, 32]), ALU.add,
                 reads=["nacum", "tot"], writes=["dend"])
            S.act(dend[:], dend[:], AF.Exp, reads=["dend"], writes=["dend"])
            if C.ssd_stop <= 1:
                continue
            for i in range(2):
                pT = ps[7][:].bitcast(BF16)[:, 0:1024].rearrange("p (c t) -> p c t", t=128)
                for half in range(2):
                    for cc in range(8):
                        S.tr(pT[:, cc, :], xsT[:, half * 8 + cc, i * 128:(i + 1) * 128], ident[:],
                             reads=["xsT", "consts"], writes=["ps7"])
                    hs = slice(half * 16, half * 16 + 16)
                    dst = xdt[:, i, half * 1024:(half + 1) * 1024].rearrange("p (h d) -> p h d", d=64)
                    src = pT.rearrange("p c (h d) -> p (c h) d", d=64)
                    S.tt("dve", dst, src, da[:, i, hs].unsqueeze(2).to_broadcast([128, 16, 64]), ALU.mult,
                         reads=["ps7", "da"], writes=["xdt"])
                    if own and C.ssd_sub >= 1:
                        S.copy("act", xtm[:, i, half * 1024:(half + 1) * 1024], pT.rearrange("p c t -> p (c t)"),
                               reads=["ps7"], writes=["xtm"])
                if C.ssd_sub < 2:
                    continue
                S.tt("dve", xdtd[:, i, :].rearrange("p (h d) -> p h d", d=64), xdt[:, i, :].rearrange("p (h d) -> p h d", d=64),
                     dend[:, i, :].unsqueeze(2).to_broadcast([128, 32, 64]), ALU.mult, reads=["xdt", "dend"], writes=["xdtd"])
                if C.ssd_sub < 3:
                    continue
                pT = ps[7][:].bitcast(BF16)[:, 0:1024].rearrange("p (c t) -> p c t", t=128)
                for g in range(8):
                    S.tr(pT[:, g, :], BTt[:, g, i * 128:(i + 1) * 128], ident[:], reads=["BTt", "consts"], writes=["ps7"])
                S.copy("act", Btm[:, i, :, :], pT, reads=["ps7"], writes=["Btm"])
            if C.ssd_stop <= 2:
                continue
            if own:
                for g in range(8):
                    if C.ssd_stop <= 3 and g > 0:
                        continue
                    S.mm(ps[4][:, 0:256], BTt[:, g, 0:128], CTt[:, g, 0:256], True, True, reads=["BTt", "CTt"], writes=["ps4"])
                    S.mm(ps[4][:, 256:384], BTt[:, g, 128:256], CTt[:, g, 128:256], True, True, reads=["BTt", "CTt"], writes=["ps4"])
                    for h4 in range(4):
                        h = g * 4 + h4
                        pL = ps[h4]; lk = "ps%d" % h4
                        if h4 == 0:
                            for q_, (src_i, mat) in enumerate(((0, tri), (0, ones), (1, tri))):
                                S.tt("dve", RA[:, q_, :, :], da[:, src_i, 32 + g * 4:36 + g * 4].unsqueeze(2).to_broadcast([128, 4, 128]),
                                     mat[:].unsqueeze(1).to_broadcast([128, 4, 128]), ALU.mult, reads=["da", "consts"], writes=["RA"])
                        S.mm(pL[:, 0:128], ones[:], RA[:, 0, h4, :], True, False, reads=["RA", "consts"], writes=[lk])
                        S.mm(pL[:, 0:128], identf[:], trineg[:], False, True, reads=["consts"], writes=[lk])
                        S.mm(pL[:, 128:256], ones[:], RA[:, 1, h4, :], True, False, reads=["RA", "consts"], writes=[lk])
                        S.mm(pL[:, 128:256], ones[:], RA[:, 2, h4, :], False, True, reads=["RA", "consts"], writes=[lk])
                        S.mm(pL[:, 256:384], ones[:], RA[:, 1, h4, :], True, False, reads=["RA", "consts"], writes=[lk])
                        S.mm(pL[:, 256:384], ones[:], RA[:, 2, h4, :], False, False, reads=["RA", "consts"], writes=[lk])
                        S.mm(pL[:, 256:384], identf[:], trineg[:], False, True, reads=["consts"], writes=[lk])
                        li = h % 2
                        S.act(Lt[li][:, 0:256], pL[:, 0:256], AF.Exp, reads=[lk, "nacum"], writes=["Lt%d" % li],
                              bias=nacum[:, 0, h:h + 1])
                        S.act(Lt[li][:, 256:384], pL[:, 256:384], AF.Exp, reads=[lk, "nacum"], writes=["Lt%d" % li],
                              bias=nacum[:, 1, h:h + 1])
                        S.tt("dve", Mt[h4][:], Lt[li][:], ps[4][:, 0:384], ALU.mult, reads=["Lt%d" % li, "ps4"],
                             writes=["Mt%d" % h4])
                    if C.ssd_stop <= 4:
                        continue
                    pY = ps[5][:, 0:512].rearrange("p (i c) -> p i c", i=2)
                    for h4 in range(4):
                        h = g * 4 + h4
                        cs = slice(h4 * 64, (h4 + 1) * 64)
                        S.mm(pY[:, 0, cs], Mt[h4][:, 0:128], xdt[:, 0, h * 64:(h + 1) * 64], True, True,
                             reads=["Mt%d" % h4, "xdt"], writes=["ps5"])
                        S.mm(pY[:, 1, cs], Mt[h4][:, 128:256], xdt[:, 0, h * 64:(h + 1) * 64], True, False,
                             reads=["Mt%d" % h4, "xdt"], writes=["ps5"])
                        S.mm(pY[:, 1, cs], Mt[h4][:, 256:384], xdt[:, 1, h * 64:(h + 1) * 64], False, True,
                             reads=["Mt%d" % h4, "xdt"], writes=["ps5"])
                    pO = ps[6][:, 0:512].rearrange("p (i c) -> p i c", i=2)
                    for i in range(2):
                        S.mm(pO[:, i, :], CTt[:, g, i * 128:(i + 1) * 128], stb[:, g, :], True, True,
                             reads=["CTt", "stb"], writes=["ps6"])
                    for i in range(2):
                        yg = ysb[:, i, g * 256:(g + 1) * 256].rearrange("p (h d) -> p h d", d=64)
                        S.tt("dve", yg, pO[:, i, :].rearrange("p (h d) -> p h d", d=64),
                             eA[:, i, g * 4:(g + 1) * 4].unsqueeze(2).to_broadcast([128, 4, 64]), ALU.mult,
                             reads=["ps6", "eA"], writes=["ysb"])
                        S.tt("dve", ysb[:, i, g * 256:(g + 1) * 256], ysb[:, i, g * 256:(g + 1) * 256], pY[:, i, :], ALU.add,
                             reads=["ysb", "ps5"], writes=["ysb"])
            if C.ssd_stop <= 5:
                continue
            for g in range(8):
                for i in range(2):
                    S.mm(ps[5][:, 0:256], Btm[:, i, g, :], xdtd[:, i, g * 256:(g + 1) * 256], i == 0, i == 1,
                         reads=["Btm", "xdtd"], writes=["ps5"])
                sg = state[:, g, :].rearrange("p (h d) -> p h d", d=64)
                S.tt("dve", sg, sg, eTot[:, g * 4:(g + 1) * 4].unsqueeze(2).to_broadcast([128, 4, 64]), ALU.mult,
                     reads=["state", "eTot"], writes=["state"])
                S.tt("dve", state[:, g, :], state[:, g, :], ps[5][:, 0:256], ALU.add, reads=["state", "ps5"], writes=["state"])
            if c == NCH // 2 - 1:
                S.ts("dve", state[:].rearrange("p g c -> p (g c)"), state[:].rearrange("p g c -> p (g c)"), pv[:, 0:1], ALU.mult,
                     reads=["state", "consts"], writes=["state"])
            S.copy("act", stb[:].rearrange("p g c -> p (g c)"), state[:].rearrange("p g c -> p (g c)"), reads=["state"], writes=["stb"])
            if C.ssd_stop <= 6:
                continue
            if own:
                for i in range(2):
                    y = ysb[:, i, :]
                    y3 = y.rearrange("p (h d) -> p h d", d=64)
                    S.tt("dve", ytmp[:].rearrange("p (h d) -> p h d", d=64), xtm[:, i, :].rearrange("p (h d) -> p h d", d=64),
                         dsk[:].unsqueeze(2).to_broadcast([128, 32, 64]), ALU.mult, reads=["xtm", "consts"], writes=["ytmp"])
                    S.tt("dve", y, y, ytmp[:], ALU.add, reads=["ysb", "ytmp"], writes=["ysb"])
                    S.tt("dve", y, y, zt[:, i, :], ALU.mult, reads=["ysb", "zt"], writes=["ysb"])
                    S.tt("dve", ytmp[:], y, y, ALU.mult, reads=["ysb"], writes=["ytmp"])
                    S.op("dve", lambda e: e.tensor_reduce(out=ssg[:], in_=ytmp[:].rearrange("p (g c) -> p g c", c=256),
                                                         axis=AX.X, op=ALU.add), reads=["ytmp"], writes=["ssg"])
                    S.act(ssg[:], ssg[:], AF.Ln, reads=["ssg"], writes=["ssg"], scale=1.0 / 256, bias=EPS)
                    S.act(ssg[:], ssg[:], AF.Exp, reads=["ssg"], writes=["ssg"], scale=-0.5)
                    S.tt("dve", ytmp[:].rearrange("p (g c) -> p g c", c=256), y.rearrange("p (g c) -> p g c", c=256),
                         ssg[:].unsqueeze(2).to_broadcast([128, 8, 256]), ALU.mult, reads=["ysb", "ssg"], writes=["ytmp"])
                    S.tt("dve", ynb[:], ytmp[:], gn[:], ALU.mult, reads=["ytmp", "consts"], writes=["ynb"])
                    for half in range(2):
                        pT = ps[7][:].bitcast(BF16)[:, 0:1024].rearrange("p (c t) -> p c t", t=128)
                        for cc in range(8):
                            S.tr(pT[:, cc, :], ynb[:, (half * 8 + cc) * 128:(half * 8 + cc + 1) * 128], ident[:],
                                 reads=["ynb", "consts"], writes=["ps7"])
                        S.copy("act", ynT[:, half * 8:(half + 1) * 8, :], pT, reads=["ps7"], writes=["ynT"])
                    tok = to0 + i * 128
                    S.dma("pool", C.ynT[:, tok:tok + 128].rearrange("(c p) t -> p c t", p=128), ynT[:], reads=["ynT"])
        S.flush()


def peer_setup(C):
    nc = C.nc

    def inp(name, shape, dt=F32):
        C.inp[name] = nc.dram_tensor(name, list(shape), dt, kind="ExternalInput").ap()
    inp("R1", [128, 32, 512], BF16); inp("R2", [128, 512], BF16)
    C.x2 = dram(C, "x2_s", [C.NO, D], F32)
    C.hT2 = dram(C, "hT2_s", [C.NO // 512, 128, 8, 512], BF16)
    C.out = nc.dram_tensor("out", [C.NO, D], F32, kind="ExternalOutput").ap()


def peer_consts():
    bf = ml_dtypes.bfloat16
    R1 = np.zeros((128, 32, 4, 128), np.float32)
    for c in range(32):
        for j in range(4):
            R1[4 * c + j, c, j, :] = 1
    R2 = np.tile(np.eye(128, dtype=np.float32), (1, 4))
    return {"R1": R1.reshape(128, 32, 512).astype(bf), "R2": R2.astype(bf)}


def phase_peer(C, S):
    nc = C.nc
    C.peer_dummy = getattr(C, "peer_dummy", PEER_DUMMY)
    NO = C.NO
    UT = C.inp["peer_uT"]; VT = C.inp["peer_v"]; WQ = C.inp["w_peer_q"]
    with ExitStack() as st:
        sb = lambda name, shape, dt: st.enter_context(nc.sbuf_tensor("pr_" + name, shape, dt))
        ident = sb("ident", [128, 128], BF16)
        S.dma("sp", ident[:], C.inp["ident_bf"][:, :], writes=["ident"])
        R1 = sb("R1", [128, 32, 512], BF16); R2 = sb("R2", [128, 512], BF16)
        S.dma("sp", R1[:], C.inp["R1"][:, :, :], writes=["R1"])
        S.dma("sp", R2[:], C.inp["R2"][:, :], writes=["R2"])
        stg = sb("stg", [128, 4096], F32)
        stg2 = sb("stg2", [128, 4096], F32)
        stg2_v = stg2[:].rearrange("p (j d) -> p j d", d=1024)
        stg_u = stg[:].rearrange("p (c n) -> p c n", n=512)
        stg_v = stg[:].rearrange("p (j d) -> p j d", d=1024)
        kT = sb("kT", [128, 16, 128], BF16)
        for hh in range(2):
            S.dma("sp", stg_u[:, :, 0:128], C.inp["keys%dT" % (hh + 1)].rearrange("h d k -> d h k"), writes=["stg"])
            S.copy("pool", kT[:].rearrange("p (h two) k -> p h two k", two=2)[:, :, hh, :], stg_u[:, :, 0:128],
                   reads=["stg"], writes=["kT"])
        ub = [sb("ub%d" % i, [128, 8, 512], BF16) for i in range(2)]
        vb = [sb("vb%d" % i, [128, 4, 1024], BF16) for i in range(2)]
        xnT = sb("xnT", [128, 8, 512], BF16)
        shr = sb("shr", [128, 8192], BF16)
        qTr = shr[:].rearrange("p (c t) -> p c t", t=512)
        sb16 = sb("sb16", [128, 16, 128], BF16); sf = sb("sf", [128, 16, 128], F32); swk = sb("swk", [128, 128], F32)
        v16 = sb("v16", [128, 16, 16], F32)
        cand = sb("cand", [128, 8, 256], F32); cwk = stg2[:, 0:2048].rearrange("p (h k) -> p h k", k=256)
        t8 = sb("t8", [128, 8, 8], F32); t8b = sb("t8b", [128, 8, 8], F32)
        thr = sb("thr", [128, 4, 8], F32); nb = sb("nb", [128, 4, 8], F32); zz = sb("zz", [128, 8], F32)
        sT = sb("sT", [128, 4, 16, 128], BF16)
        tau = sb("tau", [128, 4, 8], F32); taub = sb("taub", [128, 8], BF16)
        Eb = [sb("Eb%d" % i, [128, 512], BF16) for i in range(3)]
        Em8 = [sb("Em80", [128, 8, 512], BF16), shr[:, 0:4096].rearrange("p (h e) -> p h e", e=512)]
        gsb = [sb("gsb%d" % i, [128, 4, 512], BF16) for i in range(2)]
        hact = [sb("hact%d" % i, [128, 512], BF16) for i in range(2)]
        hTt = [sb("hTt%d" % i, [128, 4, 128], BF16) for i in range(2)]
        yacc = sb("yacc", [128, 4, 1024], F32)
        ps = C.ps
        cnt = {"e": 0, "u": 0, "it": 0}

        def peer_iter(it, c, s4, ui, vi):
            ts_ = slice(s4 * 128, (s4 + 1) * 128)
            b = it % 2
            pW = ps[4]; wk = "ps4"
            A = []
            for h in range(8):
                def ah(h=h):
                    ei = cnt["e"] % 3; cnt["e"] += 1
                    pE = ps[(2, 3, 1)[ei]]; ek = "ps%d" % (2, 3, 1)[ei]
                    S.mm(pE[:, 0:512], sT[:, s4, 2 * h, :], R1[:, c, :], True, False, reads=["sT", "R1"], writes=[ek])
                    S.mm(pE[:, 0:512], sT[:, s4, 2 * h + 1, :], R2[:], False, True, reads=["sT", "R2"], writes=[ek])
                    S.act(Eb[ei][:], pE[:, 0:512], AF.Exp, reads=[ek, "nb"], writes=["Eb%d" % ei], bias=nb[:, s4, h:h + 1])
                    S.stt("dve", Em8[b][:, h, :], pE[:, 0:512], thr[:, s4, h:h + 1], Eb[ei][:], ALU.is_ge, ALU.mult,
                          reads=[ek, "thr", "Eb%d" % ei], writes=["Em8%d" % b])
                A.append(ah)

            def b1a():
                for h in range(8):
                    S.mm(pW[:, 0:512], ident[:], Em8[b][:, h, :], h == 0, h == 7, reads=["Em8%d" % b, "ident"], writes=[wk])

            def b1():
                pass

            def b2():
                pass

            def b3():
                S.tt("dve", hact[b][:], gsb[c % 2][:, s4, :], pW[:, 0:512], ALU.mult, reads=["gsb%d" % (c % 2), wk],
                     writes=["hact%d" % b])

            def b4():
                pT = ps[0][:].bitcast(BF16)[:, 0:512].rearrange("p (j t) -> p j t", t=128)
                for j in range(4):
                    S.tr(pT[:, j, :], hact[b][:, j * 128:(j + 1) * 128], ident[:], reads=["hact%d" % b, "ident"], writes=["ps0"])

            def b5():
                pT = ps[0][:].bitcast(BF16)[:, 0:512].rearrange("p (j t) -> p j t", t=128)
                S.copy("act", hTt[b][:], pT, reads=["ps0"], writes=["hTt%d" % b])

            def b6():
                for half in range(2):
                    for j in range(4):
                        S.mm(ps[6 + half][:, 0:512], hTt[b][:, j, :], vb[vi][:, j, half * 512:(half + 1) * 512], j == 0, j == 3,
                             reads=["hTt%d" % b, "vb%d" % vi], writes=["ps%d" % (6 + half)])

            def b7():
                for half in range(2):
                    ya = yacc[:, s4, half * 512:(half + 1) * 512]
                    S.tt("dve", ya, ya, ps[6 + half][:, 0:512], ALU.add, reads=["yacc", "ps%d" % (6 + half)], writes=["yacc"])
            return A, [b1a, b1, b2, b3, b4, b5, b6, b7]

        def gelu_chunk(c, ui):
            for s4 in range(4):
                pa = ps[5] if s4 % 2 == 0 else ps[0]; pak = "ps5" if s4 % 2 == 0 else "ps0"
                for kc in range(8):
                    S.mm(pa[:, 0:512], xnT[:, kc, s4 * 128:(s4 + 1) * 128], ub[ui][:, kc, :], kc == 0, kc == 7,
                         reads=["ub%d" % ui, "xnT"], writes=[pak])
                S.act(gsb[c % 2][:, s4, :], pa[:, 0:512], AF.Gelu, reads=[pak], writes=["gsb%d" % (c % 2)])

        prevB = []
        for rd in range(NO // 512):
            S.dma("sp", xnT[:], C.hT2[rd, :, :, :], writes=["xnT"])
            S.memset("pool", yacc[:], 0.0, writes=["yacc"])
            for pc in range(4):
                S.dma("sp", stg_u, WQ[:, pc * 512:(pc + 1) * 512].rearrange("(c p) n -> p c n", p=128), writes=["stg"])
                ui = cnt["u"] % 2; cnt["u"] += 1
                S.copy("pool", ub[ui][:], stg_u, reads=["stg"], writes=["ub%d" % ui])
                for cc in range(4):
                    for kc in range(8):
                        S.mm(ps[0][:, 0:512], ub[ui][:, kc, cc * 128:(cc + 1) * 128], xnT[:, kc, :],
                             kc == 0, kc == 7, reads=["ub%d" % ui, "xnT"], writes=["ps0"])
                    S.copy("act", qTr[:, pc * 4 + cc, :], ps[0][:, 0:512], reads=["ps0"], writes=["Em81"])
            for s4 in range(4):
                ts_ = slice(s4 * 128, (s4 + 1) * 128)
                for g4 in range(4):
                    for cc in range(4):
                        ch = g4 * 4 + cc
                        S.mm(ps[1][:, cc * 128:(cc + 1) * 128], qTr[:, ch, ts_], kT[:, ch, :], True, True,
                             reads=["Em81", "kT"], writes=["ps1"])
                    S.copy("act", sb16[:, g4 * 4:(g4 + 1) * 4, :], ps[1][:, 0:512].rearrange("p (c k) -> p c k", k=128),
                           reads=["ps1"], writes=["sb16"])
                S.copy("dve", sf[:], sb16[:], reads=["sb16"], writes=["sf"])
                for ch in range(16):
                    S.op("dve", lambda e, ch=ch: e.max(out=v16[:, ch, 0:8], in_=sf[:, ch, :]), reads=["sf"], writes=["v16"])
                    S.op("dve", lambda e, ch=ch: e.match_replace(out=swk[:], in_to_replace=v16[:, ch, 0:8], in_values=sf[:, ch, :],
                                                                 imm_value=-1e30), reads=["sf", "v16"], writes=["swk"])
                    S.op("dve", lambda e, ch=ch: e.max(out=v16[:, ch, 8:16], in_=swk[:]), reads=["swk"], writes=["v16"])
                v4 = v16[:].rearrange("p (h two) k -> p h two k", two=2)
                c4 = cand[:].rearrange("p h (a b) -> p h a b", b=16)
                S.tt("dve", c4, v4[:, :, 0, :].unsqueeze(3).to_broadcast([128, 8, 16, 16]),
                     v4[:, :, 1, :].unsqueeze(2).to_broadcast([128, 8, 16, 16]), ALU.add, reads=["v16"], writes=["cand"])
                for h in range(8):
                    S.op("dve", lambda e, h=h: e.max(out=t8[:, h, :], in_=cand[:, h, :]), reads=["cand"], writes=["t8"])
                    S.op("dve", lambda e, h=h: e.match_replace(out=cwk[:, h, :], in_to_replace=t8[:, h, :], in_values=cand[:, h, :],
                                                               imm_value=-1e30), reads=["cand", "t8"], writes=["stg2"])
                    S.op("dve", lambda e, h=h: e.max(out=t8b[:, h, :], in_=cwk[:, h, :]), reads=["stg2"], writes=["t8b"])
                S.copy("dve", thr[:, s4, :], t8b[:, :, 7], reads=["t8b"], writes=["thr"])
                S.tt("dve", cwk, cand[:], t8[:, :, 0:1].to_broadcast([128, 8, 256]), ALU.subtract, reads=["cand", "t8"], writes=["stg2"])
                S.act(cwk, cwk, AF.Exp, reads=["stg2"], writes=["stg2"])
                S.tt("dve", cand[:], cand[:], thr[:, s4, :].unsqueeze(2).to_broadcast([128, 8, 256]), ALU.is_ge,
                     reads=["cand", "thr"], writes=["cand"])
                S.tt("dve", cwk, cwk, cand[:], ALU.mult, reads=["stg2", "cand"], writes=["stg2"])
                S.op("dve", lambda e: e.tensor_reduce(out=zz[:], in_=cwk, axis=AX.X, op=ALU.add), reads=["stg2"], writes=["zz"])
                S.act(zz[:], zz[:], AF.Ln, reads=["zz"], writes=["zz"])
                S.tt("dve", zz[:], zz[:], t8[:, :, 0], ALU.add, reads=["zz", "t8"], writes=["zz"])
                S.ts("dve", nb[:, s4, :], zz[:], -1.0, ALU.mult, reads=["zz"], writes=["nb"])
                for half in range(2):
                    pT = ps[1][:].bitcast(BF16)[:, 0:1024].rearrange("p (c t) -> p c t", t=128)
                    for cc in range(8):
                        S.tr(pT[:, cc, :], sb16[:, half * 8 + cc, :], ident[:], reads=["sb16", "ident"], writes=["ps1"])
                    S.copy("act", sT[:, s4, half * 8:(half + 1) * 8, :], pT, reads=["ps1"], writes=["sT"])
            def load_u(c):
                S.dma("sp", stg_u, UT[:, c * 512:(c + 1) * 512].rearrange("(kc p) n -> p kc n", p=128), writes=["stg"])
                ui_ = cnt["u"] % 2; cnt["u"] += 1
                S.copy("pool", ub[ui_][:], stg_u, reads=["stg"], writes=["ub%d" % ui_])
                return ui_

            def load_v(c):
                S.dma("sp", stg2_v, VT[c * 512:(c + 1) * 512, :].rearrange("(j p) d -> p j d", p=128), writes=["stg2"])
                S.copy("pool", vb[c % 2][:], stg2_v, reads=["stg2"], writes=["vb%d" % (c % 2)])
            events = []
            uis = {}

            def ev_load_u(c):
                uis[c] = load_u(c)
            ev_load_u(0)
            gelu_chunk(0, uis[0])
            for c in range(32):
                if c + 1 < 32:
                    events.append((c * 4 + 0 - 0.5, 0, lambda c=c: ev_load_u(c + 1)))
                    events.append((c * 4 + 2 - 0.4, 1, lambda c=c: gelu_chunk(c + 1, uis[c + 1])))
                events.append((c * 4 + 0 - 0.45, 2, lambda c=c: load_v(c)))
                for s4 in range(4):
                    it = c * 4 + s4
                    a_steps, b_steps = peer_iter(cnt["it"], c, s4, None, c % 2)
                    cnt["it"] += 1
                    for k in range(8):
                        events.append((it + k / 10.0, 3, a_steps[k]))
                    b0, _, _, b3, b4, b5, b6, b7 = b_steps
                    events.append((it + 1 + 0.35, 4, b0))
                    events.append((it + 1 + 0.45, 5, b3))
                    events.append((it + 2 + 0.05, 6, b4))
                    events.append((it + 2 + 0.15, 7, b5))
                    events.append((it + 2 + 0.36, 8, b6))
                    events.append((it + 2 + 0.46, 9, b7))
            events.sort(key=lambda e: (e[0], e[1]))
            for _, _, f in events:
                f()
            for s4 in range(4):
                tok = rd * 512 + s4 * 128
                S.dma("sp", stg[:, 0:1024], C.x2[tok:tok + 128, :], writes=["stg"])
                S.tt("dve", yacc[:, s4, :], yacc[:, s4, :], stg[:, 0:1024], ALU.add, reads=["yacc", "stg"], writes=["yacc"])
                S.dma("pool", C.out[tok:tok + 128, :], yacc[:, s4, :], reads=["yacc"])
        S.flush()


def phase_outproj(C, S):
    nc = C.nc
    NV, NO = C.NV, C.NO
    with ExitStack() as st:
        sb = lambda name, shape, dt: st.enter_context(nc.sbuf_tensor("op_" + name, shape, dt))
        ident = sb("ident", [128, 128], BF16)
        S.dma("sp", ident[:], C.inp["ident_bf"][:, :], writes=["ident"])
        stg = sb("stg", [128, 4, 1024], F32)
        Wa = sb("Wa", [128, 8, 1024], BF16); Ws = sb("Ws", [128, 16, 1024], BF16); Wo = sb("Wo", [128, 8, 1024], BF16)
        for nm, tl, nch in (("w_attn_o", Wa, 8), ("w_ssd_o", Ws, 16), ("w_out", Wo, 8)):
            for c0 in range(0, nch, 4):
                S.dma("sp", stg[:], C.inp[nm][c0 * 128:(c0 + 4) * 128, :].rearrange("(c p) n -> p c n", p=128), writes=["stg"])
                S.copy("pool", tl[:, c0:c0 + 4, :], stg[:], reads=["stg"], writes=[nm])
        aTt = [sb("aTt%d" % i, [128, 8, 128], BF16) for i in range(2)]
        yTt = [sb("yTt%d" % i, [128, 16, 128], BF16) for i in range(2)]
        ga = [sb("ga%d" % i, [128, 1024], BF16) for i in range(2)]
        gs = [sb("gs%d" % i, [128, 1024], BF16) for i in range(2)]
        xt = [sb("xt%d" % i, [128, 1024], F32) for i in range(2)]
        m1 = sb("m1", [128, 1024], F32); m2 = sb("m2", [128, 1024], F32); mb = sb("mb", [128, 1024], BF16)
        mT = sb("mT", [128, 8, 128], BF16)
        xo = [sb("xo%d" % i, [128, 1024], F32) for i in range(2)]
        ps = C.ps
        for t in range(NO // 128):
            i = t % 2
            tok = t * 128
            S.dma("sp", aTt[i][:], C.aT[:, tok:tok + 128].rearrange("(c p) t -> p c t", p=128), writes=["aTt%d" % i])
            S.dma("sp", yTt[i][:], C.ynT[:, tok:tok + 128].rearrange("(c p) t -> p c t", p=128), writes=["yTt%d" % i])
            S.dma("sp", ga[i][:], C.sga[tok:tok + 128, :], writes=["ga%d" % i])
            S.dma("sp", gs[i][:], C.sgs[tok:tok + 128, :], writes=["gs%d" % i])
            S.dma("sp", xt[i][:], C.inp["xv"][NO + tok:NO + tok + 128, :], writes=["xt%d" % i])
            for half in range(2):
                hs = slice(half * 512, (half + 1) * 512)
                for c in range(8):
                    S.mm(ps[half][:, 0:512], aTt[i][:, c, :], Wa[:, c, hs], c == 0, c == 7,
                         reads=["aTt%d" % i, "w_attn_o"], writes=["ps%d" % half])
                for c in range(16):
                    S.mm(ps[2 + half][:, 0:512], yTt[i][:, c, :], Ws[:, c, hs], c == 0, c == 15,
                         reads=["yTt%d" % i, "w_ssd_o"], writes=["ps%d" % (2 + half)])
                S.tt("dve", m1[:, hs], ps[half][:, 0:512], ga[i][:, hs], ALU.mult, reads=["ps%d" % half, "ga%d" % i], writes=["m1"])
                S.tt("dve", m2[:, hs], ps[2 + half][:, 0:512], gs[i][:, hs], ALU.mult, reads=["ps%d" % (2 + half), "gs%d" % i], writes=["m2"])
            S.tt("dve", mb[:], m1[:], m2[:], ALU.add, reads=["m1", "m2"], writes=["mb"])
            pT = ps[4][:].bitcast(BF16)[:, 0:1024].rearrange("p (c t) -> p c t", t=128)
            for c in range(8):
                S.tr(pT[:, c, :], mb[:, c * 128:(c + 1) * 128], ident[:], reads=["mb", "ident"], writes=["ps4"])
            S.copy("act", mT[:], pT, reads=["ps4"], writes=["mT"])
            for half in range(2):
                hs = slice(half * 512, (half + 1) * 512)
                for c in range(8):
                    S.mm(ps[5 + half][:, 0:512], mT[:, c, :], Wo[:, c, hs], c == 0, c == 7,
                         reads=["mT", "w_out"], writes=["ps%d" % (5 + half)])
                S.tt("dve", xo[i][:, hs], ps[5 + half][:, 0:512], xt[i][:, hs], ALU.add,
                     reads=["ps%d" % (5 + half), "xt%d" % i], writes=["xo%d" % i])
            S.dma("pool", C.x2[tok:tok + 128, :], xo[i][:], reads=["xo%d" % i])
        S.flush()


def build_all(nc, NV, st, debug=()):
    C = setup(nc, NV, debug)
    moba_setup(C); ssd_setup(C); peer_setup(C)
    S = Sched(nc, st)
    C.ps = [st.enter_context(nc.psum_tensor("ps%d" % i, [128, 512], F32)) for i in range(8)]
    phase_norm(C, S, C.inp["xv"], "norm1_g", C.hT, NV, "n1")
    phase_inproj(C, S)
    phase_moba(C, S)
    phase_ssd(C, S)
    phase_outproj(C, S)
    phase_norm(C, S, C.x2, "norm2_g", C.hT2, C.NO, "n2")
    phase_peer(C, S)
    return C, S


def make_inputs(inputs, NV, b, r, full_seq):
    bf = ml_dtypes.bfloat16
    NO = NV // 2
    f32 = lambda a: np.ascontiguousarray(np.asarray(a, dtype=np.float32))
    x = np.asarray(inputs["x"])
    ins = {}
    xv = np.zeros((NV, D), np.float32)
    if r == 0:
        xv[NO:] = x[b, 0:NO]
    else:
        xv[:] = x[b, 0:NV]
    ins["xv"] = xv
    ins["pv"] = np.full((1, 1), float(r), np.float32)
    ins["norm1_g"] = f32(inputs["norm1_g"][0:1]); ins["w_in"] = f32(inputs["w_in"][0])
    ins["q_norm_g"] = f32(inputs["q_norm_g"][0:1]); ins["k_norm_g"] = f32(inputs["k_norm_g"][0:1])
    ins["conv_wT"] = f32(np.asarray(inputs["conv_w"][0]).T); ins["conv_b"] = f32(np.asarray(inputs["conv_b"][0]).reshape(4096, 1))
    ins["dt_bias"] = f32(inputs["dt_bias"][0:1]); ins["a_log"] = f32(inputs["a_log"][0:1]); ins["d_skip"] = f32(inputs["d_skip"][0:1])
    ins["ssd_norm_g"] = f32(inputs["ssd_norm_g"][0:1]); ins["w_attn_o"] = f32(inputs["w_attn_o"][0])
    ins["w_ssd_o"] = f32(inputs["w_ssd_o"][0]); ins["w_out"] = f32(inputs["w_out"][0]); ins["norm2_g"] = f32(inputs["norm2_g"][0:1])
    ins["w_peer_q"] = f32(inputs["w_peer_q"][0])
    ins["keys1T"] = f32(np.asarray(inputs["peer_keys1"][0]).transpose(0, 2, 1))
    ins["keys2T"] = f32(np.asarray(inputs["peer_keys2"][0]).transpose(0, 2, 1))
    ins["peer_uT"] = f32(np.asarray(inputs["peer_u"][0]).T); ins["peer_v"] = f32(inputs["peer_v"][0])
    ins["ident_bf"] = np.eye(128).astype(bf); ins["ident_f"] = np.eye(128, dtype=np.float32)
    ins.update(moba_consts(NV, r)); ins.update(ssd_consts()); ins.update(peer_consts())
    return ins


NV_FULL = 8192


def kernel(**inputs):
    from concourse.bass_utils import run_bass_kernel_spmd
    nc = bass.Bass("TRN2", target_bir_lowering=False)
    with ExitStack() as st:
        C, S = build_all(nc, NV_FULL, st)
    x = np.asarray(inputs["x"])
    B = x.shape[0]
    in_maps = []
    for b in range(B):
        for r in range(2):
            in_maps.append(make_inputs(inputs, NV_FULL, b, r, NV_FULL))
    res = run_bass_kernel_spmd(nc, in_maps, core_ids=list(range(len(in_maps)))).results
    NO = NV_FULL // 2
    out = np.empty((B, NV_FULL, D), np.float32)
    for b in range(B):
        for r in range(2):
            out[b, r * NO:(r + 1) * NO] = np.asarray(res[b * 2 + r]["out"], dtype=np.float32)
    return out
```

```python
from contextlib import ExitStack
import ml_dtypes
import numpy as np
import concourse.bass as bass
import concourse.mybir as mybir

F32 = mybir.dt.float32
BF16 = mybir.dt.bfloat16
AF = mybir.ActivationFunctionType
ALU = mybir.AluOpType
AX = mybir.AxisListType

SAME_ENGINE_SYNC = True
N_DMA_SEMS = 32


class Sched:
    ENG = ("sp", "act", "dve", "pool", "pe")

    def __init__(self, nc, stack):
        self.nc = nc
        self.ops = []
        self.esem = {e: stack.enter_context(nc.semaphore("s_" + e)) for e in self.ENG}
        self.ecnt = {e: 0 for e in self.ENG}
        self.dsem = [stack.enter_context(nc.semaphore("d%d" % i)) for i in range(N_DMA_SEMS)]
        self.dcnt = [0] * N_DMA_SEMS
        self.downer = [None] * N_DMA_SEMS
        self.dnext = 0
        self.last_w = {}
        self.readers = {}
        self.waited = {e: {} for e in self.ENG}
        self.nblocks = 0
        self.nops = 0

    def _need(self, eng, ev, waits):
        if ev is None:
            return
        sem, val, src_eng, is_dma = ev
        if (not is_dma) and src_eng == eng and (eng == "pe" or not SAME_ENGINE_SYNC):
            return
        key = id(sem)
        if self.waited[eng].get(key, 0) >= val:
            return
        cur = waits.get(key)
        if cur is None or cur[1] < val:
            waits[key] = (sem, val)

    def op(self, eng, fn, reads=(), writes=(), dma=False):
        writes = list(writes) + [k for k in reads if isinstance(k, str) and k.startswith("ps") and k not in writes]
        waits = {}
        for k in reads:
            self._need(eng, self.last_w.get(k), waits)
        for k in writes:
            self._need(eng, self.last_w.get(k), waits)
            for ev in self.readers.get(k, ()):
                self._need(eng, ev, waits)
        if dma:
            half = N_DMA_SEMS // 2
            base = 0 if eng == "sp" else half
            self.dnx = getattr(self, "dnx", {})
            i = base + self.dnx.get(eng, 0)
            self.dnx[eng] = (self.dnx.get(eng, 0) + 1) % half
            if self.dcnt[i] > 0:
                self._need(eng, (self.dsem[i], 16 * self.dcnt[i], self.downer[i], True), waits)
            self.dcnt[i] += 1
            self.downer[i] = eng
            ev = (self.dsem[i], 16 * self.dcnt[i], eng, True)
            inc = (self.dsem[i], 16)
        else:
            self.ecnt[eng] += 1
            ev = (self.esem[eng], self.ecnt[eng], eng, False)
            inc = (self.esem[eng], 1)
        for (sem, val) in waits.values():
            self.waited[eng][id(sem)] = val
        for k in reads:
            self.readers.setdefault(k, []).append(ev)
        for k in writes:
            self.last_w[k] = ev
            self.readers[k] = []
        self.ops.append((eng, fn, list(waits.values()), inc))
        self.nops += 1

    def flush(self):
        fin = {}
        for i in range(N_DMA_SEMS):
            if self.dcnt[i] > 0:
                e = self.downer[i]
                if self.waited[e].get(id(self.dsem[i]), 0) < 16 * self.dcnt[i]:
                    fin.setdefault(e, []).append((self.dsem[i], 16 * self.dcnt[i]))
                    self.waited[e][id(self.dsem[i])] = 16 * self.dcnt[i]
        ops = self.ops
        self.ops = []
        if not ops and not fin:
            return
        nc = self.nc
        with nc.Block() as block:
            deco = {"sp": block.sync, "act": block.scalar, "dve": block.vector,
                    "pool": block.gpsimd, "pe": block.tensor}
            for e in self.ENG:
                mine = [o for o in ops if o[0] == e]
                tail = fin.get(e, [])
                if not mine and not tail:
                    continue

                def body(engine, mine=mine, tail=tail):
                    for (_, fn, waits, inc) in mine:
                        for (sem, val) in waits:
                            engine.wait_ge(sem, val)
                        ins = fn(engine)
                        ins.then_inc(inc[0], inc[1])
                    for (sem, val) in tail:
                        engine.wait_ge(sem, val)

                deco[e](body)
        self.nblocks += 1
        self.last_w = {}
        self.readers = {}

    def dma(self, eng, out, in_, reads=(), writes=(), **kw):
        self.op(eng, lambda e: e.dma_start(out=out, in_=in_, **kw), reads, writes, dma=True)

    def mm(self, out, lhsT, rhs, start, stop, reads=(), writes=()):
        self.op("pe", lambda e: e.matmul(out, lhsT, rhs, start=start, stop=stop), reads, writes)

    def tr(self, out, in_, ident, reads=(), writes=()):
        self.op("pe", lambda e: e.transpose(out, in_, ident), reads, writes)

    def act(self, out, in_, func, reads=(), writes=(), **kw):
        self.op("act", lambda e: e.activation(out=out, in_=in_, func=func, **kw), reads, writes)

    def tt(self, eng, out, in0, in1, op, reads=(), writes=()):
        self.op(eng, lambda e: e.tensor_tensor(out=out, in0=in0, in1=in1, op=op), reads, writes)

    def ts(self, eng, out, in0, s1, op0, s2=None, op1=None, reads=(), writes=(), **kw):
        if op1 is None:
            if op0 == ALU.pow:
                self.op(eng, lambda e: e.tensor_scalar(out=out, in0=in0, scalar1=0.0, scalar2=s1, op0=ALU.add, op1=ALU.pow, **kw),
                        reads, writes)
            else:
                self.op(eng, lambda e: e.tensor_scalar(out=out, in0=in0, scalar1=s1, scalar2=None, op0=op0, **kw),
                        reads, writes)
        else:
            self.op(eng, lambda e: e.tensor_scalar(out=out, in0=in0, scalar1=s1, scalar2=s2, op0=op0, op1=op1, **kw),
                    reads, writes)

    def stt(self, eng, out, in0, scalar, in1, op0, op1, reads=(), writes=()):
        self.op(eng, lambda e: e.scalar_tensor_tensor(out=out, in0=in0, scalar=scalar, in1=in1, op0=op0, op1=op1),
                reads, writes)

    def copy(self, eng, out, in_, reads=(), writes=()):
        if eng == "act":
            self.op(eng, lambda e: e.activation(out=out, in_=in_, func=AF.Copy), reads, writes)
        else:
            self.op(eng, lambda e: e.tensor_copy(out=out, in_=in_), reads, writes)

    def memset(self, eng, ap, val, writes=()):
        self.op(eng, lambda e: e.memset(ap, val), (), writes)


D = 1024
NH = 16
HD = 64
SH = 32
SP = 64
SG = 8
SN = 128
EPS = 1e-6
NEG = -30000.0
import os
MOBA_DUMMY = int(os.environ.get('MOBA_DUMMY', '0'))
PEER_DUMMY = int(os.environ.get('PEER_DUMMY', '0')) if 'PEER_DUMMY' in os.environ else 0
C_Q, C_K, C_V, C_Z, C_X, C_B, C_C, C_DT, C_GA, C_GS = 0, 1024, 2048, 3072, 5120, 7168, 8192, 9216, 9248, 10272
IN_COLS = 11296


class Ctx:
    pass


def dram(C, name, shape, dt):
    kind = "ExternalOutput" if name in C.debug else "Internal"
    return C.nc.dram_tensor(name, list(shape), dt, kind=kind).ap()


def setup(nc, NV, debug=()):
    C = Ctx()
    C.nc = nc
    C.NV = NV
    C.NO = NV // 2
    C.debug = set(debug)
    C.inp = {}

    def inp(name, shape, dt=F32):
        C.inp[name] = nc.dram_tensor(name, list(shape), dt, kind="ExternalInput").ap()
        return C.inp[name]
    NV_, NO = NV, C.NO
    NB = NV // 256
    C.NB = NB
    inp("xv", [NV, D])
    inp("norm1_g", [1, D]); inp("w_in", [D, IN_COLS]); inp("q_norm_g", [1, HD]); inp("k_norm_g", [1, HD])
    inp("conv_wT", [4096, 4]); inp("conv_b", [4096, 1]); inp("dt_bias", [1, SH]); inp("a_log", [1, SH])
    inp("d_skip", [1, SH]); inp("ssd_norm_g", [1, 2048]); inp("w_attn_o", [D, D]); inp("w_ssd_o", [2048, D])
    inp("w_out", [D, D]); inp("norm2_g", [1, D]); inp("w_peer_q", [D, 2048])
    inp("keys1T", [8, 128, 128]); inp("keys2T", [8, 128, 128])
    inp("peer_uT", [D, 16384]); inp("peer_v", [16384, D])
    inp("ident_bf", [128, 128], BF16); inp("ident_f", [128, 128], F32)
    inp("pv", [1, 1])
    C.hT = dram(C, "hT_s", [NV // 512, 128, 8, 512], BF16)
    C.qT = dram(C, "qT_s", [D, NO], BF16)
    C.kT = dram(C, "kT_s", [D, NV], BF16)
    C.kmT = dram(C, "kmT_s", [8, 128, NB], F32)
    C.v = dram(C, "v_s", [NV, D], BF16)
    C.z = dram(C, "z_s", [NO, 2048], BF16)
    C.xsT = dram(C, "xsT_s", [2048, NV], BF16)
    C.BT = dram(C, "BT_s", [1024, NV], BF16)
    C.CT = dram(C, "CT_s", [1024, NO], BF16)
    C.da = dram(C, "da_s", [NV, 64], F32)
    C.sga = dram(C, "sga_s", [NO, D], BF16)
    C.sgs = dram(C, "sgs_s", [NO, D], BF16)
    return C


def phase_norm(C, S, xin, gname, hT, ntok, tag):
    nc = C.nc
    with ExitStack() as st:
        sb = lambda name, shape, dt: st.enter_context(nc.sbuf_tensor(tag + name, shape, dt))
        ident = sb("ident", [128, 128], BF16)
        gT = sb("gT", [128, 8], F32)
        S.dma("sp", ident[:], C.inp["ident_bf"][:, :], writes=["ident"])
        S.dma("sp", gT[:], C.inp[gname][0, :].rearrange("(c p) -> p c", p=128), writes=["gT"],
              allow_slow_non_contiguous=True)
        xt = [sb("xt%d" % i, [128, D], F32) for i in range(2)]
        junk = sb("junk", [128, D], F32)
        ss = [sb("ss%d" % i, [128, 1], F32) for i in range(2)]
        xb = [sb("xb%d" % i, [128, D], BF16) for i in range(2)]
        ho = [sb("ho%d" % i, [128, 8, 128], BF16) for i in range(2)]
        for t in range(ntok // 128):
            i = t % 2
            pT = C.ps[t % 2][:].bitcast(BF16)[:, 0:1024].rearrange("p (c t) -> p c t", t=128)
            pk = "ps%d" % (t % 2)
            S.dma("sp", xt[i][:], xin[t * 128:(t + 1) * 128, :], writes=["xt%d" % i])
            S.act(junk[:], xt[i][:], AF.Square, reads=["xt%d" % i], writes=["junk", "ss%d" % i], accum_out=ss[i][:])
            S.act(ss[i][:], ss[i][:], AF.Ln, reads=["ss%d" % i], writes=["ss%d" % i], scale=1.0 / D, bias=EPS)
            S.act(ss[i][:], ss[i][:], AF.Exp, reads=["ss%d" % i], writes=["ss%d" % i], scale=-0.5)
            S.act(xb[i][:], xt[i][:], AF.Copy, reads=["xt%d" % i, "ss%d" % i], writes=["xb%d" % i], scale=ss[i][:])
            for c in range(8):
                S.tr(pT[:, c, :], xb[i][:, c * 128:(c + 1) * 128], ident[:], reads=["xb%d" % i, "ident"], writes=[pk])
            S.tt("dve", ho[i][:], pT, gT[:].unsqueeze(2).to_broadcast([128, 8, 128]), ALU.mult,
                 reads=[pk, "gT"], writes=["ho%d" % i])
            S.dma("pool", hT[t // 4, :, :, (t % 4) * 128:(t % 4 + 1) * 128], ho[i][:], reads=["ho%d" % i])
        S.flush()


def phase_inproj(C, S):
    nc = C.nc
    NV, NO = C.NV, C.NO
    NT = NV // 512
    NTO = NO // 512
    W = C.inp["w_in"]
    with ExitStack() as st:
        sb = lambda name, shape, dt: st.enter_context(nc.sbuf_tensor("ip_" + name, shape, dt))
        ident = sb("ident", [128, 128], BF16)
        S.dma("sp", ident[:], C.inp["ident_bf"][:, :], writes=["ident"])
        gq = sb("gq", [128, HD], F32); gk = sb("gk", [128, HD], F32)
        S.dma("sp", gq[:], C.inp["q_norm_g"][0:1, :].partition_broadcast(128), writes=["gq"])
        S.dma("sp", gk[:], C.inp["k_norm_g"][0:1, :].partition_broadcast(128), writes=["gk"])
        S.ts("dve", gq[:], gq[:], HD ** -0.5, ALU.mult, reads=["gq"], writes=["gq"])
        dtb = sb("dtb", [128, SH], F32); An = sb("An", [128, SH], F32)
        S.dma("sp", dtb[:], C.inp["dt_bias"][0:1, :].partition_broadcast(128), writes=["dtb"])
        S.dma("sp", An[:], C.inp["a_log"][0:1, :].partition_broadcast(128), writes=["An"])
        S.act(An[:], An[:], AF.Exp, reads=["An"], writes=["An"])
        S.ts("dve", An[:], An[:], -1.0, ALU.mult, reads=["An"], writes=["An"])
        cw = sb("cw", [128, 32, 4], F32); cb = sb("cb", [128, 32], F32)
        S.dma("sp", cw[:], C.inp["conv_wT"].rearrange("(c p) k -> p c k", p=128), writes=["cw"])
        S.dma("sp", cb[:], C.inp["conv_b"].rearrange("(c p) o -> p (c o)", p=128), writes=["cb"],
              allow_slow_non_contiguous=True)
        kmT = sb("kmT", [128, 8, C.NB], F32)
        S.memset("pool", kmT[:], 0.0, writes=["kmT"])
        wf = [sb("wf%d" % i, [128, 8, 512], F32) for i in range(2)]
        wb = [sb("wb%d" % i, [128, 8, 512], BF16) for i in range(2)]
        hb = [sb("hb%d" % i, [128, 8, 512], BF16) for i in range(3)]
        ev = [sb("ev%d" % i, [128, 512], F32) for i in range(2)]
        sq = sb("sq", [128, 512], F32)
        ssq = sb("ssq", [128, 8], F32)
        ob = [sb("ob%d" % i, [128, 512], BF16) for i in range(2)]
        tb = [sb("tb%d" % i, [128, 4, 128], BF16) for i in range(2)]
        kr = sb("kr", [128, 4], F32)
        cbuf = [sb("cbuf%d" % i, [128, 515], F32) for i in range(4)]
        caccs = [sb("cacc%d" % i, [128, 512], F32) for i in range(2)]
        dab = [sb("dab%d" % i, [128, 64], F32) for i in range(2)]

        blocks = []
        for j in range(2): blocks.append(("q", C_Q + 512 * j, 512, NT - NTO, j))
        for j in range(2): blocks.append(("k", C_K + 512 * j, 512, 0, j))
        for j in range(2): blocks.append(("v", C_V + 512 * j, 512, 0, j))
        for j in range(4): blocks.append(("z", C_Z + 512 * j, 512, NT - NTO, j))
        for j in range(4): blocks.append(("xs", C_X + 512 * j, 512, 0, j))
        for j in range(2): blocks.append(("B", C_B + 512 * j, 512, 0, j))
        for j in range(2): blocks.append(("C", C_C + 512 * j, 512, NT - NTO - 1, j))
        blocks.append(("dt", C_DT, 32, 0, 0))
        for j in range(2): blocks.append(("ga", C_GA + 512 * j, 512, NT - NTO, j))
        for j in range(2): blocks.append(("gs", C_GS + 512 * j, 512, NT - NTO, j))

        cnt = {"h": 0, "ps": 0, "ev": 0, "ob": 0, "tb": 0, "da": 0, "ca": 0}
        def load_w(bi):
            kind_, c0_, ncol_, _, _ = blocks[bi]
            wi_ = bi % 2
            S.dma("sp", wf[wi_][:, :, 0:ncol_], W[:, c0_:c0_ + ncol_].rearrange("(c p) n -> p c n", p=128),
                  writes=["wf%d" % wi_])
            S.copy("pool", wb[wi_][:, :, 0:ncol_], wf[wi_][:, :, 0:ncol_], reads=["wf%d" % wi_], writes=["wb%d" % wi_])
        load_w(0)
        deferred = []
        for bi, (kind, c0, ncol, t0, j) in enumerate(blocks):
            wi = bi % 2
            if bi + 1 < len(blocks):
                load_w(bi + 1)
            if kind in ("xs", "B", "C"):
                for s4 in range(4):
                    S.memset("pool", cbuf[s4][:, 0:3], 0.0, writes=["cbuf%d" % s4])
            for t in range(t0, NT):
                hi = cnt["h"] % 3; cnt["h"] += 1
                S.dma("sp", hb[hi][:], C.hT[t, :, :, :], writes=["hb%d" % hi])
                to = t - (NT - NTO)
                for s4 in range(4):
                    pi = cnt["ps"] % 4; cnt["ps"] += 1
                    ps = C.ps[pi]; pk = "ps%d" % pi
                    tok = t * 512 + s4 * 128
                    if kind in ("xs", "B", "C"):
                        for c in range(8):
                            S.mm(ps[:, 0:512], wb[wi][:, c, s4 * 128:(s4 + 1) * 128], hb[hi][:, c, :], c == 0, c == 7,
                                 reads=["wb%d" % wi, "hb%d" % hi], writes=[pk])
                        ck = "cbuf%d" % s4
                        cai = cnt["ca"] % 2; cnt["ca"] += 1
                        cacc = caccs[cai]; cak = "cacc%d" % cai
                        S.copy("act", cbuf[s4][:, 3:515], ps[:, 0:512], reads=[pk], writes=[ck])
                        chn = (c0 - C_X) // 128 + s4
                        S.ts("dve", cacc[:], cbuf[s4][:, 0:512], cw[:, chn, 0:1], ALU.mult, cb[:, chn:chn + 1], ALU.add,
                             reads=[ck, "cw", "cb"], writes=[cak])
                        for k in range(1, 4):
                            S.stt("dve", cacc[:], cbuf[s4][:, k:k + 512], cw[:, chn, k:k + 1], cacc[:], ALU.mult, ALU.add,
                                  reads=[ck, cak], writes=[cak])
                        S.copy("pool", cbuf[s4][:, 0:3], cbuf[s4][:, 512:515], reads=[ck], writes=[ck])
                        oi = cnt["ob"] % 2; cnt["ob"] += 1
                        S.act(ob[oi][:], cacc[:], AF.Silu, reads=[cak], writes=["ob%d" % oi])
                        r0 = (c0 - {"xs": C_X, "B": C_B, "C": C_C}[kind]) + s4 * 128
                        if kind == "xs":
                            S.dma("pool", C.xsT[r0:r0 + 128, t * 512:(t + 1) * 512], ob[oi][:], reads=["ob%d" % oi])
                        elif kind == "B":
                            S.dma("pool", C.BT[r0:r0 + 128, t * 512:(t + 1) * 512], ob[oi][:], reads=["ob%d" % oi])
                        elif to >= 0:
                            S.dma("pool", C.CT[r0:r0 + 128, to * 512:(to + 1) * 512], ob[oi][:], reads=["ob%d" % oi])
                        continue
                    for c in range(8):
                        S.mm(ps[:, 0:ncol], hb[hi][:, c, s4 * 128:(s4 + 1) * 128], wb[wi][:, c, 0:ncol], c == 0, c == 7,
                             reads=["wb%d" % wi, "hb%d" % hi], writes=[pk])
                    while deferred:
                        deferred.pop(0)()
                    if kind in ("q", "k"):
                        ei = cnt["ev"] % 2; cnt["ev"] += 1
                        ek = "ev%d" % ei
                        S.copy("act", ev[ei][:], ps[:, 0:512], reads=[pk], writes=[ek])
                        S.tt("dve", sq[:], ev[ei][:], ev[ei][:], ALU.mult, reads=[ek], writes=["sq"])
                        S.op("dve", lambda e: e.tensor_reduce(out=ssq[:], in_=sq[:].rearrange("p (a b) -> p a b", b=HD),
                                                             axis=AX.X, op=ALU.add), reads=["sq"], writes=["ssq"])
                        S.act(ssq[:], ssq[:], AF.Ln, reads=["ssq"], writes=["ssq"], scale=1.0 / HD, bias=EPS)
                        S.act(ssq[:], ssq[:], AF.Exp, reads=["ssq"], writes=["ssq"], scale=-0.5)
                        e3 = ev[ei][:].rearrange("p (a b) -> p a b", b=HD)
                        S.tt("dve", e3, e3, ssq[:].unsqueeze(2).to_broadcast([128, 8, HD]), ALU.mult,
                             reads=[ek, "ssq"], writes=[ek])
                        oi = cnt["ob"] % 2; cnt["ob"] += 1
                        g_ = gq if kind == "q" else gk
                        S.tt("dve", ob[oi][:].rearrange("p (a b) -> p a b", b=HD), e3,
                             g_[:].unsqueeze(1).to_broadcast([128, 8, HD]), ALU.mult,
                             reads=[ek, "gq", "gk"], writes=["ob%d" % oi])
                        def fin(kind=kind, oi=oi, j=j, to=to, s4=s4, tok=tok):
                            pti = 4 + cnt["tb"] % 2
                            ti = cnt["tb"] % 2; cnt["tb"] += 1
                            pT = C.ps[pti][:].bitcast(BF16)[:, 0:512].rearrange("p (c t) -> p c t", t=128)
                            for c in range(4):
                                S.tr(pT[:, c, :], ob[oi][:, c * 128:(c + 1) * 128], ident[:], reads=["ob%d" % oi, "ident"],
                                     writes=["ps%d" % pti])
                            S.copy("act", tb[ti][:], pT, reads=["ps%d" % pti], writes=["tb%d" % ti])
                            rows = slice(j * 512, (j + 1) * 512)
                            if kind == "q":
                                S.dma("pool", C.qT[rows, to * 512 + s4 * 128: to * 512 + (s4 + 1) * 128].rearrange("(c p) t -> p c t", p=128),
                                      tb[ti][:], reads=["tb%d" % ti])
                            else:
                                S.dma("pool", C.kT[rows, tok:tok + 128].rearrange("(c p) t -> p c t", p=128),
                                      tb[ti][:], reads=["tb%d" % ti])
                                S.op("dve", lambda e, ti=ti: e.tensor_reduce(out=kr[:], in_=tb[ti][:], axis=AX.X, op=ALU.add),
                                     reads=["tb%d" % ti], writes=["kr"])
                                blk = tok // 256
                                S.stt("dve", kmT[:, j * 4:(j + 1) * 4, blk], kr[:], 1.0 / 256, kmT[:, j * 4:(j + 1) * 4, blk],
                                      ALU.mult, ALU.add, reads=["kr", "kmT"], writes=["kmT"])
                        deferred.append(fin)
                    elif kind == "dt":
                        di = cnt["da"] % 2; cnt["da"] += 1
                        dk = "dab%d" % di
                        S.tt("dve", dab[di][:, 0:32], ps[:, 0:32], dtb[:], ALU.add, reads=[pk, "dtb"], writes=[dk])
                        S.act(dab[di][:, 0:32], dab[di][:, 0:32], AF.Exp, reads=[dk], writes=[dk])
                        S.act(dab[di][:, 0:32], dab[di][:, 0:32], AF.Ln, reads=[dk], writes=[dk], bias=1.0)
                        S.tt("dve", dab[di][:, 32:64], dab[di][:, 0:32], An[:], ALU.mult, reads=[dk, "An"], writes=[dk])
                        S.dma("pool", C.da[tok:tok + 128, :], dab[di][:], reads=[dk])
                    else:
                        oi = cnt["ob"] % 2; cnt["ob"] += 1
                        fn = {"v": AF.Copy, "z": AF.Silu, "ga": AF.Sigmoid, "gs": AF.Sigmoid}[kind]
                        S.act(ob[oi][:], ps[:, 0:512], fn, reads=[pk], writes=["ob%d" % oi])
                        if kind == "v":
                            dst = C.v[tok:tok + 128, j * 512:(j + 1) * 512]
                        else:
                            otok = to * 512 + s4 * 128
                            dst = {"z": C.z, "ga": C.sga, "gs": C.sgs}[kind][otok:otok + 128, j * 512:(j + 1) * 512]
                        S.dma("pool", dst, ob[oi][:], reads=["ob%d" % oi])
        while deferred:
            deferred.pop(0)()
        S.dma("pool", C.kmT.rearrange("c p n -> p c n"), kmT[:], reads=["kmT"])
        S.flush()


def moba_setup(C):
    nc = C.nc
    NV, NO, NB = C.NV, C.NO, C.NB

    def inp(name, shape, dt=F32):
        C.inp[name] = nc.dram_tensor(name, list(shape), dt, kind="ExternalInput").ap()
    inp("kaug_c", [33, NV], BF16)
    inp("cq", [NH, NO], BF16)
    inp("kbias", [128, NH, NV // 128])
    NBO = NO // 256
    inp("gbp", [1, NBO * 32]); inp("A01", [1, NBO * 32]); inp("Bt", [1, NBO * 32])
    inp("cm", [128, 2, 256], BF16)
    inp("sel65", [65, 64])
    C.aT = dram(C, "aT_s", [D, NO], BF16)


def moba_consts(NV, r):
    bf = ml_dtypes.bfloat16
    NO = NV // 2
    NB = NV // 256
    NBO = NO // 256
    slopes = np.exp2(-8.0 * np.arange(1, NH + 1, dtype=np.float32) / NH).astype(np.float32)
    out = {}
    ka = np.zeros((33, NV), np.float32)
    for n in range(NB):
        ka[n, n * 256:(n + 1) * 256] = 1
    ka[32] = 1
    out["kaug_c"] = ka.astype(bf)
    pos_q = (NO + np.arange(NO)).astype(np.float32)
    out["cq"] = (-slopes[:, None] * pos_q[None, :]).astype(bf)
    pos_k = (np.arange(NV // 128)[None, :] * 128 + np.arange(128)[:, None]).astype(np.float32)
    out["kbias"] = np.ascontiguousarray((slopes[None, :, None] * pos_k[:, None, :]).astype(np.float32))
    valid = np.ones(32, bool)
    valid[NB:] = False
    if r == 0:
        valid[:NB // 2] = False
    gbp = np.full((NBO, 32), NEG, np.float32); A01 = np.zeros((NBO, 32), np.float32); Bt = np.full((NBO, 32), NEG, np.float32)
    for mo in range(NBO):
        m = NB // 2 + mo
        for n in range(32):
            if n < m and valid[n]:
                gbp[mo, n] = 0; A01[mo, n] = 1
            if n == m:
                Bt[mo, n] = 0
    out["gbp"] = gbp.reshape(1, -1); out["A01"] = A01.reshape(1, -1); out["Bt"] = Bt.reshape(1, -1)
    cm = np.zeros((128, 2, 256), np.float32)
    kk = np.arange(128)[:, None]; qq = np.arange(256)[None, :]
    cm[:, 0, :] = (qq >= kk); cm[:, 1, :] = (qq >= kk + 128)
    out["cm"] = ((cm - 1.0) * 30000.0).astype(bf)
    s = np.zeros((65, 64), np.float32); s[64] = 1
    out["sel65"] = s
    return out


def phase_moba(C, S):
    nc = C.nc
    NV, NO, NB = C.NV, C.NO, C.NB
    NKT = NV // 128
    NQ = NO // 512
    NBO = NO // 256
    with ExitStack() as st:
        sb = lambda name, shape, dt: st.enter_context(nc.sbuf_tensor("mb_" + name, shape, dt))
        ident = sb("ident", [128, 128], BF16)
        S.dma("sp", ident[:], C.inp["ident_bf"][:, :], writes=["ident"])
        kaT = [sb("kaT%d" % i, [97, NV], BF16) for i in range(2)]
        qaT = [sb("qaT%d" % i, [97, NO], BF16) for i in range(2)]
        for i in range(2):
            S.dma("sp", kaT[i][64:97, :], C.inp["kaug_c"][:, :], writes=["kaT%d" % i])
        vt = sb("vt", [128, NKT, 8, 65], BF16)
        kbias = sb("kbias", [128, NH, NKT], F32)
        S.dma("sp", kbias[:], C.inp["kbias"][:, :, :], writes=["kbias"])
        gbp = sb("gbp", [128, NBO, 32], F32); A01 = sb("A01", [128, NBO, 32], F32); Bt = sb("Bt", [128, NBO, 32], F32)
        for nm, tl in (("gbp", gbp), ("A01", A01), ("Bt", Bt)):
            S.dma("sp", tl[:].rearrange("p a b -> p (a b)"), C.inp[nm][0:1, :].partition_broadcast(128), writes=[nm])
        cm = sb("cm", [128, 2, 256], BF16)
        S.dma("sp", cm[:], C.inp["cm"][:, :, :], writes=["cm"])
        sel65 = sb("sel65", [65, 64], F32)
        S.dma("sp", sel65[:], C.inp["sel65"][:, :], writes=["sel65"])
        kmf = sb("kmf", [64, NB], F32)
        kmb = sb("kmb", [64, 32], BF16)
        S.memset("pool", kmb[:], 0.0, writes=["kmb"])
        gm = sb("gm", [128, 32], F32); top8 = sb("top8", [128, 8], F32); f1 = sb("f1", [128, 32], F32)
        mbt = [sb("mbt%d" % i, [128, 96], BF16) for i in range(2)]
        for i in range(2):
            S.memset("pool", mbt[i][:], 0.0, writes=["mbt%d" % i])
        pt = [sb("pt%d" % i, [128, 512], BF16) for i in range(4)]
        oT = sb("oT", [65, 512], F32); rd = sb("rd", [64, 512], F32)
        ao = [sb("ao%d" % i, [64, 512], BF16) for i in range(2)]
        psS = [C.ps[0], C.ps[1]]; psO = [C.ps[2], C.ps[3]]; psG = C.ps[4]; psT = C.ps[5]; psD = C.ps[6]
        cnt = {"s": 0, "pt": 0, "o": 0, "ao": 0, "mb": 0}
        def load_v(g):
            S.memset("pool", vt[:, :, :, 64:65], 1.0, writes=["vt"])
            for kt0 in range(NKT):
                S.dma("sp", vt[:, kt0, :, 0:64],
                      C.v[kt0 * 128:(kt0 + 1) * 128, g * 512:(g + 1) * 512].rearrange("p (a d) -> p a d", d=64),
                      writes=["vt"])

        def prep_steps(h):
            hb = h % 2
            steps = []

            def loads():
                S.dma("sp", kaT[hb][0:64, :], C.kT[h * 64:(h + 1) * 64, :], writes=["kaT%d" % hb])
                S.dma("sp", qaT[hb][0:64, :], C.qT[h * 64:(h + 1) * 64, :], writes=["qaT%d" % hb])
                S.dma("sp", qaT[hb][96:97, :], C.inp["cq"][h:h + 1, :], writes=["qaT%d" % hb])
                S.dma("sp", kmf[:], C.kmT[h // 2, (h % 2) * 64:(h % 2) * 64 + 64, :], writes=["kmf"])
                S.copy("act", kmb[:, 0:NB], kmf[:], reads=["kmf"], writes=["kmb"])
            steps.append(loads)
            nq = NO // 128
            st1, st2, st3 = [], [], []
            for qs in range(nq):
                mo = qs // 2
                mi = qs % 2

                def s1(qs=qs, mo=mo, mi=mi):
                    S.mm(psG[:, 0:32], qaT[hb][0:64, qs * 128:(qs + 1) * 128], kmb[:], True, True,
                         reads=["qaT%d" % hb, "kmb"], writes=["psG"])
                    S.tt("dve", gm[:], psG[:, 0:32], gbp[:, mo, :], ALU.add, reads=["psG", "gbp"], writes=["gm"])
                    S.op("dve", lambda e: e.max(out=top8[:], in_=gm[:]), reads=["gm"], writes=["top8"])
                    S.ts("dve", f1[:], gm[:], top8[:, 2:3], ALU.is_ge, -NEG, ALU.mult, reads=["gm", "top8"], writes=["f1"])
                    S.tt("dve", f1[:], f1[:], A01[:, mo, :], ALU.mult, reads=["f1", "A01"], writes=["f1"])
                    S.tt("dve", mbt[mi][:, 64:96], f1[:], Bt[:, mo, :], ALU.add, reads=["f1", "Bt"], writes=["mbt%d" % mi])

                def s2(qs=qs, mi=mi):
                    pTt = psT[:].bitcast(BF16)[0:96, 0:128]
                    S.tr(pTt, mbt[mi][:], ident[:], reads=["mbt%d" % mi, "ident"], writes=["psT"])

                def s3(qs=qs):
                    S.copy("act", qaT[hb][64:96, qs * 128:(qs + 1) * 128], psT[:].bitcast(BF16)[64:96, 0:128],
                           reads=["psT"], writes=["qaT%d" % hb])
                st1.append(s1); st2.append(s2); st3.append(s3)
            for k in range(nq + 2):
                if 0 <= k - 2 < nq: steps.append(st3[k - 2])
                if 0 <= k - 1 < nq: steps.append(st2[k - 1])
                if k < nq: steps.append(st1[k])
            return steps

        def pairs(h, pending):
            hb = h % 2
            hl = h % 8
            for j in range(NQ):
                oi = cnt["o"] % 2; cnt["o"] += 1
                ok = "ps%d" % (2 + oi)
                b0 = (NO + 512 * j) // 256
                nkt = NKT // 2 + 4 * j + 4
                def emitS(kt):
                    si = cnt["s"] % 2; cnt["s"] += 1
                    sk = "ps%d" % si
                    n = kt // 2
                    lk = kaT[hb][0:97, kt * 128:(kt + 1) * 128]
                    if n >= b0:
                        c0 = (n - b0) * 256
                        c1 = 256 - c0
                        S.mm(psS[si][:, c1:c1 + 256], lk, qaT[hb][0:97, j * 512 + c1:j * 512 + c1 + 256],
                             True, True, reads=["kaT%d" % hb, "qaT%d" % hb], writes=[sk])
                        S.mm(psS[si][:, c0:c0 + 256], lk, qaT[hb][0:97, j * 512 + c0:j * 512 + c0 + 256],
                             True, False, reads=["kaT%d" % hb, "qaT%d" % hb], writes=[sk])
                        S.mm(psS[si][:, c0:c0 + 256], ident[:], cm[:, kt % 2, :],
                             False, True, reads=["ident", "cm"], writes=[sk])
                    else:
                        S.mm(psS[si][:, 0:512], lk, qaT[hb][0:97, j * 512:(j + 1) * 512],
                             True, True, reads=["kaT%d" % hb, "qaT%d" % hb], writes=[sk])
                    pi = cnt["pt"] % 4; cnt["pt"] += 1
                    pk = "pt%d" % pi
                    S.act(pt[pi][:], psS[si][:, 0:512], AF.Exp, reads=[sk, "kbias"], writes=[pk], bias=kbias[:, h, kt:kt + 1])
                    return pi
                pis = {0: emitS(0)}
                for kt in range(nkt):
                    if kt + 1 < nkt:
                        pis[kt + 1] = emitS(kt + 1)
                    pi = pis.pop(kt)
                    S.mm(psO[oi][0:65, 0:512], vt[:, kt, hl, :], pt[pi][:], kt == 0, kt == nkt - 1,
                         reads=["vt", "pt%d" % pi], writes=[ok])
                    for _ in range(MOBA_DUMMY):
                        S.mm(C.ps[7][:, 0:512], ident[:], cm[:].rearrange("p a b -> p (a b)"), True, True,
                             reads=["ident", "cm"], writes=["ps7"])
                    if pending and kt % 2 == 1:
                        pending.pop(0)()
                S.copy("act", oT[:], psO[oi][0:65, 0:512], reads=[ok], writes=["oT"])
                S.mm(psD[0:64, 0:512], sel65[:], oT[:], True, True, reads=["sel65", "oT"], writes=["psD"])
                S.op("dve", lambda e: e.reciprocal(out=rd[:], in_=psD[0:64, 0:512]), reads=["psD"], writes=["rd"])
                ai = cnt["ao"] % 2; cnt["ao"] += 1
                S.tt("dve", ao[ai][:], oT[0:64, :], rd[:], ALU.mult, reads=["oT", "rd"], writes=["ao%d" % ai])
                S.dma("pool", C.aT[h * 64:(h + 1) * 64, j * 512:(j + 1) * 512], ao[ai][:], reads=["ao%d" % ai])

        for f in prep_steps(0):
            f()
        for h in range(NH):
            if h % 8 == 0:
                load_v(h // 8)
            pending = prep_steps(h + 1) if h + 1 < NH else []
            pairs(h, pending)
            while pending:
                pending.pop(0)()
        S.flush()


def ssd_setup(C):
    nc = C.nc

    def inp(name, shape, dt=F32):
        C.inp[name] = nc.dram_tensor(name, list(shape), dt, kind="ExternalInput").ap()
    inp("tri_f", [128, 128]); inp("ones_f", [128, 128]); inp("trineg_f", [128, 128])
    C.ynT = dram(C, "ynT_s", [2048, C.NO], BF16)


def ssd_consts():
    s = np.arange(128)[:, None]; t = np.arange(128)[None, :]
    return {"tri_f": (s <= t).astype(np.float32), "ones_f": np.ones((128, 128), np.float32),
            "trineg_f": np.where(t >= s, 0.0, NEG).astype(np.float32)}


def phase_ssd(C, S):
    nc = C.nc
    C.ssd_stop = getattr(C, "ssd_stop", 99)
    C.ssd_sub = getattr(C, "ssd_sub", 99)
    NV, NO = C.NV, C.NO
    NCH = NV // 256
    with ExitStack() as st:
        sb = lambda name, shape, dt: st.enter_context(nc.sbuf_tensor("sd_" + name, shape, dt))
        ident = sb("ident", [128, 128], BF16); identf = sb("identf", [128, 128], F32)
        tri = sb("tri", [128, 128], F32); ones = sb("ones", [128, 128], F32); trineg = sb("trineg", [128, 128], F32)
        for tl, nm in ((ident, "ident_bf"), (identf, "ident_f"), (tri, "tri_f"), (ones, "ones_f"), (trineg, "trineg_f")):
            S.dma("sp", tl[:], C.inp[nm][:, :], writes=["consts"])
        dsk = sb("dsk", [128, SH], F32); gn = sb("gn", [128, 2048], F32); pv = sb("pv", [128, 1], F32)
        S.dma("sp", dsk[:], C.inp["d_skip"][0:1, :].partition_broadcast(128), writes=["consts"])
        S.dma("sp", gn[:], C.inp["ssd_norm_g"][0:1, :].partition_broadcast(128), writes=["consts"])
        S.dma("sp", pv[:], C.inp["pv"][0:1, :].partition_broadcast(128), writes=["consts"])
        state = sb("state", [128, 8, 256], F32); stb = sb("stb", [128, 8, 256], BF16)
        S.memset("pool", state[:], 0.0, writes=["state"])
        S.memset("pool", stb[:], 0.0, writes=["stb"])
        xsT = sb("xsT", [128, 16, 256], BF16); BTt = sb("BTt", [128, 8, 256], BF16); CTt = sb("CTt", [128, 8, 256], BF16)
        da = sb("da", [128, 2, 64], F32); zt = sb("zt", [128, 2, 2048], BF16)
        xdt = sb("xdt", [128, 2, 2048], BF16); xdtd = sb("xdtd", [128, 2, 2048], BF16); xtm = sb("xtm", [128, 2, 2048], BF16)
        Btm = sb("Btm", [128, 2, 8, 128], BF16)
        acum = sb("acum", [128, 2, 32], F32); nacum = sb("nacum", [128, 2, 32], F32); eA = sb("eA", [128, 2, 32], F32)
        dend = sb("dend", [128, 2, 32], F32); eTot = sb("eTot", [128, 32], F32); tot = sb("tot", [128, 32], F32)
        Lt = [sb("Lt%d" % i, [128, 384], F32) for i in range(2)]
        Mt = [sb("Mt%d" % i, [128, 384], BF16) for i in range(4)]
        ysb = sb("ysb", [128, 2, 2048], F32); ytmp = sb("ytmp", [128, 2048], F32)
        RA = sb("RA", [128, 3, 4, 128], F32)
        ssg = sb("ssg", [128, 8], F32); ynb = sb("ynb", [128, 2048], BF16); ynT = sb("ynT", [128, 16, 128], BF16)
        ps = C.ps
        for c in range(NCH):
            own = c >= NCH // 2
            t0 = c * 256
            to0 = t0 - NO
            S.dma("sp", xsT[:], C.xsT[:, t0:t0 + 256].rearrange("(c p) t -> p c t", p=128), writes=["xsT"])
            S.dma("sp", BTt[:], C.BT[:, t0:t0 + 256].rearrange("(c p) t -> p c t", p=128), writes=["BTt"])
            S.dma("sp", da[:], C.da[t0:t0 + 256, :].rearrange("(i p) c -> p i c", p=128), writes=["da"])
            if own:
                S.dma("sp", CTt[:], C.CT[:, to0:to0 + 256].rearrange("(c p) t -> p c t", p=128), writes=["CTt"])
                S.dma("sp", zt[:], C.z[to0:to0 + 256, :].rearrange("(i p) c -> p i c", p=128), writes=["zt"])
            S.mm(ps[6][:, 0:32], tri[:], da[:, 0, 32:64], True, True, reads=["consts", "da"], writes=["ps6"])
            S.mm(ps[6][:, 32:64], tri[:], da[:, 1, 32:64], True, False, reads=["consts", "da"], writes=["ps6"])
            S.mm(ps[6][:, 32:64], ones[:], da[:, 0, 32:64], False, True, reads=["consts", "da"], writes=["ps6"])
            S.mm(ps[6][:, 64:96], ones[:], da[:, 0, 32:64], True, False, reads=["consts", "da"], writes=["ps6"])
            S.mm(ps[6][:, 64:96], ones[:], da[:, 1, 32:64], False, True, reads=["consts", "da"], writes=["ps6"])
            S.copy("dve", acum[:].rearrange("p i h -> p (i h)"), ps[6][:, 0:64], reads=["ps6"], writes=["acum"])
            S.copy("dve", tot[:], ps[6][:, 64:96], reads=["ps6"], writes=["tot"])
            S.ts("dve", nacum[:], acum[:], -1.0, ALU.mult, reads=["acum"], writes=["nacum"])
            S.act(eA[:], acum[:], AF.Exp, reads=["acum"], writes=["eA"])
            S.act(eTot[:], tot[:], AF.Exp, reads=["tot"], writes=["eTot"])
            S.tt("dve", dend[:], nacum[:], tot[:].unsqueeze(1).to_broadcast([128, 2, 32]), ALU.add,
                 reads=["nacum", "tot"], writes=["dend"])
            S.act(dend[:], dend[:], AF.Exp, reads=["dend"], writes=["dend"])
            if C.ssd_stop <= 1:
                continue
            for i in range(2):
                pT = ps[7][:].bitcast(BF16)[:, 0:1024].rearrange("p (c t) -> p c t", t=128)
                for half in range(2):
                    for cc in range(8):
                        S.tr(pT[:, cc, :], xsT[:, half * 8 + cc, i * 128:(i + 1) * 128], ident[:],
                             reads=["xsT", "consts"], writes=["ps7"])
                    hs = slice(half * 16, half * 16 + 16)
                    dst = xdt[:, i, half * 1024:(half + 1) * 1024].rearrange("p (h d) -> p h d", d=64)
                    src = pT.rearrange("p c (h d) -> p (c h) d", d=64)
                    S.tt("dve", dst, src, da[:, i, hs].unsqueeze(2).to_broadcast([128, 16, 64]), ALU.mult,
                         reads=["ps7", "da"], writes=["xdt"])
                    if own and C.ssd_sub >= 1:
                        S.copy("act", xtm[:, i, half * 1024:(half + 1) * 1024], pT.rearrange("p c t -> p (c t)"),
                               reads=["ps7"], writes=["xtm"])
                if C.ssd_sub < 2:
                    continue
                S.tt("dve", xdtd[:, i, :].rearrange("p (h d) -> p h d", d=64), xdt[:, i, :].rearrange("p (h d) -> p h d", d=64),
                     dend[:, i, :].unsqueeze(2).to_broadcast([128, 32, 64]), ALU.mult, reads=["xdt", "dend"], writes=["xdtd"])
                if C.ssd_sub < 3:
                    continue
                pT = ps[7][:].bitcast(BF16)[:, 0:1024].rearrange("p (c t) -> p c t", t=128)
                for g in range(8):
                    S.tr(pT[:, g, :], BTt[:, g, i * 128:(i + 1) * 128], ident[:], reads=["BTt", "consts"], writes=["ps7"])
                S.copy("act", Btm[:, i, :, :], pT, reads=["ps7"], writes=["Btm"])
            if C.ssd_stop <= 2:
                continue
            if own:
                for g in range(8):
                    if C.ssd_stop <= 3 and g > 0:
                        continue
                    S.mm(ps[4][:, 0:256], BTt[:, g, 0:128], CTt[:, g, 0:256], True, True, reads=["BTt", "CTt"], writes=["ps4"])
                    S.mm(ps[4][:, 256:384], BTt[:, g, 128:256], CTt[:, g, 128:256], True, True, reads=["BTt", "CTt"], writes=["ps4"])
                    for h4 in range(4):
                        h = g * 4 + h4
                        pL = ps[h4]; lk = "ps%d" % h4
                        if h4 == 0:
                            for q_, (src_i, mat) in enumerate(((0, tri), (0, ones), (1, tri))):
                                S.tt("dve", RA[:, q_, :, :], da[:, src_i, 32 + g * 4:36 + g * 4].unsqueeze(2).to_broadcast([128, 4, 128]),
                                     mat[:].unsqueeze(1).to_broadcast([128, 4, 128]), ALU.mult, reads=["da", "consts"], writes=["RA"])
                        S.mm(pL[:, 0:128], ones[:], RA[:, 0, h4, :], True, False, reads=["RA", "consts"], writes=[lk])
                        S.mm(pL[:, 0:128], identf[:], trineg[:], False, True, reads=["consts"], writes=[lk])
                        S.mm(pL[:, 128:256], ones[:], RA[:, 1, h4, :], True, False, reads=["RA", "consts"], writes=[lk])
                        S.mm(pL[:, 128:256], ones[:], RA[:, 2, h4, :], False, True, reads=["RA", "consts"], writes=[lk])
                        S.mm(pL[:, 256:384], ones[:], RA[:, 1, h4, :], True, False, reads=["RA", "consts"], writes=[lk])
                        S.mm(pL[:, 256:384], ones[:], RA[:, 2, h4, :], False, False, reads=["RA", "consts"], writes=[lk])
                        S.mm(pL[:, 256:384], identf[:], trineg[:], False, True, reads=["consts"], writes=[lk])
                        li = h % 2
                        S.act(Lt[li][:, 0:256], pL[:, 0:256], AF.Exp, reads=[lk, "nacum"], writes=["Lt%d" % li],
                              bias=nacum[:, 0, h:h + 1])
                        S.act(Lt[li][:, 256:384], pL[:, 256:384], AF.Exp, reads=[lk, "nacum"], writes=["Lt%d" % li],
                              bias=nacum[:, 1, h:h + 1])
                        S.tt("dve", Mt[h4][:], Lt[li][:], ps[4][:, 0:384], ALU.mult, reads=["Lt%d" % li, "ps4"],
                             writes=["Mt%d" % h4])
                    if C.ssd_stop <= 4:
                        continue
                    pY = ps[5][:, 0:512].rearrange("p (i c) -> p i c", i=2)
                    for h4 in range(4):
                        h = g * 4 + h4
                        cs = slice(h4 * 64, (h4 + 1) * 64)
                        S.mm(pY[:, 0, cs], Mt[h4][:, 0:128], xdt[:, 0, h * 64:(h + 1) * 64], True, True,
                             reads=["Mt%d" % h4, "xdt"], writes=["ps5"])
                        S.mm(pY[:, 1, cs], Mt[h4][:, 128:256], xdt[:, 0, h * 64:(h + 1) * 64], True, False,
                             reads=["Mt%d" % h4, "xdt"], writes=["ps5"])
                        S.mm(pY[:, 1, cs], Mt[h4][:, 256:384], xdt[:, 1, h * 64:(h + 1) * 64], False, True,
                             reads=["Mt%d" % h4, "xdt"], writes=["ps5"])
                    pO = ps[6][:, 0:512].rearrange("p (i c) -> p i c", i=2)
                    for i in range(2):
                        S.mm(pO[:, i, :], CTt[:, g, i * 128:(i + 1) * 128], stb[:, g, :], True, True,
                             reads=["CTt", "stb"], writes=["ps6"])
                    for i in range(2):
                        yg = ysb[:, i, g * 256:(g + 1) * 256].rearrange("p (h d) -> p h d", d=64)
                        S.tt("dve", yg, pO[:, i, :].rearrange("p (h d) -> p h d", d=64),
                             eA[:, i, g * 4:(g + 1) * 4].unsqueeze(2).to_broadcast([128, 4, 64]), ALU.mult,
                             reads=["ps6", "eA"], writes=["ysb"])
                        S.tt("dve", ysb[:, i, g * 256:(g + 1) * 256], ysb[:, i, g * 256:(g + 1) * 256], pY[:, i, :], ALU.add,
                             reads=["ysb", "ps5"], writes=["ysb"])
            if C.ssd_stop <= 5:
                continue
            for g in range(8):
                for i in range(2):
                    S.mm(ps[5][:, 0:256], Btm[:, i, g, :], xdtd[:, i, g * 256:(g + 1) * 256], i == 0, i == 1,
                         reads=["Btm", "xdtd"], writes=["ps5"])
                sg = state[:, g, :].rearrange("p (h d) -> p h d", d=64)
                S.tt("dve", sg, sg, eTot[:, g * 4:(g + 1) * 4].unsqueeze(2).to_broadcast([128, 4, 64]), ALU.mult,
                     reads=["state", "eTot"], writes=["state"])
                S.tt("dve", state[:, g, :], state[:, g, :], ps[5][:, 0:256], ALU.add, reads=["state", "ps5"], writes=["state"])
            if c == NCH // 2 - 1:
                S.ts("dve", state[:].rearrange("p g c -> p (g c)"), state[:].rearrange("p g c -> p (g c)"), pv[:, 0:1], ALU.mult,
                     reads=["state", "consts"], writes=["state"])
            S.copy("act", stb[:].rearrange("p g c -> p (g c)"), state[:].rearrange("p g c -> p (g c)"), reads=["state"], writes=["stb"])
            if C.ssd_stop <= 6:
                continue
            if own:
                for i in range(2):
                    y = ysb[:, i, :]
                    y3 = y.rearrange("p (h d) -> p h d", d=64)
                    S.tt("dve", ytmp[:].rearrange("p (h d) -> p h d", d=64), xtm[:, i, :].rearrange("p (h d) -> p h d", d=64),
                         dsk[:].unsqueeze(2).to_broadcast([128, 32, 64]), ALU.mult, reads=["xtm", "consts"], writes=["ytmp"])
                    S.tt("dve", y, y, ytmp[:], ALU.add, reads=["ysb", "ytmp"], writes=["ysb"])
                    S.tt("dve", y, y, zt[:, i, :], ALU.mult, reads=["ysb", "zt"], writes=["ysb"])
                    S.tt("dve", ytmp[:], y, y, ALU.mult, reads=["ysb"], writes=["ytmp"])
                    S.op("dve", lambda e: e.tensor_reduce(out=ssg[:], in_=ytmp[:].rearrange("p (g c) -> p g c", c=256),
                                                         axis=AX.X, op=ALU.add), reads=["ytmp"], writes=["ssg"])
                    S.act(ssg[:], ssg[:], AF.Ln, reads=["ssg"], writes=["ssg"], scale=1.0 / 256, bias=EPS)
                    S.act(ssg[:], ssg[:], AF.Exp, reads=["ssg"], writes=["ssg"], scale=-0.5)
                    S.tt("dve", ytmp[:].rearrange("p (g c) -> p g c", c=256), y.rearrange("p (g c) -> p g c", c=256),
                         ssg[:].unsqueeze(2).to_broadcast([128, 8, 256]), ALU.mult, reads=["ysb", "ssg"], writes=["ytmp"])
                    S.tt("dve", ynb[:], ytmp[:], gn[:], ALU.mult, reads=["ytmp", "consts"], writes=["ynb"])
                    for half in range(2):
                        pT = ps[7][:].bitcast(BF16)[:, 0:1024].rearrange("p (c t) -> p c t", t=128)
                        for cc in range(8):
                            S.tr(pT[:, cc, :], ynb[:, (half * 8 + cc) * 128:(half * 8 + cc + 1) * 128], ident[:],
                                 reads=["ynb", "consts"], writes=["ps7"])
                        S.copy("act", ynT[:, half * 8:(half + 1) * 8, :], pT, reads=["ps7"], writes=["ynT"])
                    tok = to0 + i * 128
                    S.dma("pool", C.ynT[:, tok:tok + 128].rearrange("(c p) t -> p c t", p=128), ynT[:], reads=["ynT"])
        S.flush()


def peer_setup(C):
    nc = C.nc

    def inp(name, shape, dt=F32):
        C.inp[name] = nc.dram_tensor(name, list(shape), dt, kind="ExternalInput").ap()
    inp("R1", [128, 32, 512], BF16); inp("R2", [128, 512], BF16)
    C.x2 = dram(C, "x2_s", [C.NO, D], F32)
    C.hT2 = dram(C, "hT2_s", [C.NO // 512, 128, 8, 512], BF16)
    C.out = nc.dram_tensor("out", [C.NO, D], F32, kind="ExternalOutput").ap()


def peer_consts():
    bf = ml_dtypes.bfloat16
    R1 = np.zeros((128, 32, 4, 128), np.float32)
    for c in range(32):
        for j in range(4):
            R1[4 * c + j, c, j, :] = 1
    R2 = np.tile(np.eye(128, dtype=np.float32), (1, 4))
    return {"R1": R1.reshape(128, 32, 512).astype(bf), "R2": R2.astype(bf)}


def phase_peer(C, S):
    nc = C.nc
    C.peer_dummy = getattr(C, "peer_dummy", PEER_DUMMY)
    NO = C.NO
    UT = C.inp["peer_uT"]; VT = C.inp["peer_v"]; WQ = C.inp["w_peer_q"]
    with ExitStack() as st:
        sb = lambda name, shape, dt: st.enter_context(nc.sbuf_tensor("pr_" + name, shape, dt))
        ident = sb("ident", [128, 128], BF16)
        S.dma("sp", ident[:], C.inp["ident_bf"][:, :], writes=["ident"])
        R1 = sb("R1", [128, 32, 512], BF16); R2 = sb("R2", [128, 512], BF16)
        S.dma("sp", R1[:], C.inp["R1"][:, :, :], writes=["R1"])
        S.dma("sp", R2[:], C.inp["R2"][:, :], writes=["R2"])
        stg = sb("stg", [128, 4096], F32)
        stg2 = sb("stg2", [128, 4096], F32)
        stg2_v = stg2[:].rearrange("p (j d) -> p j d", d=1024)
        stg_u = stg[:].rearrange("p (c n) -> p c n", n=512)
        stg_v = stg[:].rearrange("p (j d) -> p j d", d=1024)
        kT = sb("kT", [128, 16, 128], BF16)
        for hh in range(2):
            S.dma("sp", stg_u[:, :, 0:128], C.inp["keys%dT" % (hh + 1)].rearrange("h d k -> d h k"), writes=["stg"])
            S.copy("pool", kT[:].rearrange("p (h two) k -> p h two k", two=2)[:, :, hh, :], stg_u[:, :, 0:128],
                   reads=["stg"], writes=["kT"])
        ub = [sb("ub%d" % i, [128, 8, 512], BF16) for i in range(2)]
        vb = [sb("vb%d" % i, [128, 4, 1024], BF16) for i in range(2)]
        xnT = sb("xnT", [128, 8, 512], BF16)
        shr = sb("shr", [128, 8192], BF16)
        qTr = shr[:].rearrange("p (c t) -> p c t", t=512)
        sb16 = sb("sb16", [128, 16, 128], BF16); sf = sb("sf", [128, 16, 128], F32); swk = sb("swk", [128, 128], F32)
        v16 = sb("v16", [128, 16, 16], F32)
        cand = sb("cand", [128, 8, 256], F32); cwk = stg2[:, 0:2048].rearrange("p (h k) -> p h k", k=256)
        t8 = sb("t8", [128, 8, 8], F32); t8b = sb("t8b", [128, 8, 8], F32)
        thr = sb("thr", [128, 4, 8], F32); nb = sb("nb", [128, 4, 8], F32); zz = sb("zz", [128, 8], F32)
        sT = sb("sT", [128, 4, 16, 128], BF16)
        tau = sb("tau", [128, 4, 8], F32); taub = sb("taub", [128, 8], BF16)
        Eb = [sb("Eb%d" % i, [128, 512], BF16) for i in range(3)]
        Em8 = [sb("Em80", [128, 8, 512], BF16), shr[:, 0:4096].rearrange("p (h e) -> p h e", e=512)]
        gsb = [sb("gsb%d" % i, [128, 4, 512], BF16) for i in range(2)]
        hact = [sb("hact%d" % i, [128, 512], BF16) for i in range(2)]
        hTt = [sb("hTt%d" % i, [128, 4, 128], BF16) for i in range(2)]
        yacc = sb("yacc", [128, 4, 1024], F32)
        ps = C.ps
        cnt = {"e": 0, "u": 0, "it": 0}

        def peer_iter(it, c, s4, ui, vi):
            ts_ = slice(s4 * 128, (s4 + 1) * 128)
            b = it % 2
            pW = ps[4]; wk = "ps4"
            A = []
            for h in range(8):
                def ah(h=h):
                    ei = cnt["e"] % 3; cnt["e"] += 1
                    pE = ps[(2, 3, 1)[ei]]; ek = "ps%d" % (2, 3, 1)[ei]
                    S.mm(pE[:, 0:512], sT[:, s4, 2 * h, :], R1[:, c, :], True, False, reads=["sT", "R1"], writes=[ek])
                    S.mm(pE[:, 0:512], sT[:, s4, 2 * h + 1, :], R2[:], False, True, reads=["sT", "R2"], writes=[ek])
                    S.act(Eb[ei][:], pE[:, 0:512], AF.Exp, reads=[ek, "nb"], writes=["Eb%d" % ei], bias=nb[:, s4, h:h + 1])
                    S.stt("dve", Em8[b][:, h, :], pE[:, 0:512], thr[:, s4, h:h + 1], Eb[ei][:], ALU.is_ge, ALU.mult,
                          reads=[ek, "thr", "Eb%d" % ei], writes=["Em8%d_%d" % (b, h)])
                A.append(ah)

            def b1a():
                for h in range(8):
                    S.mm(pW[:, 0:512], ident[:], Em8[b][:, h, :], h == 0, h == 7, reads=["Em8%d_%d" % (b, h), "ident"], writes=[wk])

            def b1():
                pass

            def b2():
                pass

            def b3():
                S.tt("dve", hact[b][:], gsb[c % 2][:, s4, :], pW[:, 0:512], ALU.mult, reads=["gsb%d" % (c % 2), wk],
                     writes=["hact%d" % b])

            def b4():
                pT = ps[0][:].bitcast(BF16)[:, 0:512].rearrange("p (j t) -> p j t", t=128)
                for j in range(4):
                    S.tr(pT[:, j, :], hact[b][:, j * 128:(j + 1) * 128], ident[:], reads=["hact%d" % b, "ident"], writes=["ps0"])

            def b5():
                pT = ps[0][:].bitcast(BF16)[:, 0:512].rearrange("p (j t) -> p j t", t=128)
                S.copy("act", hTt[b][:], pT, reads=["ps0"], writes=["hTt%d" % b])

            def b6():
                for half in range(2):
                    for j in range(4):
                        S.mm(ps[6 + half][:, 0:512], hTt[b][:, j, :], vb[vi][:, j, half * 512:(half + 1) * 512], j == 0, j == 3,
                             reads=["hTt%d" % b, "vb%d" % vi], writes=["ps%d" % (6 + half)])

            def b7():
                for half in range(2):
                    ya = yacc[:, s4, half * 512:(half + 1) * 512]
                    S.tt("dve", ya, ya, ps[6 + half][:, 0:512], ALU.add, reads=["yacc", "ps%d" % (6 + half)], writes=["yacc"])
            return A, [b1a, b1, b2, b3, b4, b5, b6, b7]

        def act_part(c, s4, ui):
            for kc in range(8):
                S.mm(ps[5][:, 0:512], xnT[:, kc, s4 * 128:(s4 + 1) * 128], ub[ui][:, kc, :], kc == 0, kc == 7,
                     reads=["ub%d" % ui, "xnT"], writes=["ps5"])
            S.copy("act", gsb[c % 2][:, s4, :], ps[5][:, 0:512], reads=["ps5"], writes=["gsb%d" % (c % 2)])

        def gelu_inplace(c):
            g2 = gsb[c % 2][:].rearrange("p a b -> p (a b)")
            S.act(g2, g2, AF.Gelu, reads=["gsb%d" % (c % 2)], writes=["gsb%d" % (c % 2)])

        prevB = []
        for rd in range(NO // 512):
            S.dma("sp", xnT[:], C.hT2[rd, :, :, :], writes=["xnT"])
            S.memset("pool", yacc[:], 0.0, writes=["yacc"])
            for pc in range(4):
                S.dma("sp", stg_u, WQ[:, pc * 512:(pc + 1) * 512].rearrange("(c p) n -> p c n", p=128), writes=["stg"])
                ui = cnt["u"] % 2; cnt["u"] += 1
                S.copy("pool", ub[ui][:], stg_u, reads=["stg"], writes=["ub%d" % ui])
                for cc in range(4):
                    for kc in range(8):
                        S.mm(ps[0][:, 0:512], ub[ui][:, kc, cc * 128:(cc + 1) * 128], xnT[:, kc, :],
                             kc == 0, kc == 7, reads=["ub%d" % ui, "xnT"], writes=["ps0"])
                    S.copy("act", qTr[:, pc * 4 + cc, :], ps[0][:, 0:512], reads=["ps0"], writes=["Em81_%d" % hh_ for hh_ in range(8)])
            for s4 in range(4):
                ts_ = slice(s4 * 128, (s4 + 1) * 128)
                for g4 in range(4):
                    for cc in range(4):
                        ch = g4 * 4 + cc
                        S.mm(ps[1][:, cc * 128:(cc + 1) * 128], qTr[:, ch, ts_], kT[:, ch, :], True, True,
                             reads=["Em81_%d" % hh_ for hh_ in range(8)] + ["kT"], writes=["ps1"])
                    S.copy("act", sb16[:, g4 * 4:(g4 + 1) * 4, :], ps[1][:, 0:512].rearrange("p (c k) -> p c k", k=128),
                           reads=["ps1"], writes=["sb16"])
                S.copy("dve", sf[:], sb16[:], reads=["sb16"], writes=["sf"])
                for ch in range(16):
                    S.op("dve", lambda e, ch=ch: e.max(out=v16[:, ch, 0:8], in_=sf[:, ch, :]), reads=["sf"], writes=["v16"])
                    S.op("dve", lambda e, ch=ch: e.match_replace(out=swk[:], in_to_replace=v16[:, ch, 0:8], in_values=sf[:, ch, :],
                                                                 imm_value=-1e30), reads=["sf", "v16"], writes=["swk"])
                    S.op("dve", lambda e, ch=ch: e.max(out=v16[:, ch, 8:16], in_=swk[:]), reads=["swk"], writes=["v16"])
                v4 = v16[:].rearrange("p (h two) k -> p h two k", two=2)
                c4 = cand[:].rearrange("p h (a b) -> p h a b", b=16)
                S.tt("dve", c4, v4[:, :, 0, :].unsqueeze(3).to_broadcast([128, 8, 16, 16]),
                     v4[:, :, 1, :].unsqueeze(2).to_broadcast([128, 8, 16, 16]), ALU.add, reads=["v16"], writes=["cand"])
                for h in range(8):
                    S.op("dve", lambda e, h=h: e.max(out=t8[:, h, :], in_=cand[:, h, :]), reads=["cand"], writes=["t8"])
                    S.op("dve", lambda e, h=h: e.match_replace(out=cwk[:, h, :], in_to_replace=t8[:, h, :], in_values=cand[:, h, :],
                                                               imm_value=-1e30), reads=["cand", "t8"], writes=["stg2"])
                    S.op("dve", lambda e, h=h: e.max(out=t8b[:, h, :], in_=cwk[:, h, :]), reads=["stg2"], writes=["t8b"])
                S.copy("dve", thr[:, s4, :], t8b[:, :, 7], reads=["t8b"], writes=["thr"])
                S.tt("dve", cwk, cand[:], t8[:, :, 0:1].to_broadcast([128, 8, 256]), ALU.subtract, reads=["cand", "t8"], writes=["stg2"])
                S.act(cwk, cwk, AF.Exp, reads=["stg2"], writes=["stg2"])
                S.tt("dve", cand[:], cand[:], thr[:, s4, :].unsqueeze(2).to_broadcast([128, 8, 256]), ALU.is_ge,
                     reads=["cand", "thr"], writes=["cand"])
                S.tt("dve", cwk, cwk, cand[:], ALU.mult, reads=["stg2", "cand"], writes=["stg2"])
                S.op("dve", lambda e: e.tensor_reduce(out=zz[:], in_=cwk, axis=AX.X, op=ALU.add), reads=["stg2"], writes=["zz"])
                S.act(zz[:], zz[:], AF.Ln, reads=["zz"], writes=["zz"])
                S.tt("dve", zz[:], zz[:], t8[:, :, 0], ALU.add, reads=["zz", "t8"], writes=["zz"])
                S.ts("dve", nb[:, s4, :], zz[:], -1.0, ALU.mult, reads=["zz"], writes=["nb"])
                for half in range(2):
                    pT = ps[1][:].bitcast(BF16)[:, 0:1024].rearrange("p (c t) -> p c t", t=128)
                    for cc in range(8):
                        S.tr(pT[:, cc, :], sb16[:, half * 8 + cc, :], ident[:], reads=["sb16", "ident"], writes=["ps1"])
                    S.copy("act", sT[:, s4, half * 8:(half + 1) * 8, :], pT, reads=["ps1"], writes=["sT"])
            def load_u(c):
                S.dma("sp", stg_u, UT[:, c * 512:(c + 1) * 512].rearrange("(kc p) n -> p kc n", p=128), writes=["stg"])
                ui_ = cnt["u"] % 2; cnt["u"] += 1
                S.copy("pool", ub[ui_][:], stg_u, reads=["stg"], writes=["ub%d" % ui_])
                return ui_

            def load_v(c):
                S.dma("sp", stg2_v, VT[c * 512:(c + 1) * 512, :].rearrange("(j p) d -> p j d", p=128), writes=["stg2"])
                S.copy("pool", vb[c % 2][:], stg2_v, reads=["stg2"], writes=["vb%d" % (c % 2)])
            events = []
            uis = {}

            def ev_load_u(c):
                uis[c] = load_u(c)
            ev_load_u(0)
            for s4_ in range(4):
                act_part(0, s4_, uis[0])
            gelu_inplace(0)
            for c in range(32):
                if c + 1 < 32:
                    events.append((c * 4 + 0 - 0.5, 0, lambda c=c: ev_load_u(c + 1)))
                    for s4_ in range(4):
                        events.append((c * 4 + s4_ + 0.55, 1, lambda c=c, s4_=s4_: act_part(c + 1, s4_, uis[c + 1])))
                    events.append((c * 4 + 3 + 0.75, 1, lambda c=c: gelu_inplace(c + 1)))
                events.append((c * 4 + 0 - 0.45, 2, lambda c=c: load_v(c)))
                for s4 in range(4):
                    it = c * 4 + s4
                    a_steps, b_steps = peer_iter(cnt["it"], c, s4, None, c % 2)
                    cnt["it"] += 1
                    for k in range(8):
                        events.append((it + k / 10.0, 3, a_steps[k]))
                    b0, _, _, b3, b4, b5, b6, b7 = b_steps
                    events.append((it + 1 + 0.35, 4, b0))
                    events.append((it + 1 + 0.45, 5, b3))
                    events.append((it + 2 + 0.05, 6, b4))
                    events.append((it + 2 + 0.15, 7, b5))
                    events.append((it + 2 + 0.36, 8, b6))
                    events.append((it + 2 + 0.46, 9, b7))
            events.sort(key=lambda e: (e[0], e[1]))
            for _, _, f in events:
                f()
            for s4 in range(4):
                tok = rd * 512 + s4 * 128
                S.dma("sp", stg[:, 0:1024], C.x2[tok:tok + 128, :], writes=["stg"])
                S.tt("dve", yacc[:, s4, :], yacc[:, s4, :], stg[:, 0:1024], ALU.add, reads=["yacc", "stg"], writes=["yacc"])
                S.dma("pool", C.out[tok:tok + 128, :], yacc[:, s4, :], reads=["yacc"])
        S.flush()


def phase_outproj(C, S):
    nc = C.nc
    NV, NO = C.NV, C.NO
    with ExitStack() as st:
        sb = lambda name, shape, dt: st.enter_context(nc.sbuf_tensor("op_" + name, shape, dt))
        ident = sb("ident", [128, 128], BF16)
        S.dma("sp", ident[:], C.inp["ident_bf"][:, :], writes=["ident"])
        stg = sb("stg", [128, 4, 1024], F32)
        Wa = sb("Wa", [128, 8, 1024], BF16); Ws = sb("Ws", [128, 16, 1024], BF16); Wo = sb("Wo", [128, 8, 1024], BF16)
        for nm, tl, nch in (("w_attn_o", Wa, 8), ("w_ssd_o", Ws, 16), ("w_out", Wo, 8)):
            for c0 in range(0, nch, 4):
                S.dma("sp", stg[:], C.inp[nm][c0 * 128:(c0 + 4) * 128, :].rearrange("(c p) n -> p c n", p=128), writes=["stg"])
                S.copy("pool", tl[:, c0:c0 + 4, :], stg[:], reads=["stg"], writes=[nm])
        aTt = [sb("aTt%d" % i, [128, 8, 128], BF16) for i in range(2)]
        yTt = [sb("yTt%d" % i, [128, 16, 128], BF16) for i in range(2)]
        ga = [sb("ga%d" % i, [128, 1024], BF16) for i in range(2)]
        gs = [sb("gs%d" % i, [128, 1024], BF16) for i in range(2)]
        xt = [sb("xt%d" % i, [128, 1024], F32) for i in range(2)]
        m1 = sb("m1", [128, 1024], F32); m2 = sb("m2", [128, 1024], F32); mb = sb("mb", [128, 1024], BF16)
        mT = sb("mT", [128, 8, 128], BF16)
        xo = [sb("xo%d" % i, [128, 1024], F32) for i in range(2)]
        ps = C.ps
        for t in range(NO // 128):
            i = t % 2
            tok = t * 128
            S.dma("sp", aTt[i][:], C.aT[:, tok:tok + 128].rearrange("(c p) t -> p c t", p=128), writes=["aTt%d" % i])
            S.dma("sp", yTt[i][:], C.ynT[:, tok:tok + 128].rearrange("(c p) t -> p c t", p=128), writes=["yTt%d" % i])
            S.dma("sp", ga[i][:], C.sga[tok:tok + 128, :], writes=["ga%d" % i])
            S.dma("sp", gs[i][:], C.sgs[tok:tok + 128, :], writes=["gs%d" % i])
            S.dma("sp", xt[i][:], C.inp["xv"][NO + tok:NO + tok + 128, :], writes=["xt%d" % i])
            for half in range(2):
                hs = slice(half * 512, (half + 1) * 512)
                for c in range(8):
                    S.mm(ps[half][:, 0:512], aTt[i][:, c, :], Wa[:, c, hs], c == 0, c == 7,
                         reads=["aTt%d" % i, "w_attn_o"], writes=["ps%d" % half])
                for c in range(16):
                    S.mm(ps[2 + half][:, 0:512], yTt[i][:, c, :], Ws[:, c, hs], c == 0, c == 15,
                         reads=["yTt%d" % i, "w_ssd_o"], writes=["ps%d" % (2 + half)])
                S.tt("dve", m1[:, hs], ps[half][:, 0:512], ga[i][:, hs], ALU.mult, reads=["ps%d" % half, "ga%d" % i], writes=["m1"])
                S.tt("dve", m2[:, hs], ps[2 + half][:, 0:512], gs[i][:, hs], ALU.mult, reads=["ps%d" % (2 + half), "gs%d" % i], writes=["m2"])
            S.tt("dve", mb[:], m1[:], m2[:], ALU.add, reads=["m1", "m2"], writes=["mb"])
            pT = ps[4][:].bitcast(BF16)[:, 0:1024].rearrange("p (c t) -> p c t", t=128)
            for c in range(8):
                S.tr(pT[:, c, :], mb[:, c * 128:(c + 1) * 128], ident[:], reads=["mb", "ident"], writes=["ps4"])
            S.copy("act", mT[:], pT, reads=["ps4"], writes=["mT"])
            for half in range(2):
                hs = slice(half * 512, (half + 1) * 512)
                for c in range(8):
                    S.mm(ps[5 + half][:, 0:512], mT[:, c, :], Wo[:, c, hs], c == 0, c == 7,
                         reads=["mT", "w_out"], writes=["ps%d" % (5 + half)])
                S.tt("dve", xo[i][:, hs], ps[5 + half][:, 0:512], xt[i][:, hs], ALU.add,
                     reads=["ps%d" % (5 + half), "xt%d" % i], writes=["xo%d" % i])
            S.dma("pool", C.x2[tok:tok + 128, :], xo[i][:], reads=["xo%d" % i])
        S.flush()


def build_all(nc, NV, st, debug=()):
    C = setup(nc, NV, debug)
    moba_setup(C); ssd_setup(C); peer_setup(C)
    S = Sched(nc, st)
    C.ps = [st.enter_context(nc.psum_tensor("ps%d" % i, [128, 512], F32)) for i in range(8)]
    phase_norm(C, S, C.inp["xv"], "norm1_g", C.hT, NV, "n1")
    phase_inproj(C, S)
    phase_moba(C, S)
    phase_ssd(C, S)
    phase_outproj(C, S)
    phase_norm(C, S, C.x2, "norm2_g", C.hT2, C.NO, "n2")
    phase_peer(C, S)
    return C, S


def make_inputs(inputs, NV, b, r, full_seq):
    bf = ml_dtypes.bfloat16
    NO = NV // 2
    f32 = lambda a: np.ascontiguousarray(np.asarray(a, dtype=np.float32))
    x = np.asarray(inputs["x"])
    ins = {}
    xv = np.zeros((NV, D), np.float32)
    if r == 0:
        xv[NO:] = x[b, 0:NO]
    else:
        xv[:] = x[b, 0:NV]
    ins["xv"] = xv
    ins["pv"] = np.full((1, 1), float(r), np.float32)
    ins["norm1_g"] = f32(inputs["norm1_g"][0:1]); ins["w_in"] = f32(inputs["w_in"][0])
    ins["q_norm_g"] = f32(inputs["q_norm_g"][0:1]); ins["k_norm_g"] = f32(inputs["k_norm_g"][0:1])
    ins["conv_wT"] = f32(np.asarray(inputs["conv_w"][0]).T); ins["conv_b"] = f32(np.asarray(inputs["conv_b"][0]).reshape(4096, 1))
    ins["dt_bias"] = f32(inputs["dt_bias"][0:1]); ins["a_log"] = f32(inputs["a_log"][0:1]); ins["d_skip"] = f32(inputs["d_skip"][0:1])
    ins["ssd_norm_g"] = f32(inputs["ssd_norm_g"][0:1]); ins["w_attn_o"] = f32(inputs["w_attn_o"][0])
    ins["w_ssd_o"] = f32(inputs["w_ssd_o"][0]); ins["w_out"] = f32(inputs["w_out"][0]); ins["norm2_g"] = f32(inputs["norm2_g"][0:1])
    ins["w_peer_q"] = f32(inputs["w_peer_q"][0])
    ins["keys1T"] = f32(np.asarray(inputs["peer_keys1"][0]).transpose(0, 2, 1))
    ins["keys2T"] = f32(np.asarray(inputs["peer_keys2"][0]).transpose(0, 2, 1))
    ins["peer_uT"] = f32(np.asarray(inputs["peer_u"][0]).T); ins["peer_v"] = f32(inputs["peer_v"][0])
    ins["ident_bf"] = np.eye(128).astype(bf); ins["ident_f"] = np.eye(128, dtype=np.float32)
    ins.update(moba_consts(NV, r)); ins.update(ssd_consts()); ins.update(peer_consts())
    return ins


NV_FULL = 8192


def kernel(**inputs):
    from concourse.bass_utils import run_bass_kernel_spmd
    nc = bass.Bass("TRN2", target_bir_lowering=False)
    with ExitStack() as st:
        C, S = build_all(nc, NV_FULL, st)
    x = np.asarray(inputs["x"])
    B = x.shape[0]
    in_maps = []
    for b in range(B):
        for r in range(2):
            in_maps.append(make_inputs(inputs, NV_FULL, b, r, NV_FULL))
    res = run_bass_kernel_spmd(nc, in_maps, core_ids=list(range(len(in_maps)))).results
    NO = NV_FULL // 2
    out = np.empty((B, NV_FULL, D), np.float32)
    for b in range(B):
        for r in range(2):
            out[b, r * NO:(r + 1) * NO] = np.asarray(res[b * 2 + r]["out"], dtype=np.float32)
    return out
```

```python
from contextlib import ExitStack
import ml_dtypes
import numpy as np
import concourse.bass as bass
import concourse.mybir as mybir

F32 = mybir.dt.float32
BF16 = mybir.dt.bfloat16
AF = mybir.ActivationFunctionType
ALU = mybir.AluOpType
AX = mybir.AxisListType

SAME_ENGINE_SYNC = True
N_DMA_SEMS = 32


class Sched:
    ENG = ("sp", "act", "dve", "pool", "pe")

    def __init__(self, nc, stack):
        self.nc = nc
        self.ops = []
        self.esem = {e: stack.enter_context(nc.semaphore("s_" + e)) for e in self.ENG}
        self.ecnt = {e: 0 for e in self.ENG}
        self.dsem = [stack.enter_context(nc.semaphore("d%d" % i)) for i in range(N_DMA_SEMS)]
        self.dcnt = [0] * N_DMA_SEMS
        self.downer = [None] * N_DMA_SEMS
        self.dnext = 0
        self.last_w = {}
        self.readers = {}
        self.waited = {e: {} for e in self.ENG}
        self.nblocks = 0
        self.nops = 0

    def _need(self, eng, ev, waits):
        if ev is None:
            return
        sem, val, src_eng, is_dma = ev
        if (not is_dma) and src_eng == eng and (eng == "pe" or not SAME_ENGINE_SYNC):
            return
        key = id(sem)
        if self.waited[eng].get(key, 0) >= val:
            return
        cur = waits.get(key)
        if cur is None or cur[1] < val:
            waits[key] = (sem, val)

    def op(self, eng, fn, reads=(), writes=(), dma=False):
        writes = list(writes) + [k for k in reads if isinstance(k, str) and k.startswith("ps") and k not in writes]
        waits = {}
        for k in reads:
            self._need(eng, self.last_w.get(k), waits)
        for k in writes:
            self._need(eng, self.last_w.get(k), waits)
            for ev in self.readers.get(k, ()):
                self._need(eng, ev, waits)
        if dma:
            half = N_DMA_SEMS // 2
            base = 0 if eng == "sp" else half
            self.dnx = getattr(self, "dnx", {})
            i = base + self.dnx.get(eng, 0)
            self.dnx[eng] = (self.dnx.get(eng, 0) + 1) % half
            if self.dcnt[i] > 0:
                self._need(eng, (self.dsem[i], 16 * self.dcnt[i], self.downer[i], True), waits)
            self.dcnt[i] += 1
            self.downer[i] = eng
            ev = (self.dsem[i], 16 * self.dcnt[i], eng, True)
            inc = (self.dsem[i], 16)
        else:
            self.ecnt[eng] += 1
            ev = (self.esem[eng], self.ecnt[eng], eng, False)
            inc = (self.esem[eng], 1)
        for (sem, val) in waits.values():
            self.waited[eng][id(sem)] = val
        for k in reads:
            self.readers.setdefault(k, []).append(ev)
        for k in writes:
            self.last_w[k] = ev
            self.readers[k] = []
        self.ops.append((eng, fn, list(waits.values()), inc))
        self.nops += 1

    def flush(self):
        fin = {}
        for i in range(N_DMA_SEMS):
            if self.dcnt[i] > 0:
                e = self.downer[i]
                if self.waited[e].get(id(self.dsem[i]), 0) < 16 * self.dcnt[i]:
                    fin.setdefault(e, []).append((self.dsem[i], 16 * self.dcnt[i]))
                    self.waited[e][id(self.dsem[i])] = 16 * self.dcnt[i]
        ops = self.ops
        self.ops = []
        if not ops and not fin:
            return
        nc = self.nc
        with nc.Block() as block:
            deco = {"sp": block.sync, "act": block.scalar, "dve": block.vector,
                    "pool": block.gpsimd, "pe": block.tensor}
            for e in self.ENG:
                mine = [o for o in ops if o[0] == e]
                tail = fin.get(e, [])
                if not mine and not tail:
                    continue

                def body(engine, mine=mine, tail=tail):
                    for (_, fn, waits, inc) in mine:
                        for (sem, val) in waits:
                            engine.wait_ge(sem, val)
                        ins = fn(engine)
                        ins.then_inc(inc[0], inc[1])
                    for (sem, val) in tail:
                        engine.wait_ge(sem, val)

                deco[e](body)
        self.nblocks += 1
        self.last_w = {}
        self.readers = {}

    def dma(self, eng, out, in_, reads=(), writes=(), **kw):
        self.op(eng, lambda e: e.dma_start(out=out, in_=in_, **kw), reads, writes, dma=True)

    def mm(self, out, lhsT, rhs, start, stop, reads=(), writes=()):
        self.op("pe", lambda e: e.matmul(out, lhsT, rhs, start=start, stop=stop), reads, writes)

    def tr(self, out, in_, ident, reads=(), writes=()):
        self.op("pe", lambda e: e.transpose(out, in_, ident), reads, writes)

    def act(self, out, in_, func, reads=(), writes=(), **kw):
        self.op("act", lambda e: e.activation(out=out, in_=in_, func=func, **kw), reads, writes)

    def tt(self, eng, out, in0, in1, op, reads=(), writes=()):
        self.op(eng, lambda e: e.tensor_tensor(out=out, in0=in0, in1=in1, op=op), reads, writes)

    def ts(self, eng, out, in0, s1, op0, s2=None, op1=None, reads=(), writes=(), **kw):
        if op1 is None:
            if op0 == ALU.pow:
                self.op(eng, lambda e: e.tensor_scalar(out=out, in0=in0, scalar1=0.0, scalar2=s1, op0=ALU.add, op1=ALU.pow, **kw),
                        reads, writes)
            else:
                self.op(eng, lambda e: e.tensor_scalar(out=out, in0=in0, scalar1=s1, scalar2=None, op0=op0, **kw),
                        reads, writes)
        else:
            self.op(eng, lambda e: e.tensor_scalar(out=out, in0=in0, scalar1=s1, scalar2=s2, op0=op0, op1=op1, **kw),
                    reads, writes)

    def stt(self, eng, out, in0, scalar, in1, op0, op1, reads=(), writes=()):
        self.op(eng, lambda e: e.scalar_tensor_tensor(out=out, in0=in0, scalar=scalar, in1=in1, op0=op0, op1=op1),
                reads, writes)

    def copy(self, eng, out, in_, reads=(), writes=()):
        if eng == "act":
            self.op(eng, lambda e: e.activation(out=out, in_=in_, func=AF.Copy), reads, writes)
        else:
            self.op(eng, lambda e: e.tensor_copy(out=out, in_=in_), reads, writes)

    def memset(self, eng, ap, val, writes=()):
        self.op(eng, lambda e: e.memset(ap, val), (), writes)


D = 1024
NH = 16
HD = 64
SH = 32
SP = 64
SG = 8
SN = 128
EPS = 1e-6
NEG = -30000.0
import os
MOBA_DUMMY = int(os.environ.get('MOBA_DUMMY', '0'))
PEER_DUMMY = int(os.environ.get('PEER_DUMMY', '0')) if 'PEER_DUMMY' in os.environ else 0
C_Q, C_K, C_V, C_Z, C_X, C_B, C_C, C_DT, C_GA, C_GS = 0, 1024, 2048, 3072, 5120, 7168, 8192, 9216, 9248, 10272
IN_COLS = 11296


class Ctx:
    pass


def dram(C, name, shape, dt):
    kind = "ExternalOutput" if name in C.debug else "Internal"
    return C.nc.dram_tensor(name, list(shape), dt, kind=kind).ap()


def setup(nc, NV, debug=()):
    C = Ctx()
    C.nc = nc
    C.NV = NV
    C.NO = NV // 2
    C.debug = set(debug)
    C.inp = {}

    def inp(name, shape, dt=F32):
        C.inp[name] = nc.dram_tensor(name, list(shape), dt, kind="ExternalInput").ap()
        return C.inp[name]
    NV_, NO = NV, C.NO
    NB = NV // 256
    C.NB = NB
    inp("xv", [NV, D])
    inp("norm1_g", [1, D]); inp("w_in", [D, IN_COLS]); inp("q_norm_g", [1, HD]); inp("k_norm_g", [1, HD])
    inp("conv_wT", [4096, 4]); inp("conv_b", [4096, 1]); inp("dt_bias", [1, SH]); inp("a_log", [1, SH])
    inp("d_skip", [1, SH]); inp("ssd_norm_g", [1, 2048]); inp("w_attn_o", [D, D]); inp("w_ssd_o", [2048, D])
    inp("w_out", [D, D]); inp("norm2_g", [1, D]); inp("w_peer_q", [D, 2048])
    inp("keys1T", [8, 128, 128]); inp("keys2T", [8, 128, 128])
    inp("peer_uT", [D, 16384]); inp("peer_v", [16384, D])
    inp("ident_bf", [128, 128], BF16); inp("ident_f", [128, 128], F32)
    inp("pv", [1, 1])
    C.hT = dram(C, "hT_s", [NV // 512, 128, 8, 512], BF16)
    C.qT = dram(C, "qT_s", [D, NO], BF16)
    C.kT = dram(C, "kT_s", [D, NV], BF16)
    C.kmT = dram(C, "kmT_s", [8, 128, NB], F32)
    C.v = dram(C, "v_s", [NV, D], BF16)
    C.z = dram(C, "z_s", [NO, 2048], BF16)
    C.xsT = dram(C, "xsT_s", [2048, NV], BF16)
    C.BT = dram(C, "BT_s", [1024, NV], BF16)
    C.CT = dram(C, "CT_s", [1024, NO], BF16)
    C.da = dram(C, "da_s", [NV, 64], F32)
    C.sga = dram(C, "sga_s", [NO, D], BF16)
    C.sgs = dram(C, "sgs_s", [NO, D], BF16)
    return C


def phase_norm(C, S, xin, gname, hT, ntok, tag):
    nc = C.nc
    with ExitStack() as st:
        sb = lambda name, shape, dt: st.enter_context(nc.sbuf_tensor(tag + name, shape, dt))
        ident = sb("ident", [128, 128], BF16)
        gT = sb("gT", [128, 8], F32)
        S.dma("sp", ident[:], C.inp["ident_bf"][:, :], writes=["ident"])
        S.dma("sp", gT[:], C.inp[gname][0, :].rearrange("(c p) -> p c", p=128), writes=["gT"],
              allow_slow_non_contiguous=True)
        xt = [sb("xt%d" % i, [128, D], F32) for i in range(2)]
        junk = sb("junk", [128, D], F32)
        ss = [sb("ss%d" % i, [128, 1], F32) for i in range(2)]
        xb = [sb("xb%d" % i, [128, D], BF16) for i in range(2)]
        ho = [sb("ho%d" % i, [128, 8, 128], BF16) for i in range(2)]
        for t in range(ntok // 128):
            i = t % 2
            pT = C.ps[t % 2][:].bitcast(BF16)[:, 0:1024].rearrange("p (c t) -> p c t", t=128)
            pk = "ps%d" % (t % 2)
            S.dma("sp", xt[i][:], xin[t * 128:(t + 1) * 128, :], writes=["xt%d" % i])
            S.act(junk[:], xt[i][:], AF.Square, reads=["xt%d" % i], writes=["junk", "ss%d" % i], accum_out=ss[i][:])
            S.act(ss[i][:], ss[i][:], AF.Ln, reads=["ss%d" % i], writes=["ss%d" % i], scale=1.0 / D, bias=EPS)
            S.act(ss[i][:], ss[i][:], AF.Exp, reads=["ss%d" % i], writes=["ss%d" % i], scale=-0.5)
            S.act(xb[i][:], xt[i][:], AF.Copy, reads=["xt%d" % i, "ss%d" % i], writes=["xb%d" % i], scale=ss[i][:])
            for c in range(8):
                S.tr(pT[:, c, :], xb[i][:, c * 128:(c + 1) * 128], ident[:], reads=["xb%d" % i, "ident"], writes=[pk])
            S.tt("dve", ho[i][:], pT, gT[:].unsqueeze(2).to_broadcast([128, 8, 128]), ALU.mult,
                 reads=[pk, "gT"], writes=["ho%d" % i])
            S.dma("pool", hT[t // 4, :, :, (t % 4) * 128:(t % 4 + 1) * 128], ho[i][:], reads=["ho%d" % i])
        S.flush()


def phase_inproj(C, S):
    nc = C.nc
    NV, NO = C.NV, C.NO
    NT = NV // 512
    NTO = NO // 512
    W = C.inp["w_in"]
    with ExitStack() as st:
        sb = lambda name, shape, dt: st.enter_context(nc.sbuf_tensor("ip_" + name, shape, dt))
        ident = sb("ident", [128, 128], BF16)
        S.dma("sp", ident[:], C.inp["ident_bf"][:, :], writes=["ident"])
        gq = sb("gq", [128, HD], F32); gk = sb("gk", [128, HD], F32)
        S.dma("sp", gq[:], C.inp["q_norm_g"][0:1, :].partition_broadcast(128), writes=["gq"])
        S.dma("sp", gk[:], C.inp["k_norm_g"][0:1, :].partition_broadcast(128), writes=["gk"])
        S.ts("dve", gq[:], gq[:], HD ** -0.5, ALU.mult, reads=["gq"], writes=["gq"])
        dtb = sb("dtb", [128, SH], F32); An = sb("An", [128, SH], F32)
        S.dma("sp", dtb[:], C.inp["dt_bias"][0:1, :].partition_broadcast(128), writes=["dtb"])
        S.dma("sp", An[:], C.inp["a_log"][0:1, :].partition_broadcast(128), writes=["An"])
        S.act(An[:], An[:], AF.Exp, reads=["An"], writes=["An"])
        S.ts("dve", An[:], An[:], -1.0, ALU.mult, reads=["An"], writes=["An"])
        cw = sb("cw", [128, 32, 4], F32); cb = sb("cb", [128, 32], F32)
        S.dma("sp", cw[:], C.inp["conv_wT"].rearrange("(c p) k -> p c k", p=128), writes=["cw"])
        S.dma("sp", cb[:], C.inp["conv_b"].rearrange("(c p) o -> p (c o)", p=128), writes=["cb"],
              allow_slow_non_contiguous=True)
        kmT = sb("kmT", [128, 8, C.NB], F32)
        S.memset("pool", kmT[:], 0.0, writes=["kmT"])
        wf = [sb("wf%d" % i, [128, 8, 512], F32) for i in range(2)]
        wb = [sb("wb%d" % i, [128, 8, 512], BF16) for i in range(2)]
        hb = [sb("hb%d" % i, [128, 8, 512], BF16) for i in range(3)]
        ev = [sb("ev%d" % i, [128, 512], F32) for i in range(2)]
        sq = sb("sq", [128, 512], F32)
        ssq = sb("ssq", [128, 8], F32)
        ob = [sb("ob%d" % i, [128, 512], BF16) for i in range(2)]
        tb = [sb("tb%d" % i, [128, 4, 128], BF16) for i in range(2)]
        kr = sb("kr", [128, 4], F32)
        cbuf = [sb("cbuf%d" % i, [128, 515], F32) for i in range(4)]
        caccs = [sb("cacc%d" % i, [128, 512], F32) for i in range(2)]
        dab = [sb("dab%d" % i, [128, 64], F32) for i in range(2)]

        blocks = []
        for j in range(2): blocks.append(("q", C_Q + 512 * j, 512, NT - NTO, j))
        for j in range(2): blocks.append(("k", C_K + 512 * j, 512, 0, j))
        for j in range(2): blocks.append(("v", C_V + 512 * j, 512, 0, j))
        for j in range(4): blocks.append(("z", C_Z + 512 * j, 512, NT - NTO, j))
        for j in range(4): blocks.append(("xs", C_X + 512 * j, 512, 0, j))
        for j in range(2): blocks.append(("B", C_B + 512 * j, 512, 0, j))
        for j in range(2): blocks.append(("C", C_C + 512 * j, 512, NT - NTO - 1, j))
        blocks.append(("dt", C_DT, 32, 0, 0))
        for j in range(2): blocks.append(("ga", C_GA + 512 * j, 512, NT - NTO, j))
        for j in range(2): blocks.append(("gs", C_GS + 512 * j, 512, NT - NTO, j))

        cnt = {"h": 0, "ps": 0, "ev": 0, "ob": 0, "tb": 0, "da": 0, "ca": 0}
        def load_w(bi):
            kind_, c0_, ncol_, _, _ = blocks[bi]
            wi_ = bi % 2
            S.dma("sp", wf[wi_][:, :, 0:ncol_], W[:, c0_:c0_ + ncol_].rearrange("(c p) n -> p c n", p=128),
                  writes=["wf%d" % wi_])
            S.copy("pool", wb[wi_][:, :, 0:ncol_], wf[wi_][:, :, 0:ncol_], reads=["wf%d" % wi_], writes=["wb%d" % wi_])
        load_w(0)
        deferred = []
        for bi, (kind, c0, ncol, t0, j) in enumerate(blocks):
            wi = bi % 2
            if bi + 1 < len(blocks):
                load_w(bi + 1)
            if kind in ("xs", "B", "C"):
                for s4 in range(4):
                    S.memset("pool", cbuf[s4][:, 0:3], 0.0, writes=["cbuf%d" % s4])
            for t in range(t0, NT):
                hi = cnt["h"] % 3; cnt["h"] += 1
                S.dma("sp", hb[hi][:], C.hT[t, :, :, :], writes=["hb%d" % hi])
                to = t - (NT - NTO)
                for s4 in range(4):
                    pi = cnt["ps"] % 4; cnt["ps"] += 1
                    ps = C.ps[pi]; pk = "ps%d" % pi
                    tok = t * 512 + s4 * 128
                    if kind in ("xs", "B", "C"):
                        for c in range(8):
                            S.mm(ps[:, 0:512], wb[wi][:, c, s4 * 128:(s4 + 1) * 128], hb[hi][:, c, :], c == 0, c == 7,
                                 reads=["wb%d" % wi, "hb%d" % hi], writes=[pk])
                        ck = "cbuf%d" % s4
                        cai = cnt["ca"] % 2; cnt["ca"] += 1
                        cacc = caccs[cai]; cak = "cacc%d" % cai
                        S.copy("act", cbuf[s4][:, 3:515], ps[:, 0:512], reads=[pk], writes=[ck])
                        chn = (c0 - C_X) // 128 + s4
                        S.ts("dve", cacc[:], cbuf[s4][:, 0:512], cw[:, chn, 0:1], ALU.mult, cb[:, chn:chn + 1], ALU.add,
                             reads=[ck, "cw", "cb"], writes=[cak])
                        for k in range(1, 4):
                            S.stt("dve", cacc[:], cbuf[s4][:, k:k + 512], cw[:, chn, k:k + 1], cacc[:], ALU.mult, ALU.add,
                                  reads=[ck, cak], writes=[cak])
                        S.copy("pool", cbuf[s4][:, 0:3], cbuf[s4][:, 512:515], reads=[ck], writes=[ck])
                        oi = cnt["ob"] % 2; cnt["ob"] += 1
                        S.act(ob[oi][:], cacc[:], AF.Silu, reads=[cak], writes=["ob%d" % oi])
                        r0 = (c0 - {"xs": C_X, "B": C_B, "C": C_C}[kind]) + s4 * 128
                        if kind == "xs":
                            S.dma("pool", C.xsT[r0:r0 + 128, t * 512:(t + 1) * 512], ob[oi][:], reads=["ob%d" % oi])
                        elif kind == "B":
                            S.dma("pool", C.BT[r0:r0 + 128, t * 512:(t + 1) * 512], ob[oi][:], reads=["ob%d" % oi])
                        elif to >= 0:
                            S.dma("pool", C.CT[r0:r0 + 128, to * 512:(to + 1) * 512], ob[oi][:], reads=["ob%d" % oi])
                        continue
                    for c in range(8):
                        S.mm(ps[:, 0:ncol], hb[hi][:, c, s4 * 128:(s4 + 1) * 128], wb[wi][:, c, 0:ncol], c == 0, c == 7,
                             reads=["wb%d" % wi, "hb%d" % hi], writes=[pk])
                    while deferred:
                        deferred.pop(0)()
                    if kind in ("q", "k"):
                        ei = cnt["ev"] % 2; cnt["ev"] += 1
                        ek = "ev%d" % ei
                        S.copy("act", ev[ei][:], ps[:, 0:512], reads=[pk], writes=[ek])
                        S.tt("dve", sq[:], ev[ei][:], ev[ei][:], ALU.mult, reads=[ek], writes=["sq"])
                        S.op("dve", lambda e: e.tensor_reduce(out=ssq[:], in_=sq[:].rearrange("p (a b) -> p a b", b=HD),
                                                             axis=AX.X, op=ALU.add), reads=["sq"], writes=["ssq"])
                        S.act(ssq[:], ssq[:], AF.Ln, reads=["ssq"], writes=["ssq"], scale=1.0 / HD, bias=EPS)
                        S.act(ssq[:], ssq[:], AF.Exp, reads=["ssq"], writes=["ssq"], scale=-0.5)
                        e3 = ev[ei][:].rearrange("p (a b) -> p a b", b=HD)
                        S.tt("dve", e3, e3, ssq[:].unsqueeze(2).to_broadcast([128, 8, HD]), ALU.mult,
                             reads=[ek, "ssq"], writes=[ek])
                        oi = cnt["ob"] % 2; cnt["ob"] += 1
                        g_ = gq if kind == "q" else gk
                        S.tt("dve", ob[oi][:].rearrange("p (a b) -> p a b", b=HD), e3,
                             g_[:].unsqueeze(1).to_broadcast([128, 8, HD]), ALU.mult,
                             reads=[ek, "gq", "gk"], writes=["ob%d" % oi])
                        def fin(kind=kind, oi=oi, j=j, to=to, s4=s4, tok=tok):
                            pti = 4 + cnt["tb"] % 2
                            ti = cnt["tb"] % 2; cnt["tb"] += 1
                            pT = C.ps[pti][:].bitcast(BF16)[:, 0:512].rearrange("p (c t) -> p c t", t=128)
                            for c in range(4):
                                S.tr(pT[:, c, :], ob[oi][:, c * 128:(c + 1) * 128], ident[:], reads=["ob%d" % oi, "ident"],
                                     writes=["ps%d" % pti])
                            S.copy("act", tb[ti][:], pT, reads=["ps%d" % pti], writes=["tb%d" % ti])
                            rows = slice(j * 512, (j + 1) * 512)
                            if kind == "q":
                                S.dma("pool", C.qT[rows, to * 512 + s4 * 128: to * 512 + (s4 + 1) * 128].rearrange("(c p) t -> p c t", p=128),
                                      tb[ti][:], reads=["tb%d" % ti])
                            else:
                                S.dma("pool", C.kT[rows, tok:tok + 128].rearrange("(c p) t -> p c t", p=128),
                                      tb[ti][:], reads=["tb%d" % ti])
                                S.op("dve", lambda e, ti=ti: e.tensor_reduce(out=kr[:], in_=tb[ti][:], axis=AX.X, op=ALU.add),
                                     reads=["tb%d" % ti], writes=["kr"])
                                blk = tok // 256
                                S.stt("dve", kmT[:, j * 4:(j + 1) * 4, blk], kr[:], 1.0 / 256, kmT[:, j * 4:(j + 1) * 4, blk],
                                      ALU.mult, ALU.add, reads=["kr", "kmT"], writes=["kmT"])
                        deferred.append(fin)
                    elif kind == "dt":
                        di = cnt["da"] % 2; cnt["da"] += 1
                        dk = "dab%d" % di
                        S.tt("dve", dab[di][:, 0:32], ps[:, 0:32], dtb[:], ALU.add, reads=[pk, "dtb"], writes=[dk])
                        S.act(dab[di][:, 0:32], dab[di][:, 0:32], AF.Exp, reads=[dk], writes=[dk])
                        S.act(dab[di][:, 0:32], dab[di][:, 0:32], AF.Ln, reads=[dk], writes=[dk], bias=1.0)
                        S.tt("dve", dab[di][:, 32:64], dab[di][:, 0:32], An[:], ALU.mult, reads=[dk, "An"], writes=[dk])
                        S.dma("pool", C.da[tok:tok + 128, :], dab[di][:], reads=[dk])
                    else:
                        oi = cnt["ob"] % 2; cnt["ob"] += 1
                        fn = {"v": AF.Copy, "z": AF.Silu, "ga": AF.Sigmoid, "gs": AF.Sigmoid}[kind]
                        S.act(ob[oi][:], ps[:, 0:512], fn, reads=[pk], writes=["ob%d" % oi])
                        if kind == "v":
                            dst = C.v[tok:tok + 128, j * 512:(j + 1) * 512]
                        else:
                            otok = to * 512 + s4 * 128
                            dst = {"z": C.z, "ga": C.sga, "gs": C.sgs}[kind][otok:otok + 128, j * 512:(j + 1) * 512]
                        S.dma("pool", dst, ob[oi][:], reads=["ob%d" % oi])
        while deferred:
            deferred.pop(0)()
        S.dma("pool", C.kmT.rearrange("c p n -> p c n"), kmT[:], reads=["kmT"])
        S.flush()


def moba_setup(C):
    nc = C.nc
    NV, NO, NB = C.NV, C.NO, C.NB

    def inp(name, shape, dt=F32):
        C.inp[name] = nc.dram_tensor(name, list(shape), dt, kind="ExternalInput").ap()
    inp("kaug_c", [33, NV], BF16)
    inp("cq", [NH, NO], BF16)
    inp("kbias", [128, NH, NV // 128])
    NBO = NO // 256
    inp("gbp", [1, NBO * 32]); inp("A01", [1, NBO * 32]); inp("Bt", [1, NBO * 32])
    inp("cm", [128, 2, 256], BF16)
    inp("sel65", [65, 64])
    C.aT = dram(C, "aT_s", [D, NO], BF16)


def moba_consts(NV, r):
    bf = ml_dtypes.bfloat16
    NO = NV // 2
    NB = NV // 256
    NBO = NO // 256
    slopes = np.exp2(-8.0 * np.arange(1, NH + 1, dtype=np.float32) / NH).astype(np.float32)
    out = {}
    ka = np.zeros((33, NV), np.float32)
    for n in range(NB):
        ka[n, n * 256:(n + 1) * 256] = 1
    ka[32] = 1
    out["kaug_c"] = ka.astype(bf)
    pos_q = (NO + np.arange(NO)).astype(np.float32)
    out["cq"] = (-slopes[:, None] * pos_q[None, :]).astype(bf)
    pos_k = (np.arange(NV // 128)[None, :] * 128 + np.arange(128)[:, None]).astype(np.float32)
    out["kbias"] = np.ascontiguousarray((slopes[None, :, None] * pos_k[:, None, :]).astype(np.float32))
    valid = np.ones(32, bool)
    valid[NB:] = False
    if r == 0:
        valid[:NB // 2] = False
    gbp = np.full((NBO, 32), NEG, np.float32); A01 = np.zeros((NBO, 32), np.float32); Bt = np.full((NBO, 32), NEG, np.float32)
    for mo in range(NBO):
        m = NB // 2 + mo
        for n in range(32):
            if n < m and valid[n]:
                gbp[mo, n] = 0; A01[mo, n] = 1
            if n == m:
                Bt[mo, n] = 0
    out["gbp"] = gbp.reshape(1, -1); out["A01"] = A01.reshape(1, -1); out["Bt"] = Bt.reshape(1, -1)
    cm = np.zeros((128, 2, 256), np.float32)
    kk = np.arange(128)[:, None]; qq = np.arange(256)[None, :]
    cm[:, 0, :] = (qq >= kk); cm[:, 1, :] = (qq >= kk + 128)
    out["cm"] = ((cm - 1.0) * 30000.0).astype(bf)
    s = np.zeros((65, 64), np.float32); s[64] = 1
    out["sel65"] = s
    return out


def phase_moba(C, S):
    nc = C.nc
    NV, NO, NB = C.NV, C.NO, C.NB
    NKT = NV // 128
    NQ = NO // 512
    NBO = NO // 256
    with ExitStack() as st:
        sb = lambda name, shape, dt: st.enter_context(nc.sbuf_tensor("mb_" + name, shape, dt))
        ident = sb("ident", [128, 128], BF16)
        S.dma("sp", ident[:], C.inp["ident_bf"][:, :], writes=["ident"])
        kaT = [sb("kaT%d" % i, [97, NV], BF16) for i in range(2)]
        qaT = [sb("qaT%d" % i, [97, NO], BF16) for i in range(2)]
        for i in range(2):
            S.dma("sp", kaT[i][64:97, :], C.inp["kaug_c"][:, :], writes=["kaT%d" % i])
        vt = sb("vt", [128, NKT, 8, 65], BF16)
        kbias = sb("kbias", [128, NH, NKT], F32)
        S.dma("sp", kbias[:], C.inp["kbias"][:, :, :], writes=["kbias"])
        gbp = sb("gbp", [128, NBO, 32], F32); A01 = sb("A01", [128, NBO, 32], F32); Bt = sb("Bt", [128, NBO, 32], F32)
        for nm, tl in (("gbp", gbp), ("A01", A01), ("Bt", Bt)):
            S.dma("sp", tl[:].rearrange("p a b -> p (a b)"), C.inp[nm][0:1, :].partition_broadcast(128), writes=[nm])
        cm = sb("cm", [128, 2, 256], BF16)
        S.dma("sp", cm[:], C.inp["cm"][:, :, :], writes=["cm"])
        sel65 = sb("sel65", [65, 64], F32)
        S.dma("sp", sel65[:], C.inp["sel65"][:, :], writes=["sel65"])
        kmf = sb("kmf", [64, NB], F32)
        kmb = sb("kmb", [64, 32], BF16)
        S.memset("pool", kmb[:], 0.0, writes=["kmb"])
        gm = sb("gm", [128, 32], F32); top8 = sb("top8", [128, 8], F32); f1 = sb("f1", [128, 32], F32)
        mbt = [sb("mbt%d" % i, [128, 96], BF16) for i in range(2)]
        for i in range(2):
            S.memset("pool", mbt[i][:], 0.0, writes=["mbt%d" % i])
        pt = [sb("pt%d" % i, [128, 512], BF16) for i in range(4)]
        oT = sb("oT", [65, 512], F32); rd = sb("rd", [64, 512], F32)
        ao = [sb("ao%d" % i, [64, 512], BF16) for i in range(2)]
        psS = [C.ps[0], C.ps[1]]; psO = [C.ps[2], C.ps[3]]; psG = C.ps[4]; psT = C.ps[5]; psD = C.ps[6]
        cnt = {"s": 0, "pt": 0, "o": 0, "ao": 0, "mb": 0}
        def load_v(g):
            S.memset("pool", vt[:, :, :, 64:65], 1.0, writes=["vt"])
            for kt0 in range(NKT):
                S.dma("sp", vt[:, kt0, :, 0:64],
                      C.v[kt0 * 128:(kt0 + 1) * 128, g * 512:(g + 1) * 512].rearrange("p (a d) -> p a d", d=64),
                      writes=["vt"])

        def prep_steps(h):
            hb = h % 2
            steps = []

            def loads():
                S.dma("sp", kaT[hb][0:64, :], C.kT[h * 64:(h + 1) * 64, :], writes=["kaT%d" % hb])
                S.dma("sp", qaT[hb][0:64, :], C.qT[h * 64:(h + 1) * 64, :], writes=["qaT%d" % hb])
                S.dma("sp", qaT[hb][96:97, :], C.inp["cq"][h:h + 1, :], writes=["qaT%d" % hb])
                S.dma("sp", kmf[:], C.kmT[h // 2, (h % 2) * 64:(h % 2) * 64 + 64, :], writes=["kmf"])
                S.copy("act", kmb[:, 0:NB], kmf[:], reads=["kmf"], writes=["kmb"])
            steps.append(loads)
            nq = NO // 128
            st1, st2, st3 = [], [], []
            for qs in range(nq):
                mo = qs // 2
                mi = qs % 2

                def s1(qs=qs, mo=mo, mi=mi):
                    S.mm(psG[:, 0:32], qaT[hb][0:64, qs * 128:(qs + 1) * 128], kmb[:], True, True,
                         reads=["qaT%d" % hb, "kmb"], writes=["psG"])
                    S.tt("dve", gm[:], psG[:, 0:32], gbp[:, mo, :], ALU.add, reads=["psG", "gbp"], writes=["gm"])
                    S.op("dve", lambda e: e.max(out=top8[:], in_=gm[:]), reads=["gm"], writes=["top8"])
                    S.ts("dve", f1[:], gm[:], top8[:, 2:3], ALU.is_ge, -NEG, ALU.mult, reads=["gm", "top8"], writes=["f1"])
                    S.tt("dve", f1[:], f1[:], A01[:, mo, :], ALU.mult, reads=["f1", "A01"], writes=["f1"])
                    S.tt("dve", mbt[mi][:, 64:96], f1[:], Bt[:, mo, :], ALU.add, reads=["f1", "Bt"], writes=["mbt%d" % mi])

                def s2(qs=qs, mi=mi):
                    pTt = psT[:].bitcast(BF16)[0:96, 0:128]
                    S.tr(pTt, mbt[mi][:], ident[:], reads=["mbt%d" % mi, "ident"], writes=["psT"])

                def s3(qs=qs):
                    S.copy("act", qaT[hb][64:96, qs * 128:(qs + 1) * 128], psT[:].bitcast(BF16)[64:96, 0:128],
                           reads=["psT"], writes=["qaT%d" % hb])
                st1.append(s1); st2.append(s2); st3.append(s3)
            for k in range(nq + 2):
                if 0 <= k - 2 < nq: steps.append(st3[k - 2])
                if 0 <= k - 1 < nq: steps.append(st2[k - 1])
                if k < nq: steps.append(st1[k])
            return steps

        def pairs(h, pending):
            hb = h % 2
            hl = h % 8
            for j in range(NQ):
                oi = cnt["o"] % 2; cnt["o"] += 1
                ok = "ps%d" % (2 + oi)
                b0 = (NO + 512 * j) // 256
                nkt = NKT // 2 + 4 * j + 4
                def emitS(kt):
                    si = cnt["s"] % 2; cnt["s"] += 1
                    sk = "ps%d" % si
                    n = kt // 2
                    lk = kaT[hb][0:97, kt * 128:(kt + 1) * 128]
                    if n >= b0:
                        c0 = (n - b0) * 256
                        c1 = 256 - c0
                        S.mm(psS[si][:, c1:c1 + 256], lk, qaT[hb][0:97, j * 512 + c1:j * 512 + c1 + 256],
                             True, True, reads=["kaT%d" % hb, "qaT%d" % hb], writes=[sk])
                        S.mm(psS[si][:, c0:c0 + 256], lk, qaT[hb][0:97, j * 512 + c0:j * 512 + c0 + 256],
                             True, False, reads=["kaT%d" % hb, "qaT%d" % hb], writes=[sk])
                        S.mm(psS[si][:, c0:c0 + 256], ident[:], cm[:, kt % 2, :],
                             False, True, reads=["ident", "cm"], writes=[sk])
                    else:
                        S.mm(psS[si][:, 0:512], lk, qaT[hb][0:97, j * 512:(j + 1) * 512],
                             True, True, reads=["kaT%d" % hb, "qaT%d" % hb], writes=[sk])
                    pi = cnt["pt"] % 4; cnt["pt"] += 1
                    pk = "pt%d" % pi
                    S.act(pt[pi][:], psS[si][:, 0:512], AF.Exp, reads=[sk, "kbias"], writes=[pk], bias=kbias[:, h, kt:kt + 1])
                    return pi
                pis = {0: emitS(0)}
                for kt in range(nkt):
                    if kt + 1 < nkt:
                        pis[kt + 1] = emitS(kt + 1)
                    pi = pis.pop(kt)
                    S.mm(psO[oi][0:65, 0:512], vt[:, kt, hl, :], pt[pi][:], kt == 0, kt == nkt - 1,
                         reads=["vt", "pt%d" % pi], writes=[ok])
                    for _ in range(MOBA_DUMMY):
                        S.mm(C.ps[7][:, 0:512], ident[:], cm[:].rearrange("p a b -> p (a b)"), True, True,
                             reads=["ident", "cm"], writes=["ps7"])
                    if pending and kt % 2 == 1:
                        pending.pop(0)()
                S.copy("act", oT[:], psO[oi][0:65, 0:512], reads=[ok], writes=["oT"])
                S.mm(psD[0:64, 0:512], sel65[:], oT[:], True, True, reads=["sel65", "oT"], writes=["psD"])
                S.op("dve", lambda e: e.reciprocal(out=rd[:], in_=psD[0:64, 0:512]), reads=["psD"], writes=["rd"])
                ai = cnt["ao"] % 2; cnt["ao"] += 1
                S.tt("dve", ao[ai][:], oT[0:64, :], rd[:], ALU.mult, reads=["oT", "rd"], writes=["ao%d" % ai])
                S.dma("pool", C.aT[h * 64:(h + 1) * 64, j * 512:(j + 1) * 512], ao[ai][:], reads=["ao%d" % ai])

        for f in prep_steps(0):
            f()
        for h in range(NH):
            if h % 8 == 0:
                load_v(h // 8)
            pending = prep_steps(h + 1) if h + 1 < NH else []
            pairs(h, pending)
            while pending:
                pending.pop(0)()
        S.flush()


def ssd_setup(C):
    nc = C.nc

    def inp(name, shape, dt=F32):
        C.inp[name] = nc.dram_tensor(name, list(shape), dt, kind="ExternalInput").ap()
    inp("tri_f", [128, 128]); inp("ones_f", [128, 128]); inp("trineg_f", [128, 128])
    C.ynT = dram(C, "ynT_s", [2048, C.NO], BF16)


def ssd_consts():
    s = np.arange(128)[:, None]; t = np.arange(128)[None, :]
    return {"tri_f": (s <= t).astype(np.float32), "ones_f": np.ones((128, 128), np.float32),
            "trineg_f": np.where(t >= s, 0.0, NEG).astype(np.float32)}


def phase_ssd(C, S):
    nc = C.nc
    C.ssd_stop = getattr(C, "ssd_stop", 99)
    C.ssd_sub = getattr(C, "ssd_sub", 99)
    NV, NO = C.NV, C.NO
    NCH = NV // 256
    with ExitStack() as st:
        sb = lambda name, shape, dt: st.enter_context(nc.sbuf_tensor("sd_" + name, shape, dt))
        ident = sb("ident", [128, 128], BF16); identf = sb("identf", [128, 128], F32)
        tri = sb("tri", [128, 128], F32); ones = sb("ones", [128, 128], F32); trineg = sb("trineg", [128, 128], F32)
        for tl, nm in ((ident, "ident_bf"), (identf, "ident_f"), (tri, "tri_f"), (ones, "ones_f"), (trineg, "trineg_f")):
            S.dma("sp", tl[:], C.inp[nm][:, :], writes=["consts"])
        dsk = sb("dsk", [128, SH], F32); gn = sb("gn", [128, 2048], F32); pv = sb("pv", [128, 1], F32)
        S.dma("sp", dsk[:], C.inp["d_skip"][0:1, :].partition_broadcast(128), writes=["consts"])
        S.dma("sp", gn[:], C.inp["ssd_norm_g"][0:1, :].partition_broadcast(128), writes=["consts"])
        S.dma("sp", pv[:], C.inp["pv"][0:1, :].partition_broadcast(128), writes=["consts"])
        state = sb("state", [128, 8, 256], F32); stb = sb("stb", [128, 8, 256], BF16)
        S.memset("pool", state[:], 0.0, writes=["state"])
        S.memset("pool", stb[:], 0.0, writes=["stb"])
        xsT = sb("xsT", [128, 16, 256], BF16); BTt = sb("BTt", [128, 8, 256], BF16); CTt = sb("CTt", [128, 8, 256], BF16)
        da = sb("da", [128, 2, 64], F32); zt = sb("zt", [128, 2, 2048], BF16)
        xdt = sb("xdt", [128, 2, 2048], BF16); xdtd = sb("xdtd", [128, 2, 2048], BF16); xtm = sb("xtm", [128, 2, 2048], BF16)
        Btm = sb("Btm", [128, 2, 8, 128], BF16)
        acum = sb("acum", [128, 2, 32], F32); nacum = sb("nacum", [128, 2, 32], F32); eA = sb("eA", [128, 2, 32], F32)
        dend = sb("dend", [128, 2, 32], F32); eTot = sb("eTot", [128, 32], F32); tot = sb("tot", [128, 32], F32)
        Lt = [sb("Lt%d" % i, [128, 384], F32) for i in range(2)]
        Mt = [sb("Mt%d" % i, [128, 384], BF16) for i in range(4)]
        ysb = sb("ysb", [128, 2, 2048], F32); ytmp = sb("ytmp", [128, 2048], F32)
        RA = sb("RA", [128, 3, 4, 128], F32)
        ssg = sb("ssg", [128, 8], F32); ynb = sb("ynb", [128, 2048], BF16); ynT = sb("ynT", [128, 16, 128], BF16)
        ps = C.ps
        for c in range(NCH):
            own = c >= NCH // 2
            t0 = c * 256
            to0 = t0 - NO
            S.dma("sp", xsT[:], C.xsT[:, t0:t0 + 256].rearrange("(c p) t -> p c t", p=128), writes=["xsT"])
            S.dma("sp", BTt[:], C.BT[:, t0:t0 + 256].rearrange("(c p) t -> p c t", p=128), writes=["BTt"])
            S.dma("sp", da[:], C.da[t0:t0 + 256, :].rearrange("(i p) c -> p i c", p=128), writes=["da"])
            if own:
                S.dma("sp", CTt[:], C.CT[:, to0:to0 + 256].rearrange("(c p) t -> p c t", p=128), writes=["CTt"])
                S.dma("sp", zt[:], C.z[to0:to0 + 256, :].rearrange("(i p) c -> p i c", p=128), writes=["zt"])
            S.mm(ps[6][:, 0:32], tri[:], da[:, 0, 32:64], True, True, reads=["consts", "da"], writes=["ps6"])
            S.mm(ps[6][:, 32:64], tri[:], da[:, 1, 32:64], True, False, reads=["consts", "da"], writes=["ps6"])
            S.mm(ps[6][:, 32:64], ones[:], da[:, 0, 32:64], False, True, reads=["consts", "da"], writes=["ps6"])
            S.mm(ps[6][:, 64:96], ones[:], da[:, 0, 32:64], True, False, reads=["consts", "da"], writes=["ps6"])
            S.mm(ps[6][:, 64:96], ones[:], da[:, 1, 32:64], False, True, reads=["consts", "da"], writes=["ps6"])
            S.copy("dve", acum[:].rearrange("p i h -> p (i h)"), ps[6][:, 0:64], reads=["ps6"], writes=["acum"])
            S.copy("dve", tot[:], ps[6][:, 64:96], reads=["ps6"], writes=["tot"])
            S.ts("dve", nacum[:], acum[:], -1.0, ALU.mult, reads=["acum"], writes=["nacum"])
            S.act(eA[:], acum[:], AF.Exp, reads=["acum"], writes=["eA"])
            S.act(eTot[:], tot[:], AF.Exp, reads=["tot"], writes=["eTot"])
            S.tt("dve", dend[:], nacum[:], tot[:].unsqueeze(1).to_broadcast([128, 2, 32]), ALU.add,
                 reads=["nacum", "tot"], writes=["dend"])
            S.act(dend[:], dend[:], AF.Exp, reads=["dend"], writes=["dend"])
            if C.ssd_stop <= 1:
                continue
            for i in range(2):
                pT = ps[7][:].bitcast(BF16)[:, 0:1024].rearrange("p (c t) -> p c t", t=128)
                for half in range(2):
                    for cc in range(8):
                        S.tr(pT[:, cc, :], xsT[:, half * 8 + cc, i * 128:(i + 1) * 128], ident[:],
                             reads=["xsT", "consts"], writes=["ps7"])
                    hs = slice(half * 16, half * 16 + 16)
                    dst = xdt[:, i, half * 1024:(half + 1) * 1024].rearrange("p (h d) -> p h d", d=64)
                    src = pT.rearrange("p c (h d) -> p (c h) d", d=64)
                    S.tt("dve", dst, src, da[:, i, hs].unsqueeze(2).to_broadcast([128, 16, 64]), ALU.mult,
                         reads=["ps7", "da"], writes=["xdt"])
                    if own and C.ssd_sub >= 1:
                        S.copy("act", xtm[:, i, half * 1024:(half + 1) * 1024], pT.rearrange("p c t -> p (c t)"),
                               reads=["ps7"], writes=["xtm"])
                if C.ssd_sub < 2:
                    continue
                S.tt("dve", xdtd[:, i, :].rearrange("p (h d) -> p h d", d=64), xdt[:, i, :].rearrange("p (h d) -> p h d", d=64),
                     dend[:, i, :].unsqueeze(2).to_broadcast([128, 32, 64]), ALU.mult, reads=["xdt", "dend"], writes=["xdtd"])
                if C.ssd_sub < 3:
                    continue
                pT = ps[7][:].bitcast(BF16)[:, 0:1024].rearrange("p (c t) -> p c t", t=128)
                for g in range(8):
                    S.tr(pT[:, g, :], BTt[:, g, i * 128:(i + 1) * 128], ident[:], reads=["BTt", "consts"], writes=["ps7"])
                S.copy("act", Btm[:, i, :, :], pT, reads=["ps7"], writes=["Btm"])
            if C.ssd_stop <= 2:
                continue
            if own:
                for g in range(8):
                    if C.ssd_stop <= 3 and g > 0:
                        continue
                    S.mm(ps[4][:, 0:256], BTt[:, g, 0:128], CTt[:, g, 0:256], True, True, reads=["BTt", "CTt"], writes=["ps4"])
                    S.mm(ps[4][:, 256:384], BTt[:, g, 128:256], CTt[:, g, 128:256], True, True, reads=["BTt", "CTt"], writes=["ps4"])
                    for h4 in range(4):
                        h = g * 4 + h4
                        pL = ps[h4]; lk = "ps%d" % h4
                        if h4 == 0:
                            for q_, (src_i, mat) in enumerate(((0, tri), (0, ones), (1, tri))):
                                S.tt("dve", RA[:, q_, :, :], da[:, src_i, 32 + g * 4:36 + g * 4].unsqueeze(2).to_broadcast([128, 4, 128]),
                                     mat[:].unsqueeze(1).to_broadcast([128, 4, 128]), ALU.mult, reads=["da", "consts"], writes=["RA"])
                        S.mm(pL[:, 0:128], ones[:], RA[:, 0, h4, :], True, False, reads=["RA", "consts"], writes=[lk])
                        S.mm(pL[:, 0:128], identf[:], trineg[:], False, True, reads=["consts"], writes=[lk])
                        S.mm(pL[:, 128:256], ones[:], RA[:, 1, h4, :], True, False, reads=["RA", "consts"], writes=[lk])
                        S.mm(pL[:, 128:256], ones[:], RA[:, 2, h4, :], False, True, reads=["RA", "consts"], writes=[lk])
                        S.mm(pL[:, 256:384], ones[:], RA[:, 1, h4, :], True, False, reads=["RA", "consts"], writes=[lk])
                        S.mm(pL[:, 256:384], ones[:], RA[:, 2, h4, :], False, False, reads=["RA", "consts"], writes=[lk])
                        S.mm(pL[:, 256:384], identf[:], trineg[:], False, True, reads=["consts"], writes=[lk])
                        li = h % 2
                        S.act(Lt[li][:, 0:256], pL[:, 0:256], AF.Exp, reads=[lk, "nacum"], writes=["Lt%d" % li],
                              bias=nacum[:, 0, h:h + 1])
                        S.act(Lt[li][:, 256:384], pL[:, 256:384], AF.Exp, reads=[lk, "nacum"], writes=["Lt%d" % li],
                              bias=nacum[:, 1, h:h + 1])
                        S.tt("dve", Mt[h4][:], Lt[li][:], ps[4][:, 0:384], ALU.mult, reads=["Lt%d" % li, "ps4"],
                             writes=["Mt%d" % h4])
                    if C.ssd_stop <= 4:
                        continue
                    pY = ps[5][:, 0:512].rearrange("p (i c) -> p i c", i=2)
                    for h4 in range(4):
                        h = g * 4 + h4
                        cs = slice(h4 * 64, (h4 + 1) * 64)
                        S.mm(pY[:, 0, cs], Mt[h4][:, 0:128], xdt[:, 0, h * 64:(h + 1) * 64], True, True,
                             reads=["Mt%d" % h4, "xdt"], writes=["ps5"])
                        S.mm(pY[:, 1, cs], Mt[h4][:, 128:256], xdt[:, 0, h * 64:(h + 1) * 64], True, False,
                             reads=["Mt%d" % h4, "xdt"], writes=["ps5"])
                        S.mm(pY[:, 1, cs], Mt[h4][:, 256:384], xdt[:, 1, h * 64:(h + 1) * 64], False, True,
                             reads=["Mt%d" % h4, "xdt"], writes=["ps5"])
                    pO = ps[6][:, 0:512].rearrange("p (i c) -> p i c", i=2)
                    for i in range(2):
                        S.mm(pO[:, i, :], CTt[:, g, i * 128:(i + 1) * 128], stb[:, g, :], True, True,
                             reads=["CTt", "stb"], writes=["ps6"])
                    for i in range(2):
                        yg = ysb[:, i, g * 256:(g + 1) * 256].rearrange("p (h d) -> p h d", d=64)
                        S.tt("dve", yg, pO[:, i, :].rearrange("p (h d) -> p h d", d=64),
                             eA[:, i, g * 4:(g + 1) * 4].unsqueeze(2).to_broadcast([128, 4, 64]), ALU.mult,
                             reads=["ps6", "eA"], writes=["ysb"])
                        S.tt("dve", ysb[:, i, g * 256:(g + 1) * 256], ysb[:, i, g * 256:(g + 1) * 256], pY[:, i, :], ALU.add,
                             reads=["ysb", "ps5"], writes=["ysb"])
            if C.ssd_stop <= 5:
                continue
            for g in range(8):
                for i in range(2):
                    S.mm(ps[5][:, 0:256], Btm[:, i, g, :], xdtd[:, i, g * 256:(g + 1) * 256], i == 0, i == 1,
                         reads=["Btm", "xdtd"], writes=["ps5"])
                sg = state[:, g, :].rearrange("p (h d) -> p h d", d=64)
                S.tt("dve", sg, sg, eTot[:, g * 4:(g + 1) * 4].unsqueeze(2).to_broadcast([128, 4, 64]), ALU.mult,
                     reads=["state", "eTot"], writes=["state"])
                S.tt("dve", state[:, g, :], state[:, g, :], ps[5][:, 0:256], ALU.add, reads=["state", "ps5"], writes=["state"])
            if c == NCH // 2 - 1:
                S.ts("dve", state[:].rearrange("p g c -> p (g c)"), state[:].rearrange("p g c -> p (g c)"), pv[:, 0:1], ALU.mult,
                     reads=["state", "consts"], writes=["state"])
            S.copy("act", stb[:].rearrange("p g c -> p (g c)"), state[:].rearrange("p g c -> p (g c)"), reads=["state"], writes=["stb"])
            if C.ssd_stop <= 6:
                continue
            if own:
                for i in range(2):
                    y = ysb[:, i, :]
                    y3 = y.rearrange("p (h d) -> p h d", d=64)
                    S.tt("dve", ytmp[:].rearrange("p (h d) -> p h d", d=64), xtm[:, i, :].rearrange("p (h d) -> p h d", d=64),
                         dsk[:].unsqueeze(2).to_broadcast([128, 32, 64]), ALU.mult, reads=["xtm", "consts"], writes=["ytmp"])
                    S.tt("dve", y, y, ytmp[:], ALU.add, reads=["ysb", "ytmp"], writes=["ysb"])
                    S.tt("dve", y, y, zt[:, i, :], ALU.mult, reads=["ysb", "zt"], writes=["ysb"])
                    S.tt("dve", ytmp[:], y, y, ALU.mult, reads=["ysb"], writes=["ytmp"])
                    S.op("dve", lambda e: e.tensor_reduce(out=ssg[:], in_=ytmp[:].rearrange("p (g c) -> p g c", c=256),
                                                         axis=AX.X, op=ALU.add), reads=["ytmp"], writes=["ssg"])
                    S.act(ssg[:], ssg[:], AF.Ln, reads=["ssg"], writes=["ssg"], scale=1.0 / 256, bias=EPS)
                    S.act(ssg[:], ssg[:], AF.Exp, reads=["ssg"], writes=["ssg"], scale=-0.5)
                    S.tt("dve", ytmp[:].rearrange("p (g c) -> p g c", c=256), y.rearrange("p (g c) -> p g c", c=256),
                         ssg[:].unsqueeze(2).to_broadcast([128, 8, 256]), ALU.mult, reads=["ysb", "ssg"], writes=["ytmp"])
                    S.tt("dve", ynb[:], ytmp[:], gn[:], ALU.mult, reads=["ytmp", "consts"], writes=["ynb"])
                    for half in range(2):
                        pT = ps[7][:].bitcast(BF16)[:, 0:1024].rearrange("p (c t) -> p c t", t=128)
                        for cc in range(8):
                            S.tr(pT[:, cc, :], ynb[:, (half * 8 + cc) * 128:(half * 8 + cc + 1) * 128], ident[:],
                                 reads=["ynb", "consts"], writes=["ps7"])
                        S.copy("act", ynT[:, half * 8:(half + 1) * 8, :], pT, reads=["ps7"], writes=["ynT"])
                    tok = to0 + i * 128
                    S.dma("pool", C.ynT[:, tok:tok + 128].rearrange("(c p) t -> p c t", p=128), ynT[:], reads=["ynT"])
        S.flush()


def peer_setup(C):
    nc = C.nc

    def inp(name, shape, dt=F32):
        C.inp[name] = nc.dram_tensor(name, list(shape), dt, kind="ExternalInput").ap()
    inp("R1", [128, 32, 512], BF16); inp("R2", [128, 512], BF16)
    C.x2 = dram(C, "x2_s", [C.NO, D], F32)
    C.hT2 = dram(C, "hT2_s", [C.NO // 512, 128, 8, 512], BF16)
    C.out = nc.dram_tensor("out", [C.NO, D], F32, kind="ExternalOutput").ap()


def peer_consts():
    bf = ml_dtypes.bfloat16
    R1 = np.zeros((128, 32, 4, 128), np.float32)
    for c in range(32):
        for j in range(4):
            R1[4 * c + j, c, j, :] = 1
    R2 = np.tile(np.eye(128, dtype=np.float32), (1, 4))
    return {"R1": R1.reshape(128, 32, 512).astype(bf), "R2": R2.astype(bf)}


def phase_peer(C, S):
    nc = C.nc
    C.peer_dummy = getattr(C, "peer_dummy", PEER_DUMMY)
    NO = C.NO
    UT = C.inp["peer_uT"]; VT = C.inp["peer_v"]; WQ = C.inp["w_peer_q"]
    with ExitStack() as st:
        sb = lambda name, shape, dt: st.enter_context(nc.sbuf_tensor("pr_" + name, shape, dt))
        ident = sb("ident", [128, 128], BF16)
        S.dma("sp", ident[:], C.inp["ident_bf"][:, :], writes=["ident"])
        R1 = sb("R1", [128, 32, 512], BF16); R2 = sb("R2", [128, 512], BF16)
        S.dma("sp", R1[:], C.inp["R1"][:, :, :], writes=["R1"])
        S.dma("sp", R2[:], C.inp["R2"][:, :], writes=["R2"])
        stg = sb("stg", [128, 4096], F32)
        stg2 = sb("stg2", [128, 4096], F32)
        stg2_v = stg2[:].rearrange("p (j d) -> p j d", d=1024)
        stg_u = stg[:].rearrange("p (c n) -> p c n", n=512)
        stg_v = stg[:].rearrange("p (j d) -> p j d", d=1024)
        kT = sb("kT", [128, 16, 128], BF16)
        for hh in range(2):
            S.dma("sp", stg_u[:, :, 0:128], C.inp["keys%dT" % (hh + 1)].rearrange("h d k -> d h k"), writes=["stg"])
            S.copy("pool", kT[:].rearrange("p (h two) k -> p h two k", two=2)[:, :, hh, :], stg_u[:, :, 0:128],
                   reads=["stg"], writes=["kT"])
        ub = [sb("ub%d" % i, [128, 8, 512], BF16) for i in range(2)]
        vb = [sb("vb%d" % i, [128, 4, 1024], BF16) for i in range(2)]
        xnT = sb("xnT", [128, 8, 512], BF16)
        shr = sb("shr", [128, 8192], BF16)
        qTr = shr[:].rearrange("p (c t) -> p c t", t=512)
        sb16 = sb("sb16", [128, 16, 128], BF16); sf = sb("sf", [128, 16, 128], F32); swk = sb("swk", [128, 128], F32)
        v16 = sb("v16", [128, 16, 16], F32)
        cand = sb("cand", [128, 8, 256], F32); cwk = stg2[:, 0:2048].rearrange("p (h k) -> p h k", k=256)
        t8 = sb("t8", [128, 8, 8], F32); t8b = sb("t8b", [128, 8, 8], F32)
        thr = sb("thr", [128, 4, 8], F32); nb = sb("nb", [128, 4, 8], F32); zz = sb("zz", [128, 8], F32)
        sT = sb("sT", [128, 4, 16, 128], BF16)
        tau = sb("tau", [128, 4, 8], F32); taub = sb("taub", [128, 8], BF16)
        Eb = [sb("Eb%d" % i, [128, 512], BF16) for i in range(3)]
        Em8 = [sb("Em80", [128, 8, 512], BF16), shr[:, 0:4096].rearrange("p (h e) -> p h e", e=512)]
        gsb = [sb("gsb%d" % i, [128, 4, 512], BF16) for i in range(2)]
        hact = [sb("hact%d" % i, [128, 512], BF16) for i in range(2)]
        hTt = [sb("hTt%d" % i, [128, 4, 128], BF16) for i in range(2)]
        yacc = sb("yacc", [128, 4, 1024], F32)
        ps = C.ps
        cnt = {"e": 0, "u": 0, "it": 0}

        def peer_iter(it, c, s4, ui, vi):
            ts_ = slice(s4 * 128, (s4 + 1) * 128)
            b = it % 2
            pW = ps[4]; wk = "ps4"
            A = []
            for h in range(8):
                def ah(h=h):
                    ei = cnt["e"] % 3; cnt["e"] += 1
                    pE = ps[(2, 3, 1)[ei]]; ek = "ps%d" % (2, 3, 1)[ei]
                    S.mm(pE[:, 0:512], sT[:, s4, 2 * h, :], R1[:, c, :], True, False, reads=["sT", "R1"], writes=[ek])
                    S.mm(pE[:, 0:512], sT[:, s4, 2 * h + 1, :], R2[:], False, True, reads=["sT", "R2"], writes=[ek])
                    S.act(Eb[ei][:], pE[:, 0:512], AF.Exp, reads=[ek, "nb"], writes=["Eb%d" % ei], bias=nb[:, s4, h:h + 1])
                    S.stt("dve", Em8[b][:, h, :], pE[:, 0:512], thr[:, s4, h:h + 1], Eb[ei][:], ALU.is_ge, ALU.mult,
                          reads=[ek, "thr", "Eb%d" % ei], writes=["Em8%d_%d" % (b, h)])
                A.append(ah)

            def b1a():
                for h in range(8):
                    S.mm(pW[:, 0:512], ident[:], Em8[b][:, h, :], h == 0, h == 7, reads=["Em8%d_%d" % (b, h), "ident"], writes=[wk])

            def b1():
                pass

            def b2():
                pass

            def b3():
                S.tt("dve", hact[b][:], gsb[c % 2][:, s4, :], pW[:, 0:512], ALU.mult, reads=["gsb%d" % (c % 2), wk],
                     writes=["hact%d" % b])

            def b4():
                pT = ps[0][:].bitcast(BF16)[:, 0:512].rearrange("p (j t) -> p j t", t=128)
                for j in range(4):
                    S.tr(pT[:, j, :], hact[b][:, j * 128:(j + 1) * 128], ident[:], reads=["hact%d" % b, "ident"], writes=["ps0"])

            def b5():
                pT = ps[0][:].bitcast(BF16)[:, 0:512].rearrange("p (j t) -> p j t", t=128)
                S.copy("act", hTt[b][:], pT, reads=["ps0"], writes=["hTt%d" % b])

            def b6():
                for half in range(2):
                    for j in range(4):
                        S.mm(ps[6 + half][:, 0:512], hTt[b][:, j, :], vb[vi][:, j, half * 512:(half + 1) * 512], j == 0, j == 3,
                             reads=["hTt%d" % b, "vb%d" % vi], writes=["ps%d" % (6 + half)])

            def b7():
                for half in range(2):
                    ya = yacc[:, s4, half * 512:(half + 1) * 512]
                    S.tt("dve", ya, ya, ps[6 + half][:, 0:512], ALU.add, reads=["yacc", "ps%d" % (6 + half)], writes=["yacc"])
            return A, [b1a, b1, b2, b3, b4, b5, b6, b7]

        def act_part(c, s4, ui):
            for kc in range(8):
                S.mm(ps[5][:, 0:512], xnT[:, kc, s4 * 128:(s4 + 1) * 128], ub[ui][:, kc, :], kc == 0, kc == 7,
                     reads=["ub%d" % ui, "xnT"], writes=["ps5"])
            S.copy("act", gsb[c % 2][:, s4, :], ps[5][:, 0:512], reads=["ps5"], writes=["gsb%d" % (c % 2)])

        def gelu_inplace(c):
            g2 = gsb[c % 2][:].rearrange("p a b -> p (a b)")
            S.act(g2, g2, AF.Gelu, reads=["gsb%d" % (c % 2)], writes=["gsb%d" % (c % 2)])

        prevB = []
        for rd in range(NO // 512):
            S.dma("sp", xnT[:], C.hT2[rd, :, :, :], writes=["xnT"])
            S.memset("pool", yacc[:], 0.0, writes=["yacc"])
            for pc in range(4):
                S.dma("sp", stg_u, WQ[:, pc * 512:(pc + 1) * 512].rearrange("(c p) n -> p c n", p=128), writes=["stg"])
                ui = cnt["u"] % 2; cnt["u"] += 1
                S.copy("pool", ub[ui][:], stg_u, reads=["stg"], writes=["ub%d" % ui])
                for cc in range(4):
                    for kc in range(8):
                        S.mm(ps[0][:, 0:512], ub[ui][:, kc, cc * 128:(cc + 1) * 128], xnT[:, kc, :],
                             kc == 0, kc == 7, reads=["ub%d" % ui, "xnT"], writes=["ps0"])
                    S.copy("act", qTr[:, pc * 4 + cc, :], ps[0][:, 0:512], reads=["ps0"], writes=["Em81_%d" % hh_ for hh_ in range(8)])
            for s4 in range(4):
                ts_ = slice(s4 * 128, (s4 + 1) * 128)
                for g4 in range(4):
                    for cc in range(4):
                        ch = g4 * 4 + cc
                        S.mm(ps[1][:, cc * 128:(cc + 1) * 128], qTr[:, ch, ts_], kT[:, ch, :], True, True,
                             reads=["Em81_%d" % hh_ for hh_ in range(8)] + ["kT"], writes=["ps1"])
                    S.copy("act", sb16[:, g4 * 4:(g4 + 1) * 4, :], ps[1][:, 0:512].rearrange("p (c k) -> p c k", k=128),
                           reads=["ps1"], writes=["sb16"])
                S.copy("dve", sf[:], sb16[:], reads=["sb16"], writes=["sf"])
                for ch in range(16):
                    S.op("dve", lambda e, ch=ch: e.max(out=v16[:, ch, 0:8], in_=sf[:, ch, :]), reads=["sf"], writes=["v16"])
                    S.op("dve", lambda e, ch=ch: e.match_replace(out=swk[:], in_to_replace=v16[:, ch, 0:8], in_values=sf[:, ch, :],
                                                                 imm_value=-1e30), reads=["sf", "v16"], writes=["swk"])
                    S.op("dve", lambda e, ch=ch: e.max(out=v16[:, ch, 8:16], in_=swk[:]), reads=["swk"], writes=["v16"])
                v4 = v16[:].rearrange("p (h two) k -> p h two k", two=2)
                c4 = cand[:].rearrange("p h (a b) -> p h a b", b=16)
                S.tt("dve", c4, v4[:, :, 0, :].unsqueeze(3).to_broadcast([128, 8, 16, 16]),
                     v4[:, :, 1, :].unsqueeze(2).to_broadcast([128, 8, 16, 16]), ALU.add, reads=["v16"], writes=["cand"])
                for h in range(8):
                    S.op("dve", lambda e, h=h: e.max(out=t8[:, h, :], in_=cand[:, h, :]), reads=["cand"], writes=["t8"])
                    S.op("dve", lambda e, h=h: e.match_replace(out=cwk[:, h, :], in_to_replace=t8[:, h, :], in_values=cand[:, h, :],
                                                               imm_value=-1e30), reads=["cand", "t8"], writes=["stg2"])
                    S.op("dve", lambda e, h=h: e.max(out=t8b[:, h, :], in_=cwk[:, h, :]), reads=["stg2"], writes=["t8b"])
                S.copy("dve", thr[:, s4, :], t8b[:, :, 7], reads=["t8b"], writes=["thr"])
                S.tt("dve", cwk, cand[:], t8[:, :, 0:1].to_broadcast([128, 8, 256]), ALU.subtract, reads=["cand", "t8"], writes=["stg2"])
                S.act(cwk, cwk, AF.Exp, reads=["stg2"], writes=["stg2"])
                S.tt("dve", cand[:], cand[:], thr[:, s4, :].unsqueeze(2).to_broadcast([128, 8, 256]), ALU.is_ge,
                     reads=["cand", "thr"], writes=["cand"])
                S.tt("dve", cwk, cwk, cand[:], ALU.mult, reads=["stg2", "cand"], writes=["stg2"])
                S.op("dve", lambda e: e.tensor_reduce(out=zz[:], in_=cwk, axis=AX.X, op=ALU.add), reads=["stg2"], writes=["zz"])
                S.act(zz[:], zz[:], AF.Ln, reads=["zz"], writes=["zz"])
                S.tt("dve", zz[:], zz[:], t8[:, :, 0], ALU.add, reads=["zz", "t8"], writes=["zz"])
                S.ts("dve", nb[:, s4, :], zz[:], -1.0, ALU.mult, reads=["zz"], writes=["nb"])
                for half in range(2):
                    pT = ps[1][:].bitcast(BF16)[:, 0:1024].rearrange("p (c t) -> p c t", t=128)
                    for cc in range(8):
                        S.tr(pT[:, cc, :], sb16[:, half * 8 + cc, :], ident[:], reads=["sb16", "ident"], writes=["ps1"])
                    S.copy("act", sT[:, s4, half * 8:(half + 1) * 8, :], pT, reads=["ps1"], writes=["sT"])
            def load_u(c):
                S.dma("sp", stg_u, UT[:, c * 512:(c + 1) * 512].rearrange("(kc p) n -> p kc n", p=128), writes=["stg"])
                ui_ = cnt["u"] % 2; cnt["u"] += 1
                S.copy("pool", ub[ui_][:], stg_u, reads=["stg"], writes=["ub%d" % ui_])
                return ui_

            def load_v(c):
                S.dma("sp", stg2_v, VT[c * 512:(c + 1) * 512, :].rearrange("(j p) d -> p j d", p=128), writes=["stg2"])
                S.copy("pool", vb[c % 2][:], stg2_v, reads=["stg2"], writes=["vb%d" % (c % 2)])
            events = []
            uis = {}

            def ev_load_u(c):
                uis[c] = load_u(c)
            ev_load_u(0)
            for s4_ in range(4):
                act_part(0, s4_, uis[0])
            gelu_inplace(0)
            for c in range(32):
                if c + 1 < 32:
                    events.append((c * 4 + 0 - 0.5, 0, lambda c=c: ev_load_u(c + 1)))
                    for s4_ in range(4):
                        events.append((c * 4 + s4_ + 0.65, 1, lambda c=c, s4_=s4_: act_part(c + 1, s4_, uis[c + 1])))
                    events.append((c * 4 + 3 + 0.75, 1, lambda c=c: gelu_inplace(c + 1)))
                events.append((c * 4 + 0 - 0.45, 2, lambda c=c: load_v(c)))
                for s4 in range(4):
                    it = c * 4 + s4
                    a_steps, b_steps = peer_iter(cnt["it"], c, s4, None, c % 2)
                    cnt["it"] += 1
                    for k in range(8):
                        events.append((it + k / 10.0, 3, a_steps[k]))
                    b0, _, _, b3, b4, b5, b6, b7 = b_steps
                    events.append((it + 1 + 0.45, 4, b0))
                    events.append((it + 1 + 0.52, 5, b3))
                    events.append((it + 2 + 0.05, 6, b4))
                    events.append((it + 2 + 0.15, 7, b5))
                    events.append((it + 2 + 0.25, 8, b6))
                    events.append((it + 2 + 0.32, 9, b7))
            events.sort(key=lambda e: (e[0], e[1]))
            for _, _, f in events:
                f()
            for s4 in range(4):
                tok = rd * 512 + s4 * 128
                S.dma("sp", stg[:, 0:1024], C.x2[tok:tok + 128, :], writes=["stg"])
                S.tt("dve", yacc[:, s4, :], yacc[:, s4, :], stg[:, 0:1024], ALU.add, reads=["yacc", "stg"], writes=["yacc"])
                S.dma("pool", C.out[tok:tok + 128, :], yacc[:, s4, :], reads=["yacc"])
        S.flush()


def phase_outproj(C, S):
    nc = C.nc
    NV, NO = C.NV, C.NO
    with ExitStack() as st:
        sb = lambda name, shape, dt: st.enter_context(nc.sbuf_tensor("op_" + name, shape, dt))
        ident = sb("ident", [128, 128], BF16)
        S.dma("sp", ident[:], C.inp["ident_bf"][:, :], writes=["ident"])
        stg = sb("stg", [128, 4, 1024], F32)
        Wa = sb("Wa", [128, 8, 1024], BF16); Ws = sb("Ws", [128, 16, 1024], BF16); Wo = sb("Wo", [128, 8, 1024], BF16)
        for nm, tl, nch in (("w_attn_o", Wa, 8), ("w_ssd_o", Ws, 16), ("w_out", Wo, 8)):
            for c0 in range(0, nch, 4):
                S.dma("sp", stg[:], C.inp[nm][c0 * 128:(c0 + 4) * 128, :].rearrange("(c p) n -> p c n", p=128), writes=["stg"])
                S.copy("pool", tl[:, c0:c0 + 4, :], stg[:], reads=["stg"], writes=[nm])
        aTt = [sb("aTt%d" % i, [128, 8, 128], BF16) for i in range(2)]
        yTt = [sb("yTt%d" % i, [128, 16, 128], BF16) for i in range(2)]
        ga = [sb("ga%d" % i, [128, 1024], BF16) for i in range(2)]
        gs = [sb("gs%d" % i, [128, 1024], BF16) for i in range(2)]
        xt = [sb("xt%d" % i, [128, 1024], F32) for i in range(2)]
        m1 = sb("m1", [128, 1024], F32); m2 = sb("m2", [128, 1024], F32); mb = sb("mb", [128, 1024], BF16)
        mT = sb("mT", [128, 8, 128], BF16)
        xo = [sb("xo%d" % i, [128, 1024], F32) for i in range(2)]
        ps = C.ps
        for t in range(NO // 128):
            i = t % 2
            tok = t * 128
            S.dma("sp", aTt[i][:], C.aT[:, tok:tok + 128].rearrange("(c p) t -> p c t", p=128), writes=["aTt%d" % i])
            S.dma("sp", yTt[i][:], C.ynT[:, tok:tok + 128].rearrange("(c p) t -> p c t", p=128), writes=["yTt%d" % i])
            S.dma("sp", ga[i][:], C.sga[tok:tok + 128, :], writes=["ga%d" % i])
            S.dma("sp", gs[i][:], C.sgs[tok:tok + 128, :], writes=["gs%d" % i])
            S.dma("sp", xt[i][:], C.inp["xv"][NO + tok:NO + tok + 128, :], writes=["xt%d" % i])
            for half in range(2):
                hs = slice(half * 512, (half + 1) * 512)
                for c in range(8):
                    S.mm(ps[half][:, 0:512], aTt[i][:, c, :], Wa[:, c, hs], c == 0, c == 7,
                         reads=["aTt%d" % i, "w_attn_o"], writes=["ps%d" % half])
                for c in range(16):
                    S.mm(ps[2 + half][:, 0:512], yTt[i][:, c, :], Ws[:, c, hs], c == 0, c == 15,
                         reads=["yTt%d" % i, "w_ssd_o"], writes=["ps%d" % (2 + half)])
                S.tt("dve", m1[:, hs], ps[half][:, 0:512], ga[i][:, hs], ALU.mult, reads=["ps%d" % half, "ga%d" % i], writes=["m1"])
                S.tt("dve", m2[:, hs], ps[2 + half][:, 0:512], gs[i][:, hs], ALU.mult, reads=["ps%d" % (2 + half), "gs%d" % i], writes=["m2"])
            S.tt("dve", mb[:], m1[:], m2[:], ALU.add, reads=["m1", "m2"], writes=["mb"])
            pT = ps[4][:].bitcast(BF16)[:, 0:1024].rearrange("p (c t) -> p c t", t=128)
            for c in range(8):
                S.tr(pT[:, c, :], mb[:, c * 128:(c + 1) * 128], ident[:], reads=["mb", "ident"], writes=["ps4"])
            S.copy("act", mT[:], pT, reads=["ps4"], writes=["mT"])
            for half in range(2):
                hs = slice(half * 512, (half + 1) * 512)
                for c in range(8):
                    S.mm(ps[5 + half][:, 0:512], mT[:, c, :], Wo[:, c, hs], c == 0, c == 7,
                         reads=["mT", "w_out"], writes=["ps%d" % (5 + half)])
                S.tt("dve", xo[i][:, hs], ps[5 + half][:, 0:512], xt[i][:, hs], ALU.add,
                     reads=["ps%d" % (5 + half), "xt%d" % i], writes=["xo%d" % i])
            S.dma("pool", C.x2[tok:tok + 128, :], xo[i][:], reads=["xo%d" % i])
        S.flush()


def build_all(nc, NV, st, debug=()):
    C = setup(nc, NV, debug)
    moba_setup(C); ssd_setup(C); peer_setup(C)
    S = Sched(nc, st)
    C.ps = [st.enter_context(nc.psum_tensor("ps%d" % i, [128, 512], F32)) for i in range(8)]
    phase_norm(C, S, C.inp["xv"], "norm1_g", C.hT, NV, "n1")
    phase_inproj(C, S)
    phase_moba(C, S)
    phase_ssd(C, S)
    phase_outproj(C, S)
    phase_norm(C, S, C.x2, "norm2_g", C.hT2, C.NO, "n2")
    phase_peer(C, S)
    return C, S


def make_inputs(inputs, NV, b, r, full_seq):
    bf = ml_dtypes.bfloat16
    NO = NV // 2
    f32 = lambda a: np.ascontiguousarray(np.asarray(a, dtype=np.float32))
    x = np.asarray(inputs["x"])
    ins = {}
    xv = np.zeros((NV, D), np.float32)
    if r == 0:
        xv[NO:] = x[b, 0:NO]
    else:
        xv[:] = x[b, 0:NV]
    ins["xv"] = xv
    ins["pv"] = np.full((1, 1), float(r), np.float32)
    ins["norm1_g"] = f32(inputs["norm1_g"][0:1]); ins["w_in"] = f32(inputs["w_in"][0])
    ins["q_norm_g"] = f32(inputs["q_norm_g"][0:1]); ins["k_norm_g"] = f32(inputs["k_norm_g"][0:1])
    ins["conv_wT"] = f32(np.asarray(inputs["conv_w"][0]).T); ins["conv_b"] = f32(np.asarray(inputs["conv_b"][0]).reshape(4096, 1))
    ins["dt_bias"] = f32(inputs["dt_bias"][0:1]); ins["a_log"] = f32(inputs["a_log"][0:1]); ins["d_skip"] = f32(inputs["d_skip"][0:1])
    ins["ssd_norm_g"] = f32(inputs["ssd_norm_g"][0:1]); ins["w_attn_o"] = f32(inputs["w_attn_o"][0])
    ins["w_ssd_o"] = f32(inputs["w_ssd_o"][0]); ins["w_out"] = f32(inputs["w_out"][0]); ins["norm2_g"] = f32(inputs["norm2_g"][0:1])
    ins["w_peer_q"] = f32(inputs["w_peer_q"][0])
    ins["keys1T"] = f32(np.asarray(inputs["peer_keys1"][0]).transpose(0, 2, 1))
    ins["keys2T"] = f32(np.asarray(inputs["peer_keys2"][0]).transpose(0, 2, 1))
    ins["peer_uT"] = f32(np.asarray(inputs["peer_u"][0]).T); ins["peer_v"] = f32(inputs["peer_v"][0])
    ins["ident_bf"] = np.eye(128).astype(bf); ins["ident_f"] = np.eye(128, dtype=np.float32)
    ins.update(moba_consts(NV, r)); ins.update(ssd_consts()); ins.update(peer_consts())
    return ins


NV_FULL = 8192


def kernel(**inputs):
    from concourse.bass_utils import run_bass_kernel_spmd
    nc = bass.Bass("TRN2", target_bir_lowering=False)
    with ExitStack() as st:
        C, S = build_all(nc, NV_FULL, st)
    x = np.asarray(inputs["x"])
    B = x.shape[0]
    in_maps = []
    for b in range(B):
        for r in range(2):
            in_maps.append(make_inputs(inputs, NV_FULL, b, r, NV_FULL))
    res = run_bass_kernel_spmd(nc, in_maps, core_ids=list(range(len(in_maps)))).results
    NO = NV_FULL // 2
    out = np.empty((B, NV_FULL, D), np.float32)
    for b in range(B):
        for r in range(2):
            out[b, r * NO:(r + 1) * NO] = np.asarray(res[b * 2 + r]["out"], dtype=np.float32)
    return out
```

```python
from contextlib import ExitStack
import ml_dtypes
import numpy as np
import concourse.bass as bass
import concourse.mybir as mybir

F32 = mybir.dt.float32
BF16 = mybir.dt.bfloat16
AF = mybir.ActivationFunctionType
ALU = mybir.AluOpType
AX = mybir.AxisListType

SAME_ENGINE_SYNC = True
N_DMA_SEMS = 32


class Sched:
    ENG = ("sp", "act", "dve", "pool", "pe")

    def __init__(self, nc, stack):
        self.nc = nc
        self.ops = []
        self.esem = {e: stack.enter_context(nc.semaphore("s_" + e)) for e in self.ENG}
        self.ecnt = {e: 0 for e in self.ENG}
        self.dsem = [stack.enter_context(nc.semaphore("d%d" % i)) for i in range(N_DMA_SEMS)]
        self.dcnt = [0] * N_DMA_SEMS
        self.downer = [None] * N_DMA_SEMS
        self.dnext = 0
        self.last_w = {}
        self.readers = {}
        self.waited = {e: {} for e in self.ENG}
        self.nblocks = 0
        self.nops = 0

    def _need(self, eng, ev, waits):
        if ev is None:
            return
        sem, val, src_eng, is_dma = ev
        if (not is_dma) and src_eng == eng and (eng == "pe" or not SAME_ENGINE_SYNC):
            return
        key = id(sem)
        if self.waited[eng].get(key, 0) >= val:
            return
        cur = waits.get(key)
        if cur is None or cur[1] < val:
            waits[key] = (sem, val)

    def op(self, eng, fn, reads=(), writes=(), dma=False):
        writes = list(writes) + [k for k in reads if isinstance(k, str) and k.startswith("ps") and k not in writes]
        waits = {}
        for k in reads:
            self._need(eng, self.last_w.get(k), waits)
        for k in writes:
            self._need(eng, self.last_w.get(k), waits)
            for ev in self.readers.get(k, ()):
                self._need(eng, ev, waits)
        if dma:
            half = N_DMA_SEMS // 2
            base = 0 if eng == "sp" else half
            self.dnx = getattr(self, "dnx", {})
            i = base + self.dnx.get(eng, 0)
            self.dnx[eng] = (self.dnx.get(eng, 0) + 1) % half
            if self.dcnt[i] > 0:
                self._need(eng, (self.dsem[i], 16 * self.dcnt[i], self.downer[i], True), waits)
            self.dcnt[i] += 1
            self.downer[i] = eng
            ev = (self.dsem[i], 16 * self.dcnt[i], eng, True)
            inc = (self.dsem[i], 16)
        else:
            self.ecnt[eng] += 1
            ev = (self.esem[eng], self.ecnt[eng], eng, False)
            inc = (self.esem[eng], 1)
        for (sem, val) in waits.values():
            self.waited[eng][id(sem)] = val
        for k in reads:
            self.readers.setdefault(k, []).append(ev)
        for k in writes:
            self.last_w[k] = ev
            self.readers[k] = []
        self.ops.append((eng, fn, list(waits.values()), inc))
        self.nops += 1

    def flush(self):
        fin = {}
        for i in range(N_DMA_SEMS):
            if self.dcnt[i] > 0:
                e = self.downer[i]
                if self.waited[e].get(id(self.dsem[i]), 0) < 16 * self.dcnt[i]:
                    fin.setdefault(e, []).append((self.dsem[i], 16 * self.dcnt[i]))
                    self.waited[e][id(self.dsem[i])] = 16 * self.dcnt[i]
        ops = self.ops
        self.ops = []
        if not ops and not fin:
            return
        nc = self.nc
        with nc.Block() as block:
            deco = {"sp": block.sync, "act": block.scalar, "dve": block.vector,
                    "pool": block.gpsimd, "pe": block.tensor}
            for e in self.ENG:
                mine = [o for o in ops if o[0] == e]
                tail = fin.get(e, [])
                if not mine and not tail:
                    continue

                def body(engine, mine=mine, tail=tail):
                    for (_, fn, waits, inc) in mine:
                        for (sem, val) in waits:
                            engine.wait_ge(sem, val)
                        ins = fn(engine)
                        ins.then_inc(inc[0], inc[1])
                    for (sem, val) in tail:
                        engine.wait_ge(sem, val)

                deco[e](body)
        self.nblocks += 1
        self.last_w = {}
        self.readers = {}

    def dma(self, eng, out, in_, reads=(), writes=(), **kw):
        self.op(eng, lambda e: e.dma_start(out=out, in_=in_, **kw), reads, writes, dma=True)

    def mm(self, out, lhsT, rhs, start, stop, reads=(), writes=()):
        self.op("pe", lambda e: e.matmul(out, lhsT, rhs, start=start, stop=stop), reads, writes)

    def tr(self, out, in_, ident, reads=(), writes=()):
        self.op("pe", lambda e: e.transpose(out, in_, ident), reads, writes)

    def act(self, out, in_, func, reads=(), writes=(), **kw):
        self.op("act", lambda e: e.activation(out=out, in_=in_, func=func, **kw), reads, writes)

    def tt(self, eng, out, in0, in1, op, reads=(), writes=()):
        self.op(eng, lambda e: e.tensor_tensor(out=out, in0=in0, in1=in1, op=op), reads, writes)

    def ts(self, eng, out, in0, s1, op0, s2=None, op1=None, reads=(), writes=(), **kw):
        if op1 is None:
            if op0 == ALU.pow:
                self.op(eng, lambda e: e.tensor_scalar(out=out, in0=in0, scalar1=0.0, scalar2=s1, op0=ALU.add, op1=ALU.pow, **kw),
                        reads, writes)
            else:
                self.op(eng, lambda e: e.tensor_scalar(out=out, in0=in0, scalar1=s1, scalar2=None, op0=op0, **kw),
                        reads, writes)
        else:
            self.op(eng, lambda e: e.tensor_scalar(out=out, in0=in0, scalar1=s1, scalar2=s2, op0=op0, op1=op1, **kw),
                    reads, writes)

    def stt(self, eng, out, in0, scalar, in1, op0, op1, reads=(), writes=()):
        self.op(eng, lambda e: e.scalar_tensor_tensor(out=out, in0=in0, scalar=scalar, in1=in1, op0=op0, op1=op1),
                reads, writes)

    def copy(self, eng, out, in_, reads=(), writes=()):
        if eng == "act":
            self.op(eng, lambda e: e.activation(out=out, in_=in_, func=AF.Copy), reads, writes)
        else:
            self.op(eng, lambda e: e.tensor_copy(out=out, in_=in_), reads, writes)

    def memset(self, eng, ap, val, writes=()):
        self.op(eng, lambda e: e.memset(ap, val), (), writes)


D = 1024
NH = 16
HD = 64
SH = 32
SP = 64
SG = 8
SN = 128
EPS = 1e-6
NEG = -30000.0
import os
MOBA_DUMMY = int(os.environ.get('MOBA_DUMMY', '0'))
PEER_DUMMY = int(os.environ.get('PEER_DUMMY', '0')) if 'PEER_DUMMY' in os.environ else 0
C_Q, C_K, C_V, C_Z, C_X, C_B, C_C, C_DT, C_GA, C_GS = 0, 1024, 2048, 3072, 5120, 7168, 8192, 9216, 9248, 10272
IN_COLS = 11296


class Ctx:
    pass


def dram(C, name, shape, dt):
    kind = "ExternalOutput" if name in C.debug else "Internal"
    return C.nc.dram_tensor(name, list(shape), dt, kind=kind).ap()


def setup(nc, NV, debug=()):
    C = Ctx()
    C.nc = nc
    C.NV = NV
    C.NO = NV // 2
    C.debug = set(debug)
    C.inp = {}

    def inp(name, shape, dt=F32):
        C.inp[name] = nc.dram_tensor(name, list(shape), dt, kind="ExternalInput").ap()
        return C.inp[name]
    NV_, NO = NV, C.NO
    NB = NV // 256
    C.NB = NB
    inp("xv", [NV, D])
    inp("norm1_g", [1, D]); inp("w_in", [D, IN_COLS]); inp("q_norm_g", [1, HD]); inp("k_norm_g", [1, HD])
    inp("conv_wT", [4096, 4]); inp("conv_b", [4096, 1]); inp("dt_bias", [1, SH]); inp("a_log", [1, SH])
    inp("d_skip", [1, SH]); inp("ssd_norm_g", [1, 2048]); inp("w_attn_o", [D, D]); inp("w_ssd_o", [2048, D])
    inp("w_out", [D, D]); inp("norm2_g", [1, D]); inp("w_peer_q", [D, 2048])
    inp("keys1T", [8, 128, 128]); inp("keys2T", [8, 128, 128])
    inp("peer_uT", [D, 16384]); inp("peer_v", [16384, D])
    inp("ident_bf", [128, 128], BF16); inp("ident_f", [128, 128], F32)
    inp("pv", [1, 1])
    C.hT = dram(C, "hT_s", [NV // 512, 128, 8, 512], BF16)
    C.qT = dram(C, "qT_s", [D, NO], BF16)
    C.kT = dram(C, "kT_s", [D, NV], BF16)
    C.kmT = dram(C, "kmT_s", [8, 128, NB], F32)
    C.v = dram(C, "v_s", [NV, D], BF16)
    C.z = dram(C, "z_s", [NO, 2048], BF16)
    C.xsT = dram(C, "xsT_s", [2048, NV], BF16)
    C.BT = dram(C, "BT_s", [1024, NV], BF16)
    C.CT = dram(C, "CT_s", [1024, NO], BF16)
    C.da = dram(C, "da_s", [NV, 64], F32)
    C.sga = dram(C, "sga_s", [NO, D], BF16)
    C.sgs = dram(C, "sgs_s", [NO, D], BF16)
    return C


def phase_norm(C, S, xin, gname, hT, ntok, tag):
    nc = C.nc
    with ExitStack() as st:
        sb = lambda name, shape, dt: st.enter_context(nc.sbuf_tensor(tag + name, shape, dt))
        ident = sb("ident", [128, 128], BF16)
        gT = sb("gT", [128, 8], F32)
        S.dma("sp", ident[:], C.inp["ident_bf"][:, :], writes=["ident"])
        S.dma("sp", gT[:], C.inp[gname][0, :].rearrange("(c p) -> p c", p=128), writes=["gT"],
              allow_slow_non_contiguous=True)
        xt = [sb("xt%d" % i, [128, D], F32) for i in range(2)]
        junk = sb("junk", [128, D], F32)
        ss = [sb("ss%d" % i, [128, 1], F32) for i in range(2)]
        xb = [sb("xb%d" % i, [128, D], BF16) for i in range(2)]
        ho = [sb("ho%d" % i, [128, 8, 128], BF16) for i in range(2)]
        for t in range(ntok // 128):
            i = t % 2
            pT = C.ps[t % 2][:].bitcast(BF16)[:, 0:1024].rearrange("p (c t) -> p c t", t=128)
            pk = "ps%d" % (t % 2)
            S.dma("sp", xt[i][:], xin[t * 128:(t + 1) * 128, :], writes=["xt%d" % i])
            S.act(junk[:], xt[i][:], AF.Square, reads=["xt%d" % i], writes=["junk", "ss%d" % i], accum_out=ss[i][:])
            S.act(ss[i][:], ss[i][:], AF.Ln, reads=["ss%d" % i], writes=["ss%d" % i], scale=1.0 / D, bias=EPS)
            S.act(ss[i][:], ss[i][:], AF.Exp, reads=["ss%d" % i], writes=["ss%d" % i], scale=-0.5)
            S.act(xb[i][:], xt[i][:], AF.Copy, reads=["xt%d" % i, "ss%d" % i], writes=["xb%d" % i], scale=ss[i][:])
            for c in range(8):
                S.tr(pT[:, c, :], xb[i][:, c * 128:(c + 1) * 128], ident[:], reads=["xb%d" % i, "ident"], writes=[pk])
            S.tt("dve", ho[i][:], pT, gT[:].unsqueeze(2).to_broadcast([128, 8, 128]), ALU.mult,
                 reads=[pk, "gT"], writes=["ho%d" % i])
            S.dma("pool", hT[t // 4, :, :, (t % 4) * 128:(t % 4 + 1) * 128], ho[i][:], reads=["ho%d" % i])
        S.flush()


def phase_inproj(C, S):
    nc = C.nc
    NV, NO = C.NV, C.NO
    NT = NV // 512
    NTO = NO // 512
    W = C.inp["w_in"]
    with ExitStack() as st:
        sb = lambda name, shape, dt: st.enter_context(nc.sbuf_tensor("ip_" + name, shape, dt))
        ident = sb("ident", [128, 128], BF16)
        S.dma("sp", ident[:], C.inp["ident_bf"][:, :], writes=["ident"])
        gq = sb("gq", [128, HD], F32); gk = sb("gk", [128, HD], F32)
        S.dma("sp", gq[:], C.inp["q_norm_g"][0:1, :].partition_broadcast(128), writes=["gq"])
        S.dma("sp", gk[:], C.inp["k_norm_g"][0:1, :].partition_broadcast(128), writes=["gk"])
        S.ts("dve", gq[:], gq[:], HD ** -0.5, ALU.mult, reads=["gq"], writes=["gq"])
        dtb = sb("dtb", [128, SH], F32); An = sb("An", [128, SH], F32)
        S.dma("sp", dtb[:], C.inp["dt_bias"][0:1, :].partition_broadcast(128), writes=["dtb"])
        S.dma("sp", An[:], C.inp["a_log"][0:1, :].partition_broadcast(128), writes=["An"])
        S.act(An[:], An[:], AF.Exp, reads=["An"], writes=["An"])
        S.ts("dve", An[:], An[:], -1.0, ALU.mult, reads=["An"], writes=["An"])
        cw = sb("cw", [128, 32, 4], F32); cb = sb("cb", [128, 32], F32)
        S.dma("sp", cw[:], C.inp["conv_wT"].rearrange("(c p) k -> p c k", p=128), writes=["cw"])
        S.dma("sp", cb[:], C.inp["conv_b"].rearrange("(c p) o -> p (c o)", p=128), writes=["cb"],
              allow_slow_non_contiguous=True)
        kmT = sb("kmT", [128, 8, C.NB], F32)
        S.memset("pool", kmT[:], 0.0, writes=["kmT"])
        wf = [sb("wf%d" % i, [128, 8, 512], F32) for i in range(2)]
        wb = [sb("wb%d" % i, [128, 8, 512], BF16) for i in range(2)]
        hb = [sb("hb%d" % i, [128, 8, 512], BF16) for i in range(3)]
        ev = [sb("ev%d" % i, [128, 512], F32) for i in range(2)]
        sq = sb("sq", [128, 512], F32)
        ssq = sb("ssq", [128, 8], F32)
        ob = [sb("ob%d" % i, [128, 512], BF16) for i in range(2)]
        tb = [sb("tb%d" % i, [128, 4, 128], BF16) for i in range(2)]
        kr = sb("kr", [128, 4], F32)
        cbuf = [sb("cbuf%d" % i, [128, 515], F32) for i in range(4)]
        caccs = [sb("cacc%d" % i, [128, 512], F32) for i in range(2)]
        dab = [sb("dab%d" % i, [128, 64], F32) for i in range(2)]

        blocks = []
        for j in range(2): blocks.append(("q", C_Q + 512 * j, 512, NT - NTO, j))
        for j in range(2): blocks.append(("k", C_K + 512 * j, 512, 0, j))
        for j in range(2): blocks.append(("v", C_V + 512 * j, 512, 0, j))
        for j in range(4): blocks.append(("z", C_Z + 512 * j, 512, NT - NTO, j))
        for j in range(4): blocks.append(("xs", C_X + 512 * j, 512, 0, j))
        for j in range(2): blocks.append(("B", C_B + 512 * j, 512, 0, j))
        for j in range(2): blocks.append(("C", C_C + 512 * j, 512, NT - NTO - 1, j))
        blocks.append(("dt", C_DT, 32, 0, 0))
        for j in range(2): blocks.append(("ga", C_GA + 512 * j, 512, NT - NTO, j))
        for j in range(2): blocks.append(("gs", C_GS + 512 * j, 512, NT - NTO, j))

        cnt = {"h": 0, "ps": 0, "ev": 0, "ob": 0, "tb": 0, "da": 0, "ca": 0}
        def load_w(bi):
            kind_, c0_, ncol_, _, _ = blocks[bi]
            wi_ = bi % 2
            S.dma("sp", wf[wi_][:, :, 0:ncol_], W[:, c0_:c0_ + ncol_].rearrange("(c p) n -> p c n", p=128),
                  writes=["wf%d" % wi_])
            S.copy("act", wb[wi_][:, :, 0:ncol_], wf[wi_][:, :, 0:ncol_], reads=["wf%d" % wi_], writes=["wb%d" % wi_])
        load_w(0)
        deferred = []
        cm_def = []
        for bi, (kind, c0, ncol, t0, j) in enumerate(blocks):
            wi = bi % 2
            while cm_def:
                cm_def.pop(0)()
            if bi + 1 < len(blocks):
                load_w(bi + 1)
            if kind in ("xs", "B", "C"):
                for s4 in range(4):
                    S.memset("pool", cbuf[s4][:, 0:3], 0.0, writes=["cbuf%d" % s4])
            for t in range(t0, NT):
                hi = cnt["h"] % 3; cnt["h"] += 1
                S.dma("sp", hb[hi][:], C.hT[t, :, :, :], writes=["hb%d" % hi])
                to = t - (NT - NTO)
                for s4 in range(4):
                    pi = cnt["ps"] % 4; cnt["ps"] += 1
                    ps = C.ps[pi]; pk = "ps%d" % pi
                    tok = t * 512 + s4 * 128
                    if kind in ("xs", "B", "C"):
                        for c in range(8):
                            S.mm(ps[:, 0:512], wb[wi][:, c, s4 * 128:(s4 + 1) * 128], hb[hi][:, c, :], c == 0, c == 7,
                                 reads=["wb%d" % wi, "hb%d" % hi], writes=[pk])
                        ck = "cbuf%d" % s4
                        cai = cnt["ca"] % 2; cnt["ca"] += 1
                        cacc = caccs[cai]; cak = "cacc%d" % cai
                        S.copy("act", cbuf[s4][:, 3:515], ps[:, 0:512], reads=[pk], writes=[ck])
                        while cm_def:
                            cm_def.pop(0)()
                        chn = (c0 - C_X) // 128 + s4
                        S.ts("dve", cacc[:], cbuf[s4][:, 0:512], cw[:, chn, 0:1], ALU.mult, cb[:, chn:chn + 1], ALU.add,
                             reads=[ck, "cw", "cb"], writes=[cak])
                        for k in range(1, 4):
                            S.stt("dve", cacc[:], cbuf[s4][:, k:k + 512], cw[:, chn, k:k + 1], cacc[:], ALU.mult, ALU.add,
                                  reads=[ck, cak], writes=[cak])
                        S.copy("pool", cbuf[s4][:, 0:3], cbuf[s4][:, 512:515], reads=[ck], writes=[ck])
                        def cm_fin(kind=kind, cacc=cacc, cak=cak, c0=c0, s4=s4, t=t, to=to):
                            oi = cnt["ob"] % 2; cnt["ob"] += 1
                            S.act(ob[oi][:], cacc[:], AF.Silu, reads=[cak], writes=["ob%d" % oi])
                            r0 = (c0 - {"xs": C_X, "B": C_B, "C": C_C}[kind]) + s4 * 128
                            if kind == "xs":
                                S.dma("pool", C.xsT[r0:r0 + 128, t * 512:(t + 1) * 512], ob[oi][:], reads=["ob%d" % oi])
                            elif kind == "B":
                                S.dma("pool", C.BT[r0:r0 + 128, t * 512:(t + 1) * 512], ob[oi][:], reads=["ob%d" % oi])
                            elif to >= 0:
                                S.dma("pool", C.CT[r0:r0 + 128, to * 512:(to + 1) * 512], ob[oi][:], reads=["ob%d" % oi])
                        cm_def.append(cm_fin)
                        continue
                    for c in range(8):
                        S.mm(ps[:, 0:ncol], hb[hi][:, c, s4 * 128:(s4 + 1) * 128], wb[wi][:, c, 0:ncol], c == 0, c == 7,
                             reads=["wb%d" % wi, "hb%d" % hi], writes=[pk])
                    while deferred:
                        deferred.pop(0)()
                    if kind in ("q", "k"):
                        ei = cnt["ev"] % 2; cnt["ev"] += 1
                        ek = "ev%d" % ei
                        S.copy("act", ev[ei][:], ps[:, 0:512], reads=[pk], writes=[ek])
                        S.tt("dve", sq[:], ev[ei][:], ev[ei][:], ALU.mult, reads=[ek], writes=["sq"])
                        S.op("dve", lambda e: e.tensor_reduce(out=ssq[:], in_=sq[:].rearrange("p (a b) -> p a b", b=HD),
                                                             axis=AX.X, op=ALU.add), reads=["sq"], writes=["ssq"])
                        S.act(ssq[:], ssq[:], AF.Ln, reads=["ssq"], writes=["ssq"], scale=1.0 / HD, bias=EPS)
                        S.act(ssq[:], ssq[:], AF.Exp, reads=["ssq"], writes=["ssq"], scale=-0.5)
                        e3 = ev[ei][:].rearrange("p (a b) -> p a b", b=HD)
                        S.tt("dve", e3, e3, ssq[:].unsqueeze(2).to_broadcast([128, 8, HD]), ALU.mult,
                             reads=[ek, "ssq"], writes=[ek])
                        oi = cnt["ob"] % 2; cnt["ob"] += 1
                        g_ = gq if kind == "q" else gk
                        S.tt("dve", ob[oi][:].rearrange("p (a b) -> p a b", b=HD), e3,
                             g_[:].unsqueeze(1).to_broadcast([128, 8, HD]), ALU.mult,
                             reads=[ek, "gq", "gk"], writes=["ob%d" % oi])
                        def fin(kind=kind, oi=oi, j=j, to=to, s4=s4, tok=tok):
                            pti = 4 + cnt["tb"] % 2
                            ti = cnt["tb"] % 2; cnt["tb"] += 1
                            pT = C.ps[pti][:].bitcast(BF16)[:, 0:512].rearrange("p (c t) -> p c t", t=128)
                            for c in range(4):
                                S.tr(pT[:, c, :], ob[oi][:, c * 128:(c + 1) * 128], ident[:], reads=["ob%d" % oi, "ident"],
                                     writes=["ps%d" % pti])
                            S.copy("act", tb[ti][:], pT, reads=["ps%d" % pti], writes=["tb%d" % ti])
                            rows = slice(j * 512, (j + 1) * 512)
                            if kind == "q":
                                S.dma("pool", C.qT[rows, to * 512 + s4 * 128: to * 512 + (s4 + 1) * 128].rearrange("(c p) t -> p c t", p=128),
                                      tb[ti][:], reads=["tb%d" % ti])
                            else:
                                S.dma("pool", C.kT[rows, tok:tok + 128].rearrange("(c p) t -> p c t", p=128),
                                      tb[ti][:], reads=["tb%d" % ti])
                                S.op("dve", lambda e, ti=ti: e.tensor_reduce(out=kr[:], in_=tb[ti][:], axis=AX.X, op=ALU.add),
                                     reads=["tb%d" % ti], writes=["kr"])
                                blk = tok // 256
                                S.stt("dve", kmT[:, j * 4:(j + 1) * 4, blk], kr[:], 1.0 / 256, kmT[:, j * 4:(j + 1) * 4, blk],
                                      ALU.mult, ALU.add, reads=["kr", "kmT"], writes=["kmT"])
                        deferred.append(fin)
                    elif kind == "dt":
                        di = cnt["da"] % 2; cnt["da"] += 1
                        dk = "dab%d" % di
                        S.tt("dve", dab[di][:, 0:32], ps[:, 0:32], dtb[:], ALU.add, reads=[pk, "dtb"], writes=[dk])
                        S.act(dab[di][:, 0:32], dab[di][:, 0:32], AF.Exp, reads=[dk], writes=[dk])
                        S.act(dab[di][:, 0:32], dab[di][:, 0:32], AF.Ln, reads=[dk], writes=[dk], bias=1.0)
                        S.tt("dve", dab[di][:, 32:64], dab[di][:, 0:32], An[:], ALU.mult, reads=[dk, "An"], writes=[dk])
                        S.dma("pool", C.da[tok:tok + 128, :], dab[di][:], reads=[dk])
                    else:
                        oi = cnt["ob"] % 2; cnt["ob"] += 1
                        fn = {"v": AF.Copy, "z": AF.Silu, "ga": AF.Sigmoid, "gs": AF.Sigmoid}[kind]
                        S.act(ob[oi][:], ps[:, 0:512], fn, reads=[pk], writes=["ob%d" % oi])
                        if kind == "v":
                            dst = C.v[tok:tok + 128, j * 512:(j + 1) * 512]
                        else:
                            otok = to * 512 + s4 * 128
                            dst = {"z": C.z, "ga": C.sga, "gs": C.sgs}[kind][otok:otok + 128, j * 512:(j + 1) * 512]
                        S.dma("pool", dst, ob[oi][:], reads=["ob%d" % oi])
        while deferred:
            deferred.pop(0)()
        while cm_def:
            cm_def.pop(0)()
        S.dma("pool", C.kmT.rearrange("c p n -> p c n"), kmT[:], reads=["kmT"])
        S.flush()


def moba_setup(C):
    nc = C.nc
    NV, NO, NB = C.NV, C.NO, C.NB

    def inp(name, shape, dt=F32):
        C.inp[name] = nc.dram_tensor(name, list(shape), dt, kind="ExternalInput").ap()
    inp("kaug_c", [33, NV], BF16)
    inp("cq", [NH, NO], BF16)
    inp("kbias", [128, NH, NV // 128])
    NBO = NO // 256
    inp("gbp", [1, NBO * 32]); inp("A01", [1, NBO * 32]); inp("Bt", [1, NBO * 32])
    inp("cm", [128, 2, 256], BF16)
    inp("sel65", [65, 64])
    C.aT = dram(C, "aT_s", [D, NO], BF16)


def moba_consts(NV, r):
    bf = ml_dtypes.bfloat16
    NO = NV // 2
    NB = NV // 256
    NBO = NO // 256
    slopes = np.exp2(-8.0 * np.arange(1, NH + 1, dtype=np.float32) / NH).astype(np.float32)
    out = {}
    ka = np.zeros((33, NV), np.float32)
    for n in range(NB):
        ka[n, n * 256:(n + 1) * 256] = 1
    ka[32] = 1
    out["kaug_c"] = ka.astype(bf)
    pos_q = (NO + np.arange(NO)).astype(np.float32)
    out["cq"] = (-slopes[:, None] * pos_q[None, :]).astype(bf)
    pos_k = (np.arange(NV // 128)[None, :] * 128 + np.arange(128)[:, None]).astype(np.float32)
    out["kbias"] = np.ascontiguousarray((slopes[None, :, None] * pos_k[:, None, :]).astype(np.float32))
    valid = np.ones(32, bool)
    valid[NB:] = False
    if r == 0:
        valid[:NB // 2] = False
    gbp = np.full((NBO, 32), NEG, np.float32); A01 = np.zeros((NBO, 32), np.float32); Bt = np.full((NBO, 32), NEG, np.float32)
    for mo in range(NBO):
        m = NB // 2 + mo
        for n in range(32):
            if n < m and valid[n]:
                gbp[mo, n] = 0; A01[mo, n] = 1
            if n == m:
                Bt[mo, n] = 0
    out["gbp"] = gbp.reshape(1, -1); out["A01"] = A01.reshape(1, -1); out["Bt"] = Bt.reshape(1, -1)
    cm = np.zeros((128, 2, 256), np.float32)
    kk = np.arange(128)[:, None]; qq = np.arange(256)[None, :]
    cm[:, 0, :] = (qq >= kk); cm[:, 1, :] = (qq >= kk + 128)
    out["cm"] = ((cm - 1.0) * 30000.0).astype(bf)
    s = np.zeros((65, 64), np.float32); s[64] = 1
    out["sel65"] = s
    return out


def phase_moba(C, S):
    nc = C.nc
    NV, NO, NB = C.NV, C.NO, C.NB
    NKT = NV // 128
    NQ = NO // 512
    NBO = NO // 256
    with ExitStack() as st:
        sb = lambda name, shape, dt: st.enter_context(nc.sbuf_tensor("mb_" + name, shape, dt))
        ident = sb("ident", [128, 128], BF16)
        S.dma("sp", ident[:], C.inp["ident_bf"][:, :], writes=["ident"])
        kaT = [sb("kaT%d" % i, [97, NV], BF16) for i in range(2)]
        qaT = [sb("qaT%d" % i, [97, NO], BF16) for i in range(2)]
        for i in range(2):
            S.dma("sp", kaT[i][64:97, :], C.inp["kaug_c"][:, :], writes=["kaT%d" % i])
        vt = sb("vt", [128, NKT, 8, 65], BF16)
        kbias = sb("kbias", [128, NH, NKT], F32)
        S.dma("sp", kbias[:], C.inp["kbias"][:, :, :], writes=["kbias"])
        gbp = sb("gbp", [128, NBO, 32], F32); A01 = sb("A01", [128, NBO, 32], F32); Bt = sb("Bt", [128, NBO, 32], F32)
        for nm, tl in (("gbp", gbp), ("A01", A01), ("Bt", Bt)):
            S.dma("sp", tl[:].rearrange("p a b -> p (a b)"), C.inp[nm][0:1, :].partition_broadcast(128), writes=[nm])
        cm = sb("cm", [128, 2, 256], BF16)
        S.dma("sp", cm[:], C.inp["cm"][:, :, :], writes=["cm"])
        sel65 = sb("sel65", [65, 64], F32)
        S.dma("sp", sel65[:], C.inp["sel65"][:, :], writes=["sel65"])
        kmf = sb("kmf", [64, NB], F32)
        kmb = sb("kmb", [64, 32], BF16)
        S.memset("pool", kmb[:], 0.0, writes=["kmb"])
        gm = sb("gm", [128, 32], F32); top8 = sb("top8", [128, 8], F32); f1 = sb("f1", [128, 32], F32)
        mbt = [sb("mbt%d" % i, [128, 96], BF16) for i in range(2)]
        for i in range(2):
            S.memset("pool", mbt[i][:], 0.0, writes=["mbt%d" % i])
        pt = [sb("pt%d" % i, [128, 512], BF16) for i in range(4)]
        oT = sb("oT", [65, 512], F32); rd = sb("rd", [64, 512], F32)
        ao = [sb("ao%d" % i, [64, 512], BF16) for i in range(2)]
        psS = [C.ps[0], C.ps[1]]; psO = [C.ps[2], C.ps[3]]; psG = C.ps[4]; psT = C.ps[5]; psD = C.ps[6]
        cnt = {"s": 0, "pt": 0, "o": 0, "ao": 0, "mb": 0}
        def load_v(g):
            S.memset("pool", vt[:, :, :, 64:65], 1.0, writes=["vt"])
            for kt0 in range(NKT):
                S.dma("sp", vt[:, kt0, :, 0:64],
                      C.v[kt0 * 128:(kt0 + 1) * 128, g * 512:(g + 1) * 512].rearrange("p (a d) -> p a d", d=64),
                      writes=["vt"])

        def prep_steps(h):
            hb = h % 2
            steps = []

            def loads():
                S.dma("sp", kaT[hb][0:64, :], C.kT[h * 64:(h + 1) * 64, :], writes=["kaT%d" % hb])
                S.dma("sp", qaT[hb][0:64, :], C.qT[h * 64:(h + 1) * 64, :], writes=["qaT%d" % hb])
                S.dma("sp", qaT[hb][96:97, :], C.inp["cq"][h:h + 1, :], writes=["qaT%d" % hb])
                S.dma("sp", kmf[:], C.kmT[h // 2, (h % 2) * 64:(h % 2) * 64 + 64, :], writes=["kmf"])
                S.copy("act", kmb[:, 0:NB], kmf[:], reads=["kmf"], writes=["kmb"])
            steps.append(loads)
            nq = NO // 128
            st1, st2, st3 = [], [], []
            for qs in range(nq):
                mo = qs // 2
                mi = qs % 2

                def s1(qs=qs, mo=mo, mi=mi):
                    S.mm(psG[:, 0:32], qaT[hb][0:64, qs * 128:(qs + 1) * 128], kmb[:], True, True,
                         reads=["qaT%d" % hb, "kmb"], writes=["psG"])
                    S.tt("dve", gm[:], psG[:, 0:32], gbp[:, mo, :], ALU.add, reads=["psG", "gbp"], writes=["gm"])
                    S.op("dve", lambda e: e.max(out=top8[:], in_=gm[:]), reads=["gm"], writes=["top8"])
                    S.ts("dve", f1[:], gm[:], top8[:, 2:3], ALU.is_ge, -NEG, ALU.mult, reads=["gm", "top8"], writes=["f1"])
                    S.tt("dve", f1[:], f1[:], A01[:, mo, :], ALU.mult, reads=["f1", "A01"], writes=["f1"])
                    S.tt("dve", mbt[mi][:, 64:96], f1[:], Bt[:, mo, :], ALU.add, reads=["f1", "Bt"], writes=["mbt%d" % mi])

                def s2(qs=qs, mi=mi):
                    pTt = psT[:].bitcast(BF16)[0:96, 0:128]
                    S.tr(pTt, mbt[mi][:], ident[:], reads=["mbt%d" % mi, "ident"], writes=["psT"])

                def s3(qs=qs):
                    S.copy("act", qaT[hb][64:96, qs * 128:(qs + 1) * 128], psT[:].bitcast(BF16)[64:96, 0:128],
                           reads=["psT"], writes=["qaT%d" % hb])
                st1.append(s1); st2.append(s2); st3.append(s3)
            for k in range(nq + 2):
                if 0 <= k - 2 < nq: steps.append(st3[k - 2])
                if 0 <= k - 1 < nq: steps.append(st2[k - 1])
                if k < nq: steps.append(st1[k])
            return steps

        def pairs(h, pending):
            hb = h % 2
            hl = h % 8
            for j in range(NQ):
                oi = cnt["o"] % 2; cnt["o"] += 1
                ok = "ps%d" % (2 + oi)
                b0 = (NO + 512 * j) // 256
                nkt = NKT // 2 + 4 * j + 4
                def emitS(kt):
                    si = cnt["s"] % 2; cnt["s"] += 1
                    sk = "ps%d" % si
                    n = kt // 2
                    lk = kaT[hb][0:97, kt * 128:(kt + 1) * 128]
                    if n >= b0:
                        c0 = (n - b0) * 256
                        c1 = 256 - c0
                        S.mm(psS[si][:, c1:c1 + 256], lk, qaT[hb][0:97, j * 512 + c1:j * 512 + c1 + 256],
                             True, True, reads=["kaT%d" % hb, "qaT%d" % hb], writes=[sk])
                        S.mm(psS[si][:, c0:c0 + 256], lk, qaT[hb][0:97, j * 512 + c0:j * 512 + c0 + 256],
                             True, False, reads=["kaT%d" % hb, "qaT%d" % hb], writes=[sk])
                        S.mm(psS[si][:, c0:c0 + 256], ident[:], cm[:, kt % 2, :],
                             False, True, reads=["ident", "cm"], writes=[sk])
                    else:
                        S.mm(psS[si][:, 0:512], lk, qaT[hb][0:97, j * 512:(j + 1) * 512],
                             True, True, reads=["kaT%d" % hb, "qaT%d" % hb], writes=[sk])
                    pi = cnt["pt"] % 4; cnt["pt"] += 1
                    pk = "pt%d" % pi
                    S.act(pt[pi][:], psS[si][:, 0:512], AF.Exp, reads=[sk, "kbias"], writes=[pk], bias=kbias[:, h, kt:kt + 1])
                    return pi
                pis = {0: emitS(0)}
                for kt in range(nkt):
                    if kt + 1 < nkt:
                        pis[kt + 1] = emitS(kt + 1)
                    pi = pis.pop(kt)
                    S.mm(psO[oi][0:65, 0:512], vt[:, kt, hl, :], pt[pi][:], kt == 0, kt == nkt - 1,
                         reads=["vt", "pt%d" % pi], writes=[ok])
                    for _ in range(MOBA_DUMMY):
                        S.mm(C.ps[7][:, 0:512], ident[:], cm[:].rearrange("p a b -> p (a b)"), True, True,
                             reads=["ident", "cm"], writes=["ps7"])
                    if pending and kt % 2 == 1:
                        pending.pop(0)()
                S.copy("act", oT[:], psO[oi][0:65, 0:512], reads=[ok], writes=["oT"])
                S.mm(psD[0:64, 0:512], sel65[:], oT[:], True, True, reads=["sel65", "oT"], writes=["psD"])
                S.op("dve", lambda e: e.reciprocal(out=rd[:], in_=psD[0:64, 0:512]), reads=["psD"], writes=["rd"])
                ai = cnt["ao"] % 2; cnt["ao"] += 1
                S.tt("dve", ao[ai][:], oT[0:64, :], rd[:], ALU.mult, reads=["oT", "rd"], writes=["ao%d" % ai])
                S.dma("pool", C.aT[h * 64:(h + 1) * 64, j * 512:(j + 1) * 512], ao[ai][:], reads=["ao%d" % ai])

        for f in prep_steps(0):
            f()
        for h in range(NH):
            if h % 8 == 0:
                load_v(h // 8)
            pending = prep_steps(h + 1) if h + 1 < NH else []
            pairs(h, pending)
            while pending:
                pending.pop(0)()
        S.flush()


def ssd_setup(C):
    nc = C.nc

    def inp(name, shape, dt=F32):
        C.inp[name] = nc.dram_tensor(name, list(shape), dt, kind="ExternalInput").ap()
    inp("tri_f", [128, 128]); inp("ones_f", [128, 128]); inp("trineg_f", [128, 128])
    C.ynT = dram(C, "ynT_s", [2048, C.NO], BF16)


def ssd_consts():
    s = np.arange(128)[:, None]; t = np.arange(128)[None, :]
    return {"tri_f": (s <= t).astype(np.float32), "ones_f": np.ones((128, 128), np.float32),
            "trineg_f": np.where(t >= s, 0.0, NEG).astype(np.float32)}


def phase_ssd(C, S):
    nc = C.nc
    C.ssd_stop = getattr(C, "ssd_stop", 99)
    C.ssd_sub = getattr(C, "ssd_sub", 99)
    NV, NO = C.NV, C.NO
    NCH = NV // 256
    with ExitStack() as st:
        sb = lambda name, shape, dt: st.enter_context(nc.sbuf_tensor("sd_" + name, shape, dt))
        ident = sb("ident", [128, 128], BF16); identf = sb("identf", [128, 128], F32)
        tri = sb("tri", [128, 128], F32); ones = sb("ones", [128, 128], F32); trineg = sb("trineg", [128, 128], F32)
        for tl, nm in ((ident, "ident_bf"), (identf, "ident_f"), (tri, "tri_f"), (ones, "ones_f"), (trineg, "trineg_f")):
            S.dma("sp", tl[:], C.inp[nm][:, :], writes=["consts"])
        dsk = sb("dsk", [128, SH], F32); gn = sb("gn", [128, 2048], F32); pv = sb("pv", [128, 1], F32)
        S.dma("sp", dsk[:], C.inp["d_skip"][0:1, :].partition_broadcast(128), writes=["consts"])
        S.dma("sp", gn[:], C.inp["ssd_norm_g"][0:1, :].partition_broadcast(128), writes=["consts"])
        S.dma("sp", pv[:], C.inp["pv"][0:1, :].partition_broadcast(128), writes=["consts"])
        state = sb("state", [128, 8, 256], F32); stb = sb("stb", [128, 8, 256], BF16)
        S.memset("pool", state[:], 0.0, writes=["state"])
        S.memset("pool", stb[:], 0.0, writes=["stb"])
        xsT = sb("xsT", [128, 16, 256], BF16); BTt = sb("BTt", [128, 8, 256], BF16); CTt = sb("CTt", [128, 8, 256], BF16)
        da = sb("da", [128, 2, 64], F32); zt = sb("zt", [128, 2, 2048], BF16)
        xdt = sb("xdt", [128, 2, 2048], BF16); xdtd = sb("xdtd", [128, 2, 2048], BF16); xtm = sb("xtm", [128, 2, 2048], BF16)
        Btm = sb("Btm", [128, 2, 8, 128], BF16)
        acum = sb("acum", [128, 2, 32], F32); nacum = sb("nacum", [128, 2, 32], F32); eA = sb("eA", [128, 2, 32], F32)
        dend = sb("dend", [128, 2, 32], F32); eTot = sb("eTot", [128, 32], F32); tot = sb("tot", [128, 32], F32)
        Lt = [sb("Lt%d" % i, [128, 384], F32) for i in range(2)]
        Mt = [sb("Mt%d" % i, [128, 384], BF16) for i in range(4)]
        ysb = sb("ysb", [128, 2, 2048], F32); ytmp = sb("ytmp", [128, 2048], F32)
        RA = sb("RA", [128, 3, 4, 128], F32)
        ssg = sb("ssg", [128, 8], F32); ynb = sb("ynb", [128, 2048], BF16); ynT = sb("ynT", [128, 16, 128], BF16)
        ps = C.ps
        for c in range(NCH):
            own = c >= NCH // 2
            t0 = c * 256
            to0 = t0 - NO
            S.dma("sp", xsT[:], C.xsT[:, t0:t0 + 256].rearrange("(c p) t -> p c t", p=128), writes=["xsT"])
            S.dma("sp", BTt[:], C.BT[:, t0:t0 + 256].rearrange("(c p) t -> p c t", p=128), writes=["BTt"])
            S.dma("sp", da[:], C.da[t0:t0 + 256, :].rearrange("(i p) c -> p i c", p=128), writes=["da"])
            if own:
                S.dma("sp", CTt[:], C.CT[:, to0:to0 + 256].rearrange("(c p) t -> p c t", p=128), writes=["CTt"])
                S.dma("sp", zt[:], C.z[to0:to0 + 256, :].rearrange("(i p) c -> p i c", p=128), writes=["zt"])
            S.mm(ps[6][:, 0:32], tri[:], da[:, 0, 32:64], True, True, reads=["consts", "da"], writes=["ps6"])
            S.mm(ps[6][:, 32:64], tri[:], da[:, 1, 32:64], True, False, reads=["consts", "da"], writes=["ps6"])
            S.mm(ps[6][:, 32:64], ones[:], da[:, 0, 32:64], False, True, reads=["consts", "da"], writes=["ps6"])
            S.mm(ps[6][:, 64:96], ones[:], da[:, 0, 32:64], True, False, reads=["consts", "da"], writes=["ps6"])
            S.mm(ps[6][:, 64:96], ones[:], da[:, 1, 32:64], False, True, reads=["consts", "da"], writes=["ps6"])
            S.copy("dve", acum[:].rearrange("p i h -> p (i h)"), ps[6][:, 0:64], reads=["ps6"], writes=["acum"])
            S.copy("dve", tot[:], ps[6][:, 64:96], reads=["ps6"], writes=["tot"])
            S.ts("dve", nacum[:], acum[:], -1.0, ALU.mult, reads=["acum"], writes=["nacum"])
            S.act(eA[:], acum[:], AF.Exp, reads=["acum"], writes=["eA"])
            S.act(eTot[:], tot[:], AF.Exp, reads=["tot"], writes=["eTot"])
            S.tt("dve", dend[:], nacum[:], tot[:].unsqueeze(1).to_broadcast([128, 2, 32]), ALU.add,
                 reads=["nacum", "tot"], writes=["dend"])
            S.act(dend[:], dend[:], AF.Exp, reads=["dend"], writes=["dend"])
            if C.ssd_stop <= 1:
                continue
            for i in range(2):
                pT = ps[7][:].bitcast(BF16)[:, 0:1024].rearrange("p (c t) -> p c t", t=128)
                for half in range(2):
                    for cc in range(8):
                        S.tr(pT[:, cc, :], xsT[:, half * 8 + cc, i * 128:(i + 1) * 128], ident[:],
                             reads=["xsT", "consts"], writes=["ps7"])
                    hs = slice(half * 16, half * 16 + 16)
                    dst = xdt[:, i, half * 1024:(half + 1) * 1024].rearrange("p (h d) -> p h d", d=64)
                    src = pT.rearrange("p c (h d) -> p (c h) d", d=64)
                    S.tt("dve", dst, src, da[:, i, hs].unsqueeze(2).to_broadcast([128, 16, 64]), ALU.mult,
                         reads=["ps7", "da"], writes=["xdt"])
                    if own and C.ssd_sub >= 1:
                        S.copy("act", xtm[:, i, half * 1024:(half + 1) * 1024], pT.rearrange("p c t -> p (c t)"),
                               reads=["ps7"], writes=["xtm"])
                if C.ssd_sub < 2:
                    continue
                S.tt("dve", xdtd[:, i, :].rearrange("p (h d) -> p h d", d=64), xdt[:, i, :].rearrange("p (h d) -> p h d", d=64),
                     dend[:, i, :].unsqueeze(2).to_broadcast([128, 32, 64]), ALU.mult, reads=["xdt", "dend"], writes=["xdtd"])
                if C.ssd_sub < 3:
                    continue
                pT = ps[7][:].bitcast(BF16)[:, 0:1024].rearrange("p (c t) -> p c t", t=128)
                for g in range(8):
                    S.tr(pT[:, g, :], BTt[:, g, i * 128:(i + 1) * 128], ident[:], reads=["BTt", "consts"], writes=["ps7"])
                S.copy("act", Btm[:, i, :, :], pT, reads=["ps7"], writes=["Btm"])
            if C.ssd_stop <= 2:
                continue
            if own:
                for g in range(8):
                    if C.ssd_stop <= 3 and g > 0:
                        continue
                    S.mm(ps[4][:, 0:256], BTt[:, g, 0:128], CTt[:, g, 0:256], True, True, reads=["BTt", "CTt"], writes=["ps4"])
                    S.mm(ps[4][:, 256:384], BTt[:, g, 128:256], CTt[:, g, 128:256], True, True, reads=["BTt", "CTt"], writes=["ps4"])
                    for h4 in range(4):
                        h = g * 4 + h4
                        pL = ps[h4]; lk = "ps%d" % h4
                        if h4 == 0:
                            for q_, (src_i, mat) in enumerate(((0, tri), (0, ones), (1, tri))):
                                S.tt("dve", RA[:, q_, :, :], da[:, src_i, 32 + g * 4:36 + g * 4].unsqueeze(2).to_broadcast([128, 4, 128]),
                                     mat[:].unsqueeze(1).to_broadcast([128, 4, 128]), ALU.mult, reads=["da", "consts"], writes=["RA"])
                        S.mm(pL[:, 0:128], ones[:], RA[:, 0, h4, :], True, False, reads=["RA", "consts"], writes=[lk])
                        S.mm(pL[:, 0:128], identf[:], trineg[:], False, True, reads=["consts"], writes=[lk])
                        S.mm(pL[:, 128:256], ones[:], RA[:, 1, h4, :], True, False, reads=["RA", "consts"], writes=[lk])
                        S.mm(pL[:, 128:256], ones[:], RA[:, 2, h4, :], False, True, reads=["RA", "consts"], writes=[lk])
                        S.mm(pL[:, 256:384], ones[:], RA[:, 1, h4, :], True, False, reads=["RA", "consts"], writes=[lk])
                        S.mm(pL[:, 256:384], ones[:], RA[:, 2, h4, :], False, False, reads=["RA", "consts"], writes=[lk])
                        S.mm(pL[:, 256:384], identf[:], trineg[:], False, True, reads=["consts"], writes=[lk])
                        li = h % 2
                        S.act(Lt[li][:, 0:256], pL[:, 0:256], AF.Exp, reads=[lk, "nacum"], writes=["Lt%d" % li],
                              bias=nacum[:, 0, h:h + 1])
                        S.act(Lt[li][:, 256:384], pL[:, 256:384], AF.Exp, reads=[lk, "nacum"], writes=["Lt%d" % li],
                              bias=nacum[:, 1, h:h + 1])
                        S.tt("dve", Mt[h4][:], Lt[li][:], ps[4][:, 0:384], ALU.mult, reads=["Lt%d" % li, "ps4"],
                             writes=["Mt%d" % h4])
                    if C.ssd_stop <= 4:
                        continue
                    pY = ps[5][:, 0:512].rearrange("p (i c) -> p i c", i=2)
                    for h4 in range(4):
                        h = g * 4 + h4
                        cs = slice(h4 * 64, (h4 + 1) * 64)
                        S.mm(pY[:, 0, cs], Mt[h4][:, 0:128], xdt[:, 0, h * 64:(h + 1) * 64], True, True,
                             reads=["Mt%d" % h4, "xdt"], writes=["ps5"])
                        S.mm(pY[:, 1, cs], Mt[h4][:, 128:256], xdt[:, 0, h * 64:(h + 1) * 64], True, False,
                             reads=["Mt%d" % h4, "xdt"], writes=["ps5"])
                        S.mm(pY[:, 1, cs], Mt[h4][:, 256:384], xdt[:, 1, h * 64:(h + 1) * 64], False, True,
                             reads=["Mt%d" % h4, "xdt"], writes=["ps5"])
                    pO = ps[6][:, 0:512].rearrange("p (i c) -> p i c", i=2)
                    for i in range(2):
                        S.mm(pO[:, i, :], CTt[:, g, i * 128:(i + 1) * 128], stb[:, g, :], True, True,
                             reads=["CTt", "stb"], writes=["ps6"])
                    for i in range(2):
                        yg = ysb[:, i, g * 256:(g + 1) * 256].rearrange("p (h d) -> p h d", d=64)
                        S.tt("dve", yg, pO[:, i, :].rearrange("p (h d) -> p h d", d=64),
                             eA[:, i, g * 4:(g + 1) * 4].unsqueeze(2).to_broadcast([128, 4, 64]), ALU.mult,
                             reads=["ps6", "eA"], writes=["ysb"])
                        S.tt("dve", ysb[:, i, g * 256:(g + 1) * 256], ysb[:, i, g * 256:(g + 1) * 256], pY[:, i, :], ALU.add,
                             reads=["ysb", "ps5"], writes=["ysb"])
            if C.ssd_stop <= 5:
                continue
            for g in range(8):
                for i in range(2):
                    S.mm(ps[5][:, 0:256], Btm[:, i, g, :], xdtd[:, i, g * 256:(g + 1) * 256], i == 0, i == 1,
                         reads=["Btm", "xdtd"], writes=["ps5"])
                sg = state[:, g, :].rearrange("p (h d) -> p h d", d=64)
                S.tt("dve", sg, sg, eTot[:, g * 4:(g + 1) * 4].unsqueeze(2).to_broadcast([128, 4, 64]), ALU.mult,
                     reads=["state", "eTot"], writes=["state"])
                S.tt("dve", state[:, g, :], state[:, g, :], ps[5][:, 0:256], ALU.add, reads=["state", "ps5"], writes=["state"])
            if c == NCH // 2 - 1:
                S.ts("dve", state[:].rearrange("p g c -> p (g c)"), state[:].rearrange("p g c -> p (g c)"), pv[:, 0:1], ALU.mult,
                     reads=["state", "consts"], writes=["state"])
            S.copy("act", stb[:].rearrange("p g c -> p (g c)"), state[:].rearrange("p g c -> p (g c)"), reads=["state"], writes=["stb"])
            if C.ssd_stop <= 6:
                continue
            if own:
                for i in range(2):
                    y = ysb[:, i, :]
                    y3 = y.rearrange("p (h d) -> p h d", d=64)
                    S.tt("dve", ytmp[:].rearrange("p (h d) -> p h d", d=64), xtm[:, i, :].rearrange("p (h d) -> p h d", d=64),
                         dsk[:].unsqueeze(2).to_broadcast([128, 32, 64]), ALU.mult, reads=["xtm", "consts"], writes=["ytmp"])
                    S.tt("dve", y, y, ytmp[:], ALU.add, reads=["ysb", "ytmp"], writes=["ysb"])
                    S.tt("dve", y, y, zt[:, i, :], ALU.mult, reads=["ysb", "zt"], writes=["ysb"])
                    S.tt("dve", ytmp[:], y, y, ALU.mult, reads=["ysb"], writes=["ytmp"])
                    S.op("dve", lambda e: e.tensor_reduce(out=ssg[:], in_=ytmp[:].rearrange("p (g c) -> p g c", c=256),
                                                         axis=AX.X, op=ALU.add), reads=["ytmp"], writes=["ssg"])
                    S.act(ssg[:], ssg[:], AF.Ln, reads=["ssg"], writes=["ssg"], scale=1.0 / 256, bias=EPS)
                    S.act(ssg[:], ssg[:], AF.Exp, reads=["ssg"], writes=["ssg"], scale=-0.5)
                    S.tt("dve", ytmp[:].rearrange("p (g c) -> p g c", c=256), y.rearrange("p (g c) -> p g c", c=256),
                         ssg[:].unsqueeze(2).to_broadcast([128, 8, 256]), ALU.mult, reads=["ysb", "ssg"], writes=["ytmp"])
                    S.tt("dve", ynb[:], ytmp[:], gn[:], ALU.mult, reads=["ytmp", "consts"], writes=["ynb"])
                    for half in range(2):
                        pT = ps[7][:].bitcast(BF16)[:, 0:1024].rearrange("p (c t) -> p c t", t=128)
                        for cc in range(8):
                            S.tr(pT[:, cc, :], ynb[:, (half * 8 + cc) * 128:(half * 8 + cc + 1) * 128], ident[:],
                                 reads=["ynb", "consts"], writes=["ps7"])
                        S.copy("act", ynT[:, half * 8:(half + 1) * 8, :], pT, reads=["ps7"], writes=["ynT"])
                    tok = to0 + i * 128
                    S.dma("pool", C.ynT[:, tok:tok + 128].rearrange("(c p) t -> p c t", p=128), ynT[:], reads=["ynT"])
        S.flush()


def peer_setup(C):
    nc = C.nc

    def inp(name, shape, dt=F32):
        C.inp[name] = nc.dram_tensor(name, list(shape), dt, kind="ExternalInput").ap()
    inp("R1", [128, 32, 512], BF16); inp("R2", [128, 512], BF16)
    C.x2 = dram(C, "x2_s", [C.NO, D], F32)
    C.hT2 = dram(C, "hT2_s", [C.NO // 512, 128, 8, 512], BF16)
    C.out = nc.dram_tensor("out", [C.NO, D], F32, kind="ExternalOutput").ap()


def peer_consts():
    bf = ml_dtypes.bfloat16
    R1 = np.zeros((128, 32, 4, 128), np.float32)
    for c in range(32):
        for j in range(4):
            R1[4 * c + j, c, j, :] = 1
    R2 = np.tile(np.eye(128, dtype=np.float32), (1, 4))
    return {"R1": R1.reshape(128, 32, 512).astype(bf), "R2": R2.astype(bf)}


def phase_peer(C, S):
    nc = C.nc
    C.peer_dummy = getattr(C, "peer_dummy", PEER_DUMMY)
    NO = C.NO
    UT = C.inp["peer_uT"]; VT = C.inp["peer_v"]; WQ = C.inp["w_peer_q"]
    with ExitStack() as st:
        sb = lambda name, shape, dt: st.enter_context(nc.sbuf_tensor("pr_" + name, shape, dt))
        ident = sb("ident", [128, 128], BF16)
        S.dma("sp", ident[:], C.inp["ident_bf"][:, :], writes=["ident"])
        R1 = sb("R1", [128, 32, 512], BF16); R2 = sb("R2", [128, 512], BF16)
        S.dma("sp", R1[:], C.inp["R1"][:, :, :], writes=["R1"])
        S.dma("sp", R2[:], C.inp["R2"][:, :], writes=["R2"])
        stg = sb("stg", [128, 4096], F32)
        stg2 = sb("stg2", [128, 4096], F32)
        stg2_v = stg2[:].rearrange("p (j d) -> p j d", d=1024)
        stg_u = stg[:].rearrange("p (c n) -> p c n", n=512)
        stg_v = stg[:].rearrange("p (j d) -> p j d", d=1024)
        kT = sb("kT", [128, 16, 128], BF16)
        for hh in range(2):
            S.dma("sp", stg_u[:, :, 0:128], C.inp["keys%dT" % (hh + 1)].rearrange("h d k -> d h k"), writes=["stg"])
            S.copy("pool", kT[:].rearrange("p (h two) k -> p h two k", two=2)[:, :, hh, :], stg_u[:, :, 0:128],
                   reads=["stg"], writes=["kT"])
        ub = [sb("ub%d" % i, [128, 8, 512], BF16) for i in range(2)]
        vb = [sb("vb%d" % i, [128, 4, 1024], BF16) for i in range(2)]
        xnT = sb("xnT", [128, 8, 512], BF16)
        shr = sb("shr", [128, 8192], BF16)
        qTr = shr[:].rearrange("p (c t) -> p c t", t=512)
        sb16 = sb("sb16", [128, 16, 128], BF16); sf = sb("sf", [128, 16, 128], F32); swk = sb("swk", [128, 128], F32)
        v16 = sb("v16", [128, 16, 16], F32)
        cand = sb("cand", [128, 8, 256], F32); cwk = stg2[:, 0:2048].rearrange("p (h k) -> p h k", k=256)
        t8 = sb("t8", [128, 8, 8], F32); t8b = sb("t8b", [128, 8, 8], F32)
        thr = sb("thr", [128, 4, 8], F32); nb = sb("nb", [128, 4, 8], F32); zz = sb("zz", [128, 8], F32)
        sT = sb("sT", [128, 4, 16, 128], BF16)
        tau = sb("tau", [128, 4, 8], F32); taub = sb("taub", [128, 8], BF16)
        Eb = [sb("Eb%d" % i, [128, 512], BF16) for i in range(3)]
        Em8 = [sb("Em80", [128, 8, 512], BF16), shr[:, 0:4096].rearrange("p (h e) -> p h e", e=512)]
        gsb = [sb("gsb%d" % i, [128, 4, 512], BF16) for i in range(2)]
        hact = [sb("hact%d" % i, [128, 512], BF16) for i in range(2)]
        hTt = [sb("hTt%d" % i, [128, 4, 128], BF16) for i in range(2)]
        yacc = sb("yacc", [128, 4, 1024], F32)
        ps = C.ps
        cnt = {"e": 0, "u": 0, "it": 0}

        def peer_iter(it, c, s4, ui, vi):
            ts_ = slice(s4 * 128, (s4 + 1) * 128)
            b = it % 2
            pW = ps[4]; wk = "ps4"
            A = []
            for h in range(8):
                def ah(h=h):
                    ei = cnt["e"] % 3; cnt["e"] += 1
                    pE = ps[(2, 3, 1)[ei]]; ek = "ps%d" % (2, 3, 1)[ei]
                    S.mm(pE[:, 0:512], sT[:, s4, 2 * h, :], R1[:, c, :], True, False, reads=["sT", "R1"], writes=[ek])
                    S.mm(pE[:, 0:512], sT[:, s4, 2 * h + 1, :], R2[:], False, True, reads=["sT", "R2"], writes=[ek])
                    S.act(Eb[ei][:], pE[:, 0:512], AF.Exp, reads=[ek, "nb"], writes=["Eb%d" % ei], bias=nb[:, s4, h:h + 1])
                    S.stt("dve", Em8[b][:, h, :], pE[:, 0:512], thr[:, s4, h:h + 1], Eb[ei][:], ALU.is_ge, ALU.mult,
                          reads=[ek, "thr", "Eb%d" % ei], writes=["Em8%d_%d" % (b, h)])
                A.append(ah)

            def b1a():
                for h in range(8):
                    S.mm(pW[:, 0:512], ident[:], Em8[b][:, h, :], h == 0, h == 7, reads=["Em8%d_%d" % (b, h), "ident"], writes=[wk])

            def b1():
                pass

            def b2():
                pass

            def b3():
                S.tt("dve", hact[b][:], gsb[c % 2][:, s4, :], pW[:, 0:512], ALU.mult, reads=["gsb%d" % (c % 2), wk],
                     writes=["hact%d" % b])

            def b4():
                pT = ps[0][:].bitcast(BF16)[:, 0:512].rearrange("p (j t) -> p j t", t=128)
                for j in range(4):
                    S.tr(pT[:, j, :], hact[b][:, j * 128:(j + 1) * 128], ident[:], reads=["hact%d" % b, "ident"], writes=["ps0"])

            def b5():
                pT = ps[0][:].bitcast(BF16)[:, 0:512].rearrange("p (j t) -> p j t", t=128)
                S.copy("act", hTt[b][:], pT, reads=["ps0"], writes=["hTt%d" % b])

            def b6():
                for half in range(2):
                    for j in range(4):
                        S.mm(ps[6 + half][:, 0:512], hTt[b][:, j, :], vb[vi][:, j, half * 512:(half + 1) * 512], j == 0, j == 3,
                             reads=["hTt%d" % b, "vb%d" % vi], writes=["ps%d" % (6 + half)])

            def b7():
                for half in range(2):
                    ya = yacc[:, s4, half * 512:(half + 1) * 512]
                    S.tt("dve", ya, ya, ps[6 + half][:, 0:512], ALU.add, reads=["yacc", "ps%d" % (6 + half)], writes=["yacc"])
            return A, [b1a, b1, b2, b3, b4, b5, b6, b7]

        def act_part(c, s4, ui):
            for kc in range(8):
                S.mm(ps[5][:, 0:512], xnT[:, kc, s4 * 128:(s4 + 1) * 128], ub[ui][:, kc, :], kc == 0, kc == 7,
                     reads=["ub%d" % ui, "xnT"], writes=["ps5"])
            S.copy("act", gsb[c % 2][:, s4, :], ps[5][:, 0:512], reads=["ps5"], writes=["gsb%d" % (c % 2)])

        def gelu_inplace(c):
            g2 = gsb[c % 2][:].rearrange("p a b -> p (a b)")
            S.act(g2, g2, AF.Gelu, reads=["gsb%d" % (c % 2)], writes=["gsb%d" % (c % 2)])

        prevB = []
        for rd in range(NO // 512):
            S.dma("sp", xnT[:], C.hT2[rd, :, :, :], writes=["xnT"])
            S.memset("pool", yacc[:], 0.0, writes=["yacc"])
            for pc in range(4):
                S.dma("sp", stg_u, WQ[:, pc * 512:(pc + 1) * 512].rearrange("(c p) n -> p c n", p=128), writes=["stg"])
                ui = cnt["u"] % 2; cnt["u"] += 1
                S.copy("pool", ub[ui][:], stg_u, reads=["stg"], writes=["ub%d" % ui])
                for cc in range(4):
                    for kc in range(8):
                        S.mm(ps[0][:, 0:512], ub[ui][:, kc, cc * 128:(cc + 1) * 128], xnT[:, kc, :],
                             kc == 0, kc == 7, reads=["ub%d" % ui, "xnT"], writes=["ps0"])
                    S.copy("act", qTr[:, pc * 4 + cc, :], ps[0][:, 0:512], reads=["ps0"], writes=["Em81_%d" % hh_ for hh_ in range(8)])
            for s4 in range(4):
                ts_ = slice(s4 * 128, (s4 + 1) * 128)
                for g4 in range(4):
                    for cc in range(4):
                        ch = g4 * 4 + cc
                        S.mm(ps[1][:, cc * 128:(cc + 1) * 128], qTr[:, ch, ts_], kT[:, ch, :], True, True,
                             reads=["Em81_%d" % hh_ for hh_ in range(8)] + ["kT"], writes=["ps1"])
                    S.copy("act", sb16[:, g4 * 4:(g4 + 1) * 4, :], ps[1][:, 0:512].rearrange("p (c k) -> p c k", k=128),
                           reads=["ps1"], writes=["sb16"])
                S.copy("dve", sf[:], sb16[:], reads=["sb16"], writes=["sf"])
                for ch in range(16):
                    S.op("dve", lambda e, ch=ch: e.max(out=v16[:, ch, 0:8], in_=sf[:, ch, :]), reads=["sf"], writes=["v16"])
                    S.op("dve", lambda e, ch=ch: e.match_replace(out=swk[:], in_to_replace=v16[:, ch, 0:8], in_values=sf[:, ch, :],
                                                                 imm_value=-1e30), reads=["sf", "v16"], writes=["swk"])
                    S.op("dve", lambda e, ch=ch: e.max(out=v16[:, ch, 8:16], in_=swk[:]), reads=["swk"], writes=["v16"])
                v4 = v16[:].rearrange("p (h two) k -> p h two k", two=2)
                c4 = cand[:].rearrange("p h (a b) -> p h a b", b=16)
                S.tt("dve", c4, v4[:, :, 0, :].unsqueeze(3).to_broadcast([128, 8, 16, 16]),
                     v4[:, :, 1, :].unsqueeze(2).to_broadcast([128, 8, 16, 16]), ALU.add, reads=["v16"], writes=["cand"])
                for h in range(8):
                    S.op("dve", lambda e, h=h: e.max(out=t8[:, h, :], in_=cand[:, h, :]), reads=["cand"], writes=["t8"])
                    S.op("dve", lambda e, h=h: e.match_replace(out=cwk[:, h, :], in_to_replace=t8[:, h, :], in_values=cand[:, h, :],
                                                               imm_value=-1e30), reads=["cand", "t8"], writes=["stg2"])
                    S.op("dve", lambda e, h=h: e.max(out=t8b[:, h, :], in_=cwk[:, h, :]), reads=["stg2"], writes=["t8b"])
                S.copy("dve", thr[:, s4, :], t8b[:, :, 7], reads=["t8b"], writes=["thr"])
                S.tt("dve", cwk, cand[:], t8[:, :, 0:1].to_broadcast([128, 8, 256]), ALU.subtract, reads=["cand", "t8"], writes=["stg2"])
                S.act(cwk, cwk, AF.Exp, reads=["stg2"], writes=["stg2"])
                S.tt("dve", cand[:], cand[:], thr[:, s4, :].unsqueeze(2).to_broadcast([128, 8, 256]), ALU.is_ge,
                     reads=["cand", "thr"], writes=["cand"])
                S.tt("dve", cwk, cwk, cand[:], ALU.mult, reads=["stg2", "cand"], writes=["stg2"])
                S.op("dve", lambda e: e.tensor_reduce(out=zz[:], in_=cwk, axis=AX.X, op=ALU.add), reads=["stg2"], writes=["zz"])
                S.act(zz[:], zz[:], AF.Ln, reads=["zz"], writes=["zz"])
                S.tt("dve", zz[:], zz[:], t8[:, :, 0], ALU.add, reads=["zz", "t8"], writes=["zz"])
                S.ts("dve", nb[:, s4, :], zz[:], -1.0, ALU.mult, reads=["zz"], writes=["nb"])
                for half in range(2):
                    pT = ps[1][:].bitcast(BF16)[:, 0:1024].rearrange("p (c t) -> p c t", t=128)
                    for cc in range(8):
                        S.tr(pT[:, cc, :], sb16[:, half * 8 + cc, :], ident[:], reads=["sb16", "ident"], writes=["ps1"])
                    S.copy("act", sT[:, s4, half * 8:(half + 1) * 8, :], pT, reads=["ps1"], writes=["sT"])
            def load_u(c):
                S.dma("sp", stg_u, UT[:, c * 512:(c + 1) * 512].rearrange("(kc p) n -> p kc n", p=128), writes=["stg"])
                ui_ = cnt["u"] % 2; cnt["u"] += 1
                S.copy("pool", ub[ui_][:], stg_u, reads=["stg"], writes=["ub%d" % ui_])
                return ui_

            def load_v(c):
                S.dma("sp", stg2_v, VT[c * 512:(c + 1) * 512, :].rearrange("(j p) d -> p j d", p=128), writes=["stg2"])
                S.copy("pool", vb[c % 2][:], stg2_v, reads=["stg2"], writes=["vb%d" % (c % 2)])
            events = []
            uis = {}

            def ev_load_u(c):
                uis[c] = load_u(c)
            ev_load_u(0)
            for s4_ in range(4):
                act_part(0, s4_, uis[0])
            gelu_inplace(0)
            for c in range(32):
                if c + 1 < 32:
                    events.append((c * 4 + 0 - 0.5, 0, lambda c=c: ev_load_u(c + 1)))
                    for s4_ in range(4):
                        events.append((c * 4 + s4_ + 0.65, 1, lambda c=c, s4_=s4_: act_part(c + 1, s4_, uis[c + 1])))
                    events.append((c * 4 + 3 + 0.75, 1, lambda c=c: gelu_inplace(c + 1)))
                events.append((c * 4 + 0 - 0.45, 2, lambda c=c: load_v(c)))
                for s4 in range(4):
                    it = c * 4 + s4
                    a_steps, b_steps = peer_iter(cnt["it"], c, s4, None, c % 2)
                    cnt["it"] += 1
                    for k in range(8):
                        events.append((it + k / 10.0, 3, a_steps[k]))
                    b0, _, _, b3, b4, b5, b6, b7 = b_steps
                    events.append((it + 1 + 0.45, 4, b0))
                    events.append((it + 1 + 0.52, 5, b3))
                    events.append((it + 2 + 0.05, 6, b4))
                    events.append((it + 2 + 0.15, 7, b5))
                    events.append((it + 2 + 0.25, 8, b6))
                    events.append((it + 2 + 0.32, 9, b7))
            events.sort(key=lambda e: (e[0], e[1]))
            for _, _, f in events:
                f()
            for s4 in range(4):
                tok = rd * 512 + s4 * 128
                S.dma("sp", stg[:, 0:1024], C.x2[tok:tok + 128, :], writes=["stg"])
                S.tt("dve", yacc[:, s4, :], yacc[:, s4, :], stg[:, 0:1024], ALU.add, reads=["yacc", "stg"], writes=["yacc"])
                S.dma("pool", C.out[tok:tok + 128, :], yacc[:, s4, :], reads=["yacc"])
        S.flush()


def phase_outproj(C, S):
    nc = C.nc
    NV, NO = C.NV, C.NO
    with ExitStack() as st:
        sb = lambda name, shape, dt: st.enter_context(nc.sbuf_tensor("op_" + name, shape, dt))
        ident = sb("ident", [128, 128], BF16)
        S.dma("sp", ident[:], C.inp["ident_bf"][:, :], writes=["ident"])
        stg = sb("stg", [128, 4, 1024], F32)
        Wa = sb("Wa", [128, 8, 1024], BF16); Ws = sb("Ws", [128, 16, 1024], BF16); Wo = sb("Wo", [128, 8, 1024], BF16)
        for nm, tl, nch in (("w_attn_o", Wa, 8), ("w_ssd_o", Ws, 16), ("w_out", Wo, 8)):
            for c0 in range(0, nch, 4):
                S.dma("sp", stg[:], C.inp[nm][c0 * 128:(c0 + 4) * 128, :].rearrange("(c p) n -> p c n", p=128), writes=["stg"])
                S.copy("act", tl[:, c0:c0 + 4, :], stg[:], reads=["stg"], writes=[nm])
        aTt = [sb("aTt%d" % i, [128, 8, 128], BF16) for i in range(2)]
        yTt = [sb("yTt%d" % i, [128, 16, 128], BF16) for i in range(2)]
        ga = [sb("ga%d" % i, [128, 1024], BF16) for i in range(2)]
        gs = [sb("gs%d" % i, [128, 1024], BF16) for i in range(2)]
        xt = [sb("xt%d" % i, [128, 1024], F32) for i in range(2)]
        m1 = sb("m1", [128, 1024], F32); m2 = sb("m2", [128, 1024], F32); mb = sb("mb", [128, 1024], BF16)
        mT = sb("mT", [128, 8, 128], BF16)
        xo = [sb("xo%d" % i, [128, 1024], F32) for i in range(2)]
        ps = C.ps
        for t in range(NO // 128):
            i = t % 2
            tok = t * 128
            S.dma("sp", aTt[i][:], C.aT[:, tok:tok + 128].rearrange("(c p) t -> p c t", p=128), writes=["aTt%d" % i])
            S.dma("sp", yTt[i][:], C.ynT[:, tok:tok + 128].rearrange("(c p) t -> p c t", p=128), writes=["yTt%d" % i])
            S.dma("sp", ga[i][:], C.sga[tok:tok + 128, :], writes=["ga%d" % i])
            S.dma("sp", gs[i][:], C.sgs[tok:tok + 128, :], writes=["gs%d" % i])
            S.dma("sp", xt[i][:], C.inp["xv"][NO + tok:NO + tok + 128, :], writes=["xt%d" % i])
            for half in range(2):
                hs = slice(half * 512, (half + 1) * 512)
                for c in range(8):
                    S.mm(ps[half][:, 0:512], aTt[i][:, c, :], Wa[:, c, hs], c == 0, c == 7,
                         reads=["aTt%d" % i, "w_attn_o"], writes=["ps%d" % half])
                for c in range(16):
                    S.mm(ps[2 + half][:, 0:512], yTt[i][:, c, :], Ws[:, c, hs], c == 0, c == 15,
                         reads=["yTt%d" % i, "w_ssd_o"], writes=["ps%d" % (2 + half)])
                S.tt("dve", m1[:, hs], ps[half][:, 0:512], ga[i][:, hs], ALU.mult, reads=["ps%d" % half, "ga%d" % i], writes=["m1"])
                S.tt("dve", m2[:, hs], ps[2 + half][:, 0:512], gs[i][:, hs], ALU.mult, reads=["ps%d" % (2 + half), "gs%d" % i], writes=["m2"])
            S.tt("dve", mb[:], m1[:], m2[:], ALU.add, reads=["m1", "m2"], writes=["mb"])
            pT = ps[4][:].bitcast(BF16)[:, 0:1024].rearrange("p (c t) -> p c t", t=128)
            for c in range(8):
                S.tr(pT[:, c, :], mb[:, c * 128:(c + 1) * 128], ident[:], reads=["mb", "ident"], writes=["ps4"])
            S.copy("act", mT[:], pT, reads=["ps4"], writes=["mT"])
            for half in range(2):
                hs = slice(half * 512, (half + 1) * 512)
                for c in range(8):
                    S.mm(ps[5 + half][:, 0:512], mT[:, c, :], Wo[:, c, hs], c == 0, c == 7,
                         reads=["mT", "w_out"], writes=["ps%d" % (5 + half)])
                S.tt("dve", xo[i][:, hs], ps[5 + half][:, 0:512], xt[i][:, hs], ALU.add,
                     reads=["ps%d" % (5 + half), "xt%d" % i], writes=["xo%d" % i])
            S.dma("pool", C.x2[tok:tok + 128, :], xo[i][:], reads=["xo%d" % i])
        S.flush()


def build_all(nc, NV, st, debug=()):
    C = setup(nc, NV, debug)
    moba_setup(C); ssd_setup(C); peer_setup(C)
    S = Sched(nc, st)
    C.ps = [st.enter_context(nc.psum_tensor("ps%d" % i, [128, 512], F32)) for i in range(8)]
    phase_norm(C, S, C.inp["xv"], "norm1_g", C.hT, NV, "n1")
    phase_inproj(C, S)
    phase_moba(C, S)
    phase_ssd(C, S)
    phase_outproj(C, S)
    phase_norm(C, S, C.x2, "norm2_g", C.hT2, C.NO, "n2")
    phase_peer(C, S)
    return C, S


def make_inputs(inputs, NV, b, r, full_seq):
    bf = ml_dtypes.bfloat16
    NO = NV // 2
    f32 = lambda a: np.ascontiguousarray(np.asarray(a, dtype=np.float32))
    x = np.asarray(inputs["x"])
    ins = {}
    xv = np.zeros((NV, D), np.float32)
    if r == 0:
        xv[NO:] = x[b, 0:NO]
    else:
        xv[:] = x[b, 0:NV]
    ins["xv"] = xv
    ins["pv"] = np.full((1, 1), float(r), np.float32)
    ins["norm1_g"] = f32(inputs["norm1_g"][0:1]); ins["w_in"] = f32(inputs["w_in"][0])
    ins["q_norm_g"] = f32(inputs["q_norm_g"][0:1]); ins["k_norm_g"] = f32(inputs["k_norm_g"][0:1])
    ins["conv_wT"] = f32(np.asarray(inputs["conv_w"][0]).T); ins["conv_b"] = f32(np.asarray(inputs["conv_b"][0]).reshape(4096, 1))
    ins["dt_bias"] = f32(inputs["dt_bias"][0:1]); ins["a_log"] = f32(inputs["a_log"][0:1]); ins["d_skip"] = f32(inputs["d_skip"][0:1])
    ins["ssd_norm_g"] = f32(inputs["ssd_norm_g"][0:1]); ins["w_attn_o"] = f32(inputs["w_attn_o"][0])
    ins["w_ssd_o"] = f32(inputs["w_ssd_o"][0]); ins["w_out"] = f32(inputs["w_out"][0]); ins["norm2_g"] = f32(inputs["norm2_g"][0:1])
    ins["w_peer_q"] = f32(inputs["w_peer_q"][0])
    ins["keys1T"] = f32(np.asarray(inputs["peer_keys1"][0]).transpose(0, 2, 1))
    ins["keys2T"] = f32(np.asarray(inputs["peer_keys2"][0]).transpose(0, 2, 1))
    ins["peer_uT"] = f32(np.asarray(inputs["peer_u"][0]).T); ins["peer_v"] = f32(inputs["peer_v"][0])
    ins["ident_bf"] = np.eye(128).astype(bf); ins["ident_f"] = np.eye(128, dtype=np.float32)
    ins.update(moba_consts(NV, r)); ins.update(ssd_consts()); ins.update(peer_consts())
    return ins


NV_FULL = 8192


def kernel(**inputs):
    from concourse.bass_utils import run_bass_kernel_spmd
    nc = bass.Bass("TRN2", target_bir_lowering=False)
    with ExitStack() as st:
        C, S = build_all(nc, NV_FULL, st)
    x = np.asarray(inputs["x"])
    B = x.shape[0]
    in_maps = []
    for b in range(B):
        for r in range(2):
            in_maps.append(make_inputs(inputs, NV_FULL, b, r, NV_FULL))
    res = run_bass_kernel_spmd(nc, in_maps, core_ids=list(range(len(in_maps)))).results
    NO = NV_FULL // 2
    out = np.empty((B, NV_FULL, D), np.float32)
    for b in range(B):
        for r in range(2):
            out[b, r * NO:(r + 1) * NO] = np.asarray(res[b * 2 + r]["out"], dtype=np.float32)
    return out
```

```python
from contextlib import ExitStack
import ml_dtypes
import numpy as np
import concourse.bass as bass
import concourse.mybir as mybir

F32 = mybir.dt.float32
BF16 = mybir.dt.bfloat16
AF = mybir.ActivationFunctionType
ALU = mybir.AluOpType
AX = mybir.AxisListType

SAME_ENGINE_SYNC = True
N_DMA_SEMS = 32


class Sched:
    ENG = ("sp", "act", "dve", "pool", "pe")

    def __init__(self, nc, stack):
        self.nc = nc
        self.ops = []
        self.esem = {e: stack.enter_context(nc.semaphore("s_" + e)) for e in self.ENG}
        self.ecnt = {e: 0 for e in self.ENG}
        self.dsem = [stack.enter_context(nc.semaphore("d%d" % i)) for i in range(N_DMA_SEMS)]
        self.dcnt = [0] * N_DMA_SEMS
        self.downer = [None] * N_DMA_SEMS
        self.dnext = 0
        self.last_w = {}
        self.readers = {}
        self.waited = {e: {} for e in self.ENG}
        self.nblocks = 0
        self.nops = 0

    def _need(self, eng, ev, waits):
        if ev is None:
            return
        sem, val, src_eng, is_dma = ev
        if (not is_dma) and src_eng == eng and (eng == "pe" or not SAME_ENGINE_SYNC):
            return
        key = id(sem)
        if self.waited[eng].get(key, 0) >= val:
            return
        cur = waits.get(key)
        if cur is None or cur[1] < val:
            waits[key] = (sem, val)

    def op(self, eng, fn, reads=(), writes=(), dma=False):
        writes = list(writes) + [k for k in reads if isinstance(k, str) and k.startswith("ps") and k not in writes]
        waits = {}
        for k in reads:
            self._need(eng, self.last_w.get(k), waits)
        for k in writes:
            self._need(eng, self.last_w.get(k), waits)
            for ev in self.readers.get(k, ()):
                self._need(eng, ev, waits)
        if dma:
            half = N_DMA_SEMS // 2
            base = 0 if eng == "sp" else half
            self.dnx = getattr(self, "dnx", {})
            i = base + self.dnx.get(eng, 0)
            self.dnx[eng] = (self.dnx.get(eng, 0) + 1) % half
            if self.dcnt[i] > 0:
                self._need(eng, (self.dsem[i], 16 * self.dcnt[i], self.downer[i], True), waits)
            self.dcnt[i] += 1
            self.downer[i] = eng
            ev = (self.dsem[i], 16 * self.dcnt[i], eng, True)
            inc = (self.dsem[i], 16)
        else:
            self.ecnt[eng] += 1
            ev = (self.esem[eng], self.ecnt[eng], eng, False)
            inc = (self.esem[eng], 1)
        for (sem, val) in waits.values():
            self.waited[eng][id(sem)] = val
        for k in reads:
            self.readers.setdefault(k, []).append(ev)
        for k in writes:
            self.last_w[k] = ev
            self.readers[k] = []
        self.ops.append((eng, fn, list(waits.values()), inc))
        self.nops += 1

    def flush(self):
        fin = {}
        for i in range(N_DMA_SEMS):
            if self.dcnt[i] > 0:
                e = self.downer[i]
                if self.waited[e].get(id(self.dsem[i]), 0) < 16 * self.dcnt[i]:
                    fin.setdefault(e, []).append((self.dsem[i], 16 * self.dcnt[i]))
                    self.waited[e][id(self.dsem[i])] = 16 * self.dcnt[i]
        ops = self.ops
        self.ops = []
        if not ops and not fin:
            return
        nc = self.nc
        with nc.Block() as block:
            deco = {"sp": block.sync, "act": block.scalar, "dve": block.vector,
                    "pool": block.gpsimd, "pe": block.tensor}
            for e in self.ENG:
                mine = [o for o in ops if o[0] == e]
                tail = fin.get(e, [])
                if not mine and not tail:
                    continue

                def body(engine, mine=mine, tail=tail):
                    for (_, fn, waits, inc) in mine:
                        for (sem, val) in waits:
                            engine.wait_ge(sem, val)
                        ins = fn(engine)
                        ins.then_inc(inc[0], inc[1])
                    for (sem, val) in tail:
                        engine.wait_ge(sem, val)

                deco[e](body)
        self.nblocks += 1
        self.last_w = {}
        self.readers = {}

    def dma(self, eng, out, in_, reads=(), writes=(), **kw):
        self.op(eng, lambda e: e.dma_start(out=out, in_=in_, **kw), reads, writes, dma=True)

    def mm(self, out, lhsT, rhs, start, stop, reads=(), writes=()):
        self.op("pe", lambda e: e.matmul(out, lhsT, rhs, start=start, stop=stop), reads, writes)

    def tr(self, out, in_, ident, reads=(), writes=()):
        self.op("pe", lambda e: e.transpose(out, in_, ident), reads, writes)

    def act(self, out, in_, func, reads=(), writes=(), **kw):
        self.op("act", lambda e: e.activation(out=out, in_=in_, func=func, **kw), reads, writes)

    def tt(self, eng, out, in0, in1, op, reads=(), writes=()):
        self.op(eng, lambda e: e.tensor_tensor(out=out, in0=in0, in1=in1, op=op), reads, writes)

    def ts(self, eng, out, in0, s1, op0, s2=None, op1=None, reads=(), writes=(), **kw):
        if op1 is None:
            if op0 == ALU.pow:
                self.op(eng, lambda e: e.tensor_scalar(out=out, in0=in0, scalar1=0.0, scalar2=s1, op0=ALU.add, op1=ALU.pow, **kw),
                        reads, writes)
            else:
                self.op(eng, lambda e: e.tensor_scalar(out=out, in0=in0, scalar1=s1, scalar2=None, op0=op0, **kw),
                        reads, writes)
        else:
            self.op(eng, lambda e: e.tensor_scalar(out=out, in0=in0, scalar1=s1, scalar2=s2, op0=op0, op1=op1, **kw),
                    reads, writes)

    def stt(self, eng, out, in0, scalar, in1, op0, op1, reads=(), writes=()):
        self.op(eng, lambda e: e.scalar_tensor_tensor(out=out, in0=in0, scalar=scalar, in1=in1, op0=op0, op1=op1),
                reads, writes)

    def copy(self, eng, out, in_, reads=(), writes=()):
        if eng == "act":
            self.op(eng, lambda e: e.activation(out=out, in_=in_, func=AF.Copy), reads, writes)
        else:
            self.op(eng, lambda e: e.tensor_copy(out=out, in_=in_), reads, writes)

    def memset(self, eng, ap, val, writes=()):
        self.op(eng, lambda e: e.memset(ap, val), (), writes)


D = 1024
NH = 16
HD = 64
SH = 32
SP = 64
SG = 8
SN = 128
EPS = 1e-6
NEG = -30000.0
import os
MOBA_DUMMY = int(os.environ.get('MOBA_DUMMY', '0'))
PEER_DUMMY = int(os.environ.get('PEER_DUMMY', '0')) if 'PEER_DUMMY' in os.environ else 0
C_Q, C_K, C_V, C_Z, C_X, C_B, C_C, C_DT, C_GA, C_GS = 0, 1024, 2048, 3072, 5120, 7168, 8192, 9216, 9248, 10272
IN_COLS = 11296


class Ctx:
    pass


def dram(C, name, shape, dt):
    kind = "ExternalOutput" if name in C.debug else "Internal"
    return C.nc.dram_tensor(name, list(shape), dt, kind=kind).ap()


def setup(nc, NV, debug=()):
    C = Ctx()
    C.nc = nc
    C.NV = NV
    C.NO = NV // 2
    C.debug = set(debug)
    C.inp = {}

    def inp(name, shape, dt=F32):
        C.inp[name] = nc.dram_tensor(name, list(shape), dt, kind="ExternalInput").ap()
        return C.inp[name]
    NV_, NO = NV, C.NO
    NB = NV // 256
    C.NB = NB
    inp("xv", [NV, D])
    inp("norm1_g", [1, D]); inp("w_in", [D, IN_COLS]); inp("q_norm_g", [1, HD]); inp("k_norm_g", [1, HD])
    inp("conv_wT", [4096, 4]); inp("conv_b", [4096, 1]); inp("dt_bias", [1, SH]); inp("a_log", [1, SH])
    inp("d_skip", [1, SH]); inp("ssd_norm_g", [1, 2048]); inp("w_attn_o", [D, D]); inp("w_ssd_o", [2048, D])
    inp("w_out", [D, D]); inp("norm2_g", [1, D]); inp("w_peer_q", [D, 2048])
    inp("keys1T", [8, 128, 128]); inp("keys2T", [8, 128, 128])
    inp("peer_uT", [D, 16384]); inp("peer_v", [16384, D])
    inp("ident_bf", [128, 128], BF16); inp("ident_f", [128, 128], F32)
    inp("pv", [1, 1])
    C.hT = dram(C, "hT_s", [NV // 512, 128, 8, 512], BF16)
    C.qT = dram(C, "qT_s", [D, NO], BF16)
    C.kT = dram(C, "kT_s", [D, NV], BF16)
    C.kmT = dram(C, "kmT_s", [8, 128, NB], F32)
    C.v = dram(C, "v_s", [NV, D], BF16)
    C.z = dram(C, "z_s", [NO, 2048], BF16)
    C.xsT = dram(C, "xsT_s", [2048, NV], BF16)
    C.BT = dram(C, "BT_s", [1024, NV], BF16)
    C.CT = dram(C, "CT_s", [1024, NO], BF16)
    C.da = dram(C, "da_s", [NV, 64], F32)
    C.sga = dram(C, "sga_s", [NO, D], BF16)
    C.sgs = dram(C, "sgs_s", [NO, D], BF16)
    return C


def phase_norm(C, S, xin, gname, hT, ntok, tag):
    nc = C.nc
    with ExitStack() as st:
        sb = lambda name, shape, dt: st.enter_context(nc.sbuf_tensor(tag + name, shape, dt))
        ident = sb("ident", [128, 128], BF16)
        gT = sb("gT", [128, 8], F32)
        S.dma("sp", ident[:], C.inp["ident_bf"][:, :], writes=["ident"])
        S.dma("sp", gT[:], C.inp[gname][0, :].rearrange("(c p) -> p c", p=128), writes=["gT"],
              allow_slow_non_contiguous=True)
        xt = [sb("xt%d" % i, [128, D], F32) for i in range(2)]
        junk = sb("junk", [128, D], F32)
        ss = [sb("ss%d" % i, [128, 1], F32) for i in range(2)]
        xb = [sb("xb%d" % i, [128, D], BF16) for i in range(2)]
        ho = [sb("ho%d" % i, [128, 8, 128], BF16) for i in range(2)]
        for t in range(ntok // 128):
            i = t % 2
            pT = C.ps[t % 2][:].bitcast(BF16)[:, 0:1024].rearrange("p (c t) -> p c t", t=128)
            pk = "ps%d" % (t % 2)
            S.dma("sp", xt[i][:], xin[t * 128:(t + 1) * 128, :], writes=["xt%d" % i])
            S.act(junk[:], xt[i][:], AF.Square, reads=["xt%d" % i], writes=["junk", "ss%d" % i], accum_out=ss[i][:])
            S.act(ss[i][:], ss[i][:], AF.Ln, reads=["ss%d" % i], writes=["ss%d" % i], scale=1.0 / D, bias=EPS)
            S.act(ss[i][:], ss[i][:], AF.Exp, reads=["ss%d" % i], writes=["ss%d" % i], scale=-0.5)
            S.act(xb[i][:], xt[i][:], AF.Copy, reads=["xt%d" % i, "ss%d" % i], writes=["xb%d" % i], scale=ss[i][:])
            for c in range(8):
                S.tr(pT[:, c, :], xb[i][:, c * 128:(c + 1) * 128], ident[:], reads=["xb%d" % i, "ident"], writes=[pk])
            S.tt("dve", ho[i][:], pT, gT[:].unsqueeze(2).to_broadcast([128, 8, 128]), ALU.mult,
                 reads=[pk, "gT"], writes=["ho%d" % i])
            S.dma("pool", hT[t // 4, :, :, (t % 4) * 128:(t % 4 + 1) * 128], ho[i][:], reads=["ho%d" % i])
        S.flush()


def phase_inproj(C, S):
    nc = C.nc
    NV, NO = C.NV, C.NO
    NT = NV // 512
    NTO = NO // 512
    W = C.inp["w_in"]
    with ExitStack() as st:
        sb = lambda name, shape, dt: st.enter_context(nc.sbuf_tensor("ip_" + name, shape, dt))
        ident = sb("ident", [128, 128], BF16)
        S.dma("sp", ident[:], C.inp["ident_bf"][:, :], writes=["ident"])
        gq = sb("gq", [128, HD], F32); gk = sb("gk", [128, HD], F32)
        S.dma("sp", gq[:], C.inp["q_norm_g"][0:1, :].partition_broadcast(128), writes=["gq"])
        S.dma("sp", gk[:], C.inp["k_norm_g"][0:1, :].partition_broadcast(128), writes=["gk"])
        S.ts("dve", gq[:], gq[:], HD ** -0.5, ALU.mult, reads=["gq"], writes=["gq"])
        dtb = sb("dtb", [128, SH], F32); An = sb("An", [128, SH], F32)
        S.dma("sp", dtb[:], C.inp["dt_bias"][0:1, :].partition_broadcast(128), writes=["dtb"])
        S.dma("sp", An[:], C.inp["a_log"][0:1, :].partition_broadcast(128), writes=["An"])
        S.act(An[:], An[:], AF.Exp, reads=["An"], writes=["An"])
        S.ts("dve", An[:], An[:], -1.0, ALU.mult, reads=["An"], writes=["An"])
        cw = sb("cw", [128, 32, 4], F32); cb = sb("cb", [128, 32], F32)
        S.dma("sp", cw[:], C.inp["conv_wT"].rearrange("(c p) k -> p c k", p=128), writes=["cw"])
        S.dma("sp", cb[:], C.inp["conv_b"].rearrange("(c p) o -> p (c o)", p=128), writes=["cb"],
              allow_slow_non_contiguous=True)
        kmT = sb("kmT", [128, 8, C.NB], F32)
        S.memset("pool", kmT[:], 0.0, writes=["kmT"])
        wf = [sb("wf%d" % i, [128, 8, 512], F32) for i in range(2)]
        wb = [sb("wb%d" % i, [128, 8, 512], BF16) for i in range(2)]
        hb = [sb("hb%d" % i, [128, 8, 512], BF16) for i in range(3)]
        ev = [sb("ev%d" % i, [128, 512], F32) for i in range(2)]
        sq = sb("sq", [128, 512], F32)
        ssq = sb("ssq", [128, 8], F32)
        ob = [sb("ob%d" % i, [128, 512], BF16) for i in range(2)]
        tb = [sb("tb%d" % i, [128, 4, 128], BF16) for i in range(2)]
        kr = sb("kr", [128, 4], F32)
        cbuf = [sb("cbuf%d" % i, [128, 515], F32) for i in range(4)]
        caccs = [sb("cacc%d" % i, [128, 512], F32) for i in range(2)]
        dab = [sb("dab%d" % i, [128, 64], F32) for i in range(2)]

        blocks = []
        for j in range(2): blocks.append(("q", C_Q + 512 * j, 512, NT - NTO, j))
        for j in range(2): blocks.append(("k", C_K + 512 * j, 512, 0, j))
        for j in range(2): blocks.append(("v", C_V + 512 * j, 512, 0, j))
        for j in range(4): blocks.append(("z", C_Z + 512 * j, 512, NT - NTO, j))
        for j in range(4): blocks.append(("xs", C_X + 512 * j, 512, 0, j))
        for j in range(2): blocks.append(("B", C_B + 512 * j, 512, 0, j))
        for j in range(2): blocks.append(("C", C_C + 512 * j, 512, NT - NTO - 1, j))
        blocks.append(("dt", C_DT, 32, 0, 0))
        for j in range(2): blocks.append(("ga", C_GA + 512 * j, 512, NT - NTO, j))
        for j in range(2): blocks.append(("gs", C_GS + 512 * j, 512, NT - NTO, j))

        cnt = {"h": 0, "ps": 0, "ev": 0, "ob": 0, "tb": 0, "da": 0, "ca": 0}
        def load_w(bi):
            kind_, c0_, ncol_, _, _ = blocks[bi]
            wi_ = bi % 2
            S.dma("sp", wf[wi_][:, :, 0:ncol_], W[:, c0_:c0_ + ncol_].rearrange("(c p) n -> p c n", p=128),
                  writes=["wf%d" % wi_])
            S.copy("act", wb[wi_][:, :, 0:ncol_], wf[wi_][:, :, 0:ncol_], reads=["wf%d" % wi_], writes=["wb%d" % wi_])
        load_w(0)
        deferred = []
        cm_def = []
        for bi, (kind, c0, ncol, t0, j) in enumerate(blocks):
            wi = bi % 2
            while cm_def:
                cm_def.pop(0)()
            if bi + 1 < len(blocks):
                load_w(bi + 1)
            if kind in ("xs", "B", "C"):
                for s4 in range(4):
                    S.memset("pool", cbuf[s4][:, 0:3], 0.0, writes=["cbuf%d" % s4])
            for t in range(t0, NT):
                hi = cnt["h"] % 3; cnt["h"] += 1
                S.dma("sp", hb[hi][:], C.hT[t, :, :, :], writes=["hb%d" % hi])
                to = t - (NT - NTO)
                for s4 in range(4):
                    pi = cnt["ps"] % 4; cnt["ps"] += 1
                    ps = C.ps[pi]; pk = "ps%d" % pi
                    tok = t * 512 + s4 * 128
                    if kind in ("xs", "B", "C"):
                        for c in range(8):
                            S.mm(ps[:, 0:512], wb[wi][:, c, s4 * 128:(s4 + 1) * 128], hb[hi][:, c, :], c == 0, c == 7,
                                 reads=["wb%d" % wi, "hb%d" % hi], writes=[pk])
                        ck = "cbuf%d" % s4
                        cai = cnt["ca"] % 2; cnt["ca"] += 1
                        cacc = caccs[cai]; cak = "cacc%d" % cai
                        S.copy("act", cbuf[s4][:, 3:515], ps[:, 0:512], reads=[pk], writes=[ck])
                        while cm_def:
                            cm_def.pop(0)()
                        chn = (c0 - C_X) // 128 + s4
                        S.ts("dve", cacc[:], cbuf[s4][:, 0:512], cw[:, chn, 0:1], ALU.mult, cb[:, chn:chn + 1], ALU.add,
                             reads=[ck, "cw", "cb"], writes=[cak])
                        for k in range(1, 4):
                            S.stt("dve", cacc[:], cbuf[s4][:, k:k + 512], cw[:, chn, k:k + 1], cacc[:], ALU.mult, ALU.add,
                                  reads=[ck, cak], writes=[cak])
                        S.copy("pool", cbuf[s4][:, 0:3], cbuf[s4][:, 512:515], reads=[ck], writes=[ck])
                        def cm_fin(kind=kind, cacc=cacc, cak=cak, c0=c0, s4=s4, t=t, to=to):
                            oi = cnt["ob"] % 2; cnt["ob"] += 1
                            S.act(ob[oi][:], cacc[:], AF.Silu, reads=[cak], writes=["ob%d" % oi])
                            r0 = (c0 - {"xs": C_X, "B": C_B, "C": C_C}[kind]) + s4 * 128
                            if kind == "xs":
                                S.dma("pool", C.xsT[r0:r0 + 128, t * 512:(t + 1) * 512], ob[oi][:], reads=["ob%d" % oi])
                            elif kind == "B":
                                S.dma("pool", C.BT[r0:r0 + 128, t * 512:(t + 1) * 512], ob[oi][:], reads=["ob%d" % oi])
                            elif to >= 0:
                                S.dma("pool", C.CT[r0:r0 + 128, to * 512:(to + 1) * 512], ob[oi][:], reads=["ob%d" % oi])
                        cm_def.append(cm_fin)
                        continue
                    for c in range(8):
                        S.mm(ps[:, 0:ncol], hb[hi][:, c, s4 * 128:(s4 + 1) * 128], wb[wi][:, c, 0:ncol], c == 0, c == 7,
                             reads=["wb%d" % wi, "hb%d" % hi], writes=[pk])
                    while deferred:
                        deferred.pop(0)()
                    if kind in ("q", "k"):
                        ei = cnt["ev"] % 2; cnt["ev"] += 1
                        ek = "ev%d" % ei
                        S.copy("act", ev[ei][:], ps[:, 0:512], reads=[pk], writes=[ek])
                        S.tt("dve", sq[:], ev[ei][:], ev[ei][:], ALU.mult, reads=[ek], writes=["sq"])
                        S.op("dve", lambda e: e.tensor_reduce(out=ssq[:], in_=sq[:].rearrange("p (a b) -> p a b", b=HD),
                                                             axis=AX.X, op=ALU.add), reads=["sq"], writes=["ssq"])
                        S.act(ssq[:], ssq[:], AF.Ln, reads=["ssq"], writes=["ssq"], scale=1.0 / HD, bias=EPS)
                        S.act(ssq[:], ssq[:], AF.Exp, reads=["ssq"], writes=["ssq"], scale=-0.5)
                        e3 = ev[ei][:].rearrange("p (a b) -> p a b", b=HD)
                        S.tt("dve", e3, e3, ssq[:].unsqueeze(2).to_broadcast([128, 8, HD]), ALU.mult,
                             reads=[ek, "ssq"], writes=[ek])
                        oi = cnt["ob"] % 2; cnt["ob"] += 1
                        g_ = gq if kind == "q" else gk
                        S.tt("dve", ob[oi][:].rearrange("p (a b) -> p a b", b=HD), e3,
                             g_[:].unsqueeze(1).to_broadcast([128, 8, HD]), ALU.mult,
                             reads=[ek, "gq", "gk"], writes=["ob%d" % oi])
                        def fin(kind=kind, oi=oi, j=j, to=to, s4=s4, tok=tok):
                            pti = 4 + cnt["tb"] % 2
                            ti = cnt["tb"] % 2; cnt["tb"] += 1
                            pT = C.ps[pti][:].bitcast(BF16)[:, 0:512].rearrange("p (c t) -> p c t", t=128)
                            for c in range(4):
                                S.tr(pT[:, c, :], ob[oi][:, c * 128:(c + 1) * 128], ident[:], reads=["ob%d" % oi, "ident"],
                                     writes=["ps%d" % pti])
                            S.copy("act", tb[ti][:], pT, reads=["ps%d" % pti], writes=["tb%d" % ti])
                            rows = slice(j * 512, (j + 1) * 512)
                            if kind == "q":
                                S.dma("pool", C.qT[rows, to * 512 + s4 * 128: to * 512 + (s4 + 1) * 128].rearrange("(c p) t -> p c t", p=128),
                                      tb[ti][:], reads=["tb%d" % ti])
                            else:
                                S.dma("pool", C.kT[rows, tok:tok + 128].rearrange("(c p) t -> p c t", p=128),
                                      tb[ti][:], reads=["tb%d" % ti])
                                S.op("dve", lambda e, ti=ti: e.tensor_reduce(out=kr[:], in_=tb[ti][:], axis=AX.X, op=ALU.add),
                                     reads=["tb%d" % ti], writes=["kr"])
                                blk = tok // 256
                                S.stt("dve", kmT[:, j * 4:(j + 1) * 4, blk], kr[:], 1.0 / 256, kmT[:, j * 4:(j + 1) * 4, blk],
                                      ALU.mult, ALU.add, reads=["kr", "kmT"], writes=["kmT"])
                        deferred.append(fin)
                    elif kind == "dt":
                        di = cnt["da"] % 2; cnt["da"] += 1
                        dk = "dab%d" % di
                        S.tt("dve", dab[di][:, 0:32], ps[:, 0:32], dtb[:], ALU.add, reads=[pk, "dtb"], writes=[dk])
                        S.act(dab[di][:, 0:32], dab[di][:, 0:32], AF.Exp, reads=[dk], writes=[dk])
                        S.act(dab[di][:, 0:32], dab[di][:, 0:32], AF.Ln, reads=[dk], writes=[dk], bias=1.0)
                        S.tt("dve", dab[di][:, 32:64], dab[di][:, 0:32], An[:], ALU.mult, reads=[dk, "An"], writes=[dk])
                        S.dma("pool", C.da[tok:tok + 128, :], dab[di][:], reads=[dk])
                    else:
                        oi = cnt["ob"] % 2; cnt["ob"] += 1
                        fn = {"v": AF.Copy, "z": AF.Silu, "ga": AF.Sigmoid, "gs": AF.Sigmoid}[kind]
                        S.act(ob[oi][:], ps[:, 0:512], fn, reads=[pk], writes=["ob%d" % oi])
                        if kind == "v":
                            dst = C.v[tok:tok + 128, j * 512:(j + 1) * 512]
                        else:
                            otok = to * 512 + s4 * 128
                            dst = {"z": C.z, "ga": C.sga, "gs": C.sgs}[kind][otok:otok + 128, j * 512:(j + 1) * 512]
                        S.dma("pool", dst, ob[oi][:], reads=["ob%d" % oi])
        while deferred:
            deferred.pop(0)()
        while cm_def:
            cm_def.pop(0)()
        S.dma("pool", C.kmT.rearrange("c p n -> p c n"), kmT[:], reads=["kmT"])
        S.flush()


def moba_setup(C):
    nc = C.nc
    NV, NO, NB = C.NV, C.NO, C.NB

    def inp(name, shape, dt=F32):
        C.inp[name] = nc.dram_tensor(name, list(shape), dt, kind="ExternalInput").ap()
    inp("kaug_c", [33, NV], BF16)
    inp("cq", [NH, NO], BF16)
    inp("kbias", [128, NH, NV // 128])
    NBO = NO // 256
    inp("gbp", [1, NBO * 32]); inp("A01", [1, NBO * 32]); inp("Bt", [1, NBO * 32])
    inp("cm", [128, 2, 256], BF16)
    inp("sel65", [65, 64])
    C.aT = dram(C, "aT_s", [D, NO], BF16)


def moba_consts(NV, r):
    bf = ml_dtypes.bfloat16
    NO = NV // 2
    NB = NV // 256
    NBO = NO // 256
    slopes = np.exp2(-8.0 * np.arange(1, NH + 1, dtype=np.float32) / NH).astype(np.float32)
    out = {}
    ka = np.zeros((33, NV), np.float32)
    for n in range(NB):
        ka[n, n * 256:(n + 1) * 256] = 1
    ka[32] = 1
    out["kaug_c"] = ka.astype(bf)
    pos_q = (NO + np.arange(NO)).astype(np.float32)
    out["cq"] = (-slopes[:, None] * pos_q[None, :]).astype(bf)
    pos_k = (np.arange(NV // 128)[None, :] * 128 + np.arange(128)[:, None]).astype(np.float32)
    out["kbias"] = np.ascontiguousarray((slopes[None, :, None] * pos_k[:, None, :]).astype(np.float32))
    valid = np.ones(32, bool)
    valid[NB:] = False
    if r == 0:
        valid[:NB // 2] = False
    gbp = np.full((NBO, 32), NEG, np.float32); A01 = np.zeros((NBO, 32), np.float32); Bt = np.full((NBO, 32), NEG, np.float32)
    for mo in range(NBO):
        m = NB // 2 + mo
        for n in range(32):
            if n < m and valid[n]:
                gbp[mo, n] = 0; A01[mo, n] = 1
            if n == m:
                Bt[mo, n] = 0
    out["gbp"] = gbp.reshape(1, -1); out["A01"] = A01.reshape(1, -1); out["Bt"] = Bt.reshape(1, -1)
    cm = np.zeros((128, 2, 256), np.float32)
    kk = np.arange(128)[:, None]; qq = np.arange(256)[None, :]
    cm[:, 0, :] = (qq >= kk); cm[:, 1, :] = (qq >= kk + 128)
    out["cm"] = ((cm - 1.0) * 30000.0).astype(bf)
    s = np.zeros((65, 64), np.float32); s[64] = 1
    out["sel65"] = s
    return out


def phase_moba(C, S):
    nc = C.nc
    NV, NO, NB = C.NV, C.NO, C.NB
    NKT = NV // 128
    NQ = NO // 512
    NBO = NO // 256
    with ExitStack() as st:
        sb = lambda name, shape, dt: st.enter_context(nc.sbuf_tensor("mb_" + name, shape, dt))
        ident = sb("ident", [128, 128], BF16)
        S.dma("sp", ident[:], C.inp["ident_bf"][:, :], writes=["ident"])
        kaT = [sb("kaT%d" % i, [97, NV], BF16) for i in range(2)]
        qaT = [sb("qaT%d" % i, [97, NO], BF16) for i in range(2)]
        for i in range(2):
            S.dma("sp", kaT[i][64:97, :], C.inp["kaug_c"][:, :], writes=["kaT%d" % i])
        vt = sb("vt", [128, NKT, 8, 65], BF16)
        kbias = sb("kbias", [128, NH, NKT], F32)
        S.dma("sp", kbias[:], C.inp["kbias"][:, :, :], writes=["kbias"])
        gbp = sb("gbp", [128, NBO, 32], F32); A01 = sb("A01", [128, NBO, 32], F32); Bt = sb("Bt", [128, NBO, 32], F32)
        for nm, tl in (("gbp", gbp), ("A01", A01), ("Bt", Bt)):
            S.dma("sp", tl[:].rearrange("p a b -> p (a b)"), C.inp[nm][0:1, :].partition_broadcast(128), writes=[nm])
        cm = sb("cm", [128, 2, 256], BF16)
        S.dma("sp", cm[:], C.inp["cm"][:, :, :], writes=["cm"])
        sel65 = sb("sel65", [65, 64], F32)
        S.dma("sp", sel65[:], C.inp["sel65"][:, :], writes=["sel65"])
        kmf = sb("kmf", [64, NB], F32)
        kmb = sb("kmb", [64, 32], BF16)
        S.memset("pool", kmb[:], 0.0, writes=["kmb"])
        gm = sb("gm", [128, 32], F32); top8 = sb("top8", [128, 8], F32); f1 = sb("f1", [128, 32], F32)
        mbt = [sb("mbt%d" % i, [128, 96], BF16) for i in range(2)]
        for i in range(2):
            S.memset("pool", mbt[i][:], 0.0, writes=["mbt%d" % i])
        pt = [sb("pt%d" % i, [128, 512], BF16) for i in range(4)]
        oT = sb("oT", [65, 512], F32); rd = sb("rd", [64, 512], F32)
        ao = [sb("ao%d" % i, [64, 512], BF16) for i in range(2)]
        psS = [C.ps[0], C.ps[1]]; psO = [C.ps[2], C.ps[3]]; psG = C.ps[4]; psT = C.ps[5]; psD = C.ps[6]
        cnt = {"s": 0, "pt": 0, "o": 0, "ao": 0, "mb": 0}
        def load_v(g):
            S.memset("pool", vt[:, :, :, 64:65], 1.0, writes=["vt"])
            for kt0 in range(NKT):
                S.dma("sp", vt[:, kt0, :, 0:64],
                      C.v[kt0 * 128:(kt0 + 1) * 128, g * 512:(g + 1) * 512].rearrange("p (a d) -> p a d", d=64),
                      writes=["vt"])

        def prep_steps(h):
            hb = h % 2
            steps = []

            def loads():
                S.dma("sp", kaT[hb][0:64, :], C.kT[h * 64:(h + 1) * 64, :], writes=["kaT%d" % hb])
                S.dma("sp", qaT[hb][0:64, :], C.qT[h * 64:(h + 1) * 64, :], writes=["qaT%d" % hb])
                S.dma("sp", qaT[hb][96:97, :], C.inp["cq"][h:h + 1, :], writes=["qaT%d" % hb])
                S.dma("sp", kmf[:], C.kmT[h // 2, (h % 2) * 64:(h % 2) * 64 + 64, :], writes=["kmf"])
                S.copy("act", kmb[:, 0:NB], kmf[:], reads=["kmf"], writes=["kmb"])
            steps.append(loads)
            nq = NO // 128
            st1, st2, st3 = [], [], []
            for qs in range(nq):
                mo = qs // 2
                mi = qs % 2

                def s1(qs=qs, mo=mo, mi=mi):
                    S.mm(psG[:, 0:32], qaT[hb][0:64, qs * 128:(qs + 1) * 128], kmb[:], True, True,
                         reads=["qaT%d" % hb, "kmb"], writes=["psG"])
                    S.tt("dve", gm[:], psG[:, 0:32], gbp[:, mo, :], ALU.add, reads=["psG", "gbp"], writes=["gm"])
                    S.op("dve", lambda e: e.max(out=top8[:], in_=gm[:]), reads=["gm"], writes=["top8"])
                    S.ts("dve", f1[:], gm[:], top8[:, 2:3], ALU.is_ge, -NEG, ALU.mult, reads=["gm", "top8"], writes=["f1"])
                    S.tt("dve", f1[:], f1[:], A01[:, mo, :], ALU.mult, reads=["f1", "A01"], writes=["f1"])
                    S.tt("dve", mbt[mi][:, 64:96], f1[:], Bt[:, mo, :], ALU.add, reads=["f1", "Bt"], writes=["mbt%d" % mi])

                def s2(qs=qs, mi=mi):
                    pTt = psT[:].bitcast(BF16)[0:96, 0:128]
                    S.tr(pTt, mbt[mi][:], ident[:], reads=["mbt%d" % mi, "ident"], writes=["psT"])

                def s3(qs=qs):
                    S.copy("act", qaT[hb][64:96, qs * 128:(qs + 1) * 128], psT[:].bitcast(BF16)[64:96, 0:128],
                           reads=["psT"], writes=["qaT%d" % hb])
                st1.append(s1); st2.append(s2); st3.append(s3)
            for k in range(nq + 2):
                if 0 <= k - 2 < nq: steps.append(st3[k - 2])
                if 0 <= k - 1 < nq: steps.append(st2[k - 1])
                if k < nq: steps.append(st1[k])
            return steps

        def pairs(h, pending):
            hb = h % 2
            hl = h % 8
            for j in range(NQ):
                oi = cnt["o"] % 2; cnt["o"] += 1
                ok = "ps%d" % (2 + oi)
                b0 = (NO + 512 * j) // 256
                nkt = NKT // 2 + 4 * j + 4
                def emitS(kt):
                    si = cnt["s"] % 2; cnt["s"] += 1
                    sk = "ps%d" % si
                    n = kt // 2
                    lk = kaT[hb][0:97, kt * 128:(kt + 1) * 128]
                    if n >= b0:
                        c0 = (n - b0) * 256
                        c1 = 256 - c0
                        S.mm(psS[si][:, c1:c1 + 256], lk, qaT[hb][0:97, j * 512 + c1:j * 512 + c1 + 256],
                             True, True, reads=["kaT%d" % hb, "qaT%d" % hb], writes=[sk])
                        S.mm(psS[si][:, c0:c0 + 256], lk, qaT[hb][0:97, j * 512 + c0:j * 512 + c0 + 256],
                             True, False, reads=["kaT%d" % hb, "qaT%d" % hb], writes=[sk])
                        S.mm(psS[si][:, c0:c0 + 256], ident[:], cm[:, kt % 2, :],
                             False, True, reads=["ident", "cm"], writes=[sk])
                    else:
                        S.mm(psS[si][:, 0:512], lk, qaT[hb][0:97, j * 512:(j + 1) * 512],
                             True, True, reads=["kaT%d" % hb, "qaT%d" % hb], writes=[sk])
                    pi = cnt["pt"] % 4; cnt["pt"] += 1
                    pk = "pt%d" % pi
                    S.act(pt[pi][:], psS[si][:, 0:512], AF.Exp, reads=[sk, "kbias"], writes=[pk], bias=kbias[:, h, kt:kt + 1])
                    return pi
                pis = {0: emitS(0)}
                for kt in range(nkt):
                    if kt + 1 < nkt:
                        pis[kt + 1] = emitS(kt + 1)
                    pi = pis.pop(kt)
                    S.mm(psO[oi][0:65, 0:512], vt[:, kt, hl, :], pt[pi][:], kt == 0, kt == nkt - 1,
                         reads=["vt", "pt%d" % pi], writes=[ok])
                    for _ in range(MOBA_DUMMY):
                        S.mm(C.ps[7][:, 0:512], ident[:], cm[:].rearrange("p a b -> p (a b)"), True, True,
                             reads=["ident", "cm"], writes=["ps7"])
                    if pending and kt % 2 == 1:
                        pending.pop(0)()
                S.copy("act", oT[:], psO[oi][0:65, 0:512], reads=[ok], writes=["oT"])
                S.mm(psD[0:64, 0:512], sel65[:], oT[:], True, True, reads=["sel65", "oT"], writes=["psD"])
                S.op("dve", lambda e: e.reciprocal(out=rd[:], in_=psD[0:64, 0:512]), reads=["psD"], writes=["rd"])
                ai = cnt["ao"] % 2; cnt["ao"] += 1
                S.tt("dve", ao[ai][:], oT[0:64, :], rd[:], ALU.mult, reads=["oT", "rd"], writes=["ao%d" % ai])
                S.dma("pool", C.aT[h * 64:(h + 1) * 64, j * 512:(j + 1) * 512], ao[ai][:], reads=["ao%d" % ai])

        for f in prep_steps(0):
            f()
        for h in range(NH):
            if h % 8 == 0:
                load_v(h // 8)
            pending = prep_steps(h + 1) if h + 1 < NH else []
            pairs(h, pending)
            while pending:
                pending.pop(0)()
        S.flush()


def ssd_setup(C):
    nc = C.nc

    def inp(name, shape, dt=F32):
        C.inp[name] = nc.dram_tensor(name, list(shape), dt, kind="ExternalInput").ap()
    inp("tri_f", [128, 128]); inp("ones_f", [128, 128]); inp("trineg_f", [128, 128])
    C.ynT = dram(C, "ynT_s", [2048, C.NO], BF16)


def ssd_consts():
    s = np.arange(128)[:, None]; t = np.arange(128)[None, :]
    return {"tri_f": (s <= t).astype(np.float32), "ones_f": np.ones((128, 128), np.float32),
            "trineg_f": np.where(t >= s, 0.0, NEG).astype(np.float32)}


def phase_ssd(C, S):
    nc = C.nc
    C.ssd_stop = getattr(C, "ssd_stop", 99)
    C.ssd_sub = getattr(C, "ssd_sub", 99)
    NV, NO = C.NV, C.NO
    NCH = NV // 256
    with ExitStack() as st:
        sb = lambda name, shape, dt: st.enter_context(nc.sbuf_tensor("sd_" + name, shape, dt))
        ident = sb("ident", [128, 128], BF16); identf = sb("identf", [128, 128], F32)
        tri = sb("tri", [128, 128], F32); ones = sb("ones", [128, 128], F32); trineg = sb("trineg", [128, 128], F32)
        for tl, nm in ((ident, "ident_bf"), (identf, "ident_f"), (tri, "tri_f"), (ones, "ones_f"), (trineg, "trineg_f")):
            S.dma("sp", tl[:], C.inp[nm][:, :], writes=["consts"])
        dsk = sb("dsk", [128, SH], F32); gn = sb("gn", [128, 2048], F32); pv = sb("pv", [128, 1], F32)
        S.dma("sp", dsk[:], C.inp["d_skip"][0:1, :].partition_broadcast(128), writes=["consts"])
        S.dma("sp", gn[:], C.inp["ssd_norm_g"][0:1, :].partition_broadcast(128), writes=["consts"])
        S.dma("sp", pv[:], C.inp["pv"][0:1, :].partition_broadcast(128), writes=["consts"])
        state = sb("state", [128, 8, 256], F32); stb = sb("stb", [128, 8, 256], BF16)
        S.memset("pool", state[:], 0.0, writes=["state"])
        S.memset("pool", stb[:], 0.0, writes=["stb"])
        xsT = sb("xsT", [128, 16, 256], BF16); BTt = sb("BTt", [128, 8, 256], BF16); CTt = sb("CTt", [128, 8, 256], BF16)
        da = sb("da", [128, 2, 64], F32); zt = sb("zt", [128, 2, 2048], BF16)
        xdt = sb("xdt", [128, 2, 2048], BF16); xdtd = sb("xdtd", [128, 2, 2048], BF16); xtm = sb("xtm", [128, 2, 2048], BF16)
        Btm = sb("Btm", [128, 2, 8, 128], BF16)
        acum = sb("acum", [128, 2, 32], F32); nacum = sb("nacum", [128, 2, 32], F32); eA = sb("eA", [128, 2, 32], F32)
        dend = sb("dend", [128, 2, 32], F32); eTot = sb("eTot", [128, 32], F32); tot = sb("tot", [128, 32], F32)
        Lt = [sb("Lt%d" % i, [128, 384], F32) for i in range(2)]
        Mt = [sb("Mt%d" % i, [128, 384], BF16) for i in range(4)]
        ysb = sb("ysb", [128, 2, 2048], F32); ytmp = sb("ytmp", [128, 2048], F32)
        RA = sb("RA", [128, 3, 4, 128], F32)
        ssg = sb("ssg", [128, 8], F32); ynb = sb("ynb", [128, 2048], BF16); ynT = sb("ynT", [128, 16, 128], BF16)
        ps = C.ps
        for c in range(NCH):
            own = c >= NCH // 2
            t0 = c * 256
            to0 = t0 - NO
            S.dma("sp", xsT[:], C.xsT[:, t0:t0 + 256].rearrange("(c p) t -> p c t", p=128), writes=["xsT"])
            S.dma("sp", BTt[:], C.BT[:, t0:t0 + 256].rearrange("(c p) t -> p c t", p=128), writes=["BTt"])
            S.dma("sp", da[:], C.da[t0:t0 + 256, :].rearrange("(i p) c -> p i c", p=128), writes=["da"])
            if own:
                S.dma("sp", CTt[:], C.CT[:, to0:to0 + 256].rearrange("(c p) t -> p c t", p=128), writes=["CTt"])
                S.dma("sp", zt[:], C.z[to0:to0 + 256, :].rearrange("(i p) c -> p i c", p=128), writes=["zt"])
            S.mm(ps[6][:, 0:32], tri[:], da[:, 0, 32:64], True, True, reads=["consts", "da"], writes=["ps6"])
            S.mm(ps[6][:, 32:64], tri[:], da[:, 1, 32:64], True, False, reads=["consts", "da"], writes=["ps6"])
            S.mm(ps[6][:, 32:64], ones[:], da[:, 0, 32:64], False, True, reads=["consts", "da"], writes=["ps6"])
            S.mm(ps[6][:, 64:96], ones[:], da[:, 0, 32:64], True, False, reads=["consts", "da"], writes=["ps6"])
            S.mm(ps[6][:, 64:96], ones[:], da[:, 1, 32:64], False, True, reads=["consts", "da"], writes=["ps6"])
            S.copy("dve", acum[:].rearrange("p i h -> p (i h)"), ps[6][:, 0:64], reads=["ps6"], writes=["acum"])
            S.copy("dve", tot[:], ps[6][:, 64:96], reads=["ps6"], writes=["tot"])
            S.ts("dve", nacum[:], acum[:], -1.0, ALU.mult, reads=["acum"], writes=["nacum"])
            S.act(eA[:], acum[:], AF.Exp, reads=["acum"], writes=["eA"])
            S.act(eTot[:], tot[:], AF.Exp, reads=["tot"], writes=["eTot"])
            S.tt("dve", dend[:], nacum[:], tot[:].unsqueeze(1).to_broadcast([128, 2, 32]), ALU.add,
                 reads=["nacum", "tot"], writes=["dend"])
            S.act(dend[:], dend[:], AF.Exp, reads=["dend"], writes=["dend"])
            if C.ssd_stop <= 1:
                continue
            for i in range(2):
                pT = ps[7][:].bitcast(BF16)[:, 0:1024].rearrange("p (c t) -> p c t", t=128)
                for half in range(2):
                    for cc in range(8):
                        S.tr(pT[:, cc, :], xsT[:, half * 8 + cc, i * 128:(i + 1) * 128], ident[:],
                             reads=["xsT", "consts"], writes=["ps7"])
                    hs = slice(half * 16, half * 16 + 16)
                    dst = xdt[:, i, half * 1024:(half + 1) * 1024].rearrange("p (h d) -> p h d", d=64)
                    src = pT.rearrange("p c (h d) -> p (c h) d", d=64)
                    S.tt("dve", dst, src, da[:, i, hs].unsqueeze(2).to_broadcast([128, 16, 64]), ALU.mult,
                         reads=["ps7", "da"], writes=["xdt"])
                    if own and C.ssd_sub >= 1:
                        S.copy("act", xtm[:, i, half * 1024:(half + 1) * 1024], pT.rearrange("p c t -> p (c t)"),
                               reads=["ps7"], writes=["xtm"])
                if C.ssd_sub < 2:
                    continue
                S.tt("dve", xdtd[:, i, :].rearrange("p (h d) -> p h d", d=64), xdt[:, i, :].rearrange("p (h d) -> p h d", d=64),
                     dend[:, i, :].unsqueeze(2).to_broadcast([128, 32, 64]), ALU.mult, reads=["xdt", "dend"], writes=["xdtd"])
                if C.ssd_sub < 3:
                    continue
                pT = ps[7][:].bitcast(BF16)[:, 0:1024].rearrange("p (c t) -> p c t", t=128)
                for g in range(8):
                    S.tr(pT[:, g, :], BTt[:, g, i * 128:(i + 1) * 128], ident[:], reads=["BTt", "consts"], writes=["ps7"])
                S.copy("act", Btm[:, i, :, :], pT, reads=["ps7"], writes=["Btm"])
            if C.ssd_stop <= 2:
                continue
            if own:
                for g in range(8):
                    if C.ssd_stop <= 3 and g > 0:
                        continue
                    S.mm(ps[4][:, 0:256], BTt[:, g, 0:128], CTt[:, g, 0:256], True, True, reads=["BTt", "CTt"], writes=["ps4"])
                    S.mm(ps[4][:, 256:384], BTt[:, g, 128:256], CTt[:, g, 128:256], True, True, reads=["BTt", "CTt"], writes=["ps4"])
                    for h4 in range(4):
                        h = g * 4 + h4
                        pL = ps[h4]; lk = "ps%d" % h4
                        if h4 == 0:
                            for q_, (src_i, mat) in enumerate(((0, tri), (0, ones), (1, tri))):
                                S.tt("dve", RA[:, q_, :, :], da[:, src_i, 32 + g * 4:36 + g * 4].unsqueeze(2).to_broadcast([128, 4, 128]),
                                     mat[:].unsqueeze(1).to_broadcast([128, 4, 128]), ALU.mult, reads=["da", "consts"], writes=["RA"])
                        S.mm(pL[:, 0:128], ones[:], RA[:, 0, h4, :], True, False, reads=["RA", "consts"], writes=[lk])
                        S.mm(pL[:, 0:128], identf[:], trineg[:], False, True, reads=["consts"], writes=[lk])
                        S.mm(pL[:, 128:256], ones[:], RA[:, 1, h4, :], True, False, reads=["RA", "consts"], writes=[lk])
                        S.mm(pL[:, 128:256], ones[:], RA[:, 2, h4, :], False, True, reads=["RA", "consts"], writes=[lk])
                        S.mm(pL[:, 256:384], ones[:], RA[:, 1, h4, :], True, False, reads=["RA", "consts"], writes=[lk])
                        S.mm(pL[:, 256:384], ones[:], RA[:, 2, h4, :], False, False, reads=["RA", "consts"], writes=[lk])
                        S.mm(pL[:, 256:384], identf[:], trineg[:], False, True, reads=["consts"], writes=[lk])
                        li = h % 2
                        S.act(Lt[li][:, 0:256], pL[:, 0:256], AF.Exp, reads=[lk, "nacum"], writes=["Lt%d" % li],
                              bias=nacum[:, 0, h:h + 1])
                        S.act(Lt[li][:, 256:384], pL[:, 256:384], AF.Exp, reads=[lk, "nacum"], writes=["Lt%d" % li],
                              bias=nacum[:, 1, h:h + 1])
                        S.tt("dve", Mt[h4][:], Lt[li][:], ps[4][:, 0:384], ALU.mult, reads=["Lt%d" % li, "ps4"],
                             writes=["Mt%d" % h4])
                    if C.ssd_stop <= 4:
                        continue
                    pY = ps[5][:, 0:512].rearrange("p (i c) -> p i c", i=2)
                    for h4 in range(4):
                        h = g * 4 + h4
                        cs = slice(h4 * 64, (h4 + 1) * 64)
                        S.mm(pY[:, 0, cs], Mt[h4][:, 0:128], xdt[:, 0, h * 64:(h + 1) * 64], True, True,
                             reads=["Mt%d" % h4, "xdt"], writes=["ps5"])
                        S.mm(pY[:, 1, cs], Mt[h4][:, 128:256], xdt[:, 0, h * 64:(h + 1) * 64], True, False,
                             reads=["Mt%d" % h4, "xdt"], writes=["ps5"])
                        S.mm(pY[:, 1, cs], Mt[h4][:, 256:384], xdt[:, 1, h * 64:(h + 1) * 64], False, True,
                             reads=["Mt%d" % h4, "xdt"], writes=["ps5"])
                    pO = ps[6][:, 0:512].rearrange("p (i c) -> p i c", i=2)
                    for i in range(2):
                        S.mm(pO[:, i, :], CTt[:, g, i * 128:(i + 1) * 128], stb[:, g, :], True, True,
                             reads=["CTt", "stb"], writes=["ps6"])
                    for i in range(2):
                        yg = ysb[:, i, g * 256:(g + 1) * 256].rearrange("p (h d) -> p h d", d=64)
                        S.tt("dve", yg, pO[:, i, :].rearrange("p (h d) -> p h d", d=64),
                             eA[:, i, g * 4:(g + 1) * 4].unsqueeze(2).to_broadcast([128, 4, 64]), ALU.mult,
                             reads=["ps6", "eA"], writes=["ysb"])
                        S.tt("dve", ysb[:, i, g * 256:(g + 1) * 256], ysb[:, i, g * 256:(g + 1) * 256], pY[:, i, :], ALU.add,
                             reads=["ysb", "ps5"], writes=["ysb"])
            if C.ssd_stop <= 5:
                continue
            for g in range(8):
                for i in range(2):
                    S.mm(ps[5][:, 0:256], Btm[:, i, g, :], xdtd[:, i, g * 256:(g + 1) * 256], i == 0, i == 1,
                         reads=["Btm", "xdtd"], writes=["ps5"])
                sg = state[:, g, :].rearrange("p (h d) -> p h d", d=64)
                S.tt("dve", sg, sg, eTot[:, g * 4:(g + 1) * 4].unsqueeze(2).to_broadcast([128, 4, 64]), ALU.mult,
                     reads=["state", "eTot"], writes=["state"])
                S.tt("dve", state[:, g, :], state[:, g, :], ps[5][:, 0:256], ALU.add, reads=["state", "ps5"], writes=["state"])
            if c == NCH // 2 - 1:
                S.ts("dve", state[:].rearrange("p g c -> p (g c)"), state[:].rearrange("p g c -> p (g c)"), pv[:, 0:1], ALU.mult,
                     reads=["state", "consts"], writes=["state"])
            S.copy("act", stb[:].rearrange("p g c -> p (g c)"), state[:].rearrange("p g c -> p (g c)"), reads=["state"], writes=["stb"])
            if C.ssd_stop <= 6:
                continue
            if own:
                for i in range(2):
                    y = ysb[:, i, :]
                    y3 = y.rearrange("p (h d) -> p h d", d=64)
                    S.tt("dve", ytmp[:].rearrange("p (h d) -> p h d", d=64), xtm[:, i, :].rearrange("p (h d) -> p h d", d=64),
                         dsk[:].unsqueeze(2).to_broadcast([128, 32, 64]), ALU.mult, reads=["xtm", "consts"], writes=["ytmp"])
                    S.tt("dve", y, y, ytmp[:], ALU.add, reads=["ysb", "ytmp"], writes=["ysb"])
                    S.tt("dve", y, y, zt[:, i, :], ALU.mult, reads=["ysb", "zt"], writes=["ysb"])
                    S.tt("dve", ytmp[:], y, y, ALU.mult, reads=["ysb"], writes=["ytmp"])
                    S.op("dve", lambda e: e.tensor_reduce(out=ssg[:], in_=ytmp[:].rearrange("p (g c) -> p g c", c=256),
                                                         axis=AX.X, op=ALU.add), reads=["ytmp"], writes=["ssg"])
                    S.act(ssg[:], ssg[:], AF.Ln, reads=["ssg"], writes=["ssg"], scale=1.0 / 256, bias=EPS)
                    S.act(ssg[:], ssg[:], AF.Exp, reads=["ssg"], writes=["ssg"], scale=-0.5)
                    S.tt("dve", ytmp[:].rearrange("p (g c) -> p g c", c=256), y.rearrange("p (g c) -> p g c", c=256),
                         ssg[:].unsqueeze(2).to_broadcast([128, 8, 256]), ALU.mult, reads=["ysb", "ssg"], writes=["ytmp"])
                    S.tt("dve", ynb[:], ytmp[:], gn[:], ALU.mult, reads=["ytmp", "consts"], writes=["ynb"])
                    for half in range(2):
                        pT = ps[7][:].bitcast(BF16)[:, 0:1024].rearrange("p (c t) -> p c t", t=128)
                        for cc in range(8):
                            S.tr(pT[:, cc, :], ynb[:, (half * 8 + cc) * 128:(half * 8 + cc + 1) * 128], ident[:],
                                 reads=["ynb", "consts"], writes=["ps7"])
                        S.copy("act", ynT[:, half * 8:(half + 1) * 8, :], pT, reads=["ps7"], writes=["ynT"])
                    tok = to0 + i * 128
                    S.dma("pool", C.ynT[:, tok:tok + 128].rearrange("(c p) t -> p c t", p=128), ynT[:], reads=["ynT"])
        S.flush()


def peer_setup(C):
    nc = C.nc

    def inp(name, shape, dt=F32):
        C.inp[name] = nc.dram_tensor(name, list(shape), dt, kind="ExternalInput").ap()
    inp("R1", [128, 32, 512], BF16); inp("R2", [128, 512], BF16)
    C.x2 = dram(C, "x2_s", [C.NO, D], F32)
    C.hT2 = dram(C, "hT2_s", [C.NO // 512, 128, 8, 512], BF16)
    C.out = nc.dram_tensor("out", [C.NO, D], F32, kind="ExternalOutput").ap()


def peer_consts():
    bf = ml_dtypes.bfloat16
    R1 = np.zeros((128, 32, 4, 128), np.float32)
    for c in range(32):
        for j in range(4):
            R1[4 * c + j, c, j, :] = 1
    R2 = np.tile(np.eye(128, dtype=np.float32), (1, 4))
    return {"R1": R1.reshape(128, 32, 512).astype(bf), "R2": R2.astype(bf)}


def phase_peer(C, S):
    nc = C.nc
    C.peer_dummy = getattr(C, "peer_dummy", PEER_DUMMY)
    NO = C.NO
    UT = C.inp["peer_uT"]; VT = C.inp["peer_v"]; WQ = C.inp["w_peer_q"]
    with ExitStack() as st:
        sb = lambda name, shape, dt: st.enter_context(nc.sbuf_tensor("pr_" + name, shape, dt))
        ident = sb("ident", [128, 128], BF16)
        S.dma("sp", ident[:], C.inp["ident_bf"][:, :], writes=["ident"])
        R1 = sb("R1", [128, 32, 512], BF16); R2 = sb("R2", [128, 512], BF16)
        S.dma("sp", R1[:], C.inp["R1"][:, :, :], writes=["R1"])
        S.dma("sp", R2[:], C.inp["R2"][:, :], writes=["R2"])
        stg = sb("stg", [128, 4096], F32)
        stg2 = sb("stg2", [128, 4096], F32)
        stg2_v = stg2[:].rearrange("p (j d) -> p j d", d=1024)
        stg_u = stg[:].rearrange("p (c n) -> p c n", n=512)
        stg_v = stg[:].rearrange("p (j d) -> p j d", d=1024)
        kT = sb("kT", [128, 16, 128], BF16)
        for hh in range(2):
            S.dma("sp", stg_u[:, :, 0:128], C.inp["keys%dT" % (hh + 1)].rearrange("h d k -> d h k"), writes=["stg"])
            S.copy("pool", kT[:].rearrange("p (h two) k -> p h two k", two=2)[:, :, hh, :], stg_u[:, :, 0:128],
                   reads=["stg"], writes=["kT"])
        ub = [sb("ub%d" % i, [128, 8, 512], BF16) for i in range(2)]
        vb = [sb("vb%d" % i, [128, 4, 1024], BF16) for i in range(2)]
        xnT = sb("xnT", [128, 8, 512], BF16)
        shr = sb("shr", [128, 8192], BF16)
        qTr = shr[:].rearrange("p (c t) -> p c t", t=512)
        sb16 = sb("sb16", [128, 16, 128], BF16); sf = sb("sf", [128, 16, 128], F32); swk = sb("swk", [128, 128], F32)
        v16 = sb("v16", [128, 16, 16], F32)
        cand = sb("cand", [128, 8, 256], F32); cwk = stg2[:, 0:2048].rearrange("p (h k) -> p h k", k=256)
        t8 = sb("t8", [128, 8, 8], F32); t8b = sb("t8b", [128, 8, 8], F32)
        thr = sb("thr", [128, 4, 8], F32); nb = sb("nb", [128, 4, 8], F32); zz = sb("zz", [128, 8], F32)
        sT = sb("sT", [128, 4, 16, 128], BF16)
        tau = sb("tau", [128, 4, 8], F32); taub = sb("taub", [128, 8], BF16)
        Eb = [sb("Eb%d" % i, [128, 512], BF16) for i in range(3)]
        Em8 = [sb("Em80", [128, 8, 512], BF16), shr[:, 0:4096].rearrange("p (h e) -> p h e", e=512)]
        gsb = [sb("gsb%d" % i, [128, 4, 512], BF16) for i in range(2)]
        hact = [sb("hact%d" % i, [128, 512], BF16) for i in range(2)]
        hTt = [sb("hTt%d" % i, [128, 4, 128], BF16) for i in range(2)]
        yacc = sb("yacc", [128, 4, 1024], F32)
        ps = C.ps
        cnt = {"e": 0, "u": 0, "it": 0}

        def peer_iter(it, c, s4, ui, vi):
            ts_ = slice(s4 * 128, (s4 + 1) * 128)
            b = it % 2
            pW = ps[4]; wk = "ps4"
            A = []
            for h in range(8):
                def ah(h=h):
                    ei = cnt["e"] % 3; cnt["e"] += 1
                    pE = ps[(2, 3, 1)[ei]]; ek = "ps%d" % (2, 3, 1)[ei]
                    S.mm(pE[:, 0:512], sT[:, s4, 2 * h, :], R1[:, c, :], True, False, reads=["sT", "R1"], writes=[ek])
                    S.mm(pE[:, 0:512], sT[:, s4, 2 * h + 1, :], R2[:], False, True, reads=["sT", "R2"], writes=[ek])
                    S.act(Eb[ei][:], pE[:, 0:512], AF.Exp, reads=[ek, "nb"], writes=["Eb%d" % ei], bias=nb[:, s4, h:h + 1])
                    S.stt("dve", Em8[b][:, h, :], pE[:, 0:512], thr[:, s4, h:h + 1], Eb[ei][:], ALU.is_ge, ALU.mult,
                          reads=[ek, "thr", "Eb%d" % ei], writes=["Em8%d_%d" % (b, h)])
                A.append(ah)

            def b1a():
                for h in range(8):
                    S.mm(pW[:, 0:512], ident[:], Em8[b][:, h, :], h == 0, h == 7, reads=["Em8%d_%d" % (b, h), "ident"], writes=[wk])

            def b1():
                pass

            def b2():
                pass

            def b3():
                S.tt("dve", hact[b][:], gsb[c % 2][:, s4, :], pW[:, 0:512], ALU.mult, reads=["gsb%d" % (c % 2), wk],
                     writes=["hact%d" % b])

            def b4():
                pT = ps[0][:].bitcast(BF16)[:, 0:512].rearrange("p (j t) -> p j t", t=128)
                for j in range(4):
                    S.tr(pT[:, j, :], hact[b][:, j * 128:(j + 1) * 128], ident[:], reads=["hact%d" % b, "ident"], writes=["ps0"])

            def b5():
                pT = ps[0][:].bitcast(BF16)[:, 0:512].rearrange("p (j t) -> p j t", t=128)
                S.copy("act", hTt[b][:], pT, reads=["ps0"], writes=["hTt%d" % b])

            def b6():
                for half in range(2):
                    for j in range(4):
                        S.mm(ps[6 + half][:, 0:512], hTt[b][:, j, :], vb[vi][:, j, half * 512:(half + 1) * 512], j == 0, j == 3,
                             reads=["hTt%d" % b, "vb%d" % vi], writes=["ps%d" % (6 + half)])

            def b7():
                for half in range(2):
                    ya = yacc[:, s4, half * 512:(half + 1) * 512]
                    S.tt("dve", ya, ya, ps[6 + half][:, 0:512], ALU.add, reads=["yacc", "ps%d" % (6 + half)], writes=["yacc"])
            return A, [b1a, b1, b2, b3, b4, b5, b6, b7]

        def act_part(c, s4, ui):
            for kc in range(8):
                S.mm(ps[5][:, 0:512], xnT[:, kc, s4 * 128:(s4 + 1) * 128], ub[ui][:, kc, :], kc == 0, kc == 7,
                     reads=["ub%d" % ui, "xnT"], writes=["ps5"])
            S.copy("act", gsb[c % 2][:, s4, :], ps[5][:, 0:512], reads=["ps5"], writes=["gsb%d" % (c % 2)])

        def gelu_inplace(c):
            g2 = gsb[c % 2][:].rearrange("p a b -> p (a b)")
            S.act(g2, g2, AF.Gelu, reads=["gsb%d" % (c % 2)], writes=["gsb%d" % (c % 2)])

        prevB = []
        for rd in range(NO // 512):
            S.dma("sp", xnT[:], C.hT2[rd, :, :, :], writes=["xnT"])
            S.memset("pool", yacc[:], 0.0, writes=["yacc"])
            for pc in range(4):
                S.dma("sp", stg_u, WQ[:, pc * 512:(pc + 1) * 512].rearrange("(c p) n -> p c n", p=128), writes=["stg"])
                ui = cnt["u"] % 2; cnt["u"] += 1
                S.copy("act", ub[ui][:], stg_u, reads=["stg"], writes=["ub%d" % ui])
                for cc in range(4):
                    for kc in range(8):
                        S.mm(ps[0][:, 0:512], ub[ui][:, kc, cc * 128:(cc + 1) * 128], xnT[:, kc, :],
                             kc == 0, kc == 7, reads=["ub%d" % ui, "xnT"], writes=["ps0"])
                    S.copy("act", qTr[:, pc * 4 + cc, :], ps[0][:, 0:512], reads=["ps0"], writes=["Em81_%d" % hh_ for hh_ in range(8)])
            for s4 in range(4):
                ts_ = slice(s4 * 128, (s4 + 1) * 128)
                for g4 in range(4):
                    for cc in range(4):
                        ch = g4 * 4 + cc
                        S.mm(ps[1][:, cc * 128:(cc + 1) * 128], qTr[:, ch, ts_], kT[:, ch, :], True, True,
                             reads=["Em81_%d" % hh_ for hh_ in range(8)] + ["kT"], writes=["ps1"])
                    S.copy("act", sb16[:, g4 * 4:(g4 + 1) * 4, :], ps[1][:, 0:512].rearrange("p (c k) -> p c k", k=128),
                           reads=["ps1"], writes=["sb16"])
                S.copy("dve", sf[:], sb16[:], reads=["sb16"], writes=["sf"])
                for ch in range(16):
                    S.op("dve", lambda e, ch=ch: e.max(out=v16[:, ch, 0:8], in_=sf[:, ch, :]), reads=["sf"], writes=["v16"])
                    S.op("dve", lambda e, ch=ch: e.match_replace(out=swk[:], in_to_replace=v16[:, ch, 0:8], in_values=sf[:, ch, :],
                                                                 imm_value=-1e30), reads=["sf", "v16"], writes=["swk"])
                    S.op("dve", lambda e, ch=ch: e.max(out=v16[:, ch, 8:16], in_=swk[:]), reads=["swk"], writes=["v16"])
                v4 = v16[:].rearrange("p (h two) k -> p h two k", two=2)
                c4 = cand[:].rearrange("p h (a b) -> p h a b", b=16)
                S.tt("dve", c4, v4[:, :, 0, :].unsqueeze(3).to_broadcast([128, 8, 16, 16]),
                     v4[:, :, 1, :].unsqueeze(2).to_broadcast([128, 8, 16, 16]), ALU.add, reads=["v16"], writes=["cand"])
                for h in range(8):
                    S.op("dve", lambda e, h=h: e.max(out=t8[:, h, :], in_=cand[:, h, :]), reads=["cand"], writes=["t8"])
                    S.op("dve", lambda e, h=h: e.match_replace(out=cwk[:, h, :], in_to_replace=t8[:, h, :], in_values=cand[:, h, :],
                                                               imm_value=-1e30), reads=["cand", "t8"], writes=["stg2"])
                    S.op("dve", lambda e, h=h: e.max(out=t8b[:, h, :], in_=cwk[:, h, :]), reads=["stg2"], writes=["t8b"])
                S.copy("dve", thr[:, s4, :], t8b[:, :, 7], reads=["t8b"], writes=["thr"])
                S.tt("dve", cwk, cand[:], t8[:, :, 0:1].to_broadcast([128, 8, 256]), ALU.subtract, reads=["cand", "t8"], writes=["stg2"])
                S.act(cwk, cwk, AF.Exp, reads=["stg2"], writes=["stg2"])
                S.tt("dve", cand[:], cand[:], thr[:, s4, :].unsqueeze(2).to_broadcast([128, 8, 256]), ALU.is_ge,
                     reads=["cand", "thr"], writes=["cand"])
                S.tt("dve", cwk, cwk, cand[:], ALU.mult, reads=["stg2", "cand"], writes=["stg2"])
                S.op("dve", lambda e: e.tensor_reduce(out=zz[:], in_=cwk, axis=AX.X, op=ALU.add), reads=["stg2"], writes=["zz"])
                S.act(zz[:], zz[:], AF.Ln, reads=["zz"], writes=["zz"])
                S.tt("dve", zz[:], zz[:], t8[:, :, 0], ALU.add, reads=["zz", "t8"], writes=["zz"])
                S.ts("dve", nb[:, s4, :], zz[:], -1.0, ALU.mult, reads=["zz"], writes=["nb"])
                for half in range(2):
                    pT = ps[1][:].bitcast(BF16)[:, 0:1024].rearrange("p (c t) -> p c t", t=128)
                    for cc in range(8):
                        S.tr(pT[:, cc, :], sb16[:, half * 8 + cc, :], ident[:], reads=["sb16", "ident"], writes=["ps1"])
                    S.copy("act", sT[:, s4, half * 8:(half + 1) * 8, :], pT, reads=["ps1"], writes=["sT"])
            def load_u(c):
                S.dma("sp", stg_u, UT[:, c * 512:(c + 1) * 512].rearrange("(kc p) n -> p kc n", p=128), writes=["stg"])
                ui_ = cnt["u"] % 2; cnt["u"] += 1
                S.copy("pool", ub[ui_][:], stg_u, reads=["stg"], writes=["ub%d" % ui_])
                return ui_

            def load_v(c):
                S.dma("sp", stg2_v, VT[c * 512:(c + 1) * 512, :].rearrange("(j p) d -> p j d", p=128), writes=["stg2"])
                S.copy("pool", vb[c % 2][:], stg2_v, reads=["stg2"], writes=["vb%d" % (c % 2)])
            events = []
            uis = {}

            def ev_load_u(c):
                uis[c] = load_u(c)
            ev_load_u(0)
            for s4_ in range(4):
                act_part(0, s4_, uis[0])
            gelu_inplace(0)
            for c in range(32):
                if c + 1 < 32:
                    events.append((c * 4 + 0 - 0.5, 0, lambda c=c: ev_load_u(c + 1)))
                    for s4_ in range(4):
                        events.append((c * 4 + s4_ + 0.65, 1, lambda c=c, s4_=s4_: act_part(c + 1, s4_, uis[c + 1])))
                    events.append((c * 4 + 3 + 0.75, 1, lambda c=c: gelu_inplace(c + 1)))
                events.append((c * 4 + 0 - 0.45, 2, lambda c=c: load_v(c)))
                for s4 in range(4):
                    it = c * 4 + s4
                    a_steps, b_steps = peer_iter(cnt["it"], c, s4, None, c % 2)
                    cnt["it"] += 1
                    for k in range(8):
                        events.append((it + k / 10.0, 3, a_steps[k]))
                    b0, _, _, b3, b4, b5, b6, b7 = b_steps
                    events.append((it + 1 + 0.45, 4, b0))
                    events.append((it + 1 + 0.52, 5, b3))
                    events.append((it + 2 + 0.05, 6, b4))
                    events.append((it + 2 + 0.15, 7, b5))
                    events.append((it + 2 + 0.25, 8, b6))
                    events.append((it + 2 + 0.32, 9, b7))
            events.sort(key=lambda e: (e[0], e[1]))
            for _, _, f in events:
                f()
            for s4 in range(4):
                tok = rd * 512 + s4 * 128
                S.dma("sp", stg[:, 0:1024], C.x2[tok:tok + 128, :], writes=["stg"])
                S.tt("dve", yacc[:, s4, :], yacc[:, s4, :], stg[:, 0:1024], ALU.add, reads=["yacc", "stg"], writes=["yacc"])
                S.dma("pool", C.out[tok:tok + 128, :], yacc[:, s4, :], reads=["yacc"])
        S.flush()


def phase_outproj(C, S):
    nc = C.nc
    NV, NO = C.NV, C.NO
    with ExitStack() as st:
        sb = lambda name, shape, dt: st.enter_context(nc.sbuf_tensor("op_" + name, shape, dt))
        ident = sb("ident", [128, 128], BF16)
        S.dma("sp", ident[:], C.inp["ident_bf"][:, :], writes=["ident"])
        stg = sb("stg", [128, 4, 1024], F32)
        Wa = sb("Wa", [128, 8, 1024], BF16); Ws = sb("Ws", [128, 16, 1024], BF16); Wo = sb("Wo", [128, 8, 1024], BF16)
        for nm, tl, nch in (("w_attn_o", Wa, 8), ("w_ssd_o", Ws, 16), ("w_out", Wo, 8)):
            for c0 in range(0, nch, 4):
                S.dma("sp", stg[:], C.inp[nm][c0 * 128:(c0 + 4) * 128, :].rearrange("(c p) n -> p c n", p=128), writes=["stg"])
                S.copy("act", tl[:, c0:c0 + 4, :], stg[:], reads=["stg"], writes=[nm])
        aTt = [sb("aTt%d" % i, [128, 8, 128], BF16) for i in range(2)]
        yTt = [sb("yTt%d" % i, [128, 16, 128], BF16) for i in range(2)]
        ga = [sb("ga%d" % i, [128, 1024], BF16) for i in range(2)]
        gs = [sb("gs%d" % i, [128, 1024], BF16) for i in range(2)]
        xt = [sb("xt%d" % i, [128, 1024], F32) for i in range(2)]
        m1 = sb("m1", [128, 1024], F32); m2 = sb("m2", [128, 1024], F32); mb = sb("mb", [128, 1024], BF16)
        mT = sb("mT", [128, 8, 128], BF16)
        xo = [sb("xo%d" % i, [128, 1024], F32) for i in range(2)]
        ps = C.ps
        for t in range(NO // 128):
            i = t % 2
            tok = t * 128
            S.dma("sp", aTt[i][:], C.aT[:, tok:tok + 128].rearrange("(c p) t -> p c t", p=128), writes=["aTt%d" % i])
            S.dma("sp", yTt[i][:], C.ynT[:, tok:tok + 128].rearrange("(c p) t -> p c t", p=128), writes=["yTt%d" % i])
            S.dma("sp", ga[i][:], C.sga[tok:tok + 128, :], writes=["ga%d" % i])
            S.dma("sp", gs[i][:], C.sgs[tok:tok + 128, :], writes=["gs%d" % i])
            S.dma("sp", xt[i][:], C.inp["xv"][NO + tok:NO + tok + 128, :], writes=["xt%d" % i])
            for half in range(2):
                hs = slice(half * 512, (half + 1) * 512)
                for c in range(8):
                    S.mm(ps[half][:, 0:512], aTt[i][:, c, :], Wa[:, c, hs], c == 0, c == 7,
                         reads=["aTt%d" % i, "w_attn_o"], writes=["ps%d" % half])
                for c in range(16):
                    S.mm(ps[2 + half][:, 0:512], yTt[i][:, c, :], Ws[:, c, hs], c == 0, c == 15,
                         reads=["yTt%d" % i, "w_ssd_o"], writes=["ps%d" % (2 + half)])
                S.tt("dve", m1[:, hs], ps[half][:, 0:512], ga[i][:, hs], ALU.mult, reads=["ps%d" % half, "ga%d" % i], writes=["m1"])
                S.tt("dve", m2[:, hs], ps[2 + half][:, 0:512], gs[i][:, hs], ALU.mult, reads=["ps%d" % (2 + half), "gs%d" % i], writes=["m2"])
            S.tt("dve", mb[:], m1[:], m2[:], ALU.add, reads=["m1", "m2"], writes=["mb"])
            pT = ps[4][:].bitcast(BF16)[:, 0:1024].rearrange("p (c t) -> p c t", t=128)
            for c in range(8):
                S.tr(pT[:, c, :], mb[:, c * 128:(c + 1) * 128], ident[:], reads=["mb", "ident"], writes=["ps4"])
            S.copy("act", mT[:], pT, reads=["ps4"], writes=["mT"])
            for half in range(2):
                hs = slice(half * 512, (half + 1) * 512)
                for c in range(8):
                    S.mm(ps[5 + half][:, 0:512], mT[:, c, :], Wo[:, c, hs], c == 0, c == 7,
                         reads=["mT", "w_out"], writes=["ps%d" % (5 + half)])
                S.tt("dve", xo[i][:, hs], ps[5 + half][:, 0:512], xt[i][:, hs], ALU.add,
                     reads=["ps%d" % (5 + half), "xt%d" % i], writes=["xo%d" % i])
            S.dma("pool", C.x2[tok:tok + 128, :], xo[i][:], reads=["xo%d" % i])
        S.flush()


def build_all(nc, NV, st, debug=()):
    C = setup(nc, NV, debug)
    moba_setup(C); ssd_setup(C); peer_setup(C)
    S = Sched(nc, st)
    C.ps = [st.enter_context(nc.psum_tensor("ps%d" % i, [128, 512], F32)) for i in range(8)]
    phase_norm(C, S, C.inp["xv"], "norm1_g", C.hT, NV, "n1")
    phase_inproj(C, S)
    phase_moba(C, S)
    phase_ssd(C, S)
    phase_outproj(C, S)
    phase_norm(C, S, C.x2, "norm2_g", C.hT2, C.NO, "n2")
    phase_peer(C, S)
    return C, S


def make_inputs(inputs, NV, b, r, full_seq):
    bf = ml_dtypes.bfloat16
    NO = NV // 2
    f32 = lambda a: np.ascontiguousarray(np.asarray(a, dtype=np.float32))
    x = np.asarray(inputs["x"])
    ins = {}
    xv = np.zeros((NV, D), np.float32)
    if r == 0:
        xv[NO:] = x[b, 0:NO]
    else:
        xv[:] = x[b, 0:NV]
    ins["xv"] = xv
    ins["pv"] = np.full((1, 1), float(r), np.float32)
    ins["norm1_g"] = f32(inputs["norm1_g"][0:1]); ins["w_in"] = f32(inputs["w_in"][0])
    ins["q_norm_g"] = f32(inputs["q_norm_g"][0:1]); ins["k_norm_g"] = f32(inputs["k_norm_g"][0:1])
    ins["conv_wT"] = f32(np.asarray(inputs["conv_w"][0]).T); ins["conv_b"] = f32(np.asarray(inputs["conv_b"][0]).reshape(4096, 1))
    ins["dt_bias"] = f32(inputs["dt_bias"][0:1]); ins["a_log"] = f32(inputs["a_log"][0:1]); ins["d_skip"] = f32(inputs["d_skip"][0:1])
    ins["ssd_norm_g"] = f32(inputs["ssd_norm_g"][0:1]); ins["w_attn_o"] = f32(inputs["w_attn_o"][0])
    ins["w_ssd_o"] = f32(inputs["w_ssd_o"][0]); ins["w_out"] = f32(inputs["w_out"][0]); ins["norm2_g"] = f32(inputs["norm2_g"][0:1])
    ins["w_peer_q"] = f32(inputs["w_peer_q"][0])
    ins["keys1T"] = f32(np.asarray(inputs["peer_keys1"][0]).transpose(0, 2, 1))
    ins["keys2T"] = f32(np.asarray(inputs["peer_keys2"][0]).transpose(0, 2, 1))
    ins["peer_uT"] = f32(np.asarray(inputs["peer_u"][0]).T); ins["peer_v"] = f32(inputs["peer_v"][0])
    ins["ident_bf"] = np.eye(128).astype(bf); ins["ident_f"] = np.eye(128, dtype=np.float32)
    ins.update(moba_consts(NV, r)); ins.update(ssd_consts()); ins.update(peer_consts())
    return ins


NV_FULL = 8192


def kernel(**inputs):
    from concourse.bass_utils import run_bass_kernel_spmd
    nc = bass.Bass("TRN2", target_bir_lowering=False)
    with ExitStack() as st:
        C, S = build_all(nc, NV_FULL, st)
    x = np.asarray(inputs["x"])
    B = x.shape[0]
    in_maps = []
    for b in range(B):
        for r in range(2):
            in_maps.append(make_inputs(inputs, NV_FULL, b, r, NV_FULL))
    res = run_bass_kernel_spmd(nc, in_maps, core_ids=list(range(len(in_maps)))).results
    NO = NV_FULL // 2
    out = np.empty((B, NV_FULL, D), np.float32)
    for b in range(B):
        for r in range(2):
            out[b, r * NO:(r + 1) * NO] = np.asarray(res[b * 2 + r]["out"], dtype=np.float32)
    return out
```

```python
from contextlib import ExitStack
import ml_dtypes
import numpy as np
import concourse.bass as bass
import concourse.mybir as mybir

F32 = mybir.dt.float32
BF16 = mybir.dt.bfloat16
AF = mybir.ActivationFunctionType
ALU = mybir.AluOpType
AX = mybir.AxisListType

SAME_ENGINE_SYNC = True
N_DMA_SEMS = 32


class Sched:
    ENG = ("sp", "act", "dve", "pool", "pe")

    def __init__(self, nc, stack):
        self.nc = nc
        self.ops = []
        self.esem = {e: stack.enter_context(nc.semaphore("s_" + e)) for e in self.ENG}
        self.ecnt = {e: 0 for e in self.ENG}
        self.dsem = [stack.enter_context(nc.semaphore("d%d" % i)) for i in range(N_DMA_SEMS)]
        self.dcnt = [0] * N_DMA_SEMS
        self.downer = [None] * N_DMA_SEMS
        self.dnext = 0
        self.last_w = {}
        self.readers = {}
        self.waited = {e: {} for e in self.ENG}
        self.nblocks = 0
        self.nops = 0

    def _need(self, eng, ev, waits):
        if ev is None:
            return
        sem, val, src_eng, is_dma = ev
        if (not is_dma) and src_eng == eng and (eng == "pe" or not SAME_ENGINE_SYNC):
            return
        key = id(sem)
        if self.waited[eng].get(key, 0) >= val:
            return
        cur = waits.get(key)
        if cur is None or cur[1] < val:
            waits[key] = (sem, val)

    def op(self, eng, fn, reads=(), writes=(), dma=False):
        writes = list(writes) + [k for k in reads if isinstance(k, str) and k.startswith("ps") and k not in writes]
        waits = {}
        for k in reads:
            self._need(eng, self.last_w.get(k), waits)
        for k in writes:
            self._need(eng, self.last_w.get(k), waits)
            for ev in self.readers.get(k, ()):
                self._need(eng, ev, waits)
        if dma:
            half = N_DMA_SEMS // 2
            base = 0 if eng == "sp" else half
            self.dnx = getattr(self, "dnx", {})
            i = base + self.dnx.get(eng, 0)
            self.dnx[eng] = (self.dnx.get(eng, 0) + 1) % half
            if self.dcnt[i] > 0:
                self._need(eng, (self.dsem[i], 16 * self.dcnt[i], self.downer[i], True), waits)
            self.dcnt[i] += 1
            self.downer[i] = eng
            ev = (self.dsem[i], 16 * self.dcnt[i], eng, True)
            inc = (self.dsem[i], 16)
        else:
            self.ecnt[eng] += 1
            ev = (self.esem[eng], self.ecnt[eng], eng, False)
            inc = (self.esem[eng], 1)
        for (sem, val) in waits.values():
            self.waited[eng][id(sem)] = val
        for k in reads:
            self.readers.setdefault(k, []).append(ev)
        for k in writes:
            self.last_w[k] = ev
            self.readers[k] = []
        self.ops.append((eng, fn, list(waits.values()), inc))
        self.nops += 1

    def flush(self):
        fin = {}
        for i in range(N_DMA_SEMS):
            if self.dcnt[i] > 0:
                e = self.downer[i]
                if self.waited[e].get(id(self.dsem[i]), 0) < 16 * self.dcnt[i]:
                    fin.setdefault(e, []).append((self.dsem[i], 16 * self.dcnt[i]))
                    self.waited[e][id(self.dsem[i])] = 16 * self.dcnt[i]
        ops = self.ops
        self.ops = []
        if not ops and not fin:
            return
        nc = self.nc
        with nc.Block() as block:
            deco = {"sp": block.sync, "act": block.scalar, "dve": block.vector,
                    "pool": block.gpsimd, "pe": block.tensor}
            for e in self.ENG:
                mine = [o for o in ops if o[0] == e]
                tail = fin.get(e, [])
                if not mine and not tail:
                    continue

                def body(engine, mine=mine, tail=tail):
                    for (_, fn, waits, inc) in mine:
                        for (sem, val) in waits:
                            engine.wait_ge(sem, val)
                        ins = fn(engine)
                        ins.then_inc(inc[0], inc[1])
                    for (sem, val) in tail:
                        engine.wait_ge(sem, val)

                deco[e](body)
        self.nblocks += 1
        self.last_w = {}
        self.readers = {}

    def dma(self, eng, out, in_, reads=(), writes=(), **kw):
        self.op(eng, lambda e: e.dma_start(out=out, in_=in_, **kw), reads, writes, dma=True)

    def mm(self, out, lhsT, rhs, start, stop, reads=(), writes=()):
        self.op("pe", lambda e: e.matmul(out, lhsT, rhs, start=start, stop=stop), reads, writes)

    def tr(self, out, in_, ident, reads=(), writes=()):
        self.op("pe", lambda e: e.transpose(out, in_, ident), reads, writes)

    def act(self, out, in_, func, reads=(), writes=(), **kw):
        self.op("act", lambda e: e.activation(out=out, in_=in_, func=func, **kw), reads, writes)

    def tt(self, eng, out, in0, in1, op, reads=(), writes=()):
        self.op(eng, lambda e: e.tensor_tensor(out=out, in0=in0, in1=in1, op=op), reads, writes)

    def ts(self, eng, out, in0, s1, op0, s2=None, op1=None, reads=(), writes=(), **kw):
        if op1 is None:
            if op0 == ALU.pow:
                self.op(eng, lambda e: e.tensor_scalar(out=out, in0=in0, scalar1=0.0, scalar2=s1, op0=ALU.add, op1=ALU.pow, **kw),
                        reads, writes)
            else:
                self.op(eng, lambda e: e.tensor_scalar(out=out, in0=in0, scalar1=s1, scalar2=None, op0=op0, **kw),
                        reads, writes)
        else:
            self.op(eng, lambda e: e.tensor_scalar(out=out, in0=in0, scalar1=s1, scalar2=s2, op0=op0, op1=op1, **kw),
                    reads, writes)

    def stt(self, eng, out, in0, scalar, in1, op0, op1, reads=(), writes=()):
        self.op(eng, lambda e: e.scalar_tensor_tensor(out=out, in0=in0, scalar=scalar, in1=in1, op0=op0, op1=op1),
                reads, writes)

    def copy(self, eng, out, in_, reads=(), writes=()):
        if eng == "act":
            self.op(eng, lambda e: e.activation(out=out, in_=in_, func=AF.Copy), reads, writes)
        else:
            self.op(eng, lambda e: e.tensor_copy(out=out, in_=in_), reads, writes)

    def memset(self, eng, ap, val, writes=()):
        self.op(eng, lambda e: e.memset(ap, val), (), writes)


D = 1024
NH = 16
HD = 64
SH = 32
SP = 64
SG = 8
SN = 128
EPS = 1e-6
NEG = -30000.0
import os
MOBA_DUMMY = int(os.environ.get('MOBA_DUMMY', '0'))
PEER_DUMMY = int(os.environ.get('PEER_DUMMY', '0')) if 'PEER_DUMMY' in os.environ else 0
C_Q, C_K, C_V, C_Z, C_X, C_B, C_C, C_DT, C_GA, C_GS = 0, 1024, 2048, 3072, 5120, 7168, 8192, 9216, 9248, 10272
IN_COLS = 11296


class Ctx:
    pass


def dram(C, name, shape, dt):
    kind = "ExternalOutput" if name in C.debug else "Internal"
    return C.nc.dram_tensor(name, list(shape), dt, kind=kind).ap()


def setup(nc, NV, debug=()):
    C = Ctx()
    C.nc = nc
    C.NV = NV
    C.NO = NV // 2
    C.debug = set(debug)
    C.inp = {}

    def inp(name, shape, dt=F32):
        C.inp[name] = nc.dram_tensor(name, list(shape), dt, kind="ExternalInput").ap()
        return C.inp[name]
    NV_, NO = NV, C.NO
    NB = NV // 256
    C.NB = NB
    inp("xv", [NV, D])
    inp("norm1_g", [1, D]); inp("w_in", [D, IN_COLS]); inp("q_norm_g", [1, HD]); inp("k_norm_g", [1, HD])
    inp("conv_wT", [4096, 4]); inp("conv_b", [4096, 1]); inp("dt_bias", [1, SH]); inp("a_log", [1, SH])
    inp("d_skip", [1, SH]); inp("ssd_norm_g", [1, 2048]); inp("w_attn_o", [D, D]); inp("w_ssd_o", [2048, D])
    inp("w_out", [D, D]); inp("norm2_g", [1, D]); inp("w_peer_q", [D, 2048])
    inp("keys1T", [8, 128, 128]); inp("keys2T", [8, 128, 128])
    inp("peer_uT", [D, 16384]); inp("peer_v", [16384, D])
    inp("ident_bf", [128, 128], BF16); inp("ident_f", [128, 128], F32)
    inp("pv", [1, 1])
    C.hT = dram(C, "hT_s", [NV // 512, 128, 8, 512], BF16)
    C.qT = dram(C, "qT_s", [D, NO], BF16)
    C.kT = dram(C, "kT_s", [D, NV], BF16)
    C.kmT = dram(C, "kmT_s", [8, 128, NB], F32)
    C.v = dram(C, "v_s", [NV, D], BF16)
    C.z = dram(C, "z_s", [NO, 2048], BF16)
    C.xsT = dram(C, "xsT_s", [2048, NV], BF16)
    C.BT = dram(C, "BT_s", [1024, NV], BF16)
    C.CT = dram(C, "CT_s", [1024, NO], BF16)
    C.da = dram(C, "da_s", [NV, 64], F32)
    C.sga = dram(C, "sga_s", [NO, D], BF16)
    C.sgs = dram(C, "sgs_s", [NO, D], BF16)
    return C


def phase_norm(C, S, xin, gname, hT, ntok, tag):
    nc = C.nc
    with ExitStack() as st:
        sb = lambda name, shape, dt: st.enter_context(nc.sbuf_tensor(tag + name, shape, dt))
        ident = sb("ident", [128, 128], BF16)
        gT = sb("gT", [128, 8], F32)
        S.dma("sp", ident[:], C.inp["ident_bf"][:, :], writes=["ident"])
        S.dma("sp", gT[:], C.inp[gname][0, :].rearrange("(c p) -> p c", p=128), writes=["gT"],
              allow_slow_non_contiguous=True)
        xt = [sb("xt%d" % i, [128, D], F32) for i in range(2)]
        junk = sb("junk", [128, D], F32)
        ss = [sb("ss%d" % i, [128, 1], F32) for i in range(2)]
        xb = [sb("xb%d" % i, [128, D], BF16) for i in range(2)]
        ho = [sb("ho%d" % i, [128, 8, 128], BF16) for i in range(2)]
        for t in range(ntok // 128):
            i = t % 2
            pT = C.ps[t % 2][:].bitcast(BF16)[:, 0:1024].rearrange("p (c t) -> p c t", t=128)
            pk = "ps%d" % (t % 2)
            S.dma("sp", xt[i][:], xin[t * 128:(t + 1) * 128, :], writes=["xt%d" % i])
            S.act(junk[:], xt[i][:], AF.Square, reads=["xt%d" % i], writes=["junk", "ss%d" % i], accum_out=ss[i][:])
            S.act(ss[i][:], ss[i][:], AF.Ln, reads=["ss%d" % i], writes=["ss%d" % i], scale=1.0 / D, bias=EPS)
            S.act(ss[i][:], ss[i][:], AF.Exp, reads=["ss%d" % i], writes=["ss%d" % i], scale=-0.5)
            S.act(xb[i][:], xt[i][:], AF.Copy, reads=["xt%d" % i, "ss%d" % i], writes=["xb%d" % i], scale=ss[i][:])
            for c in range(8):
                S.tr(pT[:, c, :], xb[i][:, c * 128:(c + 1) * 128], ident[:], reads=["xb%d" % i, "ident"], writes=[pk])
            S.tt("dve", ho[i][:], pT, gT[:].unsqueeze(2).to_broadcast([128, 8, 128]), ALU.mult,
                 reads=[pk, "gT"], writes=["ho%d" % i])
            S.dma("pool", hT[t // 4, :, :, (t % 4) * 128:(t % 4 + 1) * 128], ho[i][:], reads=["ho%d" % i])
        S.flush()


def phase_inproj(C, S):
    nc = C.nc
    NV, NO = C.NV, C.NO
    NT = NV // 512
    NTO = NO // 512
    W = C.inp["w_in"]
    with ExitStack() as st:
        sb = lambda name, shape, dt: st.enter_context(nc.sbuf_tensor("ip_" + name, shape, dt))
        ident = sb("ident", [128, 128], BF16)
        S.dma("sp", ident[:], C.inp["ident_bf"][:, :], writes=["ident"])
        gq = sb("gq", [128, HD], F32); gk = sb("gk", [128, HD], F32)
        S.dma("sp", gq[:], C.inp["q_norm_g"][0:1, :].partition_broadcast(128), writes=["gq"])
        S.dma("sp", gk[:], C.inp["k_norm_g"][0:1, :].partition_broadcast(128), writes=["gk"])
        S.ts("dve", gq[:], gq[:], HD ** -0.5, ALU.mult, reads=["gq"], writes=["gq"])
        dtb = sb("dtb", [128, SH], F32); An = sb("An", [128, SH], F32)
        S.dma("sp", dtb[:], C.inp["dt_bias"][0:1, :].partition_broadcast(128), writes=["dtb"])
        S.dma("sp", An[:], C.inp["a_log"][0:1, :].partition_broadcast(128), writes=["An"])
        S.act(An[:], An[:], AF.Exp, reads=["An"], writes=["An"])
        S.ts("dve", An[:], An[:], -1.0, ALU.mult, reads=["An"], writes=["An"])
        cw = sb("cw", [128, 32, 4], F32); cb = sb("cb", [128, 32], F32)
        S.dma("sp", cw[:], C.inp["conv_wT"].rearrange("(c p) k -> p c k", p=128), writes=["cw"])
        S.dma("sp", cb[:], C.inp["conv_b"].rearrange("(c p) o -> p (c o)", p=128), writes=["cb"],
              allow_slow_non_contiguous=True)
        kmT = sb("kmT", [128, 8, C.NB], F32)
        S.memset("pool", kmT[:], 0.0, writes=["kmT"])
        wf = [sb("wf%d" % i, [128, 8, 512], F32) for i in range(2)]
        wb = [sb("wb%d" % i, [128, 8, 512], BF16) for i in range(2)]
        hb = [sb("hb%d" % i, [128, 8, 512], BF16) for i in range(3)]
        ev = [sb("ev%d" % i, [128, 512], F32) for i in range(2)]
        sq = sb("sq", [128, 512], F32)
        ssq = sb("ssq", [128, 8], F32)
        ob = [sb("ob%d" % i, [128, 512], BF16) for i in range(2)]
        tb = [sb("tb%d" % i, [128, 4, 128], BF16) for i in range(2)]
        kr = sb("kr", [128, 4], F32)
        cbuf = [sb("cbuf%d" % i, [128, 515], F32) for i in range(4)]
        caccs = [sb("cacc%d" % i, [128, 512], F32) for i in range(2)]
        dab = [sb("dab%d" % i, [128, 64], F32) for i in range(2)]

        blocks = []
        for j in range(2): blocks.append(("q", C_Q + 512 * j, 512, NT - NTO, j))
        for j in range(2): blocks.append(("k", C_K + 512 * j, 512, 0, j))
        for j in range(2): blocks.append(("v", C_V + 512 * j, 512, 0, j))
        for j in range(4): blocks.append(("z", C_Z + 512 * j, 512, NT - NTO, j))
        for j in range(4): blocks.append(("xs", C_X + 512 * j, 512, 0, j))
        for j in range(2): blocks.append(("B", C_B + 512 * j, 512, 0, j))
        for j in range(2): blocks.append(("C", C_C + 512 * j, 512, NT - NTO - 1, j))
        blocks.append(("dt", C_DT, 32, 0, 0))
        for j in range(2): blocks.append(("ga", C_GA + 512 * j, 512, NT - NTO, j))
        for j in range(2): blocks.append(("gs", C_GS + 512 * j, 512, NT - NTO, j))

        cnt = {"h": 0, "ps": 0, "ev": 0, "ob": 0, "tb": 0, "da": 0, "ca": 0}
        def load_w(bi):
            kind_, c0_, ncol_, _, _ = blocks[bi]
            wi_ = bi % 2
            S.dma("sp", wf[wi_][:, :, 0:ncol_], W[:, c0_:c0_ + ncol_].rearrange("(c p) n -> p c n", p=128),
                  writes=["wf%d" % wi_])
            S.copy("act", wb[wi_][:, :, 0:ncol_], wf[wi_][:, :, 0:ncol_], reads=["wf%d" % wi_], writes=["wb%d" % wi_])
        load_w(0)
        deferred = []
        cm_def = []
        for bi, (kind, c0, ncol, t0, j) in enumerate(blocks):
            wi = bi % 2
            while cm_def:
                cm_def.pop(0)()
            if bi + 1 < len(blocks):
                load_w(bi + 1)
            if kind in ("xs", "B", "C"):
                for s4 in range(4):
                    S.memset("pool", cbuf[s4][:, 0:3], 0.0, writes=["cbuf%d" % s4])
            for t in range(t0, NT):
                hi = cnt["h"] % 3; cnt["h"] += 1
                S.dma("sp", hb[hi][:], C.hT[t, :, :, :], writes=["hb%d" % hi])
                to = t - (NT - NTO)
                for s4 in range(4):
                    pi = cnt["ps"] % 4; cnt["ps"] += 1
                    ps = C.ps[pi]; pk = "ps%d" % pi
                    tok = t * 512 + s4 * 128
                    if kind in ("xs", "B", "C"):
                        for c in range(8):
                            S.mm(ps[:, 0:512], wb[wi][:, c, s4 * 128:(s4 + 1) * 128], hb[hi][:, c, :], c == 0, c == 7,
                                 reads=["wb%d" % wi, "hb%d" % hi], writes=[pk])
                        ck = "cbuf%d" % s4
                        cai = cnt["ca"] % 2; cnt["ca"] += 1
                        cacc = caccs[cai]; cak = "cacc%d" % cai
                        S.copy("act", cbuf[s4][:, 3:515], ps[:, 0:512], reads=[pk], writes=[ck])
                        while cm_def:
                            cm_def.pop(0)()
                        chn = (c0 - C_X) // 128 + s4
                        S.ts("dve", cacc[:], cbuf[s4][:, 0:512], cw[:, chn, 0:1], ALU.mult, cb[:, chn:chn + 1], ALU.add,
                             reads=[ck, "cw", "cb"], writes=[cak])
                        for k in range(1, 4):
                            S.stt("dve", cacc[:], cbuf[s4][:, k:k + 512], cw[:, chn, k:k + 1], cacc[:], ALU.mult, ALU.add,
                                  reads=[ck, cak], writes=[cak])
                        S.copy("pool", cbuf[s4][:, 0:3], cbuf[s4][:, 512:515], reads=[ck], writes=[ck])
                        def cm_fin(kind=kind, cacc=cacc, cak=cak, c0=c0, s4=s4, t=t, to=to):
                            oi = cnt["ob"] % 2; cnt["ob"] += 1
                            S.act(ob[oi][:], cacc[:], AF.Silu, reads=[cak], writes=["ob%d" % oi])
                            r0 = (c0 - {"xs": C_X, "B": C_B, "C": C_C}[kind]) + s4 * 128
                            if kind == "xs":
                                S.dma("pool", C.xsT[r0:r0 + 128, t * 512:(t + 1) * 512], ob[oi][:], reads=["ob%d" % oi])
                            elif kind == "B":
                                S.dma("pool", C.BT[r0:r0 + 128, t * 512:(t + 1) * 512], ob[oi][:], reads=["ob%d" % oi])
                            elif to >= 0:
                                S.dma("pool", C.CT[r0:r0 + 128, to * 512:(to + 1) * 512], ob[oi][:], reads=["ob%d" % oi])
                        cm_def.append(cm_fin)
                        continue
                    for c in range(8):
                        S.mm(ps[:, 0:ncol], hb[hi][:, c, s4 * 128:(s4 + 1) * 128], wb[wi][:, c, 0:ncol], c == 0, c == 7,
                             reads=["wb%d" % wi, "hb%d" % hi], writes=[pk])
                    while deferred:
                        deferred.pop(0)()
                    if kind in ("q", "k"):
                        ei = cnt["ev"] % 2; cnt["ev"] += 1
                        ek = "ev%d" % ei
                        S.copy("act", ev[ei][:], ps[:, 0:512], reads=[pk], writes=[ek])
                        S.tt("dve", sq[:], ev[ei][:], ev[ei][:], ALU.mult, reads=[ek], writes=["sq"])
                        S.op("dve", lambda e: e.tensor_reduce(out=ssq[:], in_=sq[:].rearrange("p (a b) -> p a b", b=HD),
                                                             axis=AX.X, op=ALU.add), reads=["sq"], writes=["ssq"])
                        S.act(ssq[:], ssq[:], AF.Ln, reads=["ssq"], writes=["ssq"], scale=1.0 / HD, bias=EPS)
                        S.act(ssq[:], ssq[:], AF.Exp, reads=["ssq"], writes=["ssq"], scale=-0.5)
                        e3 = ev[ei][:].rearrange("p (a b) -> p a b", b=HD)
                        S.tt("dve", e3, e3, ssq[:].unsqueeze(2).to_broadcast([128, 8, HD]), ALU.mult,
                             reads=[ek, "ssq"], writes=[ek])
                        oi = cnt["ob"] % 2; cnt["ob"] += 1
                        g_ = gq if kind == "q" else gk
                        S.tt("dve", ob[oi][:].rearrange("p (a b) -> p a b", b=HD), e3,
                             g_[:].unsqueeze(1).to_broadcast([128, 8, HD]), ALU.mult,
                             reads=[ek, "gq", "gk"], writes=["ob%d" % oi])
                        def fin(kind=kind, oi=oi, j=j, to=to, s4=s4, tok=tok):
                            pti = 4 + cnt["tb"] % 2
                            ti = cnt["tb"] % 2; cnt["tb"] += 1
                            pT = C.ps[pti][:].bitcast(BF16)[:, 0:512].rearrange("p (c t) -> p c t", t=128)
                            for c in range(4):
                                S.tr(pT[:, c, :], ob[oi][:, c * 128:(c + 1) * 128], ident[:], reads=["ob%d" % oi, "ident"],
                                     writes=["ps%d" % pti])
                            S.copy("act", tb[ti][:], pT, reads=["ps%d" % pti], writes=["tb%d" % ti])
                            rows = slice(j * 512, (j + 1) * 512)
                            if kind == "q":
                                S.dma("pool", C.qT[rows, to * 512 + s4 * 128: to * 512 + (s4 + 1) * 128].rearrange("(c p) t -> p c t", p=128),
                                      tb[ti][:], reads=["tb%d" % ti])
                            else:
                                S.dma("pool", C.kT[rows, tok:tok + 128].rearrange("(c p) t -> p c t", p=128),
                                      tb[ti][:], reads=["tb%d" % ti])
                                S.op("dve", lambda e, ti=ti: e.tensor_reduce(out=kr[:], in_=tb[ti][:], axis=AX.X, op=ALU.add),
                                     reads=["tb%d" % ti], writes=["kr"])
                                blk = tok // 256
                                S.stt("dve", kmT[:, j * 4:(j + 1) * 4, blk], kr[:], 1.0 / 256, kmT[:, j * 4:(j + 1) * 4, blk],
                                      ALU.mult, ALU.add, reads=["kr", "kmT"], writes=["kmT"])
                        deferred.append(fin)
                    elif kind == "dt":
                        di = cnt["da"] % 2; cnt["da"] += 1
                        dk = "dab%d" % di
                        S.tt("dve", dab[di][:, 0:32], ps[:, 0:32], dtb[:], ALU.add, reads=[pk, "dtb"], writes=[dk])
                        S.act(dab[di][:, 0:32], dab[di][:, 0:32], AF.Exp, reads=[dk], writes=[dk])
                        S.act(dab[di][:, 0:32], dab[di][:, 0:32], AF.Ln, reads=[dk], writes=[dk], bias=1.0)
                        S.tt("dve", dab[di][:, 32:64], dab[di][:, 0:32], An[:], ALU.mult, reads=[dk, "An"], writes=[dk])
                        S.dma("pool", C.da[tok:tok + 128, :], dab[di][:], reads=[dk])
                    else:
                        oi = cnt["ob"] % 2; cnt["ob"] += 1
                        fn = {"v": AF.Copy, "z": AF.Silu, "ga": AF.Sigmoid, "gs": AF.Sigmoid}[kind]
                        S.act(ob[oi][:], ps[:, 0:512], fn, reads=[pk], writes=["ob%d" % oi])
                        if kind == "v":
                            dst = C.v[tok:tok + 128, j * 512:(j + 1) * 512]
                        else:
                            otok = to * 512 + s4 * 128
                            dst = {"z": C.z, "ga": C.sga, "gs": C.sgs}[kind][otok:otok + 128, j * 512:(j + 1) * 512]
                        S.dma("pool", dst, ob[oi][:], reads=["ob%d" % oi])
        while deferred:
            deferred.pop(0)()
        while cm_def:
            cm_def.pop(0)()
        S.dma("pool", C.kmT.rearrange("c p n -> p c n"), kmT[:], reads=["kmT"])
        S.flush()


def moba_setup(C):
    nc = C.nc
    NV, NO, NB = C.NV, C.NO, C.NB

    def inp(name, shape, dt=F32):
        C.inp[name] = nc.dram_tensor(name, list(shape), dt, kind="ExternalInput").ap()
    inp("kaug_c", [33, NV], BF16)
    inp("cq", [NH, NO], BF16)
    inp("kbias", [128, NH, NV // 128])
    NBO = NO // 256
    inp("gbp", [1, NBO * 32]); inp("A01", [1, NBO * 32]); inp("Bt", [1, NBO * 32])
    inp("cm", [128, 2, 256], BF16)
    inp("sel65", [65, 64])
    C.aT = dram(C, "aT_s", [D, NO], BF16)


def moba_consts(NV, r):
    bf = ml_dtypes.bfloat16
    NO = NV // 2
    NB = NV // 256
    NBO = NO // 256
    slopes = np.exp2(-8.0 * np.arange(1, NH + 1, dtype=np.float32) / NH).astype(np.float32)
    out = {}
    ka = np.zeros((33, NV), np.float32)
    for n in range(NB):
        ka[n, n * 256:(n + 1) * 256] = 1
    ka[32] = 1
    out["kaug_c"] = ka.astype(bf)
    pos_q = (NO + np.arange(NO)).astype(np.float32)
    out["cq"] = (-slopes[:, None] * pos_q[None, :]).astype(bf)
    pos_k = (np.arange(NV // 128)[None, :] * 128 + np.arange(128)[:, None]).astype(np.float32)
    out["kbias"] = np.ascontiguousarray((slopes[None, :, None] * pos_k[:, None, :]).astype(np.float32))
    valid = np.ones(32, bool)
    valid[NB:] = False
    if r == 0:
        valid[:NB // 2] = False
    gbp = np.full((NBO, 32), NEG, np.float32); A01 = np.zeros((NBO, 32), np.float32); Bt = np.full((NBO, 32), NEG, np.float32)
    for mo in range(NBO):
        m = NB // 2 + mo
        for n in range(32):
            if n < m and valid[n]:
                gbp[mo, n] = 0; A01[mo, n] = 1
            if n == m:
                Bt[mo, n] = 0
    out["gbp"] = gbp.reshape(1, -1); out["A01"] = A01.reshape(1, -1); out["Bt"] = Bt.reshape(1, -1)
    cm = np.zeros((128, 2, 256), np.float32)
    kk = np.arange(128)[:, None]; qq = np.arange(256)[None, :]
    cm[:, 0, :] = (qq >= kk); cm[:, 1, :] = (qq >= kk + 128)
    out["cm"] = ((cm - 1.0) * 30000.0).astype(bf)
    s = np.zeros((65, 64), np.float32); s[64] = 1
    out["sel65"] = s
    return out


def phase_moba(C, S):
    nc = C.nc
    NV, NO, NB = C.NV, C.NO, C.NB
    NKT = NV // 128
    NQ = NO // 512
    NBO = NO // 256
    with ExitStack() as st:
        sb = lambda name, shape, dt: st.enter_context(nc.sbuf_tensor("mb_" + name, shape, dt))
        ident = sb("ident", [128, 128], BF16)
        S.dma("sp", ident[:], C.inp["ident_bf"][:, :], writes=["ident"])
        kaT = [sb("kaT%d" % i, [97, NV], BF16) for i in range(2)]
        qaT = [sb("qaT%d" % i, [97, NO], BF16) for i in range(2)]
        for i in range(2):
            S.dma("sp", kaT[i][64:97, :], C.inp["kaug_c"][:, :], writes=["kaT%d" % i])
        vt = sb("vt", [128, NKT, 8, 65], BF16)
        kbias = sb("kbias", [128, NH, NKT], F32)
        S.dma("sp", kbias[:], C.inp["kbias"][:, :, :], writes=["kbias"])
        gbp = sb("gbp", [128, NBO, 32], F32); A01 = sb("A01", [128, NBO, 32], F32); Bt = sb("Bt", [128, NBO, 32], F32)
        for nm, tl in (("gbp", gbp), ("A01", A01), ("Bt", Bt)):
            S.dma("sp", tl[:].rearrange("p a b -> p (a b)"), C.inp[nm][0:1, :].partition_broadcast(128), writes=[nm])
        cm = sb("cm", [128, 2, 256], BF16)
        S.dma("sp", cm[:], C.inp["cm"][:, :, :], writes=["cm"])
        sel65 = sb("sel65", [65, 64], F32)
        S.dma("sp", sel65[:], C.inp["sel65"][:, :], writes=["sel65"])
        kmf = sb("kmf", [64, NB], F32)
        kmb = sb("kmb", [64, 32], BF16)
        S.memset("pool", kmb[:], 0.0, writes=["kmb"])
        gm = sb("gm", [128, 32], F32); top8 = sb("top8", [128, 8], F32); f1 = sb("f1", [128, 32], F32)
        mbt = [sb("mbt%d" % i, [128, 96], BF16) for i in range(2)]
        for i in range(2):
            S.memset("pool", mbt[i][:], 0.0, writes=["mbt%d" % i])
        pt = [sb("pt%d" % i, [128, 512], BF16) for i in range(4)]
        oT = sb("oT", [65, 512], F32); rd = sb("rd", [64, 512], F32)
        ao = [sb("ao%d" % i, [64, 512], BF16) for i in range(2)]
        psS = [C.ps[0], C.ps[1]]; psO = [C.ps[2], C.ps[3]]; psG = C.ps[4]; psT = C.ps[5]; psD = C.ps[6]
        cnt = {"s": 0, "pt": 0, "o": 0, "ao": 0, "mb": 0}
        def load_v(g):
            if g == 0:
                S.memset("pool", vt[:, :, :, 64:65], 1.0, writes=["vt%d" % k_ for k_ in range(NKT)])
            for kt0 in range(NKT):
                S.dma("sp", vt[:, kt0, :, 0:64],
                      C.v[kt0 * 128:(kt0 + 1) * 128, g * 512:(g + 1) * 512].rearrange("p (a d) -> p a d", d=64),
                      writes=["vt%d" % kt0])

        def prep_steps(h):
            hb = h % 2
            steps = []

            def loads():
                S.dma("sp", kaT[hb][0:64, :], C.kT[h * 64:(h + 1) * 64, :], writes=["kaT%d" % hb])
                S.dma("sp", qaT[hb][0:64, :], C.qT[h * 64:(h + 1) * 64, :], writes=["qaT%d" % hb])
                S.dma("sp", qaT[hb][96:97, :], C.inp["cq"][h:h + 1, :], writes=["qaT%d" % hb])
                S.dma("sp", kmf[:], C.kmT[h // 2, (h % 2) * 64:(h % 2) * 64 + 64, :], writes=["kmf"])
                S.copy("act", kmb[:, 0:NB], kmf[:], reads=["kmf"], writes=["kmb"])
            steps.append(loads)
            nq = NO // 128
            st1, st2, st3 = [], [], []
            for qs in range(nq):
                mo = qs // 2
                mi = qs % 2

                def s1(qs=qs, mo=mo, mi=mi):
                    S.mm(psG[:, 0:32], qaT[hb][0:64, qs * 128:(qs + 1) * 128], kmb[:], True, True,
                         reads=["qaT%d" % hb, "kmb"], writes=["psG"])
                    S.tt("dve", gm[:], psG[:, 0:32], gbp[:, mo, :], ALU.add, reads=["psG", "gbp"], writes=["gm"])
                    S.op("dve", lambda e: e.max(out=top8[:], in_=gm[:]), reads=["gm"], writes=["top8"])
                    S.ts("dve", f1[:], gm[:], top8[:, 2:3], ALU.is_ge, -NEG, ALU.mult, reads=["gm", "top8"], writes=["f1"])
                    S.tt("dve", f1[:], f1[:], A01[:, mo, :], ALU.mult, reads=["f1", "A01"], writes=["f1"])
                    S.tt("dve", mbt[mi][:, 64:96], f1[:], Bt[:, mo, :], ALU.add, reads=["f1", "Bt"], writes=["mbt%d" % mi])

                def s2(qs=qs, mi=mi):
                    pTt = psT[:].bitcast(BF16)[0:96, 0:128]
                    S.tr(pTt, mbt[mi][:], ident[:], reads=["mbt%d" % mi, "ident"], writes=["psT"])

                def s3(qs=qs):
                    S.copy("act", qaT[hb][64:96, qs * 128:(qs + 1) * 128], psT[:].bitcast(BF16)[64:96, 0:128],
                           reads=["psT"], writes=["qaT%d" % hb])
                st1.append(s1); st2.append(s2); st3.append(s3)
            for k in range(nq + 2):
                if 0 <= k - 2 < nq: steps.append(st3[k - 2])
                if 0 <= k - 1 < nq: steps.append(st2[k - 1])
                if k < nq: steps.append(st1[k])
            return steps

        def pairs(h, pending):
            hb = h % 2
            hl = h % 8
            for j in range(NQ):
                oi = cnt["o"] % 2; cnt["o"] += 1
                ok = "ps%d" % (2 + oi)
                b0 = (NO + 512 * j) // 256
                nkt = NKT // 2 + 4 * j + 4
                def emitS(kt):
                    si = cnt["s"] % 2; cnt["s"] += 1
                    sk = "ps%d" % si
                    n = kt // 2
                    lk = kaT[hb][0:97, kt * 128:(kt + 1) * 128]
                    if n >= b0:
                        c0 = (n - b0) * 256
                        c1 = 256 - c0
                        S.mm(psS[si][:, c1:c1 + 256], lk, qaT[hb][0:97, j * 512 + c1:j * 512 + c1 + 256],
                             True, True, reads=["kaT%d" % hb, "qaT%d" % hb], writes=[sk])
                        S.mm(psS[si][:, c0:c0 + 256], lk, qaT[hb][0:97, j * 512 + c0:j * 512 + c0 + 256],
                             True, False, reads=["kaT%d" % hb, "qaT%d" % hb], writes=[sk])
                        S.mm(psS[si][:, c0:c0 + 256], ident[:], cm[:, kt % 2, :],
                             False, True, reads=["ident", "cm"], writes=[sk])
                    else:
                        S.mm(psS[si][:, 0:512], lk, qaT[hb][0:97, j * 512:(j + 1) * 512],
                             True, True, reads=["kaT%d" % hb, "qaT%d" % hb], writes=[sk])
                    pi = cnt["pt"] % 4; cnt["pt"] += 1
                    pk = "pt%d" % pi
                    S.act(pt[pi][:], psS[si][:, 0:512], AF.Exp, reads=[sk, "kbias"], writes=[pk], bias=kbias[:, h, kt:kt + 1])
                    return pi
                pis = {0: emitS(0)}
                for kt in range(nkt):
                    if kt + 1 < nkt:
                        pis[kt + 1] = emitS(kt + 1)
                    pi = pis.pop(kt)
                    S.mm(psO[oi][0:65, 0:512], vt[:, kt, hl, :], pt[pi][:], kt == 0, kt == nkt - 1,
                         reads=["vt%d" % kt, "pt%d" % pi], writes=[ok])
                    for _ in range(MOBA_DUMMY):
                        S.mm(C.ps[7][:, 0:512], ident[:], cm[:].rearrange("p a b -> p (a b)"), True, True,
                             reads=["ident", "cm"], writes=["ps7"])
                    if pending and kt % 2 == 1:
                        pending.pop(0)()
                S.copy("act", oT[:], psO[oi][0:65, 0:512], reads=[ok], writes=["oT"])
                S.mm(psD[0:64, 0:512], sel65[:], oT[:], True, True, reads=["sel65", "oT"], writes=["psD"])
                S.op("dve", lambda e: e.reciprocal(out=rd[:], in_=psD[0:64, 0:512]), reads=["psD"], writes=["rd"])
                ai = cnt["ao"] % 2; cnt["ao"] += 1
                S.tt("dve", ao[ai][:], oT[0:64, :], rd[:], ALU.mult, reads=["oT", "rd"], writes=["ao%d" % ai])
                S.dma("pool", C.aT[h * 64:(h + 1) * 64, j * 512:(j + 1) * 512], ao[ai][:], reads=["ao%d" % ai])

        for f in prep_steps(0):
            f()
        for h in range(NH):
            if h % 8 == 0:
                load_v(h // 8)
            pending = prep_steps(h + 1) if h + 1 < NH else []
            pairs(h, pending)
            while pending:
                pending.pop(0)()
        S.flush()


def ssd_setup(C):
    nc = C.nc

    def inp(name, shape, dt=F32):
        C.inp[name] = nc.dram_tensor(name, list(shape), dt, kind="ExternalInput").ap()
    inp("tri_f", [128, 128]); inp("ones_f", [128, 128]); inp("trineg_f", [128, 128])
    C.ynT = dram(C, "ynT_s", [2048, C.NO], BF16)


def ssd_consts():
    s = np.arange(128)[:, None]; t = np.arange(128)[None, :]
    return {"tri_f": (s <= t).astype(np.float32), "ones_f": np.ones((128, 128), np.float32),
            "trineg_f": np.where(t >= s, 0.0, NEG).astype(np.float32)}


def phase_ssd(C, S):
    nc = C.nc
    C.ssd_stop = getattr(C, "ssd_stop", 99)
    C.ssd_sub = getattr(C, "ssd_sub", 99)
    NV, NO = C.NV, C.NO
    NCH = NV // 256
    with ExitStack() as st:
        sb = lambda name, shape, dt: st.enter_context(nc.sbuf_tensor("sd_" + name, shape, dt))
        ident = sb("ident", [128, 128], BF16); identf = sb("identf", [128, 128], F32)
        tri = sb("tri", [128, 128], F32); ones = sb("ones", [128, 128], F32); trineg = sb("trineg", [128, 128], F32)
        for tl, nm in ((ident, "ident_bf"), (identf, "ident_f"), (tri, "tri_f"), (ones, "ones_f"), (trineg, "trineg_f")):
            S.dma("sp", tl[:], C.inp[nm][:, :], writes=["consts"])
        dsk = sb("dsk", [128, SH], F32); gn = sb("gn", [128, 2048], F32); pv = sb("pv", [128, 1], F32)
        S.dma("sp", dsk[:], C.inp["d_skip"][0:1, :].partition_broadcast(128), writes=["consts"])
        S.dma("sp", gn[:], C.inp["ssd_norm_g"][0:1, :].partition_broadcast(128), writes=["consts"])
        S.dma("sp", pv[:], C.inp["pv"][0:1, :].partition_broadcast(128), writes=["consts"])
        state = sb("state", [128, 8, 256], F32); stb = sb("stb", [128, 8, 256], BF16)
        S.memset("pool", state[:], 0.0, writes=["state"])
        S.memset("pool", stb[:], 0.0, writes=["stb"])
        xsT = sb("xsT", [128, 16, 256], BF16); BTt = sb("BTt", [128, 8, 256], BF16); CTt = sb("CTt", [128, 8, 256], BF16)
        da = sb("da", [128, 2, 64], F32); zt = sb("zt", [128, 2, 2048], BF16)
        xdt = sb("xdt", [128, 2, 2048], BF16); xdtd = sb("xdtd", [128, 2, 2048], BF16); xtm = sb("xtm", [128, 2, 2048], BF16)
        Btm = sb("Btm", [128, 2, 8, 128], BF16)
        acum = sb("acum", [128, 2, 32], F32); nacum = sb("nacum", [128, 2, 32], F32); eA = sb("eA", [128, 2, 32], F32)
        dend = sb("dend", [128, 2, 32], F32); eTot = sb("eTot", [128, 32], F32); tot = sb("tot", [128, 32], F32)
        Lt = [sb("Lt%d" % i, [128, 384], F32) for i in range(2)]
        Mt = [sb("Mt%d" % i, [128, 384], BF16) for i in range(4)]
        ysb = sb("ysb", [128, 2, 2048], F32); ytmp = sb("ytmp", [128, 2048], F32)
        RA = sb("RA", [128, 3, 4, 128], F32)
        ssg = sb("ssg", [128, 8], F32); ynb = sb("ynb", [128, 2048], BF16); ynT = sb("ynT", [128, 16, 128], BF16)
        ps = C.ps
        for c in range(NCH):
            own = c >= NCH // 2
            t0 = c * 256
            to0 = t0 - NO
            S.dma("sp", xsT[:], C.xsT[:, t0:t0 + 256].rearrange("(c p) t -> p c t", p=128), writes=["xsT"])
            S.dma("sp", BTt[:], C.BT[:, t0:t0 + 256].rearrange("(c p) t -> p c t", p=128), writes=["BTt"])
            S.dma("sp", da[:], C.da[t0:t0 + 256, :].rearrange("(i p) c -> p i c", p=128), writes=["da"])
            if own:
                S.dma("sp", CTt[:], C.CT[:, to0:to0 + 256].rearrange("(c p) t -> p c t", p=128), writes=["CTt"])
                S.dma("sp", zt[:], C.z[to0:to0 + 256, :].rearrange("(i p) c -> p i c", p=128), writes=["zt"])
            S.mm(ps[6][:, 0:32], tri[:], da[:, 0, 32:64], True, True, reads=["consts", "da"], writes=["ps6"])
            S.mm(ps[6][:, 32:64], tri[:], da[:, 1, 32:64], True, False, reads=["consts", "da"], writes=["ps6"])
            S.mm(ps[6][:, 32:64], ones[:], da[:, 0, 32:64], False, True, reads=["consts", "da"], writes=["ps6"])
            S.mm(ps[6][:, 64:96], ones[:], da[:, 0, 32:64], True, False, reads=["consts", "da"], writes=["ps6"])
            S.mm(ps[6][:, 64:96], ones[:], da[:, 1, 32:64], False, True, reads=["consts", "da"], writes=["ps6"])
            S.copy("dve", acum[:].rearrange("p i h -> p (i h)"), ps[6][:, 0:64], reads=["ps6"], writes=["acum"])
            S.copy("dve", tot[:], ps[6][:, 64:96], reads=["ps6"], writes=["tot"])
            S.ts("dve", nacum[:], acum[:], -1.0, ALU.mult, reads=["acum"], writes=["nacum"])
            S.act(eA[:], acum[:], AF.Exp, reads=["acum"], writes=["eA"])
            S.act(eTot[:], tot[:], AF.Exp, reads=["tot"], writes=["eTot"])
            S.tt("dve", dend[:], nacum[:], tot[:].unsqueeze(1).to_broadcast([128, 2, 32]), ALU.add,
                 reads=["nacum", "tot"], writes=["dend"])
            S.act(dend[:], dend[:], AF.Exp, reads=["dend"], writes=["dend"])
            if C.ssd_stop <= 1:
                continue
            for i in range(2):
                pT = ps[7][:].bitcast(BF16)[:, 0:1024].rearrange("p (c t) -> p c t", t=128)
                for half in range(2):
                    for cc in range(8):
                        S.tr(pT[:, cc, :], xsT[:, half * 8 + cc, i * 128:(i + 1) * 128], ident[:],
                             reads=["xsT", "consts"], writes=["ps7"])
                    hs = slice(half * 16, half * 16 + 16)
                    dst = xdt[:, i, half * 1024:(half + 1) * 1024].rearrange("p (h d) -> p h d", d=64)
                    src = pT.rearrange("p c (h d) -> p (c h) d", d=64)
                    S.tt("dve", dst, src, da[:, i, hs].unsqueeze(2).to_broadcast([128, 16, 64]), ALU.mult,
                         reads=["ps7", "da"], writes=["xdt"])
                    if own and C.ssd_sub >= 1:
                        S.copy("act", xtm[:, i, half * 1024:(half + 1) * 1024], pT.rearrange("p c t -> p (c t)"),
                               reads=["ps7"], writes=["xtm"])
                if C.ssd_sub < 2:
                    continue
                S.tt("dve", xdtd[:, i, :].rearrange("p (h d) -> p h d", d=64), xdt[:, i, :].rearrange("p (h d) -> p h d", d=64),
                     dend[:, i, :].unsqueeze(2).to_broadcast([128, 32, 64]), ALU.mult, reads=["xdt", "dend"], writes=["xdtd"])
                if C.ssd_sub < 3:
                    continue
                pT = ps[7][:].bitcast(BF16)[:, 0:1024].rearrange("p (c t) -> p c t", t=128)
                for g in range(8):
                    S.tr(pT[:, g, :], BTt[:, g, i * 128:(i + 1) * 128], ident[:], reads=["BTt", "consts"], writes=["ps7"])
                S.copy("act", Btm[:, i, :, :], pT, reads=["ps7"], writes=["Btm"])
            if C.ssd_stop <= 2:
                continue
            if own:
                for g in range(8):
                    if C.ssd_stop <= 3 and g > 0:
                        continue
                    S.mm(ps[4][:, 0:256], BTt[:, g, 0:128], CTt[:, g, 0:256], True, True, reads=["BTt", "CTt"], writes=["ps4"])
                    S.mm(ps[4][:, 256:384], BTt[:, g, 128:256], CTt[:, g, 128:256], True, True, reads=["BTt", "CTt"], writes=["ps4"])
                    for h4 in range(4):
                        h = g * 4 + h4
                        pL = ps[h4]; lk = "ps%d" % h4
                        if h4 == 0:
                            for q_, (src_i, mat) in enumerate(((0, tri), (0, ones), (1, tri))):
                                S.tt("dve", RA[:, q_, :, :], da[:, src_i, 32 + g * 4:36 + g * 4].unsqueeze(2).to_broadcast([128, 4, 128]),
                                     mat[:].unsqueeze(1).to_broadcast([128, 4, 128]), ALU.mult, reads=["da", "consts"], writes=["RA"])
                        S.mm(pL[:, 0:128], ones[:], RA[:, 0, h4, :], True, False, reads=["RA", "consts"], writes=[lk])
                        S.mm(pL[:, 0:128], identf[:], trineg[:], False, True, reads=["consts"], writes=[lk])
                        S.mm(pL[:, 128:256], ones[:], RA[:, 1, h4, :], True, False, reads=["RA", "consts"], writes=[lk])
                        S.mm(pL[:, 128:256], ones[:], RA[:, 2, h4, :], False, True, reads=["RA", "consts"], writes=[lk])
                        S.mm(pL[:, 256:384], ones[:], RA[:, 1, h4, :], True, False, reads=["RA", "consts"], writes=[lk])
                        S.mm(pL[:, 256:384], ones[:], RA[:, 2, h4, :], False, False, reads=["RA", "consts"], writes=[lk])
                        S.mm(pL[:, 256:384], identf[:], trineg[:], False, True, reads=["consts"], writes=[lk])
                        li = h % 2
                        S.act(Lt[li][:, 0:256], pL[:, 0:256], AF.Exp, reads=[lk, "nacum"], writes=["Lt%d" % li],
                              bias=nacum[:, 0, h:h + 1])
                        S.act(Lt[li][:, 256:384], pL[:, 256:384], AF.Exp, reads=[lk, "nacum"], writes=["Lt%d" % li],
                              bias=nacum[:, 1, h:h + 1])
                        S.tt("dve", Mt[h4][:], Lt[li][:], ps[4][:, 0:384], ALU.mult, reads=["Lt%d" % li, "ps4"],
                             writes=["Mt%d" % h4])
                    if C.ssd_stop <= 4:
                        continue
                    pY = ps[5][:, 0:512].rearrange("p (i c) -> p i c", i=2)
                    for h4 in range(4):
                        h = g * 4 + h4
                        cs = slice(h4 * 64, (h4 + 1) * 64)
                        S.mm(pY[:, 0, cs], Mt[h4][:, 0:128], xdt[:, 0, h * 64:(h + 1) * 64], True, True,
                             reads=["Mt%d" % h4, "xdt"], writes=["ps5"])
                        S.mm(pY[:, 1, cs], Mt[h4][:, 128:256], xdt[:, 0, h * 64:(h + 1) * 64], True, False,
                             reads=["Mt%d" % h4, "xdt"], writes=["ps5"])
                        S.mm(pY[:, 1, cs], Mt[h4][:, 256:384], xdt[:, 1, h * 64:(h + 1) * 64], False, True,
                             reads=["Mt%d" % h4, "xdt"], writes=["ps5"])
                    pO = ps[6][:, 0:512].rearrange("p (i c) -> p i c", i=2)
                    for i in range(2):
                        S.mm(pO[:, i, :], CTt[:, g, i * 128:(i + 1) * 128], stb[:, g, :], True, True,
                             reads=["CTt", "stb"], writes=["ps6"])
                    for i in range(2):
                        yg = ysb[:, i, g * 256:(g + 1) * 256].rearrange("p (h d) -> p h d", d=64)
                        S.tt("dve", yg, pO[:, i, :].rearrange("p (h d) -> p h d", d=64),
                             eA[:, i, g * 4:(g + 1) * 4].unsqueeze(2).to_broadcast([128, 4, 64]), ALU.mult,
                             reads=["ps6", "eA"], writes=["ysb"])
                        S.tt("dve", ysb[:, i, g * 256:(g + 1) * 256], ysb[:, i, g * 256:(g + 1) * 256], pY[:, i, :], ALU.add,
                             reads=["ysb", "ps5"], writes=["ysb"])
            if C.ssd_stop <= 5:
                continue
            for g in range(8):
                for i in range(2):
                    S.mm(ps[5][:, 0:256], Btm[:, i, g, :], xdtd[:, i, g * 256:(g + 1) * 256], i == 0, i == 1,
                         reads=["Btm", "xdtd"], writes=["ps5"])
                sg = state[:, g, :].rearrange("p (h d) -> p h d", d=64)
                S.tt("dve", sg, sg, eTot[:, g * 4:(g + 1) * 4].unsqueeze(2).to_broadcast([128, 4, 64]), ALU.mult,
                     reads=["state", "eTot"], writes=["state"])
                S.tt("dve", state[:, g, :], state[:, g, :], ps[5][:, 0:256], ALU.add, reads=["state", "ps5"], writes=["state"])
            if c == NCH // 2 - 1:
                S.ts("dve", state[:].rearrange("p g c -> p (g c)"), state[:].rearrange("p g c -> p (g c)"), pv[:, 0:1], ALU.mult,
                     reads=["state", "consts"], writes=["state"])
            S.copy("act", stb[:].rearrange("p g c -> p (g c)"), state[:].rearrange("p g c -> p (g c)"), reads=["state"], writes=["stb"])
            if C.ssd_stop <= 6:
                continue
            if own:
                for i in range(2):
                    y = ysb[:, i, :]
                    y3 = y.rearrange("p (h d) -> p h d", d=64)
                    S.tt("dve", ytmp[:].rearrange("p (h d) -> p h d", d=64), xtm[:, i, :].rearrange("p (h d) -> p h d", d=64),
                         dsk[:].unsqueeze(2).to_broadcast([128, 32, 64]), ALU.mult, reads=["xtm", "consts"], writes=["ytmp"])
                    S.tt("dve", y, y, ytmp[:], ALU.add, reads=["ysb", "ytmp"], writes=["ysb"])
                    S.tt("dve", y, y, zt[:, i, :], ALU.mult, reads=["ysb", "zt"], writes=["ysb"])
                    S.tt("dve", ytmp[:], y, y, ALU.mult, reads=["ysb"], writes=["ytmp"])
                    S.op("dve", lambda e: e.tensor_reduce(out=ssg[:], in_=ytmp[:].rearrange("p (g c) -> p g c", c=256),
                                                         axis=AX.X, op=ALU.add), reads=["ytmp"], writes=["ssg"])
                    S.act(ssg[:], ssg[:], AF.Ln, reads=["ssg"], writes=["ssg"], scale=1.0 / 256, bias=EPS)
                    S.act(ssg[:], ssg[:], AF.Exp, reads=["ssg"], writes=["ssg"], scale=-0.5)
                    S.tt("dve", ytmp[:].rearrange("p (g c) -> p g c", c=256), y.rearrange("p (g c) -> p g c", c=256),
                         ssg[:].unsqueeze(2).to_broadcast([128, 8, 256]), ALU.mult, reads=["ysb", "ssg"], writes=["ytmp"])
                    S.tt("dve", ynb[:], ytmp[:], gn[:], ALU.mult, reads=["ytmp", "consts"], writes=["ynb"])
                    for half in range(2):
                        pT = ps[7][:].bitcast(BF16)[:, 0:1024].rearrange("p (c t) -> p c t", t=128)
                        for cc in range(8):
                            S.tr(pT[:, cc, :], ynb[:, (half * 8 + cc) * 128:(half * 8 + cc + 1) * 128], ident[:],
                                 reads=["ynb", "consts"], writes=["ps7"])
                        S.copy("act", ynT[:, half * 8:(half + 1) * 8, :], pT, reads=["ps7"], writes=["ynT"])
                    tok = to0 + i * 128
                    S.dma("pool", C.ynT[:, tok:tok + 128].rearrange("(c p) t -> p c t", p=128), ynT[:], reads=["ynT"])
        S.flush()


def peer_setup(C):
    nc = C.nc

    def inp(name, shape, dt=F32):
        C.inp[name] = nc.dram_tensor(name, list(shape), dt, kind="ExternalInput").ap()
    inp("R1", [128, 32, 512], BF16); inp("R2", [128, 512], BF16)
    C.x2 = dram(C, "x2_s", [C.NO, D], F32)
    C.hT2 = dram(C, "hT2_s", [C.NO // 512, 128, 8, 512], BF16)
    C.out = nc.dram_tensor("out", [C.NO, D], F32, kind="ExternalOutput").ap()


def peer_consts():
    bf = ml_dtypes.bfloat16
    R1 = np.zeros((128, 32, 4, 128), np.float32)
    for c in range(32):
        for j in range(4):
            R1[4 * c + j, c, j, :] = 1
    R2 = np.tile(np.eye(128, dtype=np.float32), (1, 4))
    return {"R1": R1.reshape(128, 32, 512).astype(bf), "R2": R2.astype(bf)}


def phase_peer(C, S):
    nc = C.nc
    C.peer_dummy = getattr(C, "peer_dummy", PEER_DUMMY)
    NO = C.NO
    UT = C.inp["peer_uT"]; VT = C.inp["peer_v"]; WQ = C.inp["w_peer_q"]
    with ExitStack() as st:
        sb = lambda name, shape, dt: st.enter_context(nc.sbuf_tensor("pr_" + name, shape, dt))
        ident = sb("ident", [128, 128], BF16)
        S.dma("sp", ident[:], C.inp["ident_bf"][:, :], writes=["ident"])
        R1 = sb("R1", [128, 32, 512], BF16); R2 = sb("R2", [128, 512], BF16)
        S.dma("sp", R1[:], C.inp["R1"][:, :, :], writes=["R1"])
        S.dma("sp", R2[:], C.inp["R2"][:, :], writes=["R2"])
        stg = sb("stg", [128, 4096], F32)
        stg2 = sb("stg2", [128, 4096], F32)
        stg2_v = stg2[:].rearrange("p (j d) -> p j d", d=1024)
        stg_u = stg[:].rearrange("p (c n) -> p c n", n=512)
        stg_v = stg[:].rearrange("p (j d) -> p j d", d=1024)
        kT = sb("kT", [128, 16, 128], BF16)
        for hh in range(2):
            S.dma("sp", stg_u[:, :, 0:128], C.inp["keys%dT" % (hh + 1)].rearrange("h d k -> d h k"), writes=["stg"])
            S.copy("pool", kT[:].rearrange("p (h two) k -> p h two k", two=2)[:, :, hh, :], stg_u[:, :, 0:128],
                   reads=["stg"], writes=["kT"])
        ub = [sb("ub%d" % i, [128, 8, 512], BF16) for i in range(2)]
        vb = [sb("vb%d" % i, [128, 4, 1024], BF16) for i in range(2)]
        xnT = sb("xnT", [128, 8, 512], BF16)
        shr = sb("shr", [128, 8192], BF16)
        qTr = shr[:].rearrange("p (c t) -> p c t", t=512)
        sb16 = sb("sb16", [128, 16, 128], BF16); sf = sb("sf", [128, 16, 128], F32); swk = sb("swk", [128, 128], F32)
        v16 = sb("v16", [128, 16, 16], F32)
        cand = sb("cand", [128, 8, 256], F32); cwk = stg2[:, 0:2048].rearrange("p (h k) -> p h k", k=256)
        t8 = sb("t8", [128, 8, 8], F32); t8b = sb("t8b", [128, 8, 8], F32)
        thr = sb("thr", [128, 4, 8], F32); nb = sb("nb", [128, 4, 8], F32); zz = sb("zz", [128, 8], F32)
        sT = sb("sT", [128, 4, 16, 128], BF16)
        tau = sb("tau", [128, 4, 8], F32); taub = sb("taub", [128, 8], BF16)
        Eb = [sb("Eb%d" % i, [128, 512], BF16) for i in range(3)]
        Em8 = [sb("Em80", [128, 8, 512], BF16), shr[:, 0:4096].rearrange("p (h e) -> p h e", e=512)]
        gsb = [sb("gsb%d" % i, [128, 4, 512], BF16) for i in range(2)]
        hact = [sb("hact%d" % i, [128, 512], BF16) for i in range(2)]
        hTt = [sb("hTt%d" % i, [128, 4, 128], BF16) for i in range(2)]
        yacc = sb("yacc", [128, 4, 1024], F32)
        ps = C.ps
        cnt = {"e": 0, "u": 0, "it": 0}

        def peer_iter(it, c, s4, ui, vi):
            ts_ = slice(s4 * 128, (s4 + 1) * 128)
            b = it % 2
            pW = ps[4]; wk = "ps4"
            A = []
            for h in range(8):
                def ah(h=h):
                    ei = cnt["e"] % 3; cnt["e"] += 1
                    pE = ps[(2, 3, 1)[ei]]; ek = "ps%d" % (2, 3, 1)[ei]
                    S.mm(pE[:, 0:512], sT[:, s4, 2 * h, :], R1[:, c, :], True, False, reads=["sT", "R1"], writes=[ek])
                    S.mm(pE[:, 0:512], sT[:, s4, 2 * h + 1, :], R2[:], False, True, reads=["sT", "R2"], writes=[ek])
                    S.act(Eb[ei][:], pE[:, 0:512], AF.Exp, reads=[ek, "nb"], writes=["Eb%d" % ei], bias=nb[:, s4, h:h + 1])
                    S.stt("dve", Em8[b][:, h, :], pE[:, 0:512], thr[:, s4, h:h + 1], Eb[ei][:], ALU.is_ge, ALU.mult,
                          reads=[ek, "thr", "Eb%d" % ei], writes=["Em8%d_%d" % (b, h)])
                A.append(ah)

            def b1a():
                for h in range(8):
                    S.mm(pW[:, 0:512], ident[:], Em8[b][:, h, :], h == 0, h == 7, reads=["Em8%d_%d" % (b, h), "ident"], writes=[wk])

            def b1():
                pass

            def b2():
                pass

            def b3():
                S.tt("dve", hact[b][:], gsb[c % 2][:, s4, :], pW[:, 0:512], ALU.mult, reads=["gsb%d" % (c % 2), wk],
                     writes=["hact%d" % b])

            def b4():
                pT = ps[0][:].bitcast(BF16)[:, 0:512].rearrange("p (j t) -> p j t", t=128)
                for j in range(4):
                    S.tr(pT[:, j, :], hact[b][:, j * 128:(j + 1) * 128], ident[:], reads=["hact%d" % b, "ident"], writes=["ps0"])

            def b5():
                pT = ps[0][:].bitcast(BF16)[:, 0:512].rearrange("p (j t) -> p j t", t=128)
                S.copy("act", hTt[b][:], pT, reads=["ps0"], writes=["hTt%d" % b])

            def b6():
                for half in range(2):
                    for j in range(4):
                        S.mm(ps[6 + half][:, 0:512], hTt[b][:, j, :], vb[vi][:, j, half * 512:(half + 1) * 512], j == 0, j == 3,
                             reads=["hTt%d" % b, "vb%d" % vi], writes=["ps%d" % (6 + half)])

            def b7():
                for half in range(2):
                    ya = yacc[:, s4, half * 512:(half + 1) * 512]
                    S.tt("dve", ya, ya, ps[6 + half][:, 0:512], ALU.add, reads=["yacc", "ps%d" % (6 + half)], writes=["yacc"])
            return A, [b1a, b1, b2, b3, b4, b5, b6, b7]

        def act_part(c, s4, ui):
            for kc in range(8):
                S.mm(ps[5][:, 0:512], xnT[:, kc, s4 * 128:(s4 + 1) * 128], ub[ui][:, kc, :], kc == 0, kc == 7,
                     reads=["ub%d" % ui, "xnT"], writes=["ps5"])
            S.copy("act", gsb[c % 2][:, s4, :], ps[5][:, 0:512], reads=["ps5"], writes=["gsb%d" % (c % 2)])

        def gelu_inplace(c):
            g2 = gsb[c % 2][:].rearrange("p a b -> p (a b)")
            S.act(g2, g2, AF.Gelu, reads=["gsb%d" % (c % 2)], writes=["gsb%d" % (c % 2)])

        prevB = []
        for rd in range(NO // 512):
            S.dma("sp", xnT[:], C.hT2[rd, :, :, :], writes=["xnT"])
            S.memset("pool", yacc[:], 0.0, writes=["yacc"])
            for pc in range(4):
                S.dma("sp", stg_u, WQ[:, pc * 512:(pc + 1) * 512].rearrange("(c p) n -> p c n", p=128), writes=["stg"])
                ui = cnt["u"] % 2; cnt["u"] += 1
                S.copy("act", ub[ui][:], stg_u, reads=["stg"], writes=["ub%d" % ui])
                for cc in range(4):
                    for kc in range(8):
                        S.mm(ps[0][:, 0:512], ub[ui][:, kc, cc * 128:(cc + 1) * 128], xnT[:, kc, :],
                             kc == 0, kc == 7, reads=["ub%d" % ui, "xnT"], writes=["ps0"])
                    S.copy("act", qTr[:, pc * 4 + cc, :], ps[0][:, 0:512], reads=["ps0"], writes=["Em81_%d" % hh_ for hh_ in range(8)])
            for s4 in range(4):
                ts_ = slice(s4 * 128, (s4 + 1) * 128)
                for g4 in range(4):
                    for cc in range(4):
                        ch = g4 * 4 + cc
                        S.mm(ps[1][:, cc * 128:(cc + 1) * 128], qTr[:, ch, ts_], kT[:, ch, :], True, True,
                             reads=["Em81_%d" % hh_ for hh_ in range(8)] + ["kT"], writes=["ps1"])
                    S.copy("act", sb16[:, g4 * 4:(g4 + 1) * 4, :], ps[1][:, 0:512].rearrange("p (c k) -> p c k", k=128),
                           reads=["ps1"], writes=["sb16"])
                S.copy("dve", sf[:], sb16[:], reads=["sb16"], writes=["sf"])
                for ch in range(16):
                    S.op("dve", lambda e, ch=ch: e.max(out=v16[:, ch, 0:8], in_=sf[:, ch, :]), reads=["sf"], writes=["v16"])
                    S.op("dve", lambda e, ch=ch: e.match_replace(out=swk[:], in_to_replace=v16[:, ch, 0:8], in_values=sf[:, ch, :],
                                                                 imm_value=-1e30), reads=["sf", "v16"], writes=["swk"])
                    S.op("dve", lambda e, ch=ch: e.max(out=v16[:, ch, 8:16], in_=swk[:]), reads=["swk"], writes=["v16"])
                v4 = v16[:].rearrange("p (h two) k -> p h two k", two=2)
                c4 = cand[:].rearrange("p h (a b) -> p h a b", b=16)
                S.tt("dve", c4, v4[:, :, 0, :].unsqueeze(3).to_broadcast([128, 8, 16, 16]),
                     v4[:, :, 1, :].unsqueeze(2).to_broadcast([128, 8, 16, 16]), ALU.add, reads=["v16"], writes=["cand"])
                for h in range(8):
                    S.op("dve", lambda e, h=h: e.max(out=t8[:, h, :], in_=cand[:, h, :]), reads=["cand"], writes=["t8"])
                    S.op("dve", lambda e, h=h: e.match_replace(out=cwk[:, h, :], in_to_replace=t8[:, h, :], in_values=cand[:, h, :],
                                                               imm_value=-1e30), reads=["cand", "t8"], writes=["stg2"])
                    S.op("dve", lambda e, h=h: e.max(out=t8b[:, h, :], in_=cwk[:, h, :]), reads=["stg2"], writes=["t8b"])
                S.copy("dve", thr[:, s4, :], t8b[:, :, 7], reads=["t8b"], writes=["thr"])
                S.tt("dve", cwk, cand[:], t8[:, :, 0:1].to_broadcast([128, 8, 256]), ALU.subtract, reads=["cand", "t8"], writes=["stg2"])
                S.act(cwk, cwk, AF.Exp, reads=["stg2"], writes=["stg2"])
                S.tt("dve", cand[:], cand[:], thr[:, s4, :].unsqueeze(2).to_broadcast([128, 8, 256]), ALU.is_ge,
                     reads=["cand", "thr"], writes=["cand"])
                S.tt("dve", cwk, cwk, cand[:], ALU.mult, reads=["stg2", "cand"], writes=["stg2"])
                S.op("dve", lambda e: e.tensor_reduce(out=zz[:], in_=cwk, axis=AX.X, op=ALU.add), reads=["stg2"], writes=["zz"])
                S.act(zz[:], zz[:], AF.Ln, reads=["zz"], writes=["zz"])
                S.tt("dve", zz[:], zz[:], t8[:, :, 0], ALU.add, reads=["zz", "t8"], writes=["zz"])
                S.ts("dve", nb[:, s4, :], zz[:], -1.0, ALU.mult, reads=["zz"], writes=["nb"])
                for half in range(2):
                    pT = ps[1][:].bitcast(BF16)[:, 0:1024].rearrange("p (c t) -> p c t", t=128)
                    for cc in range(8):
                        S.tr(pT[:, cc, :], sb16[:, half * 8 + cc, :], ident[:], reads=["sb16", "ident"], writes=["ps1"])
                    S.copy("act", sT[:, s4, half * 8:(half + 1) * 8, :], pT, reads=["ps1"], writes=["sT"])
            def load_u(c):
                S.dma("sp", stg_u, UT[:, c * 512:(c + 1) * 512].rearrange("(kc p) n -> p kc n", p=128), writes=["stg"])
                ui_ = cnt["u"] % 2; cnt["u"] += 1
                S.copy("pool", ub[ui_][:], stg_u, reads=["stg"], writes=["ub%d" % ui_])
                return ui_

            def load_v(c):
                S.dma("sp", stg2_v, VT[c * 512:(c + 1) * 512, :].rearrange("(j p) d -> p j d", p=128), writes=["stg2"])
                S.copy("pool", vb[c % 2][:], stg2_v, reads=["stg2"], writes=["vb%d" % (c % 2)])
            events = []
            uis = {}

            def ev_load_u(c):
                uis[c] = load_u(c)
            ev_load_u(0)
            for s4_ in range(4):
                act_part(0, s4_, uis[0])
            gelu_inplace(0)
            for c in range(32):
                if c + 1 < 32:
                    events.append((c * 4 + 0 - 0.5, 0, lambda c=c: ev_load_u(c + 1)))
                    for s4_ in range(4):
                        events.append((c * 4 + s4_ + 0.65, 1, lambda c=c, s4_=s4_: act_part(c + 1, s4_, uis[c + 1])))
                    events.append((c * 4 + 3 + 0.75, 1, lambda c=c: gelu_inplace(c + 1)))
                events.append((c * 4 + 0 - 0.45, 2, lambda c=c: load_v(c)))
                for s4 in range(4):
                    it = c * 4 + s4
                    a_steps, b_steps = peer_iter(cnt["it"], c, s4, None, c % 2)
                    cnt["it"] += 1
                    for k in range(8):
                        events.append((it + k / 10.0, 3, a_steps[k]))
                    b0, _, _, b3, b4, b5, b6, b7 = b_steps
                    events.append((it + 1 + 0.45, 4, b0))
                    events.append((it + 1 + 0.52, 5, b3))
                    events.append((it + 2 + 0.05, 6, b4))
                    events.append((it + 2 + 0.15, 7, b5))
                    events.append((it + 2 + 0.25, 8, b6))
                    events.append((it + 2 + 0.32, 9, b7))
            events.sort(key=lambda e: (e[0], e[1]))
            for _, _, f in events:
                f()
            for s4 in range(4):
                tok = rd * 512 + s4 * 128
                S.dma("sp", stg[:, 0:1024], C.x2[tok:tok + 128, :], writes=["stg"])
                S.tt("dve", yacc[:, s4, :], yacc[:, s4, :], stg[:, 0:1024], ALU.add, reads=["yacc", "stg"], writes=["yacc"])
                S.dma("pool", C.out[tok:tok + 128, :], yacc[:, s4, :], reads=["yacc"])
        S.flush()


def phase_outproj(C, S):
    nc = C.nc
    NV, NO = C.NV, C.NO
    with ExitStack() as st:
        sb = lambda name, shape, dt: st.enter_context(nc.sbuf_tensor("op_" + name, shape, dt))
        ident = sb("ident", [128, 128], BF16)
        S.dma("sp", ident[:], C.inp["ident_bf"][:, :], writes=["ident"])
        stg = sb("stg", [128, 4, 1024], F32)
        Wa = sb("Wa", [128, 8, 1024], BF16); Ws = sb("Ws", [128, 16, 1024], BF16); Wo = sb("Wo", [128, 8, 1024], BF16)
        for nm, tl, nch in (("w_attn_o", Wa, 8), ("w_ssd_o", Ws, 16), ("w_out", Wo, 8)):
            for c0 in range(0, nch, 4):
                S.dma("sp", stg[:], C.inp[nm][c0 * 128:(c0 + 4) * 128, :].rearrange("(c p) n -> p c n", p=128), writes=["stg"])
                S.copy("act", tl[:, c0:c0 + 4, :], stg[:], reads=["stg"], writes=[nm])
        aTt = [sb("aTt%d" % i, [128, 8, 128], BF16) for i in range(2)]
        yTt = [sb("yTt%d" % i, [128, 16, 128], BF16) for i in range(2)]
        ga = [sb("ga%d" % i, [128, 1024], BF16) for i in range(2)]
        gs = [sb("gs%d" % i, [128, 1024], BF16) for i in range(2)]
        xt = [sb("xt%d" % i, [128, 1024], F32) for i in range(2)]
        m1 = sb("m1", [128, 1024], F32); m2 = sb("m2", [128, 1024], F32); mb = sb("mb", [128, 1024], BF16)
        mT = sb("mT", [128, 8, 128], BF16)
        xo = [sb("xo%d" % i, [128, 1024], F32) for i in range(2)]
        ps = C.ps
        for t in range(NO // 128):
            i = t % 2
            tok = t * 128
            S.dma("sp", aTt[i][:], C.aT[:, tok:tok + 128].rearrange("(c p) t -> p c t", p=128), writes=["aTt%d" % i])
            S.dma("sp", yTt[i][:], C.ynT[:, tok:tok + 128].rearrange("(c p) t -> p c t", p=128), writes=["yTt%d" % i])
            S.dma("sp", ga[i][:], C.sga[tok:tok + 128, :], writes=["ga%d" % i])
            S.dma("sp", gs[i][:], C.sgs[tok:tok + 128, :], writes=["gs%d" % i])
            S.dma("sp", xt[i][:], C.inp["xv"][NO + tok:NO + tok + 128, :], writes=["xt%d" % i])
            for half in range(2):
                hs = slice(half * 512, (half + 1) * 512)
                for c in range(8):
                    S.mm(ps[half][:, 0:512], aTt[i][:, c, :], Wa[:, c, hs], c == 0, c == 7,
                         reads=["aTt%d" % i, "w_attn_o"], writes=["ps%d" % half])
                for c in range(16):
                    S.mm(ps[2 + half][:, 0:512], yTt[i][:, c, :], Ws[:, c, hs], c == 0, c == 15,
                         reads=["yTt%d" % i, "w_ssd_o"], writes=["ps%d" % (2 + half)])
                S.tt("dve", m1[:, hs], ps[half][:, 0:512], ga[i][:, hs], ALU.mult, reads=["ps%d" % half, "ga%d" % i], writes=["m1"])
                S.tt("dve", m2[:, hs], ps[2 + half][:, 0:512], gs[i][:, hs], ALU.mult, reads=["ps%d" % (2 + half), "gs%d" % i], writes=["m2"])
            S.tt("dve", mb[:], m1[:], m2[:], ALU.add, reads=["m1", "m2"], writes=["mb"])
            pT = ps[4][:].bitcast(BF16)[:, 0:1024].rearrange("p (c t) -> p c t", t=128)
            for c in range(8):
                S.tr(pT[:, c, :], mb[:, c * 128:(c + 1) * 128], ident[:], reads=["mb", "ident"], writes=["ps4"])
            S.copy("act", mT[:], pT, reads=["ps4"], writes=["mT"])
            for half in range(2):
                hs = slice(half * 512, (half + 1) * 512)
                for c in range(8):
                    S.mm(ps[5 + half][:, 0:512], mT[:, c, :], Wo[:, c, hs], c == 0, c == 7,
                         reads=["mT", "w_out"], writes=["ps%d" % (5 + half)])
                S.tt("dve", xo[i][:, hs], ps[5 + half][:, 0:512], xt[i][:, hs], ALU.add,
                     reads=["ps%d" % (5 + half), "xt%d" % i], writes=["xo%d" % i])
            S.dma("pool", C.x2[tok:tok + 128, :], xo[i][:], reads=["xo%d" % i])
        S.flush()


def build_all(nc, NV, st, debug=()):
    C = setup(nc, NV, debug)
    moba_setup(C); ssd_setup(C); peer_setup(C)
    S = Sched(nc, st)
    C.ps = [st.enter_context(nc.psum_tensor("ps%d" % i, [128, 512], F32)) for i in range(8)]
    phase_norm(C, S, C.inp["xv"], "norm1_g", C.hT, NV, "n1")
    phase_inproj(C, S)
    phase_moba(C, S)
    phase_ssd(C, S)
    phase_outproj(C, S)
    phase_norm(C, S, C.x2, "norm2_g", C.hT2, C.NO, "n2")
    phase_peer(C, S)
    return C, S


def make_inputs(inputs, NV, b, r, full_seq):
    bf = ml_dtypes.bfloat16
    NO = NV // 2
    f32 = lambda a: np.ascontiguousarray(np.asarray(a, dtype=np.float32))
    x = np.asarray(inputs["x"])
    ins = {}
    xv = np.zeros((NV, D), np.float32)
    if r == 0:
        xv[NO:] = x[b, 0:NO]
    else:
        xv[:] = x[b, 0:NV]
    ins["xv"] = xv
    ins["pv"] = np.full((1, 1), float(r), np.float32)
    ins["norm1_g"] = f32(inputs["norm1_g"][0:1]); ins["w_in"] = f32(inputs["w_in"][0])
    ins["q_norm_g"] = f32(inputs["q_norm_g"][0:1]); ins["k_norm_g"] = f32(inputs["k_norm_g"][0:1])
    ins["conv_wT"] = f32(np.asarray(inputs["conv_w"][0]).T); ins["conv_b"] = f32(np.asarray(inputs["conv_b"][0]).reshape(4096, 1))
    ins["dt_bias"] = f32(inputs["dt_bias"][0:1]); ins["a_log"] = f32(inputs["a_log"][0:1]); ins["d_skip"] = f32(inputs["d_skip"][0:1])
    ins["ssd_norm_g"] = f32(inputs["ssd_norm_g"][0:1]); ins["w_attn_o"] = f32(inputs["w_attn_o"][0])
    ins["w_ssd_o"] = f32(inputs["w_ssd_o"][0]); ins["w_out"] = f32(inputs["w_out"][0]); ins["norm2_g"] = f32(inputs["norm2_g"][0:1])
    ins["w_peer_q"] = f32(inputs["w_peer_q"][0])
    ins["keys1T"] = f32(np.asarray(inputs["peer_keys1"][0]).transpose(0, 2, 1))
    ins["keys2T"] = f32(np.asarray(inputs["peer_keys2"][0]).transpose(0, 2, 1))
    ins["peer_uT"] = f32(np.asarray(inputs["peer_u"][0]).T); ins["peer_v"] = f32(inputs["peer_v"][0])
    ins["ident_bf"] = np.eye(128).astype(bf); ins["ident_f"] = np.eye(128, dtype=np.float32)
    ins.update(moba_consts(NV, r)); ins.update(ssd_consts()); ins.update(peer_consts())
    return ins


NV_FULL = 8192


def kernel(**inputs):
    from concourse.bass_utils import run_bass_kernel_spmd
    nc = bass.Bass("TRN2", target_bir_lowering=False)
    with ExitStack() as st:
        C, S = build_all(nc, NV_FULL, st)
    x = np.asarray(inputs["x"])
    B = x.shape[0]
    in_maps = []
    for b in range(B):
        for r in range(2):
            in_maps.append(make_inputs(inputs, NV_FULL, b, r, NV_FULL))
    res = run_bass_kernel_spmd(nc, in_maps, core_ids=list(range(len(in_maps)))).results
    NO = NV_FULL // 2
    out = np.empty((B, NV_FULL, D), np.float32)
    for b in range(B):
        for r in range(2):
            out[b, r * NO:(r + 1) * NO] = np.asarray(res[b * 2 + r]["out"], dtype=np.float32)
    return out
```
